# Optimizing a Trainium2 kernel written in Bass

```python
import math
import jax, jax.numpy as jnp
from jax import lax
import numpy as np

D_MODEL = 1024
BATCH = 32
SEQ = 2048
DEPTH = 1

N_MEM = 256
ATTN_HEADS = 8
HEAD_DIM = 64
D_ATTN = ATTN_HEADS * HEAD_DIM
D_CONV = D_MODEL - D_ATTN
CONV_WIDTH = 31
Q_BLOCK = 128
MEM_HEADS = 4
MEM_HEAD_DIM = D_MODEL // MEM_HEADS
N_GROUPS = 4
EXPERTS_PER_GROUP = 8
N_EXPERTS = N_GROUPS * EXPERTS_PER_GROUP
TOP_K_IN_GROUP = 2
D_EXPERT = D_MODEL // 2
EPS = 1e-6
OFF_Q = 0
OFF_K = OFF_Q + D_ATTN
OFF_V = OFF_K + D_ATTN
OFF_F = OFF_V + D_ATTN
OFF_C = OFF_F + ATTN_HEADS
D_IN = OFF_C + 2 * D_CONV

kernel_name = "hybrid_fox_conformer_hmoe_block"


def rmsnorm(x, g):
    xf = x.astype(jnp.float32)
    y = xf * lax.rsqrt(jnp.mean(xf * xf, axis=-1, keepdims=True) + EPS)
    return (y * g.astype(jnp.float32)).astype(x.dtype)


def layernorm(x, g, b):
    xf = x.astype(jnp.float32)
    mu = jnp.mean(xf, axis=-1, keepdims=True)
    xc = xf - mu
    y = xc * lax.rsqrt(jnp.mean(xc * xc, axis=-1, keepdims=True) + EPS)
    return (y * g.astype(jnp.float32) + b.astype(jnp.float32)).astype(x.dtype)


def forgetting_attention(q, k, v, log_f):
    S = q.shape[2]
    c = jnp.cumsum(log_f, axis=-1)
    scale = 1.0 / math.sqrt(HEAD_DIM)
    outs = []
    for i in range(S // Q_BLOCK):
        lo, hi = i * Q_BLOCK, (i + 1) * Q_BLOCK
        s = jnp.einsum('bhqd,bhkd->bhqk', q[:, :, lo:hi], k[:, :, :hi]).astype(jnp.float32) * scale
        s = s + c[:, :, lo:hi, None] - c[:, :, None, :hi]
        causal = (lo + jnp.arange(Q_BLOCK))[:, None] >= jnp.arange(hi)[None, :]
        s = jnp.where(causal, s, -jnp.inf)
        p = jax.nn.softmax(s, axis=-1).astype(v.dtype)
        outs.append(jnp.einsum('bhqk,bhkd->bhqd', p, v[:, :, :hi]))
    return jnp.concatenate(outs, axis=2)


def conformer_conv(u, b_glu, w_dw, b_dw, ln_g, ln_b):
    u = u + b_glu
    a, gate = jnp.split(u, 2, axis=-1)
    z = a * jax.nn.sigmoid(gate)
    zp = jnp.pad(z, ((0, 0), (CONV_WIDTH - 1, 0), (0, 0)))
    z = lax.conv_general_dilated(zp, w_dw[:, None, :], window_strides=(1,), padding='VALID',
                                 dimension_numbers=('NWC', 'WIO', 'NWC'),
                                 feature_group_count=D_CONV) + b_dw
    z = layernorm(z, ln_g, ln_b)
    return jax.nn.silu(z)


def parallel_mixer(h, w_in, b_forget, b_glu, w_dw, b_dw, conv_ln_g, conv_ln_b, attn_out_g, w_out):
    B, S, _ = h.shape
    proj = h @ w_in
    def heads(t):
        return t.reshape(B, S, ATTN_HEADS, HEAD_DIM).transpose(0, 2, 1, 3)
    q = heads(proj[..., OFF_Q:OFF_K])
    k = heads(proj[..., OFF_K:OFF_V])
    v = heads(proj[..., OFF_V:OFF_F])
    log_f = jax.nn.log_sigmoid((proj[..., OFF_F:OFF_C] + b_forget).astype(jnp.float32))
    log_f = log_f.transpose(0, 2, 1)
    attn = forgetting_attention(q, k, v, log_f)
    attn = attn.transpose(0, 2, 1, 3).reshape(B, S, D_ATTN)
    attn = rmsnorm(attn, attn_out_g)
    conv = conformer_conv(proj[..., OFF_C:], b_glu, w_dw, b_dw, conv_ln_g, conv_ln_b)
    return jnp.concatenate([attn, conv], axis=-1) @ w_out


def memory_cross_attention(h, mem_n, w_mq, w_mkv, w_mo):
    B, S, _ = h.shape
    q = (h @ w_mq).reshape(B, S, MEM_HEADS, MEM_HEAD_DIM)
    kv = mem_n @ w_mkv
    k, v = jnp.split(kv, 2, axis=-1)
    k = k.reshape(B, -1, MEM_HEADS, MEM_HEAD_DIM)
    v = v.reshape(B, -1, MEM_HEADS, MEM_HEAD_DIM)
    s = jnp.einsum('bshd,bmhd->bhsm', q, k).astype(jnp.float32) / math.sqrt(MEM_HEAD_DIM)
    p = jax.nn.softmax(s, axis=-1).astype(v.dtype)
    o = jnp.einsum('bhsm,bmhd->bshd', p, v).reshape(B, S, D_MODEL)
    return o @ w_mo


def hierarchical_moe(h, w_route_group, b_route_group, w_route_expert, b_route_expert,
                     w_gate, w_up, w_down):
    B, S, D = h.shape
    t = h.reshape(-1, D)
    p_group = jax.nn.softmax((t @ w_route_group).astype(jnp.float32) + b_route_group, axis=-1)
    g_top, g_idx = lax.top_k(p_group, 1)
    e_logits = jnp.einsum('td,gde->tge', t, w_route_expert).astype(jnp.float32) + b_route_expert
    e_logits = jnp.take_along_axis(e_logits, g_idx[:, :, None], axis=1)[:, 0]
    p_exp = jax.nn.softmax(e_logits, axis=-1)
    e_top, e_idx = lax.top_k(p_exp, TOP_K_IN_GROUP)
    w = g_top * e_top / jnp.sum(e_top, axis=-1, keepdims=True)
    ids = g_idx * EXPERTS_PER_GROUP + e_idx
    gates = jnp.sum(jax.nn.one_hot(ids, N_EXPERTS, dtype=jnp.float32) * w[..., None], axis=1)
    gates = gates.astype(h.dtype).T

    def expert_step(y, args):
        wg, wu, wd, ge = args
        out = (jax.nn.silu(t @ wg) * (t @ wu)) @ wd
        return y + ge[:, None] * out, None

    y, _ = lax.scan(expert_step, jnp.zeros_like(t), (w_gate, w_up, w_down, gates))
    return y.reshape(B, S, D)


def setup_inputs(seed: int = 0) -> dict:
    key = jax.random.key(seed)
    ks = iter(jax.random.split(key, 32))
    f32 = jnp.float32

    def nrm(shape, fan_in):
        return jax.random.normal(next(ks), shape, f32) * (fan_in ** -0.5)

    def gain(shape):
        return 1.0 + 0.02 * jax.random.normal(next(ks), shape, f32)

    def small(shape):
        return 0.01 * jax.random.normal(next(ks), shape, f32)

    L = DEPTH
    return {
        "x": jax.random.normal(next(ks), (BATCH, SEQ, D_MODEL), f32),
        "mem": jax.random.normal(next(ks), (BATCH, N_MEM, D_MODEL), f32),
        "norm_mix_g": gain((L, D_MODEL)),
        "w_in": nrm((L, D_MODEL, D_IN), D_MODEL),
        "b_forget": jax.random.uniform(next(ks), (L, ATTN_HEADS), f32, 1.0, 6.0),
        "b_glu": small((L, 2 * D_CONV)),
        "w_dw": nrm((L, CONV_WIDTH, D_CONV), CONV_WIDTH),
        "b_dw": small((L, D_CONV)),
        "conv_ln_g": gain((L, D_CONV)),
        "conv_ln_b": small((L, D_CONV)),
        "attn_out_g": gain((L, D_ATTN)),
        "w_out": nrm((L, D_ATTN + D_CONV, D_MODEL), D_ATTN + D_CONV),
        "norm_mem_g": gain((L, D_MODEL)),
        "mem_norm_g": gain((L, D_MODEL)),
        "w_mq": nrm((L, D_MODEL, D_MODEL), D_MODEL),
        "w_mkv": nrm((L, D_MODEL, 2 * D_MODEL), D_MODEL),
        "w_mo": nrm((L, D_MODEL, D_MODEL), D_MODEL),
        "norm_ffn_g": gain((L, D_MODEL)),
        "w_route_group": nrm((L, D_MODEL, N_GROUPS), D_MODEL),
        "b_route_group": small((L, N_GROUPS)),
        "w_route_expert": nrm((L, N_GROUPS, D_MODEL, EXPERTS_PER_GROUP), D_MODEL),
        "b_route_expert": small((L, N_GROUPS, EXPERTS_PER_GROUP)),
        "w_gate": nrm((L, N_EXPERTS, D_MODEL, D_EXPERT), D_MODEL),
        "w_up": nrm((L, N_EXPERTS, D_MODEL, D_EXPERT), D_MODEL),
        "w_down": nrm((L, N_EXPERTS, D_EXPERT, D_MODEL), D_EXPERT),
        "final_g": gain((D_MODEL,)),
    }


def reference(x, mem, norm_mix_g, w_in, b_forget, b_glu, w_dw, b_dw, conv_ln_g, conv_ln_b,
              attn_out_g, w_out, norm_mem_g, mem_norm_g, w_mq, w_mkv, w_mo, norm_ffn_g,
              w_route_group, b_route_group, w_route_expert, b_route_expert,
              w_gate, w_up, w_down, final_g):
    for l in range(DEPTH):
        h = rmsnorm(x, norm_mix_g[l])
        x = x + parallel_mixer(h, w_in[l], b_forget[l], b_glu[l], w_dw[l], b_dw[l],
                               conv_ln_g[l], conv_ln_b[l], attn_out_g[l], w_out[l])
        h = rmsnorm(x, norm_mem_g[l])
        mem_n = rmsnorm(mem, mem_norm_g[l])
        x = x + memory_cross_attention(h, mem_n, w_mq[l], w_mkv[l], w_mo[l])
        h = rmsnorm(x, norm_ffn_g[l])
        x = x + hierarchical_moe(h, w_route_group[l], b_route_group[l], w_route_expert[l],
                                 b_route_expert[l], w_gate[l], w_up[l], w_down[l])
    return rmsnorm(x, final_g)
```

```python
import numpy as np
import ml_dtypes
from concourse.bass_utils import run_bass_kernel_spmd
from contextlib import ExitStack
import concourse.bass as bass
import concourse.mybir as mybir

F32 = mybir.dt.float32
BF16 = mybir.dt.bfloat16
I32 = mybir.dt.int32
ALU = mybir.AluOpType
AF = mybir.ActivationFunctionType
AX = mybir.AxisListType
_DSZ = {F32: 4, BF16: 2, I32: 4, mybir.dt.uint32: 4, mybir.dt.float16: 2, mybir.dt.uint8: 1,
        mybir.dt.int8: 1, mybir.dt.uint16: 2, mybir.dt.int16: 2}


def region(ap):
    dsz = _DSZ[ap.dtype]
    dims = ap.ap
    off = ap.offset
    sp = str(ap.space)
    if sp in ("SB", "PSUM"):
        shp = ap.tensor.shape
        rowlen = 1
        for s in list(shp)[1:]:
            rowlen *= s
        p0 = off // rowlen
        c0 = off % rowlen
        pstep, pcnt = dims[0]
        p1 = p0 + ((pcnt - 1) * pstep) // rowlen + 1 if pstep else p0 + 1
        ext = 0
        for st, cn in dims[1:]:
            ext += (cn - 1) * abs(st)
        if sp == "PSUM":
            return (sp + ":" + ap.tensor.name, 0, 128, 0, 1 << 20)
        return (sp + ":" + ap.tensor.name, p0, p1, c0 * dsz, (c0 + ext + 1) * dsz)
    else:
        ext = 0
        for st, cn in dims:
            ext += (cn - 1) * abs(st)
        return ("D:" + ap.tensor.name, 0, 1, off * dsz, (off + ext + 1) * dsz)


class Op:
    __slots__ = ("eng", "fn", "reads", "writes", "dma", "deps", "sig", "dsem", "dval",
                 "waits", "idx", "prewait")


class Prog:
    ENGS = ("pe", "act", "dve", "pool", "sp")
    EPOCH = 30000

    def __init__(self, nc, ndma_sems=56):
        self.nc = nc
        self.ops = []
        self.recs = {}
        self.ndma = ndma_sems
        self.extra_regions = {}

    def add(self, eng, fn, reads=(), writes=(), dma=False):
        op = Op()
        op.eng = eng; op.fn = fn; op.dma = dma
        op.reads = [r if isinstance(r, tuple) else region(r) for r in reads]
        op.writes = [r if isinstance(r, tuple) else region(r) for r in writes]
        op.idx = len(self.ops)
        op.sig = None; op.dsem = None; op.dval = None; op.waits = []; op.prewait = None
        deps = set()
        for (sp, p0, p1, lo, hi) in op.reads:
            for r in self.recs.get(sp, ()):
                if r[5] and r[0] < p1 and p0 < r[1] and r[2] < hi and lo < r[3]:
                    deps.add(r[4])
        for (sp, p0, p1, lo, hi) in op.writes:
            lst = self.recs.get(sp)
            if lst is None:
                lst = self.recs[sp] = []
            keep = []
            for r in lst:
                if r[0] < p1 and p0 < r[1] and r[2] < hi and lo < r[3]:
                    deps.add(r[4])
                    if r[0] >= p0 and r[1] <= p1 and r[2] >= lo and r[3] <= hi:
                        continue
                keep.append(r)
            keep.append([p0, p1, lo, hi, op.idx, True])
            self.recs[sp] = keep
        for (sp, p0, p1, lo, hi) in op.reads:
            lst = self.recs.get(sp)
            if lst is None:
                lst = self.recs[sp] = []
            if not dma:
                for r in lst:
                    if (not r[5]) and r[0] == p0 and r[1] == p1 and r[2] == lo and r[3] == hi \
                            and (not self.ops[r[4]].dma) and self.ops[r[4]].eng == eng:
                        r[4] = op.idx
                        break
                else:
                    lst.append([p0, p1, lo, hi, op.idx, False])
            else:
                lst.append([p0, p1, lo, hi, op.idx, False])
        deps.discard(op.idx)
        op.deps = deps
        self.ops.append(op)
        return op

    def matmul(self, out, lhsT, rhs, start=True, stop=True, **kw):
        return self.add("pe", lambda e: e.matmul(out, lhsT, rhs, start=start, stop=stop, **kw),
                        reads=[lhsT, rhs], writes=[out])

    def transpose(self, out, in_, ident):
        return self.add("pe", lambda e: e.transpose(out, in_, ident), reads=[in_, ident], writes=[out])

    def activation(self, out, in_, func, bias=None, scale=None, accum_out=None, eng="act"):
        reads = [in_]
        kw = {}
        if bias is not None:
            kw["bias"] = bias
            if not isinstance(bias, (int, float)):
                reads.append(bias)
        if scale is not None:
            kw["scale"] = scale
            if not isinstance(scale, (int, float)):
                reads.append(scale)
        writes = [out]
        if accum_out is not None:
            kw["accum_out"] = accum_out
            writes.append(accum_out)
        return self.add(eng, lambda e: e.activation(out, in_, func, **kw), reads=reads, writes=writes)

    def tensor_scalar(self, out, in0, s1, s2, op0, op1=None, eng="dve", accum_out=None):
        reads = [in0]
        for s in (s1, s2):
            if s is not None and not isinstance(s, (int, float)):
                reads.append(s)
        kw = {}
        if op1 is not None:
            kw["op1"] = op1
        writes = [out]
        if accum_out is not None:
            kw["accum_out"] = accum_out
            writes.append(accum_out)
        return self.add(eng, lambda e: e.tensor_scalar(out, in0, s1, s2, op0, **kw), reads=reads, writes=writes)

    def tensor_tensor(self, out, in0, in1, op, eng="dve"):
        return self.add(eng, lambda e: e.tensor_tensor(out, in0, in1, op), reads=[in0, in1], writes=[out])

    def stt(self, out, in0, scalar, in1, op0, op1, eng="dve", accum_out=None):
        reads = [in0, in1]
        if not isinstance(scalar, (int, float)):
            reads.append(scalar)
        kw = {}
        writes = [out]
        if accum_out is not None:
            kw["accum_out"] = accum_out
            writes.append(accum_out)
        return self.add(eng, lambda e: e.scalar_tensor_tensor(out, in0, scalar, in1, op0, op1, **kw),
                        reads=reads, writes=writes)

    def copy(self, out, in_, eng="dve"):
        if eng == "act":
            return self.add(eng, lambda e: e.copy(out, in_), reads=[in_], writes=[out])
        return self.add(eng, lambda e: e.tensor_copy(out, in_), reads=[in_], writes=[out])

    def memset(self, out, val, eng="dve"):
        return self.add(eng, lambda e: e.memset(out, val), reads=[], writes=[out])

    def reduce(self, out, in_, op, axis=None, eng="dve"):
        ax = axis if axis is not None else AX.X
        return self.add(eng, lambda e: e.tensor_reduce(out, in_, ax, op), reads=[in_], writes=[out])

    def dma(self, out, in_, q="sp", **kw):
        return self.add(q, lambda e: e.dma_start(out=out, in_=in_, **kw), reads=[in_], writes=[out], dma=True)

    def finalize(self):
        ops = self.ops
        need = [False] * len(ops)
        for op in ops:
            for d in op.deps:
                p = ops[d]
                if p.dma:
                    continue
                if p.eng == op.eng and not op.dma:
                    if p.eng == "pe":
                        continue
                    continue_flag = True
                    for (sp, p0, p1, lo, hi) in op.reads:
                        for (sp2, q0, q1, lo2, hi2) in p.writes:
                            if sp == sp2 and q0 < p1 and p0 < q1 and lo2 < hi and lo < hi2:
                                continue_flag = False
                    if continue_flag:
                        continue
                need[d] = True
        cnt = {e: 0 for e in self.ENGS}
        for op in ops:
            if (not op.dma) and need[op.idx]:
                cnt[op.eng] += 1
                op.sig = cnt[op.eng]
        dcount = [0] * self.ndma
        k = 0
        for op in ops:
            if op.dma:
                s = k % self.ndma
                k += 1
                dcount[s] += 1
                op.dsem = s
                op.dval = 16 * dcount[s]
                op.prewait = (("dma", s), 16 * (dcount[s] - 1)) if dcount[s] > 1 else None
        self.dfinal = [16 * c for c in dcount]
        seen = {e: {} for e in self.ENGS}
        pos = {e: 0 for e in self.ENGS}
        opos = {}
        for op in ops:
            pos[op.eng] += 1
            opos[op.idx] = pos[op.eng]
        nw = 0
        for op in ops:
            sn = seen[op.eng]
            w = {}
            if op.prewait is not None:
                key, val = op.prewait
                if sn.get(key, 0) < val:
                    w[key] = val
            for d in op.deps:
                p = ops[d]
                if p.dma:
                    key, val = ("dma", p.dsem), p.dval
                else:
                    if p.sig is None:
                        continue
                    if p.eng == op.eng and not op.dma:
                        if p.eng == "pe":
                            continue
                        if opos[op.idx] - opos[p.idx] > 3:
                            continue
                    ep = (p.sig - 1) // self.EPOCH
                    key, val = ("eng", p.eng), ep * 100000 + (p.sig - 1) % self.EPOCH + 1
                if sn.get(key, 0) < val and w.get(key, 0) < val:
                    w[key] = val
            for key, val in w.items():
                sn[key] = val
            op.waits = list(w.items())
            nw += len(op.waits)
        self.n_waits = nw
        self.n_epochs = {e: (cnt[e] - 1) // self.EPOCH + 1 if cnt[e] else 1 for e in self.ENGS}
        return cnt

    def emit(self):
        nc = self.nc
        cnt = self.finalize()
        ops = self.ops
        with ExitStack() as st:
            esem = {}
            for e in self.ENGS:
                for ep in range(self.n_epochs[e]):
                    esem[(e, ep)] = st.enter_context(nc.semaphore("s_%s%d" % (e, ep)))
            dsem = [st.enter_context(nc.semaphore("d%d" % i)) for i in range(self.ndma)]
            block = st.enter_context(nc.Block())
            by_eng = {e: [op for op in ops if op.eng == e] for e in self.ENGS}
            EP = self.EPOCH

            def run(eng_obj, lst, is_sp):
                for op in lst:
                    for key, val in op.waits:
                        if key[0] == "dma":
                            eng_obj.wait_ge(dsem[key[1]], val)
                        else:
                            ep, v = divmod(val, 100000)
                            eng_obj.wait_ge(esem[(key[1], ep)], v)
                    inst = op.fn(eng_obj)
                    if op.dma:
                        inst.then_inc(dsem[op.dsem], 16)
                    elif op.sig is not None:
                        ep = (op.sig - 1) // EP
                        inst.then_inc(esem[(op.eng, ep)], 1)
                if is_sp:
                    for i, v in enumerate(self.dfinal):
                        if v:
                            eng_obj.wait_ge(dsem[i], v)

            @block.sync
            def _(e):
                run(e, by_eng["sp"], True)

            @block.tensor
            def _(e):
                run(e, by_eng["pe"], False)

            @block.scalar
            def _(e):
                run(e, by_eng["act"], False)

            @block.vector
            def _(e):
                run(e, by_eng["dve"], False)

            @block.gpsimd
            def _(e):
                run(e, by_eng["pool"], False)
        return cnt

S = 2048
D = 1024
NT = S // 128
NTILES = 96
TS = 256
NSLOT = NTILES * TS
GM, GQ, GK, GA, BG, BD, WDW = 0, 8, 16, 24, 28, 36, 40
NV = 40 + 4 * 31
RG, RF, RLG, RLB, RBF, RBR = 0, 1024, 2048, 2560, 3072, 3200
NR = 3236
IDF, LTRI, ONESF, THR, PCOL, CBASE, IOTA, CEPS, NCF = 0, 128, 256, 384, 480, 481, 485, 517, 525
IDB, NEGM, UST, ONEB, LTRIB, NCB = 0, 128, 256, 384, 512, 640


def _prod(s):
    r = 1
    for a in s:
        r *= a
    return r


class Arena:
    def __init__(self, nc, st, name, nbytes):
        self.t = st.enter_context(nc.sbuf_tensor(name, [128, nbytes // 4], F32))
        self.cap = nbytes
        self.off = 0

    def alloc(self, nbytes):
        off = (self.off + 63) // 64 * 64
        self.off = off + nbytes
        assert self.off <= self.cap, ("arena overflow", self.off, self.cap)
        return off

    def view(self, off, dtype, shape):
        n = _prod(shape)
        dsz = _DSZ[dtype]
        nb = (n * dsz + 3) // 4
        v = self.t[:, off // 4: off // 4 + nb]
        if dtype != F32:
            v = v.bitcast(dtype)
        v = v[:, 0:n]
        if len(shape) == 2:
            v = v.rearrange("p (a b) -> p a b", a=shape[0], b=shape[1])
        elif len(shape) == 3:
            v = v.rearrange("p (a b c) -> p a b c", a=shape[0], b=shape[1], c=shape[2])
        return v

    def new(self, dtype, shape):
        off = self.alloc(_prod(shape) * _DSZ[dtype])
        return self.view(off, dtype, shape)


import os as _os
BP = set(_os.environ.get('BPARTS', 'conv,convT,qk,mask,exp,pv,norm,tail,catT').split(','))


def build(nseq=4, debug=(), stop_after=None, n_moe_tiles=NTILES, skip_moe=False):
    nc = bass.Bass("TRN2", target_bir_lowering=False)
    NTOK = nseq * S
    NTT = NTOK // 128

    def din(name, shape, dt=F32):
        return nc.dram_tensor(name, shape, dt, kind="ExternalInput").ap()

    def scr(name, shape, dt):
        if name in debug:
            return nc.dram_tensor(name, shape, dt, kind="ExternalOutput").ap()
        return nc.dram_tensor(name, shape, dt).ap()

    x = din("x", [NTOK, D])
    mem = din("mem", [nseq * 256, D])
    w_in = din("w_in", [D, 2568])
    w_out = din("w_out", [D, D])
    w_mq = din("w_mq", [D, D])
    w_mkv = din("w_mkv", [D, 2 * D])
    w_mo = din("w_mo", [D, D])
    w_route = din("w_route", [128, 8 * 36])
    w_gate = din("w_gate", [32 * 128, 4096])
    w_up = din("w_up", [32 * 128, 4096])
    w_down = din("w_down", [32 * 512, 1024])
    vecs = din("vecs", [128, NV])
    rows = din("rows", [128, NR])
    constf = din("constf", [128, NCF])
    constb = din("constb", [128, NCB], BF16)
    consti = din("consti", [128, 64], I32)
    out = nc.dram_tensor("out", [NTOK, D], F32, kind="ExternalOutput").ap()

    w_in_bf = scr("w_in_bf", [D, 2568], BF16)
    w_out_bf = scr("w_out_bf", [D, D], BF16)
    w_mq_bf = scr("w_mq_bf", [D, D], BF16)
    w_mkv_bf = scr("w_mkv_bf", [D, 2 * D], BF16)
    w_mo_bf = scr("w_mo_bf", [D, D], BF16)
    x2s = scr("x2s", [NTOK, D], F32)
    h3s = scr("h3s", [NTOK, D], BF16)
    Ysc = scr("Ysc", [NSLOT, D], F32)
    s2t = scr("s2t", [NSLOT, 1], I32)
    dbg = {}
    for name, shape, dt in (("d_cat", [S, D], BF16), ("d_x1", [S, D], F32), ("d_lg", [NTOK, 36], F32),
                            ("d_q", [128, 4 * 2048], BF16), ("d_attn", [S, 512], F32), ("d_z", [128, 4 * 2080], BF16),
                            ("d_B", [128, 2048], F32), ("d_rt", [128, 64 * 8], F32), ("d_te", [128, 96], F32),
                            ("d_conv", [S, 512], F32)):
        if name in debug:
            dbg[name] = nc.dram_tensor(name, shape, dt, kind="ExternalOutput").ap()

    P = Prog(nc)
    st = ExitStack()
    A = Arena(nc, st, "arena", 210944)
    PS = [st.enter_context(nc.psum_tensor("ps%d" % i, [128, 512], F32)) for i in range(8)]

    def psf(i):
        return PS[i][:, :]

    def psb(i):
        return PS[i][:, :].bitcast(BF16)

    cf = A.new(F32, [NCF])
    cb = A.new(BF16, [NCB])
    ci = A.new(I32, [64])
    vv = A.new(F32, [NV])
    rA = A.new(F32, [1188])
    LG_, LB_, BF_, BR_ = 0, 512, 1024, 1152
    A1 = A.new(BF16, [64, 32])
    A2 = A.new(BF16, [64, 32])
    G1 = A.new(F32, [64]); G2 = A.new(F32, [64]); P1 = A.new(F32, [64]); P2 = A.new(F32, [64])
    POS1 = A.new(I32, [64]); POS2 = A.new(I32, [64])
    base = A.new(F32, [32])
    wr_bf = A.new(BF16, [8, 36])
    v_ = None
    identf = cf[:, IDF:IDF + 128]
    identb = cb[:, IDB:IDB + 128]
    negm = cb[:, NEGM:NEGM + 128]
    eps1024 = cf[:, CEPS:CEPS + 1]
    eps512 = cf[:, CEPS + 1:CEPS + 2]
    one_c = cf[:, CEPS + 2:CEPS + 3]
    epsln = cf[:, CEPS + 3:CEPS + 4]
    P.dma(cf, constf); P.dma(cb, constb); P.dma(ci, consti); P.dma(vv, vecs)
    P.dma(rA, rows[:, RLG:RLG + 1188])
    P.memset(base, 0.0)
    P.tensor_scalar(vv[:, GM:GM + 24], vv[:, GM:GM + 24], 32.0, None, ALU.mult)
    P.tensor_scalar(vv[:, GA:GA + 4], vv[:, GA:GA + 4], float(np.sqrt(512.0)), None, ALU.mult)
    mark_persist = A.off

    Wreg_off = A.alloc(49152)
    Win = A.view(Wreg_off, BF16, [8, 2568])
    Woqm = A.view(Wreg_off, BF16, [3, 8, 1024])
    xT = A.new(BF16, [8, 2048])
    K2T = A.new(BF16, [8, 256])
    V2 = A.new(BF16, [2, 4, 257])
    Bt = A.new(F32, [8, 16, 16])
    nlf = A.new(F32, [16, 8]); incl = A.new(F32, [16, 8]); Cc = A.new(F32, [16, 8]); Cend = A.new(F32, [16, 8])
    U_off = A.alloc(66048 + 64)
    qT = A.view(U_off, BF16, [4, 2048])
    kT = A.view(U_off + 16384, BF16, [4, 2048])
    Wkv = A.view(U_off, BF16, [8, 2048])
    vS = A.view(U_off + 32768, BF16, [16, 8, 65])
    zT = A.view(U_off + 32768 + 16640, BF16, [4, 2080])
    memT = A.view(U_off + 32768 + 16640, BF16, [8, 256])
    T_off = A.alloc(24320)
    xin = [A.view(T_off + i * 4096, F32, [1024]) for i in range(2)]
    xn = [A.view(T_off + 8192 + i * 2048, BF16, [1024]) for i in range(2)]
    sig = [A.view(T_off + 12288 + i * 2048, F32, [512]) for i in range(2)]
    stA = A.view(T_off + 16384, F32, [16])
    PT = [A.view(T_off + i * 256, BF16, [128]) for i in range(6)]
    attn = A.view(T_off + 1536, F32, [512])
    cat = [A.view(T_off + 3584 + i * 2048, BF16, [1024]) for i in range(2)]
    cacc = A.view(T_off + 7680, F32, [1, 512])
    chi = A.view(T_off + 9728, BF16, [4, 512])
    clo = A.view(T_off + 13824, BF16, [4, 512])
    cz = [A.view(T_off + 17920 + i * 2048, F32, [512]) for i in range(2)]
    stB = A.view(T_off + 22016, F32, [64])
    ctmp = A.view(T_off + 22272, F32, [512])
    o_ = U_off
    x1 = A.view(o_, F32, [2, 1024]); o_ += 8192
    xr = [A.view(o_ + i * 4096, F32, [1024]) for i in range(2)]; o_ += 8192
    xn2 = [A.view(o_ + i * 2048, BF16, [1024]) for i in range(2)]; o_ += 4096
    h2T = A.view(o_, BF16, [8, 256]); o_ += 4096
    q2T = A.view(o_, BF16, [8, 256]); o_ += 4096
    P2T = [A.view(o_ + i * 512, BF16, [256]) for i in range(8)]; o_ += 4096
    oo = A.view(o_, BF16, [2, 1024]); o_ += 4096
    oT = A.view(o_, BF16, [8, 256]); o_ += 4096
    x2t = [A.view(o_ + i * 4096, F32, [1024]) for i in range(2)]; o_ += 8192
    h3t = [A.view(o_ + i * 2048, BF16, [1024]) for i in range(2)]; o_ += 4096
    h3T = A.view(o_, BF16, [8, 128]); o_ += 2048
    rG = A.view(o_, F32, [1024]); o_ += 4096
    rt = A.view(o_, F32, [256]); o_ += 1024
    rtb = A.view(o_, BF16, [64]); o_ += 128
    stC = A.view(o_, F32, [32]); o_ += 128
    assert o_ <= U_off + 66048

    st0 = [A.view(U_off + i * 15488, F32, [2568]) for i in range(2)]
    st0b = [A.view(U_off + 10304 + i * 15488, BF16, [2568]) for i in range(2)]
    k = 0
    for (src, dst, ncol, gcol, nsc) in ((w_in, w_in_bf, 2568, GM, 8), (w_mkv, w_mkv_bf, 2048, GK, 8),
                                       (w_mq, w_mq_bf, 1024, GQ, 8), (w_out, w_out_bf, 1024, GA, 4),
                                       (w_mo, w_mo_bf, 1024, None, 0)):
        for c in range(8):
            sb_, sbb = st0[k % 2], st0b[k % 2]
            P.dma(sb_[:, 0:ncol], src[c * 128:(c + 1) * 128, :])
            eng = "dve" if k % 2 == 0 else "pool"
            if c < nsc:
                P.tensor_scalar(sbb[:, 0:ncol], sb_[:, 0:ncol], vv[:, gcol + c:gcol + c + 1], None, ALU.mult, eng=eng)
            else:
                P.copy(sbb[:, 0:ncol], sb_[:, 0:ncol], eng=eng)
            P.dma(dst[c * 128:(c + 1) * 128, :], sbb[:, 0:ncol])
            k += 1
    P.dma(st0[0][:, 0:288], w_route)
    P.copy(wr_bf, st0[0][:, 0:288].rearrange("p (c n) -> p c n", c=8))

    if stop_after == "phase0":
        P.dma(out[0:128, :], st0[0][:, 0:1024])
        P.emit()
        return nc
    bank = [0]

    def nb():
        bank[0] = (bank[0] + 1) % 8
        return bank[0]

    def rstd_from_ss(dst, ss, epsc):
        P.activation(dst, ss, AF.Ln, bias=epsc, scale=1.0)
        P.activation(dst, dst, AF.Exp, scale=-0.5)


    def recip(o, i, eng="dve"):
        P.add(eng, lambda e: e.reciprocal(o, i), reads=[i], writes=[o])

    def route_tile(gi, pl, tt):
        lg = rt[:, 0:36]
        P.tensor_tensor(lg, pl, rA[:, BR_:BR_ + 36], ALU.add)
        if "d_lg" in dbg:
            P.dma(dbg["d_lg"][gi * 128:(gi + 1) * 128, :], lg)
        gmax, ngmax, gsum, gtop = rt[:, 40:41], rt[:, 41:42], rt[:, 42:43], rt[:, 43:44]
        ohg = rt[:, 44:48]
        P.reduce(gmax, lg[:, 0:4], ALU.max)
        P.tensor_scalar(ohg, lg[:, 0:4], gmax, None, ALU.is_equal)
        P.tensor_scalar(ngmax, gmax, -1.0, None, ALU.mult)
        P.activation(rt[:, 48:52], lg[:, 0:4], AF.Exp, bias=ngmax, scale=1.0, accum_out=gsum)
        recip(gtop, gsum)
        esel = rt[:, 52:60]
        P.tensor_scalar(esel, lg[:, 4:12], ohg[:, 0:1], None, ALU.mult)
        for g in range(1, 4):
            P.stt(esel, lg[:, 4 + 8 * g:12 + 8 * g], ohg[:, g:g + 1], esel, ALU.mult, ALU.add)
        top8 = rt[:, 60:68]
        P.add("dve", lambda e: e.max(top8, esel), reads=[esel], writes=[top8])
        oh1, oh2 = rt[:, 68:76], rt[:, 76:84]
        P.tensor_scalar(oh1, esel, top8[:, 0:1], None, ALU.is_equal)
        P.tensor_scalar(oh2, esel, top8[:, 1:2], None, ALU.is_equal)
        dd, w1 = rt[:, 84:85], rt[:, 85:86]
        P.tensor_tensor(dd, top8[:, 1:2], top8[:, 0:1], ALU.subtract)
        P.activation(dd, dd, AF.Exp)
        P.tensor_scalar(dd, dd, 1.0, None, ALU.add)
        recip(w1, dd)
        P.tensor_tensor(G1[:, gi:gi + 1], gtop, w1, ALU.mult)
        P.tensor_tensor(G2[:, gi:gi + 1], gtop, G1[:, gi:gi + 1], ALU.subtract)
        a1, a2 = rt[:, 96:128], rt[:, 128:160]
        for a_, oh in ((a1, oh1), (a2, oh2)):
            P.tensor_tensor(a_.rearrange("p (g e) -> p g e", g=4), ohg.unsqueeze(2).to_broadcast([128, 4, 8]),
                            oh.unsqueeze(1).to_broadcast([128, 4, 8]), ALU.mult)
        P.copy(A1[:, gi, :], a1)
        P.copy(A2[:, gi, :], a2)
        cmb = rtb[:, 0:32]
        P.tensor_tensor(cmb, a1, a2, ALU.add)
        pr = psf(7)[:, 384 + 64 * tt:448 + 64 * tt]
        P.matmul(pr[:, 0:32], cb[:, UST:UST + 128], cmb)
        P.matmul(pr[:, 32:64], cb[:, ONEB:ONEB + 128], cmb)
        rk, jk = rt[:, 160:192], rt[:, 192:224]
        P.tensor_tensor(rk, pr[:, 0:32], base, ALU.add)
        P.tensor_tensor(jk, a1, rk, ALU.mult)
        P.reduce(P1[:, gi:gi + 1], jk, ALU.add)
        P.tensor_tensor(jk, a2, rk, ALU.mult)
        P.reduce(P2[:, gi:gi + 1], jk, ALU.add)
        P.tensor_tensor(base, base, pr[:, 32:64], ALU.add)

    def sumsq(junk, src, ss):
        P.activation(junk, src, AF.Square, accum_out=ss)

    def transposeT(src_bf, dstT, col0, ev, b=None):
        if b is None:
            b = 6 + (nb() % 2)
        pv = psb(b)
        for c in range(8):
            P.transpose(pv[:, c * 128:(c + 1) * 128], src_bf[:, c * 128:(c + 1) * 128], identb)
        P.copy(dstT[:, :, col0:col0 + 128], pv.rearrange("p (c t) -> p c t", c=8), eng=ev)


    for b in range(nseq):
        tok0 = b * S
        for c in range(8):
            P.dma(Wkv[:, c, :], w_mkv_bf[c * 128:(c + 1) * 128, :])
        for mt in range(2):
            xi, xb = xin[mt], xn[mt]
            P.dma(xi, mem[b * 256 + mt * 128: b * 256 + (mt + 1) * 128, :])
            ss, rs = stA[:, 2 * mt:2 * mt + 1], stA[:, 2 * mt + 1:2 * mt + 2]
            sumsq(xb, xi, ss)
            rstd_from_ss(rs, ss, eps1024)
            P.tensor_scalar(xb, xi, rs, None, ALU.mult)
            transposeT(xb, memT, mt * 128, "dve")
        for cc in range(8):
            bk = nb() % 6
            pv = psf(bk)[:, 0:256]
            for c in range(8):
                P.matmul(pv, Wkv[:, c, cc * 128:(cc + 1) * 128], memT[:, c, :], start=(c == 0), stop=(c == 7))
            P.copy(K2T[:, cc, :], pv, eng="act" if cc % 2 else "dve")
        for mt in range(2):
            for nh in range(2):
                bk = nb() % 6
                pv = psf(bk)
                for c in range(8):
                    P.matmul(pv, memT[:, c, mt * 128:(mt + 1) * 128], Wkv[:, c, 1024 + nh * 512:1024 + (nh + 1) * 512],
                             start=(c == 0), stop=(c == 7))
                P.copy(V2[:, mt, 2 * nh:2 * nh + 2, 0:256], pv.rearrange("p (h d) -> p h d", h=2),
                       eng="act" if nh % 2 else "dve")
        P.memset(V2[:, :, :, 256:257], 1.0)
        if stop_after == "A0":
            P.dma(out[0:128, :], xin[0])
            P.emit()
            return nc
        for c in range(8):
            P.dma(Win[:, c, :], w_in_bf[c * 128:(c + 1) * 128, :])
        for i in range(NT):
            xi, xb = xin[i % 2], xn[i % 2]
            P.dma(xi, x[tok0 + i * 128: tok0 + (i + 1) * 128, :])
            ss, rs = stA[:, 4 + 2 * (i % 2):5 + 2 * (i % 2)], stA[:, 5 + 2 * (i % 2):6 + 2 * (i % 2)]
            sumsq(xb, xi, ss)
            rstd_from_ss(rs, ss, eps1024)
            P.tensor_scalar(xb, xi, rs, None, ALU.mult)
            transposeT(xb, xT, i * 128, "act" if i % 2 else "dve")
        if stop_after == "A1":
            P.dma(out[0:128, :], xin[0])
            P.emit()
            return nc
        P.memset(zT[:, :, 0:30], 0.0)
        P.memset(zT[:, :, 2078:2080], 0.0)
        P.memset(vS[:, :, :, 64:65], 1.0)
        OFF_K, OFF_V, OFF_F, OFF_C = 512, 1024, 1536, 1544
        for tb in range(4):
            tsl = slice(tb * 512, (tb + 1) * 512)
            for cc in range(8):
                bk = nb() % 6
                pv = psf(bk)
                for c in range(8):
                    P.matmul(pv, Win[:, c, cc * 128:(cc + 1) * 128], xT[:, c, tsl], start=(c == 0), stop=(c == 7))
                dst = qT[:, cc, tsl] if cc < 4 else kT[:, cc - 4, tsl]
                P.copy(dst, pv, eng="act" if cc % 2 else "dve")
            for i4 in range(4):
                bg = nb() % 6
                pg = psf(bg)
                for c in range(8):
                    P.matmul(pg, Win[:, c, OFF_C + 512 + i4 * 128:OFF_C + 512 + (i4 + 1) * 128], xT[:, c, tsl],
                             start=(c == 0), stop=(c == 7))
                sg = sig[i4 % 2]
                P.activation(sg, pg, AF.Sigmoid, bias=vv[:, BG + 4 + i4:BG + 5 + i4], scale=1.0)
                ba = nb() % 6
                pa = psf(ba)
                for c in range(8):
                    P.matmul(pa, Win[:, c, OFF_C + i4 * 128:OFF_C + (i4 + 1) * 128], xT[:, c, tsl],
                             start=(c == 0), stop=(c == 7))
                P.stt(zT[:, i4, 30 + tb * 512:30 + (tb + 1) * 512], pa, vv[:, BG + i4:BG + i4 + 1], sg, ALU.add, ALU.mult)
        pf_ = psf(6)
        for i in range(NT):
            bk = nb() % 6
            pv = psf(bk)
            for c in range(8):
                P.matmul(pv, xT[:, c, i * 128:(i + 1) * 128], Win[:, c, OFF_V:OFF_V + 512], start=(c == 0), stop=(c == 7))
            P.copy(vS[:, i, :, 0:64], pv.rearrange("p (h d) -> p h d", h=8), eng="act" if i % 2 else "dve")
            for c in range(8):
                P.matmul(pf_[:, i * 8:(i + 1) * 8], xT[:, c, i * 128:(i + 1) * 128], Win[:, c, OFF_F:OFF_F + 8],
                         start=(c == 0), stop=(c == 7))
        if stop_after == "A2":
            P.dma(out[0:128, :], xin[0])
            P.emit()
            return nc
        nlf2 = nlf.rearrange("p a b -> p (a b)")
        P.tensor_tensor(nlf2, pf_[:, 0:128], rA[:, BF_:BF_ + 128], ALU.add)
        P.activation(nlf2, nlf2, AF.Exp, scale=-1.0)
        P.activation(nlf2, nlf2, AF.Ln, bias=one_c, scale=1.0)
        if stop_after == "A3a":
            P.dma(out[0:128, :], xin[0])
            P.emit()
            return nc
        P.copy(incl[:, 0, :], nlf[:, 0, :])
        for i in range(1, NT):
            P.tensor_tensor(incl[:, i, :], incl[:, i - 1, :], nlf[:, i, :], ALU.add)
        if stop_after == "A3b":
            P.dma(out[0:128, :], xin[0])
            P.emit()
            return nc
        pc1 = psf(7)[:, 0:128]
        pc2 = psf(7)[:, 128:256]
        for (pdst, lmat, srcf) in ((pc1, cb[:, ONEB:ONEB + 128], incl.rearrange("p a b -> p (a b)")),
                                   (pc2, cb[:, LTRIB:LTRIB + 128], nlf2)):
            rres = cz[0][:, 0:128]
            for t3 in range(3):
                piece = xn[0][:, t3 * 128:(t3 + 1) * 128]
                P.copy(piece, srcf if t3 == 0 else rres)
                if t3 < 2:
                    P.tensor_tensor(rres, srcf if t3 == 0 else rres, piece, ALU.subtract)
                P.matmul(pdst, lmat, piece, start=(t3 == 0), stop=(t3 == 2))
        P.copy(Cend.rearrange("p a b -> p (a b)"), pc1)
        P.copy(Cc.rearrange("p a b -> p (a b)"), pc2)
        if stop_after == "A3c":
            P.dma(out[0:128, :], xin[0])
            P.emit()
            return nc
        P.tensor_tensor(Cc[:, 1:16, :], Cc[:, 1:16, :], Cend[:, 0:15, :], ALU.add)
        if stop_after == "A3d":
            P.dma(out[0:128, :], xin[0])
            P.emit()
            return nc
        for h in range(8):
            P.tensor_tensor(Bt[:, h, :, :], Cc[:, :, h:h + 1].to_broadcast([128, 16, 16]),
                            Cend[:, :, h].unsqueeze(1).to_broadcast([128, 16, 16]), ALU.subtract)
        if "d_q" in dbg:
            P.dma(dbg["d_q"], qT.rearrange("p a b -> p (a b)"))
        if "d_z" in dbg:
            P.dma(dbg["d_z"], zT.rearrange("p a b -> p (a b)"))
        if "d_B" in dbg:
            P.dma(dbg["d_B"], Bt.rearrange("p a b c -> p (a b c)"))
        if stop_after == "A3":
            P.dma(out[0:128, :], xin[0])
            P.emit()
            return nc
        for wi, wsrc in enumerate((w_out_bf, w_mq_bf, w_mo_bf)):
            for c in range(8):
                P.dma(Woqm[:, wi, c, :], wsrc[c * 128:(c + 1) * 128, :])

        catT = xT
        LAG = 3
        for j in range(NT):
            if j % 4 == 0 and 'conv' in BP:
                c0 = j * 128
                for i4 in range(4):
                    acc = cacc[:, 0, :]
                    P.tensor_scalar(acc, zT[:, i4, c0:c0 + 512], vv[:, WDW + i4 * 31:WDW + i4 * 31 + 1],
                                    vv[:, BD + i4:BD + i4 + 1], ALU.mult, ALU.add, eng="pool")
                    for kk in range(1, 31):
                        P.tensor_scalar(ctmp, zT[:, i4, c0 + kk:c0 + kk + 512],
                                        vv[:, WDW + i4 * 31 + kk:WDW + i4 * 31 + kk + 1], None, ALU.mult, eng="pool")
                        P.tensor_tensor(acc, acc, ctmp, ALU.add, eng="pool")
                    P.copy(chi[:, i4, :], acc, eng="pool")
                    P.tensor_tensor(ctmp, acc, chi[:, i4, :], ALU.subtract, eng="pool")
                    P.copy(clo[:, i4, :], ctmp, eng="pool")
            pcv = psf(6)
            for i4 in range(4 if 'convT' in BP else 0):
                tsl_ = slice((j % 4) * 128, (j % 4 + 1) * 128)
                P.matmul(pcv[:, i4 * 128:(i4 + 1) * 128], chi[:, i4, tsl_], identb, start=True, stop=False)
                P.matmul(pcv[:, i4 * 128:(i4 + 1) * 128], clo[:, i4, tsl_], identb, start=False, stop=True)
            accb = [psf(2), psf(3)]
            pairs = [(h, kt) for h in range(8) for kt in range(j + 1)]
            stslots = {}
            n_pairs = len(pairs)

            def emit_qk(n):
                h, kt = pairs[n]
                stv = psf((0, 1, 4, 5)[n % 4])[:, 0:128]
                r0 = 0 if 'evenonly' in BP else (h % 2) * 64
                if 'qk' in BP:
                    P.matmul(stv, kT[r0:r0 + 64, h // 2, kt * 128:(kt + 1) * 128], qT[r0:r0 + 64, h // 2, j * 128:(j + 1) * 128],
                             start=True, stop=(kt != j or 'mask' not in BP))
                if kt == j and 'mask' in BP:
                    P.matmul(stv, identb, negm, start=False, stop=True)
                pt = PT[n % 6]
                if 'exp' in BP:
                    if 'expdve' in BP:
                        P.copy(pt, stv, eng='dve')
                    elif 'expcopy' in BP:
                        P.copy(pt, stv, eng='act')
                    elif 'nobias' in BP:
                        P.activation(pt, stv, AF.Exp, scale=0.125)
                    else:
                        P.activation(pt, stv, AF.Exp, bias=Bt[:, h, kt, j:j + 1], scale=0.125)

            def emit_pv(n):
                h, kt = pairs[n]
                pt = PT[n % 6]
                av = accb[h // 4][:, (h % 4) * 65:(h % 4) * 65 + 65]
                if 'pv' in BP:
                    P.matmul(av, pt, vS[:, kt, h, :], start=(kt == 0), stop=(kt == j))

            for n in range(n_pairs + LAG):
                if n < n_pairs:
                    emit_qk(n)
                if n >= LAG:
                    emit_pv(n - LAG)
            ct = cat[j % 2]
            rden = stB[:, 0:8]
            for hh in range(2):
                av = accb[hh][:, 0:260].rearrange("p (h d) -> p h d", h=4)
                P.add("dve", lambda e, o=rden[:, hh * 4:(hh + 1) * 4], i=av[:, :, 64]: e.reciprocal(o, i),
                      reads=[av[:, :, 64]], writes=[rden[:, hh * 4:(hh + 1) * 4]])
                P.tensor_tensor(attn[:, hh * 256:(hh + 1) * 256].rearrange("p (h d) -> p h d", h=4), av[:, :, 0:64],
                                rden[:, hh * 4:(hh + 1) * 4].unsqueeze(2).to_broadcast([128, 4, 64]), ALU.mult)
            ssa, rsa = stB[:, 8:9], stB[:, 9:10]
            sumsq(cz[0], attn, ssa)
            rstd_from_ss(rsa, ssa, eps512)
            P.tensor_scalar(ct[:, 0:512], attn, rsa, None, ALU.mult)
            if "d_attn" in dbg:
                P.dma(dbg["d_attn"][j * 128:(j + 1) * 128, :], attn)
            s1, nm, sq, rsd = stB[:, 10:11], stB[:, 11:12], stB[:, 12:13], stB[:, 13:14]
            xc, tmp = cz[0], cz[1]
            if "d_conv" in dbg:
                P.copy(tmp, pcv)
                P.dma(dbg["d_conv"][j * 128:(j + 1) * 128, :], tmp)
            P.reduce(s1, pcv, ALU.add)
            P.tensor_scalar(nm, s1, -1.0 / 512.0, None, ALU.mult)
            P.tensor_scalar(xc, pcv, nm, None, ALU.add)
            sumsq(tmp, xc, sq)
            P.activation(rsd, sq, AF.Ln, bias=epsln, scale=1.0 / 512.0)
            P.activation(rsd, rsd, AF.Exp, scale=-0.5)
            P.stt(xc, xc, rsd, rA[:, LG_:LG_ + 512], ALU.mult, ALU.mult)
            P.tensor_tensor(xc, xc, rA[:, LB_:LB_ + 512], ALU.add)
            P.activation(tmp, xc, AF.Exp, scale=-1.0)
            P.tensor_scalar(tmp, tmp, 1.0, None, ALU.add, eng="pool")
            P.add("dve", lambda e, o=tmp: e.reciprocal(o, o), reads=[tmp], writes=[tmp])
            P.tensor_tensor(ct[:, 512:1024], xc, tmp, ALU.mult)
            if "d_cat" in dbg:
                P.dma(dbg["d_cat"][j * 128:(j + 1) * 128, :], ct)
            transposeT(ct, catT, j * 128, "act" if j % 2 else "dve", b=7)

        if stop_after == "B":
            P.dma(out[0:128, :], xin[0])
            P.emit()
            return nc
        P.dma(rG, rows[:, RG:RG + 1024])
        P.tensor_scalar(rG, rG, 32.0, None, ALU.mult)
        Wout, Wmq, Wmo = Woqm[:, 0], Woqm[:, 1], Woqm[:, 2]
        for blk in range(NT // 2):
            for tt in range(2):
                i = blk * 2 + tt
                P.dma(xr[tt], x[tok0 + i * 128: tok0 + (i + 1) * 128, :])
                for nh in range(2):
                    pv = psf(nb() % 6)
                    for c in range(8):
                        P.matmul(pv, catT[:, c, i * 128:(i + 1) * 128], Wout[:, c, nh * 512:(nh + 1) * 512],
                                 start=(c == 0), stop=(c == 7))
                    P.tensor_tensor(x1[:, tt, nh * 512:(nh + 1) * 512], pv, xr[tt][:, nh * 512:(nh + 1) * 512], ALU.add)
                if "d_x1" in dbg:
                    P.dma(dbg["d_x1"][i * 128:(i + 1) * 128, :], x1[:, tt, :])
                ss, rs = stC[:, 2 * tt:2 * tt + 1], stC[:, 2 * tt + 1:2 * tt + 2]
                sumsq(xn2[tt], x1[:, tt, :], ss)
                rstd_from_ss(rs, ss, eps1024)
                P.tensor_scalar(xn2[tt], x1[:, tt, :], rs, None, ALU.mult)
                transposeT(xn2[tt], h2T, tt * 128, "act" if tt else "dve")
            for cc in range(8):
                pv = psf(nb() % 6)[:, 0:256]
                for c in range(8):
                    P.matmul(pv, Wmq[:, c, cc * 128:(cc + 1) * 128], h2T[:, c, :], start=(c == 0), stop=(c == 7))
                P.copy(q2T[:, cc, :], pv, eng="act" if cc % 2 else "dve")
            for h in range(4):
                for mt in range(2):
                    pv = psf(nb() % 6)[:, 0:256]
                    for k2 in range(2):
                        P.matmul(pv, K2T[:, 2 * h + k2, mt * 128:(mt + 1) * 128], q2T[:, 2 * h + k2, :],
                                 start=(k2 == 0), stop=(k2 == 1))
                    P.activation(P2T[(h % 2) * 2 + mt + 4 * (blk % 2)], pv, AF.Exp, scale=1.0 / 16.0)
                for tt in range(2):
                    pa = psf(nb() % 6)[:, 0:257]
                    for mt in range(2):
                        P.matmul(pa, P2T[(h % 2) * 2 + mt + 4 * (blk % 2)][:, tt * 128:(tt + 1) * 128], V2[:, mt, h, :],
                                 start=(mt == 0), stop=(mt == 1))
                    rd = stC[:, 8 + tt:9 + tt]
                    P.add("dve", lambda e, o=rd, i_=pa[:, 256:257]: e.reciprocal(o, i_), reads=[pa[:, 256:257]], writes=[rd])
                    P.tensor_scalar(oo[:, tt, h * 256:(h + 1) * 256], pa[:, 0:256], rd, None, ALU.mult)
            for tt in range(2):
                transposeT(oo[:, tt, :], oT, tt * 128, "act" if tt else "dve")
            for tt in range(2):
                i = blk * 2 + tt
                gi = b * NT + i
                xo = x2t[tt]
                for nh in range(2):
                    pv = psf(nb() % 6)
                    for c in range(8):
                        P.matmul(pv, oT[:, c, tt * 128:(tt + 1) * 128], Wmo[:, c, nh * 512:(nh + 1) * 512],
                                 start=(c == 0), stop=(c == 7))
                    P.tensor_tensor(xo[:, nh * 512:(nh + 1) * 512], pv, x1[:, tt, nh * 512:(nh + 1) * 512], ALU.add)
                P.dma(x2s[tok0 + i * 128: tok0 + (i + 1) * 128, :], xo)
                ss, rs = stC[:, 4 + 2 * tt:5 + 2 * tt], stC[:, 5 + 2 * tt:6 + 2 * tt]
                hb = h3t[tt]
                sumsq(hb, xo, ss)
                rstd_from_ss(rs, ss, eps1024)
                P.stt(hb.rearrange("t (c p) -> t p c", c=8), xo.rearrange("t (p c) -> t p c", c=8), rs,
                      rG.rearrange("t (p c) -> t p c", c=8), ALU.mult, ALU.mult)
                P.dma(h3s[tok0 + i * 128: tok0 + (i + 1) * 128, :], hb)
                transposeT(hb, h3T, 0, "act" if tt else "dve")
                pl = psf(7)[:, 256 + 64 * tt:256 + 64 * tt + 36]
                for c in range(8):
                    P.matmul(pl, h3T[:, c, :], wr_bf[:, c, :], start=(c == 0), stop=(c == 7))
                route_tile(gi, pl, tt)

    if "d_rt" in dbg:
        P.dma(dbg["d_rt"][:, 0:NTT], G1[:, 0:NTT]); P.dma(dbg["d_rt"][:, 64:64 + NTT], G2[:, 0:NTT])
        P.dma(dbg["d_rt"][:, 128:128 + NTT], P1[:, 0:NTT]); P.dma(dbg["d_rt"][:, 192:192 + NTT], P2[:, 0:NTT])
        P.dma(dbg["d_rt"][:, 256:288], base)
    if stop_after == "phase1":
        P.dma(out[0:128, :], x2t[0])
        P.emit()
        return nc

    A.off = mark_persist
    pcf = A.new(F32, [32]); pci = A.new(I32, [32]); offs = A.new(F32, [32]); ends = A.new(F32, [32])
    te = A.new(F32, [96]); gidx = A.new(I32, [96]); didx = A.new(I32, [96, 4]); tf = A.new(F32, [96])
    zero_i = A.new(I32, [192]); big = A.new(F32, [64, 32]); posf = A.new(F32, [64])
    P.tensor_scalar(pcf, base, 127.5, 1.0 / 256.0, ALU.add, ALU.mult)
    P.copy(pci, pcf)
    P.copy(pcf, pci)
    P.tensor_scalar(pcf, pcf, 256.0, None, ALU.mult)
    P.memset(offs[:, 0:1], 0.0)
    for e in range(1, 32):
        P.tensor_tensor(offs[:, e:e + 1], offs[:, e - 1:e], pcf[:, e - 1:e], ALU.add)
    P.tensor_tensor(ends, offs, pcf, ALU.add)
    P.memset(te, 0.0)
    for e in range(32):
        P.stt(te, cf[:, THR:THR + 96], ends[:, e:e + 1], te, ALU.is_ge, ALU.add)
    P.tensor_scalar(te, te, 31.0, None, ALU.min)
    if "d_te" in dbg:
        P.dma(dbg["d_te"], te)
    P.tensor_scalar(tf, te, 128.0, cf[:, PCOL:PCOL + 1], ALU.mult, ALU.add)
    P.copy(gidx, tf)
    for c in range(4):
        P.tensor_scalar(tf, te, 512.0, cf[:, CBASE + c:CBASE + c + 1], ALU.mult, ALU.add)
        P.copy(didx[:, :, c], tf)
    for (Ax, Px, POSx) in ((A1, P1, POS1), (A2, P2, POS2)):
        P.tensor_tensor(big[:, 0:NTT, :], Ax[:, 0:NTT, :], offs.unsqueeze(1).to_broadcast([128, NTT, 32]), ALU.mult)
        P.reduce(posf[:, 0:NTT], big[:, 0:NTT, :], ALU.add)
        P.tensor_tensor(posf[:, 0:NTT], posf[:, 0:NTT], Px[:, 0:NTT], ALU.add)
        P.copy(POSx[:, 0:NTT], posf[:, 0:NTT])
    P.memset(zero_i, 0)
    P.dma(s2t.rearrange("(p n) o -> p (n o)", p=128), zero_i)
    FAKE = "D:s2t_scatter"
    nsc = 0
    for gi in range(NTT):
        for POSx in (POS1, POS2):
            P.add("pool", lambda e, po=POSx[:, gi:gi + 1], ti=ci[:, gi:gi + 1]: e.indirect_dma_start(
                out=s2t, out_offset=bass.IndirectOffsetOnAxis(ap=po, axis=0), in_=ti, in_offset=None),
                reads=[POSx[:, gi:gi + 1], ci[:, gi:gi + 1], s2t], writes=[(FAKE, 0, 1, nsc, nsc + 1)], dma=True)
            nsc += 1

    wg = [A.new(BF16, [8, 512]) for _ in range(3)]
    wu = [A.new(BF16, [8, 512]) for _ in range(3)]
    wd = [A.new(BF16, [4, 1024]) for _ in range(3)]
    sidx = [A.new(I32, [2]) for _ in range(3)]
    hs = [A.new(BF16, [2, 1024]) for _ in range(2)]
    hTs = [A.new(BF16, [8, 256]) for _ in range(2)]
    sil = [A.new(F32, [256]) for _ in range(2)]
    hid = [A.new(BF16, [4, 256]) for _ in range(2)]
    yt = [A.new(F32, [1024]) for _ in range(3)]
    s2t_v = s2t.rearrange("(j g p) o -> j p (g o)", g=2, p=128)
    ny = 0
    for j in range(n_moe_tiles):
        k3 = j % 3
        for (wdst, wsrc) in ((wg[k3], w_gate), (wu[k3], w_up)):
            P.add("pool", lambda e, o=wdst.rearrange("p a b -> p (a b)"), s=wsrc, ix=gidx[:, j:j + 1]: e.indirect_dma_start(
                out=o, out_offset=None, in_=s, in_offset=bass.IndirectOffsetOnAxis(ap=ix, axis=0)),
                reads=[gidx[:, j:j + 1], wsrc], writes=[wdst], dma=True)
        for c in range(4):
            P.add("pool", lambda e, o=wd[k3][:, c, :], ix=didx[:, j, c:c + 1]: e.indirect_dma_start(
                out=o, out_offset=None, in_=w_down, in_offset=bass.IndirectOffsetOnAxis(ap=ix, axis=0)),
                reads=[didx[:, j, c:c + 1], w_down], writes=[wd[k3][:, c, :]], dma=True)
        P.add("sp", lambda e, o=sidx[k3], s=s2t_v[j]: e.dma_start(out=o, in_=s, allow_slow_non_contiguous=True),
              reads=[s2t_v[j], (FAKE, 0, 1, 0, 1 << 20)], writes=[sidx[k3]], dma=True)
        hsj, hTj, hidj = hs[j % 2], hTs[j % 2], hid[j % 2]
        for g in range(2):
            P.add("pool", lambda e, o=hsj[:, g, :], ix=sidx[k3][:, g:g + 1]: e.indirect_dma_start(
                out=o, out_offset=None, in_=h3s, in_offset=bass.IndirectOffsetOnAxis(ap=ix, axis=0)),
                reads=[sidx[k3][:, g:g + 1], h3s], writes=[hsj[:, g, :]], dma=True)
            transposeT(hsj[:, g, :], hTj, g * 128, "act" if g else "dve")
        for fc in range(4):
            pg = psf(nb() % 6)[:, 0:256]
            for c in range(8):
                P.matmul(pg, wg[k3][:, c, fc * 128:(fc + 1) * 128], hTj[:, c, :], start=(c == 0), stop=(c == 7))
            pu = psf(nb() % 6)[:, 0:256]
            for c in range(8):
                P.matmul(pu, wu[k3][:, c, fc * 128:(fc + 1) * 128], hTj[:, c, :], start=(c == 0), stop=(c == 7))
            sl = sil[fc % 2]
            P.activation(sl, pg, AF.Silu)
            P.tensor_tensor(hidj[:, fc, :], sl, pu, ALU.mult)
        for g in range(2):
            y_ = yt[ny % 3]
            ny += 1
            for nh in range(2):
                pv = psf(nb() % 6)
                for fc in range(4):
                    P.matmul(pv, hidj[:, fc, g * 128:(g + 1) * 128], wd[k3][:, fc, nh * 512:(nh + 1) * 512],
                             start=(fc == 0), stop=(fc == 3))
                P.copy(y_[:, nh * 512:(nh + 1) * 512], pv, eng="act" if nh else "dve")
            P.dma(Ysc[j * 256 + g * 128: j * 256 + (g + 1) * 128, :], y_)

    fg = A.new(F32, [1024])
    x2b = [A.new(F32, [1024]) for _ in range(2)]
    y1b = [A.new(F32, [1024]) for _ in range(2)]
    y2b = [A.new(F32, [1024]) for _ in range(2)]
    otb = [A.new(F32, [1024]) for _ in range(2)]
    st4 = A.new(F32, [8])
    P.dma(fg, rows[:, RF:RF + 1024])
    P.tensor_scalar(fg, fg, 32.0, None, ALU.mult)
    for gi in range(NTT):
        k2 = gi % 2
        P.dma(x2b[k2], x2s[gi * 128:(gi + 1) * 128, :])
        for (yb, POSx) in ((y1b[k2], POS1), (y2b[k2], POS2)):
            P.add("pool", lambda e, o=yb, ix=POSx[:, gi:gi + 1]: e.indirect_dma_start(
                out=o, out_offset=None, in_=Ysc, in_offset=bass.IndirectOffsetOnAxis(ap=ix, axis=0)),
                reads=[POSx[:, gi:gi + 1], Ysc], writes=[yb], dma=True)
        acc = x2b[k2]
        if skip_moe:
            pass
        else:
            P.stt(acc, y1b[k2], G1[:, gi:gi + 1], acc, ALU.mult, ALU.add)
            P.stt(acc, y2b[k2], G2[:, gi:gi + 1], acc, ALU.mult, ALU.add)
        ss, rs = st4[:, 2 * k2:2 * k2 + 1], st4[:, 2 * k2 + 1:2 * k2 + 2]
        sumsq(otb[k2], acc, ss)
        rstd_from_ss(rs, ss, eps1024)
        P.stt(otb[k2], acc, rs, fg, ALU.mult, ALU.mult)
        P.dma(out[gi * 128:(gi + 1) * 128, :], otb[k2])
    cnt = P.emit()
    st.close()
    return nc


def _consts():
    p = np.arange(128)
    cfm = np.zeros((128, NCF), np.float32)
    cfm[:, IDF:IDF + 128] = np.eye(128)
    cfm[:, LTRI:LTRI + 128] = (p[:, None] <= p[None, :])
    cfm[:, ONESF:ONESF + 128] = 1.0
    cfm[:, THR:THR + 96] = 256.0 * np.arange(96)[None, :]
    cfm[:, PCOL] = p
    cfm[:, CBASE:CBASE + 4] = np.arange(4)[None, :] * 128 + p[:, None]
    cfm[:, IOTA:IOTA + 32] = np.arange(32)[None, :]
    cfm[:, CEPS:CEPS + 4] = np.array([1024e-6, 512e-6, 1.0, 1e-6], np.float32)[None, :]
    cbm = np.zeros((128, NCB), np.float32)
    cbm[:, IDB:IDB + 128] = np.eye(128)
    cbm[:, NEGM:NEGM + 128] = np.where(p[:, None] > p[None, :], -30000.0, 0.0)
    cbm[:, UST:UST + 128] = (p[:, None] < p[None, :])
    cbm[:, ONEB:ONEB + 128] = 1.0
    cbm[:, LTRIB:LTRIB + 128] = (p[:, None] <= p[None, :])
    cim = (np.arange(64)[None, :] * 128 + p[:, None]).astype(np.int32)
    return cfm, cbm.astype(ml_dtypes.bfloat16), cim


def _host_inputs(core, nseq, x, mem, norm_mix_g, w_in, b_forget, b_glu, w_dw, b_dw, conv_ln_g, conv_ln_b,
                 attn_out_g, w_out, norm_mem_g, mem_norm_g, w_mq, w_mkv, w_mo, norm_ffn_g,
                 w_route_group, b_route_group, w_route_expert, b_route_expert, w_gate, w_up, w_down, final_g):
    f = lambda a: np.ascontiguousarray(np.asarray(a, dtype=np.float32))
    cfm, cbm, cim = _consts()
    vecs = np.zeros((128, NV), np.float32)
    vecs[:, GM:GM + 8] = f(norm_mix_g)[0].reshape(8, 128).T
    vecs[:, GQ:GQ + 8] = f(norm_mem_g)[0].reshape(8, 128).T
    vecs[:, GK:GK + 8] = f(mem_norm_g)[0].reshape(8, 128).T
    vecs[:, GA:GA + 4] = f(attn_out_g)[0].reshape(4, 128).T
    vecs[:, BG:BG + 8] = f(b_glu)[0].reshape(8, 128).T
    vecs[:, BD:BD + 4] = f(b_dw)[0].reshape(4, 128).T
    vecs[:, WDW:WDW + 124] = f(w_dw)[0].reshape(31, 4, 128).transpose(2, 1, 0).reshape(128, 124)
    rows = np.zeros((1, NR), np.float32)
    rows[0, RG:RG + 1024] = f(norm_ffn_g)[0]
    rows[0, RF:RF + 1024] = f(final_g)
    rows[0, RLG:RLG + 512] = f(conv_ln_g)[0]
    rows[0, RLB:RLB + 512] = f(conv_ln_b)[0]
    rows[0, RBF:RBF + 128] = np.tile(f(b_forget)[0], 16)
    rows[0, RBR:RBR + 4] = f(b_route_group)[0]
    rows[0, RBR + 4:RBR + 36] = f(b_route_expert)[0].reshape(32)
    rows = np.ascontiguousarray(np.broadcast_to(rows, (128, NR)))
    wr = np.concatenate([f(w_route_group)[0], f(w_route_expert)[0].transpose(1, 0, 2).reshape(1024, 32)], axis=1)
    return {
        "x": f(x[core * nseq:(core + 1) * nseq]).reshape(nseq * S, D),
        "mem": f(mem[core * nseq:(core + 1) * nseq]).reshape(nseq * 256, D),
        "w_in": f(w_in)[0], "w_out": f(w_out)[0], "w_mq": f(w_mq)[0], "w_mkv": f(w_mkv)[0], "w_mo": f(w_mo)[0],
        "w_route": np.ascontiguousarray(wr.reshape(128, 8 * 36)),
        "w_gate": f(w_gate)[0].reshape(32 * 128, 4096), "w_up": f(w_up)[0].reshape(32 * 128, 4096),
        "w_down": f(w_down)[0].reshape(32 * 512, 1024),
        "vecs": vecs, "rows": rows, "constf": cfm, "constb": cbm, "consti": cim,
    }


_NC_CACHE = {}


def kernel(**inputs):
    n_cores = 8
    nseq = 4
    if "full" not in _NC_CACHE:
        _NC_CACHE["full"] = build(nseq=nseq)
    nc = _NC_CACHE["full"]
    in_maps = [_host_inputs(c, nseq, **inputs) for c in range(n_cores)]
    res = run_bass_kernel_spmd(nc, in_maps, core_ids=list(range(n_cores)))
    outs = [np.asarray(r["out"]).reshape(nseq, S, D) for r in res.results]
    return np.concatenate(outs, axis=0).astype(np.float32)
```

```python
import numpy as np
import ml_dtypes
from concourse.bass_utils import run_bass_kernel_spmd
from contextlib import ExitStack
import concourse.bass as bass
import concourse.mybir as mybir

F32 = mybir.dt.float32
BF16 = mybir.dt.bfloat16
I32 = mybir.dt.int32
ALU = mybir.AluOpType
AF = mybir.ActivationFunctionType
AX = mybir.AxisListType
_DSZ = {F32: 4, BF16: 2, I32: 4, mybir.dt.uint32: 4, mybir.dt.float16: 2, mybir.dt.uint8: 1,
        mybir.dt.int8: 1, mybir.dt.uint16: 2, mybir.dt.int16: 2}


def region(ap):
    dsz = _DSZ[ap.dtype]
    dims = ap.ap
    off = ap.offset
    sp = str(ap.space)
    if sp in ("SB", "PSUM"):
        shp = ap.tensor.shape
        rowlen = 1
        for s in list(shp)[1:]:
            rowlen *= s
        p0 = off // rowlen
        c0 = off % rowlen
        pstep, pcnt = dims[0]
        p1 = p0 + ((pcnt - 1) * pstep) // rowlen + 1 if pstep else p0 + 1
        ext = 0
        for st, cn in dims[1:]:
            ext += (cn - 1) * abs(st)
        if sp == "PSUM":
            return (sp + ":" + ap.tensor.name, 0, 128, 0, 1 << 20)
        return (sp + ":" + ap.tensor.name, p0, p1, c0 * dsz, (c0 + ext + 1) * dsz)
    else:
        ext = 0
        for st, cn in dims:
            ext += (cn - 1) * abs(st)
        return ("D:" + ap.tensor.name, 0, 1, off * dsz, (off + ext + 1) * dsz)


class Op:
    __slots__ = ("eng", "fn", "reads", "writes", "dma", "deps", "sig", "dsem", "dval",
                 "waits", "idx", "prewait")


class Prog:
    ENGS = ("pe", "act", "dve", "pool", "sp")
    EPOCH = 30000

    def __init__(self, nc, ndma_sems=56):
        self.nc = nc
        self.ops = []
        self.recs = {}
        self.ndma = ndma_sems
        self.extra_regions = {}

    def add(self, eng, fn, reads=(), writes=(), dma=False):
        op = Op()
        op.eng = eng; op.fn = fn; op.dma = dma
        op.reads = [r if isinstance(r, tuple) else region(r) for r in reads]
        op.writes = [r if isinstance(r, tuple) else region(r) for r in writes]
        op.idx = len(self.ops)
        op.sig = None; op.dsem = None; op.dval = None; op.waits = []; op.prewait = None
        deps = set()
        for (sp, p0, p1, lo, hi) in op.reads:
            for r in self.recs.get(sp, ()):
                if r[5] and r[0] < p1 and p0 < r[1] and r[2] < hi and lo < r[3]:
                    deps.add(r[4])
        for (sp, p0, p1, lo, hi) in op.writes:
            lst = self.recs.get(sp)
            if lst is None:
                lst = self.recs[sp] = []
            keep = []
            for r in lst:
                if r[0] < p1 and p0 < r[1] and r[2] < hi and lo < r[3]:
                    deps.add(r[4])
                    if r[0] >= p0 and r[1] <= p1 and r[2] >= lo and r[3] <= hi:
                        continue
                keep.append(r)
            keep.append([p0, p1, lo, hi, op.idx, True])
            self.recs[sp] = keep
        for (sp, p0, p1, lo, hi) in op.reads:
            lst = self.recs.get(sp)
            if lst is None:
                lst = self.recs[sp] = []
            if not dma:
                for r in lst:
                    if (not r[5]) and r[0] == p0 and r[1] == p1 and r[2] == lo and r[3] == hi \
                            and (not self.ops[r[4]].dma) and self.ops[r[4]].eng == eng:
                        r[4] = op.idx
                        break
                else:
                    lst.append([p0, p1, lo, hi, op.idx, False])
            else:
                lst.append([p0, p1, lo, hi, op.idx, False])
        deps.discard(op.idx)
        op.deps = deps
        self.ops.append(op)
        return op

    def matmul(self, out, lhsT, rhs, start=True, stop=True, **kw):
        return self.add("pe", lambda e: e.matmul(out, lhsT, rhs, start=start, stop=stop, **kw),
                        reads=[lhsT, rhs], writes=[out])

    def transpose(self, out, in_, ident):
        return self.add("pe", lambda e: e.transpose(out, in_, ident), reads=[in_, ident], writes=[out])

    def activation(self, out, in_, func, bias=None, scale=None, accum_out=None, eng="act"):
        reads = [in_]
        kw = {}
        if bias is not None:
            kw["bias"] = bias
            if not isinstance(bias, (int, float)):
                reads.append(bias)
        if scale is not None:
            kw["scale"] = scale
            if not isinstance(scale, (int, float)):
                reads.append(scale)
        writes = [out]
        if accum_out is not None:
            kw["accum_out"] = accum_out
            writes.append(accum_out)
        return self.add(eng, lambda e: e.activation(out, in_, func, **kw), reads=reads, writes=writes)

    def tensor_scalar(self, out, in0, s1, s2, op0, op1=None, eng="dve", accum_out=None):
        reads = [in0]
        for s in (s1, s2):
            if s is not None and not isinstance(s, (int, float)):
                reads.append(s)
        kw = {}
        if op1 is not None:
            kw["op1"] = op1
        writes = [out]
        if accum_out is not None:
            kw["accum_out"] = accum_out
            writes.append(accum_out)
        return self.add(eng, lambda e: e.tensor_scalar(out, in0, s1, s2, op0, **kw), reads=reads, writes=writes)

    def tensor_tensor(self, out, in0, in1, op, eng="dve"):
        return self.add(eng, lambda e: e.tensor_tensor(out, in0, in1, op), reads=[in0, in1], writes=[out])

    def stt(self, out, in0, scalar, in1, op0, op1, eng="dve", accum_out=None):
        reads = [in0, in1]
        if not isinstance(scalar, (int, float)):
            reads.append(scalar)
        kw = {}
        writes = [out]
        if accum_out is not None:
            kw["accum_out"] = accum_out
            writes.append(accum_out)
        return self.add(eng, lambda e: e.scalar_tensor_tensor(out, in0, scalar, in1, op0, op1, **kw),
                        reads=reads, writes=writes)

    def copy(self, out, in_, eng="dve"):
        if eng == "act":
            return self.add(eng, lambda e: e.copy(out, in_), reads=[in_], writes=[out])
        return self.add(eng, lambda e: e.tensor_copy(out, in_), reads=[in_], writes=[out])

    def memset(self, out, val, eng="dve"):
        return self.add(eng, lambda e: e.memset(out, val), reads=[], writes=[out])

    def reduce(self, out, in_, op, axis=None, eng="dve"):
        ax = axis if axis is not None else AX.X
        return self.add(eng, lambda e: e.tensor_reduce(out, in_, ax, op), reads=[in_], writes=[out])

    def dma(self, out, in_, q="sp", **kw):
        return self.add(q, lambda e: e.dma_start(out=out, in_=in_, **kw), reads=[in_], writes=[out], dma=True)

    def finalize(self):
        ops = self.ops
        need = [False] * len(ops)
        for op in ops:
            for d in op.deps:
                p = ops[d]
                if p.dma:
                    continue
                if p.eng == op.eng and not op.dma:
                    if p.eng == "pe":
                        continue
                    continue_flag = True
                    for (sp, p0, p1, lo, hi) in op.reads:
                        for (sp2, q0, q1, lo2, hi2) in p.writes:
                            if sp == sp2 and q0 < p1 and p0 < q1 and lo2 < hi and lo < hi2:
                                continue_flag = False
                    if continue_flag:
                        continue
                need[d] = True
        cnt = {e: 0 for e in self.ENGS}
        for op in ops:
            if (not op.dma) and need[op.idx]:
                cnt[op.eng] += 1
                op.sig = cnt[op.eng]
        dcount = [0] * self.ndma
        k = 0
        for op in ops:
            if op.dma:
                s = k % self.ndma
                k += 1
                dcount[s] += 1
                op.dsem = s
                op.dval = 16 * dcount[s]
                op.prewait = (("dma", s), 16 * (dcount[s] - 1)) if dcount[s] > 1 else None
        self.dfinal = [16 * c for c in dcount]
        seen = {e: {} for e in self.ENGS}
        pos = {e: 0 for e in self.ENGS}
        opos = {}
        for op in ops:
            pos[op.eng] += 1
            opos[op.idx] = pos[op.eng]
        nw = 0
        for op in ops:
            sn = seen[op.eng]
            w = {}
            if op.prewait is not None:
                key, val = op.prewait
                if sn.get(key, 0) < val:
                    w[key] = val
            for d in op.deps:
                p = ops[d]
                if p.dma:
                    key, val = ("dma", p.dsem), p.dval
                else:
                    if p.sig is None:
                        continue
                    if p.eng == op.eng and not op.dma:
                        if p.eng == "pe":
                            continue
                        if opos[op.idx] - opos[p.idx] > 3:
                            continue
                    ep = (p.sig - 1) // self.EPOCH
                    key, val = ("eng", p.eng), ep * 100000 + (p.sig - 1) % self.EPOCH + 1
                if sn.get(key, 0) < val and w.get(key, 0) < val:
                    w[key] = val
            for key, val in w.items():
                sn[key] = val
            op.waits = list(w.items())
            nw += len(op.waits)
        self.n_waits = nw
        self.n_epochs = {e: (cnt[e] - 1) // self.EPOCH + 1 if cnt[e] else 1 for e in self.ENGS}
        return cnt

    def emit(self):
        nc = self.nc
        cnt = self.finalize()
        ops = self.ops
        with ExitStack() as st:
            esem = {}
            for e in self.ENGS:
                for ep in range(self.n_epochs[e]):
                    esem[(e, ep)] = st.enter_context(nc.semaphore("s_%s%d" % (e, ep)))
            dsem = [st.enter_context(nc.semaphore("d%d" % i)) for i in range(self.ndma)]
            block = st.enter_context(nc.Block())
            by_eng = {e: [op for op in ops if op.eng == e] for e in self.ENGS}
            EP = self.EPOCH

            def run(eng_obj, lst, is_sp):
                for op in lst:
                    for key, val in op.waits:
                        if key[0] == "dma":
                            eng_obj.wait_ge(dsem[key[1]], val)
                        else:
                            ep, v = divmod(val, 100000)
                            eng_obj.wait_ge(esem[(key[1], ep)], v)
                    inst = op.fn(eng_obj)
                    if op.dma:
                        inst.then_inc(dsem[op.dsem], 16)
                    elif op.sig is not None:
                        ep = (op.sig - 1) // EP
                        inst.then_inc(esem[(op.eng, ep)], 1)
                if is_sp:
                    for i, v in enumerate(self.dfinal):
                        if v:
                            eng_obj.wait_ge(dsem[i], v)

            @block.sync
            def _(e):
                run(e, by_eng["sp"], True)

            @block.tensor
            def _(e):
                run(e, by_eng["pe"], False)

            @block.scalar
            def _(e):
                run(e, by_eng["act"], False)

            @block.vector
            def _(e):
                run(e, by_eng["dve"], False)

            @block.gpsimd
            def _(e):
                run(e, by_eng["pool"], False)
        return cnt

S = 2048
D = 1024
NT = S // 128
NTILES = 96
TS = 256
NSLOT = NTILES * TS
GM, GQ, GK, GA, BG, BD, WDW = 0, 8, 16, 24, 28, 36, 40
NV = 40 + 4 * 31
RG, RF, RLG, RLB, RBF, RBR = 0, 1024, 2048, 2560, 3072, 3200
NR = 3236
IDF, LTRI, ONESF, THR, PCOL, CBASE, IOTA, CEPS, NCF = 0, 128, 256, 384, 480, 481, 485, 517, 525
IDB, NEGM, UST, ONEB, LTRIB, NCB = 0, 128, 256, 384, 512, 640


def _prod(s):
    r = 1
    for a in s:
        r *= a
    return r


class Arena:
    def __init__(self, nc, st, name, nbytes):
        self.t = st.enter_context(nc.sbuf_tensor(name, [128, nbytes // 4], F32))
        self.cap = nbytes
        self.off = 0

    def alloc(self, nbytes):
        off = (self.off + 63) // 64 * 64
        self.off = off + nbytes
        assert self.off <= self.cap, ("arena overflow", self.off, self.cap)
        return off

    def view(self, off, dtype, shape):
        n = _prod(shape)
        dsz = _DSZ[dtype]
        nb = (n * dsz + 3) // 4
        v = self.t[:, off // 4: off // 4 + nb]
        if dtype != F32:
            v = v.bitcast(dtype)
        v = v[:, 0:n]
        if len(shape) == 2:
            v = v.rearrange("p (a b) -> p a b", a=shape[0], b=shape[1])
        elif len(shape) == 3:
            v = v.rearrange("p (a b c) -> p a b c", a=shape[0], b=shape[1], c=shape[2])
        return v

    def new(self, dtype, shape):
        off = self.alloc(_prod(shape) * _DSZ[dtype])
        return self.view(off, dtype, shape)


import os as _os
BP = set(_os.environ.get('BPARTS', 'conv,convT,qk,mask,exp,pv,norm,tail,catT').split(','))


def build(nseq=4, debug=(), stop_after=None, n_moe_tiles=NTILES, skip_moe=False):
    nc = bass.Bass("TRN2", target_bir_lowering=False)
    NTOK = nseq * S
    NTT = NTOK // 128

    def din(name, shape, dt=F32):
        return nc.dram_tensor(name, shape, dt, kind="ExternalInput").ap()

    def scr(name, shape, dt):
        if name in debug:
            return nc.dram_tensor(name, shape, dt, kind="ExternalOutput").ap()
        return nc.dram_tensor(name, shape, dt).ap()

    x = din("x", [NTOK, D])
    mem = din("mem", [nseq * 256, D])
    w_in = din("w_in", [D, 2568])
    w_out = din("w_out", [D, D])
    w_mq = din("w_mq", [D, D])
    w_mkv = din("w_mkv", [D, 2 * D])
    w_mo = din("w_mo", [D, D])
    w_route = din("w_route", [128, 8 * 36])
    w_gate = din("w_gate", [32 * 128, 4096])
    w_up = din("w_up", [32 * 128, 4096])
    w_down = din("w_down", [32 * 512, 1024])
    vecs = din("vecs", [128, NV])
    rows = din("rows", [128, NR])
    constf = din("constf", [128, NCF])
    constb = din("constb", [128, NCB], BF16)
    consti = din("consti", [128, 64], I32)
    out = nc.dram_tensor("out", [NTOK, D], F32, kind="ExternalOutput").ap()

    w_in_bf = scr("w_in_bf", [D, 2568], BF16)
    w_out_bf = scr("w_out_bf", [D, D], BF16)
    w_mq_bf = scr("w_mq_bf", [D, D], BF16)
    w_mkv_bf = scr("w_mkv_bf", [D, 2 * D], BF16)
    w_mo_bf = scr("w_mo_bf", [D, D], BF16)
    x2s = scr("x2s", [NTOK, D], F32)
    h3s = scr("h3s", [NTOK, D], BF16)
    Ysc = scr("Ysc", [NSLOT, D], F32)
    s2t = scr("s2t", [NSLOT, 1], I32)
    dbg = {}
    for name, shape, dt in (("d_cat", [S, D], BF16), ("d_x1", [S, D], F32), ("d_lg", [NTOK, 36], F32),
                            ("d_q", [128, 4 * 2048], BF16), ("d_attn", [S, 512], F32), ("d_z", [128, 4 * 2080], BF16),
                            ("d_B", [128, 2048], F32), ("d_rt", [128, 64 * 8], F32), ("d_te", [128, 96], F32),
                            ("d_conv", [S, 512], F32)):
        if name in debug:
            dbg[name] = nc.dram_tensor(name, shape, dt, kind="ExternalOutput").ap()

    P = Prog(nc)
    st = ExitStack()
    A = Arena(nc, st, "arena", 210944)
    PS = [st.enter_context(nc.psum_tensor("ps%d" % i, [128, 512], F32)) for i in range(8)]

    def psf(i):
        return PS[i][:, :]

    def psb(i):
        return PS[i][:, :].bitcast(BF16)

    cf = A.new(F32, [NCF])
    cb = A.new(BF16, [NCB])
    ci = A.new(I32, [64])
    vv = A.new(F32, [NV])
    rA = A.new(F32, [1188])
    LG_, LB_, BF_, BR_ = 0, 512, 1024, 1152
    A1 = A.new(BF16, [64, 32])
    A2 = A.new(BF16, [64, 32])
    G1 = A.new(F32, [64]); G2 = A.new(F32, [64]); P1 = A.new(F32, [64]); P2 = A.new(F32, [64])
    POS1 = A.new(I32, [64]); POS2 = A.new(I32, [64])
    base = A.new(F32, [32])
    wr_bf = A.new(BF16, [8, 36])
    v_ = None
    identf = cf[:, IDF:IDF + 128]
    identb = cb[:, IDB:IDB + 128]
    negm = cb[:, NEGM:NEGM + 128]
    eps1024 = cf[:, CEPS:CEPS + 1]
    eps512 = cf[:, CEPS + 1:CEPS + 2]
    one_c = cf[:, CEPS + 2:CEPS + 3]
    epsln = cf[:, CEPS + 3:CEPS + 4]
    P.dma(cf, constf); P.dma(cb, constb); P.dma(ci, consti); P.dma(vv, vecs)
    P.dma(rA, rows[:, RLG:RLG + 1188])
    P.memset(base, 0.0)
    P.tensor_scalar(vv[:, GM:GM + 24], vv[:, GM:GM + 24], 32.0, None, ALU.mult)
    P.tensor_scalar(vv[:, GA:GA + 4], vv[:, GA:GA + 4], float(np.sqrt(512.0)), None, ALU.mult)
    mark_persist = A.off

    Wreg_off = A.alloc(49152)
    Win = A.view(Wreg_off, BF16, [8, 2568])
    Woqm = A.view(Wreg_off, BF16, [3, 8, 1024])
    xT = A.new(BF16, [8, 2048])
    K2T = A.new(BF16, [8, 256])
    V2 = A.new(BF16, [2, 4, 257])
    Bt = A.new(F32, [8, 16, 16])
    nlf = A.new(F32, [16, 8]); incl = A.new(F32, [16, 8]); Cc = A.new(F32, [16, 8]); Cend = A.new(F32, [16, 8])
    U_off = A.alloc(66048 + 64)
    qT = A.view(U_off, BF16, [4, 2048])
    kT = A.view(U_off + 16384, BF16, [4, 2048])
    Wkv = A.view(U_off, BF16, [8, 2048])
    vS = A.view(U_off + 32768, BF16, [16, 8, 65])
    zT = A.view(U_off + 32768 + 16640, BF16, [4, 2080])
    memT = A.view(U_off + 32768 + 16640, BF16, [8, 256])
    T_off = A.alloc(24320)
    xin = [A.view(T_off + i * 4096, F32, [1024]) for i in range(2)]
    xn = [A.view(T_off + 8192 + i * 2048, BF16, [1024]) for i in range(2)]
    sig = [A.view(T_off + 12288 + i * 2048, F32, [512]) for i in range(2)]
    stA = A.view(T_off + 16384, F32, [16])
    PT = [A.view(T_off + i * 256, BF16, [128]) for i in range(6)]
    attn = A.view(T_off + 1536, F32, [512])
    cat = [A.view(T_off + 3584 + i * 2048, BF16, [1024]) for i in range(2)]
    cacc = A.view(T_off + 7680, F32, [1, 512])
    chi = A.view(T_off + 9728, BF16, [4, 512])
    clo = A.view(T_off + 13824, BF16, [4, 512])
    cz = [A.view(T_off + 17920 + i * 2048, F32, [512]) for i in range(2)]
    stB = A.view(T_off + 22016, F32, [64])
    ctmp = A.view(T_off + 22272, F32, [512])
    o_ = U_off
    x1 = A.view(o_, F32, [2, 1024]); o_ += 8192
    xr = [A.view(o_ + i * 4096, F32, [1024]) for i in range(2)]; o_ += 8192
    xn2 = [A.view(o_ + i * 2048, BF16, [1024]) for i in range(2)]; o_ += 4096
    h2T = A.view(o_, BF16, [8, 256]); o_ += 4096
    q2T = A.view(o_, BF16, [8, 256]); o_ += 4096
    P2T = [A.view(o_ + i * 512, BF16, [256]) for i in range(8)]; o_ += 4096
    oo = A.view(o_, BF16, [2, 1024]); o_ += 4096
    oT = A.view(o_, BF16, [8, 256]); o_ += 4096
    x2t = [A.view(o_ + i * 4096, F32, [1024]) for i in range(2)]; o_ += 8192
    h3t = [A.view(o_ + i * 2048, BF16, [1024]) for i in range(2)]; o_ += 4096
    h3T = A.view(o_, BF16, [8, 128]); o_ += 2048
    rG = A.view(o_, F32, [1024]); o_ += 4096
    rt = A.view(o_, F32, [256]); o_ += 1024
    rtb = A.view(o_, BF16, [64]); o_ += 128
    stC = A.view(o_, F32, [32]); o_ += 128
    assert o_ <= U_off + 66048

    st0 = [A.view(U_off + i * 15488, F32, [2568]) for i in range(2)]
    st0b = [A.view(U_off + 10304 + i * 15488, BF16, [2568]) for i in range(2)]
    k = 0
    for (src, dst, ncol, gcol, nsc) in ((w_in, w_in_bf, 2568, GM, 8), (w_mkv, w_mkv_bf, 2048, GK, 8),
                                       (w_mq, w_mq_bf, 1024, GQ, 8), (w_out, w_out_bf, 1024, GA, 4),
                                       (w_mo, w_mo_bf, 1024, None, 0)):
        for c in range(8):
            sb_, sbb = st0[k % 2], st0b[k % 2]
            P.dma(sb_[:, 0:ncol], src[c * 128:(c + 1) * 128, :])
            eng = "dve" if k % 2 == 0 else "pool"
            if c < nsc:
                P.tensor_scalar(sbb[:, 0:ncol], sb_[:, 0:ncol], vv[:, gcol + c:gcol + c + 1], None, ALU.mult, eng=eng)
            else:
                P.copy(sbb[:, 0:ncol], sb_[:, 0:ncol], eng=eng)
            P.dma(dst[c * 128:(c + 1) * 128, :], sbb[:, 0:ncol])
            k += 1
    P.dma(st0[0][:, 0:288], w_route)
    P.copy(wr_bf, st0[0][:, 0:288].rearrange("p (c n) -> p c n", c=8))

    if stop_after == "phase0":
        P.dma(out[0:128, :], st0[0][:, 0:1024])
        P.emit()
        return nc
    bank = [0]

    def nb():
        bank[0] = (bank[0] + 1) % 8
        return bank[0]

    def rstd_from_ss(dst, ss, epsc):
        P.activation(dst, ss, AF.Ln, bias=epsc, scale=1.0)
        P.activation(dst, dst, AF.Exp, scale=-0.5)


    def recip(o, i, eng="dve"):
        P.add(eng, lambda e: e.reciprocal(o, i), reads=[i], writes=[o])

    def route_tile(gi, pl, tt):
        lg = rt[:, 0:36]
        P.tensor_tensor(lg, pl, rA[:, BR_:BR_ + 36], ALU.add)
        if "d_lg" in dbg:
            P.dma(dbg["d_lg"][gi * 128:(gi + 1) * 128, :], lg)
        gmax, ngmax, gsum, gtop = rt[:, 40:41], rt[:, 41:42], rt[:, 42:43], rt[:, 43:44]
        ohg = rt[:, 44:48]
        P.reduce(gmax, lg[:, 0:4], ALU.max)
        P.tensor_scalar(ohg, lg[:, 0:4], gmax, None, ALU.is_equal)
        P.tensor_scalar(ngmax, gmax, -1.0, None, ALU.mult)
        P.activation(rt[:, 48:52], lg[:, 0:4], AF.Exp, bias=ngmax, scale=1.0, accum_out=gsum)
        recip(gtop, gsum)
        esel = rt[:, 52:60]
        P.tensor_scalar(esel, lg[:, 4:12], ohg[:, 0:1], None, ALU.mult)
        for g in range(1, 4):
            P.stt(esel, lg[:, 4 + 8 * g:12 + 8 * g], ohg[:, g:g + 1], esel, ALU.mult, ALU.add)
        top8 = rt[:, 60:68]
        P.add("dve", lambda e: e.max(top8, esel), reads=[esel], writes=[top8])
        oh1, oh2 = rt[:, 68:76], rt[:, 76:84]
        P.tensor_scalar(oh1, esel, top8[:, 0:1], None, ALU.is_equal)
        P.tensor_scalar(oh2, esel, top8[:, 1:2], None, ALU.is_equal)
        dd, w1 = rt[:, 84:85], rt[:, 85:86]
        P.tensor_tensor(dd, top8[:, 1:2], top8[:, 0:1], ALU.subtract)
        P.activation(dd, dd, AF.Exp)
        P.tensor_scalar(dd, dd, 1.0, None, ALU.add)
        recip(w1, dd)
        P.tensor_tensor(G1[:, gi:gi + 1], gtop, w1, ALU.mult)
        P.tensor_tensor(G2[:, gi:gi + 1], gtop, G1[:, gi:gi + 1], ALU.subtract)
        a1, a2 = rt[:, 96:128], rt[:, 128:160]
        for a_, oh in ((a1, oh1), (a2, oh2)):
            P.tensor_tensor(a_.rearrange("p (g e) -> p g e", g=4), ohg.unsqueeze(2).to_broadcast([128, 4, 8]),
                            oh.unsqueeze(1).to_broadcast([128, 4, 8]), ALU.mult)
        P.copy(A1[:, gi, :], a1)
        P.copy(A2[:, gi, :], a2)
        cmb = rtb[:, 0:32]
        P.tensor_tensor(cmb, a1, a2, ALU.add)
        pr = psf(7)[:, 384 + 64 * tt:448 + 64 * tt]
        P.matmul(pr[:, 0:32], cb[:, UST:UST + 128], cmb)
        P.matmul(pr[:, 32:64], cb[:, ONEB:ONEB + 128], cmb)
        rk, jk = rt[:, 160:192], rt[:, 192:224]
        P.tensor_tensor(rk, pr[:, 0:32], base, ALU.add)
        P.tensor_tensor(jk, a1, rk, ALU.mult)
        P.reduce(P1[:, gi:gi + 1], jk, ALU.add)
        P.tensor_tensor(jk, a2, rk, ALU.mult)
        P.reduce(P2[:, gi:gi + 1], jk, ALU.add)
        P.tensor_tensor(base, base, pr[:, 32:64], ALU.add)

    def sumsq(junk, src, ss):
        P.activation(junk, src, AF.Square, accum_out=ss)

    def transposeT(src_bf, dstT, col0, ev, b=None):
        if b is None:
            b = 6 + (nb() % 2)
        pv = psb(b)
        for c in range(8):
            P.transpose(pv[:, c * 128:(c + 1) * 128], src_bf[:, c * 128:(c + 1) * 128], identb)
        P.copy(dstT[:, :, col0:col0 + 128], pv.rearrange("p (c t) -> p c t", c=8), eng=ev)


    for b in range(nseq):
        tok0 = b * S
        for c in range(8):
            P.dma(Wkv[:, c, :], w_mkv_bf[c * 128:(c + 1) * 128, :])
        for mt in range(2):
            xi, xb = xin[mt], xn[mt]
            P.dma(xi, mem[b * 256 + mt * 128: b * 256 + (mt + 1) * 128, :])
            ss, rs = stA[:, 2 * mt:2 * mt + 1], stA[:, 2 * mt + 1:2 * mt + 2]
            sumsq(xb, xi, ss)
            rstd_from_ss(rs, ss, eps1024)
            P.tensor_scalar(xb, xi, rs, None, ALU.mult)
            transposeT(xb, memT, mt * 128, "dve")
        for cc in range(8):
            bk = nb() % 6
            pv = psf(bk)[:, 0:256]
            for c in range(8):
                P.matmul(pv, Wkv[:, c, cc * 128:(cc + 1) * 128], memT[:, c, :], start=(c == 0), stop=(c == 7))
            P.copy(K2T[:, cc, :], pv, eng="act" if cc % 2 else "dve")
        for mt in range(2):
            for nh in range(2):
                bk = nb() % 6
                pv = psf(bk)
                for c in range(8):
                    P.matmul(pv, memT[:, c, mt * 128:(mt + 1) * 128], Wkv[:, c, 1024 + nh * 512:1024 + (nh + 1) * 512],
                             start=(c == 0), stop=(c == 7))
                P.copy(V2[:, mt, 2 * nh:2 * nh + 2, 0:256], pv.rearrange("p (h d) -> p h d", h=2),
                       eng="act" if nh % 2 else "dve")
        P.memset(V2[:, :, :, 256:257], 1.0)
        if stop_after == "A0":
            P.dma(out[0:128, :], xin[0])
            P.emit()
            return nc
        for c in range(8):
            P.dma(Win[:, c, :], w_in_bf[c * 128:(c + 1) * 128, :])
        for i in range(NT):
            xi, xb = xin[i % 2], xn[i % 2]
            P.dma(xi, x[tok0 + i * 128: tok0 + (i + 1) * 128, :])
            ss, rs = stA[:, 4 + 2 * (i % 2):5 + 2 * (i % 2)], stA[:, 5 + 2 * (i % 2):6 + 2 * (i % 2)]
            sumsq(xb, xi, ss)
            rstd_from_ss(rs, ss, eps1024)
            P.tensor_scalar(xb, xi, rs, None, ALU.mult)
            transposeT(xb, xT, i * 128, "act" if i % 2 else "dve")
        if stop_after == "A1":
            P.dma(out[0:128, :], xin[0])
            P.emit()
            return nc
        P.memset(zT[:, :, 0:30], 0.0)
        P.memset(zT[:, :, 2078:2080], 0.0)
        P.memset(vS[:, :, :, 64:65], 1.0)
        OFF_K, OFF_V, OFF_F, OFF_C = 512, 1024, 1536, 1544
        for tb in range(4):
            tsl = slice(tb * 512, (tb + 1) * 512)
            for cc in range(8):
                bk = nb() % 6
                pv = psf(bk)
                for c in range(8):
                    P.matmul(pv, Win[:, c, cc * 128:(cc + 1) * 128], xT[:, c, tsl], start=(c == 0), stop=(c == 7))
                dst = qT[:, cc, tsl] if cc < 4 else kT[:, cc - 4, tsl]
                P.copy(dst, pv, eng="act" if cc % 2 else "dve")
            for i4 in range(4):
                bg = nb() % 6
                pg = psf(bg)
                for c in range(8):
                    P.matmul(pg, Win[:, c, OFF_C + 512 + i4 * 128:OFF_C + 512 + (i4 + 1) * 128], xT[:, c, tsl],
                             start=(c == 0), stop=(c == 7))
                sg = sig[i4 % 2]
                P.activation(sg, pg, AF.Sigmoid, bias=vv[:, BG + 4 + i4:BG + 5 + i4], scale=1.0)
                ba = nb() % 6
                pa = psf(ba)
                for c in range(8):
                    P.matmul(pa, Win[:, c, OFF_C + i4 * 128:OFF_C + (i4 + 1) * 128], xT[:, c, tsl],
                             start=(c == 0), stop=(c == 7))
                P.stt(zT[:, i4, 30 + tb * 512:30 + (tb + 1) * 512], pa, vv[:, BG + i4:BG + i4 + 1], sg, ALU.add, ALU.mult)
        pf_ = psf(6)
        for i in range(NT):
            bk = nb() % 6
            pv = psf(bk)
            for c in range(8):
                P.matmul(pv, xT[:, c, i * 128:(i + 1) * 128], Win[:, c, OFF_V:OFF_V + 512], start=(c == 0), stop=(c == 7))
            P.copy(vS[:, i, :, 0:64], pv.rearrange("p (h d) -> p h d", h=8), eng="act" if i % 2 else "dve")
            for c in range(8):
                P.matmul(pf_[:, i * 8:(i + 1) * 8], xT[:, c, i * 128:(i + 1) * 128], Win[:, c, OFF_F:OFF_F + 8],
                         start=(c == 0), stop=(c == 7))
        if stop_after == "A2":
            P.dma(out[0:128, :], xin[0])
            P.emit()
            return nc
        nlf2 = nlf.rearrange("p a b -> p (a b)")
        P.tensor_tensor(nlf2, pf_[:, 0:128], rA[:, BF_:BF_ + 128], ALU.add)
        P.activation(nlf2, nlf2, AF.Exp, scale=-1.0)
        P.activation(nlf2, nlf2, AF.Ln, bias=one_c, scale=1.0)
        if stop_after == "A3a":
            P.dma(out[0:128, :], xin[0])
            P.emit()
            return nc
        P.copy(incl[:, 0, :], nlf[:, 0, :])
        for i in range(1, NT):
            P.tensor_tensor(incl[:, i, :], incl[:, i - 1, :], nlf[:, i, :], ALU.add)
        if stop_after == "A3b":
            P.dma(out[0:128, :], xin[0])
            P.emit()
            return nc
        pc1 = psf(7)[:, 0:128]
        pc2 = psf(7)[:, 128:256]
        for (pdst, lmat, srcf) in ((pc1, cb[:, ONEB:ONEB + 128], incl.rearrange("p a b -> p (a b)")),
                                   (pc2, cb[:, LTRIB:LTRIB + 128], nlf2)):
            rres = cz[0][:, 0:128]
            for t3 in range(3):
                piece = xn[0][:, t3 * 128:(t3 + 1) * 128]
                P.copy(piece, srcf if t3 == 0 else rres)
                if t3 < 2:
                    P.tensor_tensor(rres, srcf if t3 == 0 else rres, piece, ALU.subtract)
                P.matmul(pdst, lmat, piece, start=(t3 == 0), stop=(t3 == 2))
        P.copy(Cend.rearrange("p a b -> p (a b)"), pc1)
        P.copy(Cc.rearrange("p a b -> p (a b)"), pc2)
        if stop_after == "A3c":
            P.dma(out[0:128, :], xin[0])
            P.emit()
            return nc
        P.tensor_tensor(Cc[:, 1:16, :], Cc[:, 1:16, :], Cend[:, 0:15, :], ALU.add)
        if stop_after == "A3d":
            P.dma(out[0:128, :], xin[0])
            P.emit()
            return nc
        for h in range(8):
            P.tensor_tensor(Bt[:, h, :, :], Cc[:, :, h:h + 1].to_broadcast([128, 16, 16]),
                            Cend[:, :, h].unsqueeze(1).to_broadcast([128, 16, 16]), ALU.subtract)
        if "d_q" in dbg:
            P.dma(dbg["d_q"], qT.rearrange("p a b -> p (a b)"))
        if "d_z" in dbg:
            P.dma(dbg["d_z"], zT.rearrange("p a b -> p (a b)"))
        if "d_B" in dbg:
            P.dma(dbg["d_B"], Bt.rearrange("p a b c -> p (a b c)"))
        if stop_after == "A3":
            P.dma(out[0:128, :], xin[0])
            P.emit()
            return nc
        for wi, wsrc in enumerate((w_out_bf, w_mq_bf, w_mo_bf)):
            for c in range(8):
                P.dma(Woqm[:, wi, c, :], wsrc[c * 128:(c + 1) * 128, :])

        catT = xT
        LAG = 3
        for j in range(NT):
            if j % 4 == 0 and 'conv' in BP:
                c0 = j * 128
                for i4 in range(4):
                    acc = cacc[:, 0, :] if i4 % 2 == 0 else ctmp
                    P.tensor_scalar(acc, zT[:, i4, c0:c0 + 512], vv[:, WDW + i4 * 31:WDW + i4 * 31 + 1],
                                    vv[:, BD + i4:BD + i4 + 1], ALU.mult, ALU.add)
                    for kk in range(1, 31):
                        P.stt(acc, zT[:, i4, c0 + kk:c0 + kk + 512], vv[:, WDW + i4 * 31 + kk:WDW + i4 * 31 + kk + 1], acc,
                              ALU.mult, ALU.add)
                    P.copy(chi[:, i4, :], acc, eng="act")
                    P.tensor_tensor(acc, acc, chi[:, i4, :], ALU.subtract)
                    P.copy(clo[:, i4, :], acc, eng="pool")
            pcv = psf(6)
            for i4 in range(4 if 'convT' in BP else 0):
                tsl_ = slice((j % 4) * 128, (j % 4 + 1) * 128)
                P.matmul(pcv[:, i4 * 128:(i4 + 1) * 128], chi[:, i4, tsl_], identb, start=True, stop=False)
                P.matmul(pcv[:, i4 * 128:(i4 + 1) * 128], clo[:, i4, tsl_], identb, start=False, stop=True)
            accb = [psf(2), psf(3)]
            pairs = [(h, kt) for h in range(8) for kt in range(j + 1)]
            stslots = {}
            n_pairs = len(pairs)

            def emit_qk(n):
                h, kt = pairs[n]
                stv = psf((0, 1, 4, 5)[n % 4])[:, 0:128]
                r0 = 0 if 'evenonly' in BP else (h % 2) * 64
                if 'qk' in BP:
                    P.matmul(stv, kT[r0:r0 + 64, h // 2, kt * 128:(kt + 1) * 128], qT[r0:r0 + 64, h // 2, j * 128:(j + 1) * 128],
                             start=True, stop=(kt != j or 'mask' not in BP))
                if kt == j and 'mask' in BP:
                    P.matmul(stv, identb, negm, start=False, stop=True)
                pt = PT[n % 6]
                if 'exp' in BP:
                    if 'expdve' in BP:
                        P.copy(pt, stv, eng='dve')
                    elif 'expcopy' in BP:
                        P.copy(pt, stv, eng='act')
                    elif 'nobias' in BP:
                        P.activation(pt, stv, AF.Exp, scale=0.125)
                    else:
                        P.activation(pt, stv, AF.Exp, bias=Bt[:, h, kt, j:j + 1], scale=0.125)

            def emit_pv(n):
                h, kt = pairs[n]
                pt = PT[n % 6]
                av = accb[h // 4][:, (h % 4) * 65:(h % 4) * 65 + 65]
                if 'pv' in BP:
                    P.matmul(av, pt, vS[:, kt, h, :], start=(kt == 0), stop=(kt == j))

            for n in range(n_pairs + LAG):
                if n < n_pairs:
                    emit_qk(n)
                if n >= LAG:
                    emit_pv(n - LAG)
            ct = cat[j % 2]
            rden = stB[:, 0:8]
            for hh in range(2):
                av = accb[hh][:, 0:260].rearrange("p (h d) -> p h d", h=4)
                P.add("dve", lambda e, o=rden[:, hh * 4:(hh + 1) * 4], i=av[:, :, 64]: e.reciprocal(o, i),
                      reads=[av[:, :, 64]], writes=[rden[:, hh * 4:(hh + 1) * 4]])
                P.tensor_tensor(attn[:, hh * 256:(hh + 1) * 256].rearrange("p (h d) -> p h d", h=4), av[:, :, 0:64],
                                rden[:, hh * 4:(hh + 1) * 4].unsqueeze(2).to_broadcast([128, 4, 64]), ALU.mult)
            ssa, rsa = stB[:, 8:9], stB[:, 9:10]
            sumsq(cz[0], attn, ssa)
            rstd_from_ss(rsa, ssa, eps512)
            P.tensor_scalar(ct[:, 0:512], attn, rsa, None, ALU.mult)
            if "d_attn" in dbg:
                P.dma(dbg["d_attn"][j * 128:(j + 1) * 128, :], attn)
            s1, nm, sq, rsd = stB[:, 10:11], stB[:, 11:12], stB[:, 12:13], stB[:, 13:14]
            xc, tmp = cz[0], cz[1]
            if "d_conv" in dbg:
                P.copy(tmp, pcv)
                P.dma(dbg["d_conv"][j * 128:(j + 1) * 128, :], tmp)
            P.reduce(s1, pcv, ALU.add)
            P.tensor_scalar(nm, s1, -1.0 / 512.0, None, ALU.mult)
            P.tensor_scalar(xc, pcv, nm, None, ALU.add)
            sumsq(tmp, xc, sq)
            P.activation(rsd, sq, AF.Ln, bias=epsln, scale=1.0 / 512.0)
            P.activation(rsd, rsd, AF.Exp, scale=-0.5)
            P.stt(xc, xc, rsd, rA[:, LG_:LG_ + 512], ALU.mult, ALU.mult)
            P.tensor_tensor(xc, xc, rA[:, LB_:LB_ + 512], ALU.add)
            P.activation(tmp, xc, AF.Exp, scale=-1.0)
            P.tensor_scalar(tmp, tmp, 1.0, None, ALU.add, eng="pool")
            P.add("dve", lambda e, o=tmp: e.reciprocal(o, o), reads=[tmp], writes=[tmp])
            P.tensor_tensor(ct[:, 512:1024], xc, tmp, ALU.mult)
            if "d_cat" in dbg:
                P.dma(dbg["d_cat"][j * 128:(j + 1) * 128, :], ct)
            transposeT(ct, catT, j * 128, "act" if j % 2 else "dve", b=7)

        if stop_after == "B":
            P.dma(out[0:128, :], xin[0])
            P.emit()
            return nc
        P.dma(rG, rows[:, RG:RG + 1024])
        P.tensor_scalar(rG, rG, 32.0, None, ALU.mult)
        Wout, Wmq, Wmo = Woqm[:, 0], Woqm[:, 1], Woqm[:, 2]
        for blk in range(NT // 2):
            for tt in range(2):
                i = blk * 2 + tt
                P.dma(xr[tt], x[tok0 + i * 128: tok0 + (i + 1) * 128, :])
                for nh in range(2):
                    pv = psf(nb() % 6)
                    for c in range(8):
                        P.matmul(pv, catT[:, c, i * 128:(i + 1) * 128], Wout[:, c, nh * 512:(nh + 1) * 512],
                                 start=(c == 0), stop=(c == 7))
                    P.tensor_tensor(x1[:, tt, nh * 512:(nh + 1) * 512], pv, xr[tt][:, nh * 512:(nh + 1) * 512], ALU.add)
                if "d_x1" in dbg:
                    P.dma(dbg["d_x1"][i * 128:(i + 1) * 128, :], x1[:, tt, :])
                ss, rs = stC[:, 2 * tt:2 * tt + 1], stC[:, 2 * tt + 1:2 * tt + 2]
                sumsq(xn2[tt], x1[:, tt, :], ss)
                rstd_from_ss(rs, ss, eps1024)
                P.tensor_scalar(xn2[tt], x1[:, tt, :], rs, None, ALU.mult)
                transposeT(xn2[tt], h2T, tt * 128, "act" if tt else "dve")
            for cc in range(8):
                pv = psf(nb() % 6)[:, 0:256]
                for c in range(8):
                    P.matmul(pv, Wmq[:, c, cc * 128:(cc + 1) * 128], h2T[:, c, :], start=(c == 0), stop=(c == 7))
                P.copy(q2T[:, cc, :], pv, eng="act" if cc % 2 else "dve")
            for h in range(4):
                for mt in range(2):
                    pv = psf(nb() % 6)[:, 0:256]
                    for k2 in range(2):
                        P.matmul(pv, K2T[:, 2 * h + k2, mt * 128:(mt + 1) * 128], q2T[:, 2 * h + k2, :],
                                 start=(k2 == 0), stop=(k2 == 1))
                    P.activation(P2T[(h % 2) * 2 + mt + 4 * (blk % 2)], pv, AF.Exp, scale=1.0 / 16.0)
                for tt in range(2):
                    pa = psf(nb() % 6)[:, 0:257]
                    for mt in range(2):
                        P.matmul(pa, P2T[(h % 2) * 2 + mt + 4 * (blk % 2)][:, tt * 128:(tt + 1) * 128], V2[:, mt, h, :],
                                 start=(mt == 0), stop=(mt == 1))
                    rd = stC[:, 8 + tt:9 + tt]
                    P.add("dve", lambda e, o=rd, i_=pa[:, 256:257]: e.reciprocal(o, i_), reads=[pa[:, 256:257]], writes=[rd])
                    P.tensor_scalar(oo[:, tt, h * 256:(h + 1) * 256], pa[:, 0:256], rd, None, ALU.mult)
            for tt in range(2):
                transposeT(oo[:, tt, :], oT, tt * 128, "act" if tt else "dve")
            for tt in range(2):
                i = blk * 2 + tt
                gi = b * NT + i
                xo = x2t[tt]
                for nh in range(2):
                    pv = psf(nb() % 6)
                    for c in range(8):
                        P.matmul(pv, oT[:, c, tt * 128:(tt + 1) * 128], Wmo[:, c, nh * 512:(nh + 1) * 512],
                                 start=(c == 0), stop=(c == 7))
                    P.tensor_tensor(xo[:, nh * 512:(nh + 1) * 512], pv, x1[:, tt, nh * 512:(nh + 1) * 512], ALU.add)
                P.dma(x2s[tok0 + i * 128: tok0 + (i + 1) * 128, :], xo)
                ss, rs = stC[:, 4 + 2 * tt:5 + 2 * tt], stC[:, 5 + 2 * tt:6 + 2 * tt]
                hb = h3t[tt]
                sumsq(hb, xo, ss)
                rstd_from_ss(rs, ss, eps1024)
                P.stt(hb.rearrange("t (c p) -> t p c", c=8), xo.rearrange("t (p c) -> t p c", c=8), rs,
                      rG.rearrange("t (p c) -> t p c", c=8), ALU.mult, ALU.mult)
                P.dma(h3s[tok0 + i * 128: tok0 + (i + 1) * 128, :], hb)
                transposeT(hb, h3T, 0, "act" if tt else "dve")
                pl = psf(7)[:, 256 + 64 * tt:256 + 64 * tt + 36]
                for c in range(8):
                    P.matmul(pl, h3T[:, c, :], wr_bf[:, c, :], start=(c == 0), stop=(c == 7))
                route_tile(gi, pl, tt)

    if "d_rt" in dbg:
        P.dma(dbg["d_rt"][:, 0:NTT], G1[:, 0:NTT]); P.dma(dbg["d_rt"][:, 64:64 + NTT], G2[:, 0:NTT])
        P.dma(dbg["d_rt"][:, 128:128 + NTT], P1[:, 0:NTT]); P.dma(dbg["d_rt"][:, 192:192 + NTT], P2[:, 0:NTT])
        P.dma(dbg["d_rt"][:, 256:288], base)
    if stop_after == "phase1":
        P.dma(out[0:128, :], x2t[0])
        P.emit()
        return nc

    A.off = mark_persist
    pcf = A.new(F32, [32]); pci = A.new(I32, [32]); offs = A.new(F32, [32]); ends = A.new(F32, [32])
    te = A.new(F32, [96]); gidx = A.new(I32, [96]); didx = A.new(I32, [96, 4]); tf = A.new(F32, [96])
    zero_i = A.new(I32, [192]); big = A.new(F32, [64, 32]); posf = A.new(F32, [64])
    P.tensor_scalar(pcf, base, 127.5, 1.0 / 256.0, ALU.add, ALU.mult)
    P.copy(pci, pcf)
    P.copy(pcf, pci)
    P.tensor_scalar(pcf, pcf, 256.0, None, ALU.mult)
    P.memset(offs[:, 0:1], 0.0)
    for e in range(1, 32):
        P.tensor_tensor(offs[:, e:e + 1], offs[:, e - 1:e], pcf[:, e - 1:e], ALU.add)
    P.tensor_tensor(ends, offs, pcf, ALU.add)
    P.memset(te, 0.0)
    for e in range(32):
        P.stt(te, cf[:, THR:THR + 96], ends[:, e:e + 1], te, ALU.is_ge, ALU.add)
    P.tensor_scalar(te, te, 31.0, None, ALU.min)
    if "d_te" in dbg:
        P.dma(dbg["d_te"], te)
    P.tensor_scalar(tf, te, 128.0, cf[:, PCOL:PCOL + 1], ALU.mult, ALU.add)
    P.copy(gidx, tf)
    for c in range(4):
        P.tensor_scalar(tf, te, 512.0, cf[:, CBASE + c:CBASE + c + 1], ALU.mult, ALU.add)
        P.copy(didx[:, :, c], tf)
    for (Ax, Px, POSx) in ((A1, P1, POS1), (A2, P2, POS2)):
        P.tensor_tensor(big[:, 0:NTT, :], Ax[:, 0:NTT, :], offs.unsqueeze(1).to_broadcast([128, NTT, 32]), ALU.mult)
        P.reduce(posf[:, 0:NTT], big[:, 0:NTT, :], ALU.add)
        P.tensor_tensor(posf[:, 0:NTT], posf[:, 0:NTT], Px[:, 0:NTT], ALU.add)
        P.copy(POSx[:, 0:NTT], posf[:, 0:NTT])
    P.memset(zero_i, 0)
    P.dma(s2t.rearrange("(p n) o -> p (n o)", p=128), zero_i)
    FAKE = "D:s2t_scatter"
    nsc = 0
    for gi in range(NTT):
        for POSx in (POS1, POS2):
            P.add("pool", lambda e, po=POSx[:, gi:gi + 1], ti=ci[:, gi:gi + 1]: e.indirect_dma_start(
                out=s2t, out_offset=bass.IndirectOffsetOnAxis(ap=po, axis=0), in_=ti, in_offset=None),
                reads=[POSx[:, gi:gi + 1], ci[:, gi:gi + 1], s2t], writes=[(FAKE, 0, 1, nsc, nsc + 1)], dma=True)
            nsc += 1

    wg = [A.new(BF16, [8, 512]) for _ in range(3)]
    wu = [A.new(BF16, [8, 512]) for _ in range(3)]
    wd = [A.new(BF16, [4, 1024]) for _ in range(3)]
    sidx = [A.new(I32, [2]) for _ in range(3)]
    hs = [A.new(BF16, [2, 1024]) for _ in range(2)]
    hTs = [A.new(BF16, [8, 256]) for _ in range(2)]
    sil = [A.new(F32, [256]) for _ in range(2)]
    hid = [A.new(BF16, [4, 256]) for _ in range(2)]
    yt = [A.new(F32, [1024]) for _ in range(3)]
    s2t_v = s2t.rearrange("(j g p) o -> j p (g o)", g=2, p=128)
    ny = 0
    for j in range(n_moe_tiles):
        k3 = j % 3
        for (wdst, wsrc) in ((wg[k3], w_gate), (wu[k3], w_up)):
            P.add("pool", lambda e, o=wdst.rearrange("p a b -> p (a b)"), s=wsrc, ix=gidx[:, j:j + 1]: e.indirect_dma_start(
                out=o, out_offset=None, in_=s, in_offset=bass.IndirectOffsetOnAxis(ap=ix, axis=0)),
                reads=[gidx[:, j:j + 1], wsrc], writes=[wdst], dma=True)
        for c in range(4):
            P.add("pool", lambda e, o=wd[k3][:, c, :], ix=didx[:, j, c:c + 1]: e.indirect_dma_start(
                out=o, out_offset=None, in_=w_down, in_offset=bass.IndirectOffsetOnAxis(ap=ix, axis=0)),
                reads=[didx[:, j, c:c + 1], w_down], writes=[wd[k3][:, c, :]], dma=True)
        P.add("sp", lambda e, o=sidx[k3], s=s2t_v[j]: e.dma_start(out=o, in_=s, allow_slow_non_contiguous=True),
              reads=[s2t_v[j], (FAKE, 0, 1, 0, 1 << 20)], writes=[sidx[k3]], dma=True)
        hsj, hTj, hidj = hs[j % 2], hTs[j % 2], hid[j % 2]
        for g in range(2):
            P.add("pool", lambda e, o=hsj[:, g, :], ix=sidx[k3][:, g:g + 1]: e.indirect_dma_start(
                out=o, out_offset=None, in_=h3s, in_offset=bass.IndirectOffsetOnAxis(ap=ix, axis=0)),
                reads=[sidx[k3][:, g:g + 1], h3s], writes=[hsj[:, g, :]], dma=True)
            transposeT(hsj[:, g, :], hTj, g * 128, "act" if g else "dve")
        for fc in range(4):
            pg = psf(nb() % 6)[:, 0:256]
            for c in range(8):
                P.matmul(pg, wg[k3][:, c, fc * 128:(fc + 1) * 128], hTj[:, c, :], start=(c == 0), stop=(c == 7))
            pu = psf(nb() % 6)[:, 0:256]
            for c in range(8):
                P.matmul(pu, wu[k3][:, c, fc * 128:(fc + 1) * 128], hTj[:, c, :], start=(c == 0), stop=(c == 7))
            sl = sil[fc % 2]
            P.activation(sl, pg, AF.Silu)
            P.tensor_tensor(hidj[:, fc, :], sl, pu, ALU.mult)
        for g in range(2):
            y_ = yt[ny % 3]
            ny += 1
            for nh in range(2):
                pv = psf(nb() % 6)
                for fc in range(4):
                    P.matmul(pv, hidj[:, fc, g * 128:(g + 1) * 128], wd[k3][:, fc, nh * 512:(nh + 1) * 512],
                             start=(fc == 0), stop=(fc == 3))
                P.copy(y_[:, nh * 512:(nh + 1) * 512], pv, eng="act" if nh else "dve")
            P.dma(Ysc[j * 256 + g * 128: j * 256 + (g + 1) * 128, :], y_)

    fg = A.new(F32, [1024])
    x2b = [A.new(F32, [1024]) for _ in range(2)]
    y1b = [A.new(F32, [1024]) for _ in range(2)]
    y2b = [A.new(F32, [1024]) for _ in range(2)]
    otb = [A.new(F32, [1024]) for _ in range(2)]
    st4 = A.new(F32, [8])
    P.dma(fg, rows[:, RF:RF + 1024])
    P.tensor_scalar(fg, fg, 32.0, None, ALU.mult)
    for gi in range(NTT):
        k2 = gi % 2
        P.dma(x2b[k2], x2s[gi * 128:(gi + 1) * 128, :])
        for (yb, POSx) in ((y1b[k2], POS1), (y2b[k2], POS2)):
            P.add("pool", lambda e, o=yb, ix=POSx[:, gi:gi + 1]: e.indirect_dma_start(
                out=o, out_offset=None, in_=Ysc, in_offset=bass.IndirectOffsetOnAxis(ap=ix, axis=0)),
                reads=[POSx[:, gi:gi + 1], Ysc], writes=[yb], dma=True)
        acc = x2b[k2]
        if skip_moe:
            pass
        else:
            P.stt(acc, y1b[k2], G1[:, gi:gi + 1], acc, ALU.mult, ALU.add)
            P.stt(acc, y2b[k2], G2[:, gi:gi + 1], acc, ALU.mult, ALU.add)
        ss, rs = st4[:, 2 * k2:2 * k2 + 1], st4[:, 2 * k2 + 1:2 * k2 + 2]
        sumsq(otb[k2], acc, ss)
        rstd_from_ss(rs, ss, eps1024)
        P.stt(otb[k2], acc, rs, fg, ALU.mult, ALU.mult)
        P.dma(out[gi * 128:(gi + 1) * 128, :], otb[k2])
    cnt = P.emit()
    st.close()
    return nc


def _consts():
    p = np.arange(128)
    cfm = np.zeros((128, NCF), np.float32)
    cfm[:, IDF:IDF + 128] = np.eye(128)
    cfm[:, LTRI:LTRI + 128] = (p[:, None] <= p[None, :])
    cfm[:, ONESF:ONESF + 128] = 1.0
    cfm[:, THR:THR + 96] = 256.0 * np.arange(96)[None, :]
    cfm[:, PCOL] = p
    cfm[:, CBASE:CBASE + 4] = np.arange(4)[None, :] * 128 + p[:, None]
    cfm[:, IOTA:IOTA + 32] = np.arange(32)[None, :]
    cfm[:, CEPS:CEPS + 4] = np.array([1024e-6, 512e-6, 1.0, 1e-6], np.float32)[None, :]
    cbm = np.zeros((128, NCB), np.float32)
    cbm[:, IDB:IDB + 128] = np.eye(128)
    cbm[:, NEGM:NEGM + 128] = np.where(p[:, None] > p[None, :], -30000.0, 0.0)
    cbm[:, UST:UST + 128] = (p[:, None] < p[None, :])
    cbm[:, ONEB:ONEB + 128] = 1.0
    cbm[:, LTRIB:LTRIB + 128] = (p[:, None] <= p[None, :])
    cim = (np.arange(64)[None, :] * 128 + p[:, None]).astype(np.int32)
    return cfm, cbm.astype(ml_dtypes.bfloat16), cim


def _host_inputs(core, nseq, x, mem, norm_mix_g, w_in, b_forget, b_glu, w_dw, b_dw, conv_ln_g, conv_ln_b,
                 attn_out_g, w_out, norm_mem_g, mem_norm_g, w_mq, w_mkv, w_mo, norm_ffn_g,
                 w_route_group, b_route_group, w_route_expert, b_route_expert, w_gate, w_up, w_down, final_g):
    f = lambda a: np.ascontiguousarray(np.asarray(a, dtype=np.float32))
    cfm, cbm, cim = _consts()
    vecs = np.zeros((128, NV), np.float32)
    vecs[:, GM:GM + 8] = f(norm_mix_g)[0].reshape(8, 128).T
    vecs[:, GQ:GQ + 8] = f(norm_mem_g)[0].reshape(8, 128).T
    vecs[:, GK:GK + 8] = f(mem_norm_g)[0].reshape(8, 128).T
    vecs[:, GA:GA + 4] = f(attn_out_g)[0].reshape(4, 128).T
    vecs[:, BG:BG + 8] = f(b_glu)[0].reshape(8, 128).T
    vecs[:, BD:BD + 4] = f(b_dw)[0].reshape(4, 128).T
    vecs[:, WDW:WDW + 124] = f(w_dw)[0].reshape(31, 4, 128).transpose(2, 1, 0).reshape(128, 124)
    rows = np.zeros((1, NR), np.float32)
    rows[0, RG:RG + 1024] = f(norm_ffn_g)[0]
    rows[0, RF:RF + 1024] = f(final_g)
    rows[0, RLG:RLG + 512] = f(conv_ln_g)[0]
    rows[0, RLB:RLB + 512] = f(conv_ln_b)[0]
    rows[0, RBF:RBF + 128] = np.tile(f(b_forget)[0], 16)
    rows[0, RBR:RBR + 4] = f(b_route_group)[0]
    rows[0, RBR + 4:RBR + 36] = f(b_route_expert)[0].reshape(32)
    rows = np.ascontiguousarray(np.broadcast_to(rows, (128, NR)))
    wr = np.concatenate([f(w_route_group)[0], f(w_route_expert)[0].transpose(1, 0, 2).reshape(1024, 32)], axis=1)
    return {
        "x": f(x[core * nseq:(core + 1) * nseq]).reshape(nseq * S, D),
        "mem": f(mem[core * nseq:(core + 1) * nseq]).reshape(nseq * 256, D),
        "w_in": f(w_in)[0], "w_out": f(w_out)[0], "w_mq": f(w_mq)[0], "w_mkv": f(w_mkv)[0], "w_mo": f(w_mo)[0],
        "w_route": np.ascontiguousarray(wr.reshape(128, 8 * 36)),
        "w_gate": f(w_gate)[0].reshape(32 * 128, 4096), "w_up": f(w_up)[0].reshape(32 * 128, 4096),
        "w_down": f(w_down)[0].reshape(32 * 512, 1024),
        "vecs": vecs, "rows": rows, "constf": cfm, "constb": cbm, "consti": cim,
    }


_NC_CACHE = {}


def kernel(**inputs):
    n_cores = 8
    nseq = 4
    if "full" not in _NC_CACHE:
        _NC_CACHE["full"] = build(nseq=nseq)
    nc = _NC_CACHE["full"]
    in_maps = [_host_inputs(c, nseq, **inputs) for c in range(n_cores)]
    res = run_bass_kernel_spmd(nc, in_maps, core_ids=list(range(n_cores)))
    outs = [np.asarray(r["out"]).reshape(nseq, S, D) for r in res.results]
    return np.concatenate(outs, axis=0).astype(np.float32)
```

```python
import numpy as np
import ml_dtypes
from concourse.bass_utils import run_bass_kernel_spmd
from contextlib import ExitStack
import concourse.bass as bass
import concourse.mybir as mybir

F32 = mybir.dt.float32
BF16 = mybir.dt.bfloat16
I32 = mybir.dt.int32
ALU = mybir.AluOpType
AF = mybir.ActivationFunctionType
AX = mybir.AxisListType
_DSZ = {F32: 4, BF16: 2, I32: 4, mybir.dt.uint32: 4, mybir.dt.float16: 2, mybir.dt.uint8: 1,
        mybir.dt.int8: 1, mybir.dt.uint16: 2, mybir.dt.int16: 2}


def region(ap):
    dsz = _DSZ[ap.dtype]
    dims = ap.ap
    off = ap.offset
    sp = str(ap.space)
    if sp in ("SB", "PSUM"):
        shp = ap.tensor.shape
        rowlen = 1
        for s in list(shp)[1:]:
            rowlen *= s
        p0 = off // rowlen
        c0 = off % rowlen
        pstep, pcnt = dims[0]
        p1 = p0 + ((pcnt - 1) * pstep) // rowlen + 1 if pstep else p0 + 1
        ext = 0
        for st, cn in dims[1:]:
            ext += (cn - 1) * abs(st)
        if sp == "PSUM":
            return (sp + ":" + ap.tensor.name, 0, 128, 0, 1 << 20)
        return (sp + ":" + ap.tensor.name, p0, p1, c0 * dsz, (c0 + ext + 1) * dsz)
    else:
        ext = 0
        for st, cn in dims:
            ext += (cn - 1) * abs(st)
        return ("D:" + ap.tensor.name, 0, 1, off * dsz, (off + ext + 1) * dsz)


class Op:
    __slots__ = ("eng", "fn", "reads", "writes", "dma", "deps", "sig", "dsem", "dval",
                 "waits", "idx", "prewait")


class Prog:
    ENGS = ("pe", "act", "dve", "pool", "sp")
    EPOCH = 30000

    def __init__(self, nc, ndma_sems=56):
        self.nc = nc
        self.ops = []
        self.recs = {}
        self.ndma = ndma_sems
        self.extra_regions = {}

    def add(self, eng, fn, reads=(), writes=(), dma=False):
        op = Op()
        op.eng = eng; op.fn = fn; op.dma = dma
        op.reads = [r if isinstance(r, tuple) else region(r) for r in reads]
        op.writes = [r if isinstance(r, tuple) else region(r) for r in writes]
        op.idx = len(self.ops)
        op.sig = None; op.dsem = None; op.dval = None; op.waits = []; op.prewait = None
        deps = set()
        for (sp, p0, p1, lo, hi) in op.reads:
            for r in self.recs.get(sp, ()):
                if r[5] and r[0] < p1 and p0 < r[1] and r[2] < hi and lo < r[3]:
                    deps.add(r[4])
        for (sp, p0, p1, lo, hi) in op.writes:
            lst = self.recs.get(sp)
            if lst is None:
                lst = self.recs[sp] = []
            keep = []
            for r in lst:
                if r[0] < p1 and p0 < r[1] and r[2] < hi and lo < r[3]:
                    deps.add(r[4])
                    if r[0] >= p0 and r[1] <= p1 and r[2] >= lo and r[3] <= hi:
                        continue
                keep.append(r)
            keep.append([p0, p1, lo, hi, op.idx, True])
            self.recs[sp] = keep
        for (sp, p0, p1, lo, hi) in op.reads:
            lst = self.recs.get(sp)
            if lst is None:
                lst = self.recs[sp] = []
            if not dma:
                for r in lst:
                    if (not r[5]) and r[0] == p0 and r[1] == p1 and r[2] == lo and r[3] == hi \
                            and (not self.ops[r[4]].dma) and self.ops[r[4]].eng == eng:
                        r[4] = op.idx
                        break
                else:
                    lst.append([p0, p1, lo, hi, op.idx, False])
            else:
                lst.append([p0, p1, lo, hi, op.idx, False])
        deps.discard(op.idx)
        op.deps = deps
        self.ops.append(op)
        return op

    def matmul(self, out, lhsT, rhs, start=True, stop=True, **kw):
        return self.add("pe", lambda e: e.matmul(out, lhsT, rhs, start=start, stop=stop, **kw),
                        reads=[lhsT, rhs], writes=[out])

    def transpose(self, out, in_, ident):
        return self.add("pe", lambda e: e.transpose(out, in_, ident), reads=[in_, ident], writes=[out])

    def activation(self, out, in_, func, bias=None, scale=None, accum_out=None, eng="act"):
        reads = [in_]
        kw = {}
        if bias is not None:
            kw["bias"] = bias
            if not isinstance(bias, (int, float)):
                reads.append(bias)
        if scale is not None:
            kw["scale"] = scale
            if not isinstance(scale, (int, float)):
                reads.append(scale)
        writes = [out]
        if accum_out is not None:
            kw["accum_out"] = accum_out
            writes.append(accum_out)
        return self.add(eng, lambda e: e.activation(out, in_, func, **kw), reads=reads, writes=writes)

    def tensor_scalar(self, out, in0, s1, s2, op0, op1=None, eng="dve", accum_out=None):
        reads = [in0]
        for s in (s1, s2):
            if s is not None and not isinstance(s, (int, float)):
                reads.append(s)
        kw = {}
        if op1 is not None:
            kw["op1"] = op1
        writes = [out]
        if accum_out is not None:
            kw["accum_out"] = accum_out
            writes.append(accum_out)
        return self.add(eng, lambda e: e.tensor_scalar(out, in0, s1, s2, op0, **kw), reads=reads, writes=writes)

    def tensor_tensor(self, out, in0, in1, op, eng="dve"):
        return self.add(eng, lambda e: e.tensor_tensor(out, in0, in1, op), reads=[in0, in1], writes=[out])

    def stt(self, out, in0, scalar, in1, op0, op1, eng="dve", accum_out=None):
        reads = [in0, in1]
        if not isinstance(scalar, (int, float)):
            reads.append(scalar)
        kw = {}
        writes = [out]
        if accum_out is not None:
            kw["accum_out"] = accum_out
            writes.append(accum_out)
        return self.add(eng, lambda e: e.scalar_tensor_tensor(out, in0, scalar, in1, op0, op1, **kw),
                        reads=reads, writes=writes)

    def copy(self, out, in_, eng="dve"):
        if eng == "act":
            return self.add(eng, lambda e: e.copy(out, in_), reads=[in_], writes=[out])
        return self.add(eng, lambda e: e.tensor_copy(out, in_), reads=[in_], writes=[out])

    def memset(self, out, val, eng="dve"):
        return self.add(eng, lambda e: e.memset(out, val), reads=[], writes=[out])

    def reduce(self, out, in_, op, axis=None, eng="dve"):
        ax = axis if axis is not None else AX.X
        return self.add(eng, lambda e: e.tensor_reduce(out, in_, ax, op), reads=[in_], writes=[out])

    def dma(self, out, in_, q="sp", **kw):
        return self.add(q, lambda e: e.dma_start(out=out, in_=in_, **kw), reads=[in_], writes=[out], dma=True)

    def finalize(self):
        ops = self.ops
        need = [False] * len(ops)
        for op in ops:
            for d in op.deps:
                p = ops[d]
                if p.dma:
                    continue
                if p.eng == op.eng and not op.dma:
                    if p.eng == "pe":
                        continue
                    continue_flag = True
                    for (sp, p0, p1, lo, hi) in op.reads:
                        for (sp2, q0, q1, lo2, hi2) in p.writes:
                            if sp == sp2 and q0 < p1 and p0 < q1 and lo2 < hi and lo < hi2:
                                continue_flag = False
                    if continue_flag:
                        continue
                need[d] = True
        cnt = {e: 0 for e in self.ENGS}
        for op in ops:
            if (not op.dma) and need[op.idx]:
                cnt[op.eng] += 1
                op.sig = cnt[op.eng]
        dcount = [0] * self.ndma
        k = 0
        for op in ops:
            if op.dma:
                s = k % self.ndma
                k += 1
                dcount[s] += 1
                op.dsem = s
                op.dval = 16 * dcount[s]
                op.prewait = (("dma", s), 16 * (dcount[s] - 1)) if dcount[s] > 1 else None
        self.dfinal = [16 * c for c in dcount]
        seen = {e: {} for e in self.ENGS}
        pos = {e: 0 for e in self.ENGS}
        opos = {}
        for op in ops:
            pos[op.eng] += 1
            opos[op.idx] = pos[op.eng]
        nw = 0
        for op in ops:
            sn = seen[op.eng]
            w = {}
            if op.prewait is not None:
                key, val = op.prewait
                if sn.get(key, 0) < val:
                    w[key] = val
            for d in op.deps:
                p = ops[d]
                if p.dma:
                    key, val = ("dma", p.dsem), p.dval
                else:
                    if p.sig is None:
                        continue
                    if p.eng == op.eng and not op.dma:
                        if p.eng == "pe":
                            continue
                        if opos[op.idx] - opos[p.idx] > 3:
                            continue
                    ep = (p.sig - 1) // self.EPOCH
                    key, val = ("eng", p.eng), ep * 100000 + (p.sig - 1) % self.EPOCH + 1
                if sn.get(key, 0) < val and w.get(key, 0) < val:
                    w[key] = val
            for key, val in w.items():
                sn[key] = val
            op.waits = list(w.items())
            nw += len(op.waits)
        self.n_waits = nw
        self.n_epochs = {e: (cnt[e] - 1) // self.EPOCH + 1 if cnt[e] else 1 for e in self.ENGS}
        return cnt

    def emit(self):
        nc = self.nc
        cnt = self.finalize()
        ops = self.ops
        with ExitStack() as st:
            esem = {}
            for e in self.ENGS:
                for ep in range(self.n_epochs[e]):
                    esem[(e, ep)] = st.enter_context(nc.semaphore("s_%s%d" % (e, ep)))
            dsem = [st.enter_context(nc.semaphore("d%d" % i)) for i in range(self.ndma)]
            block = st.enter_context(nc.Block())
            by_eng = {e: [op for op in ops if op.eng == e] for e in self.ENGS}
            EP = self.EPOCH

            def run(eng_obj, lst, is_sp):
                for op in lst:
                    for key, val in op.waits:
                        if key[0] == "dma":
                            eng_obj.wait_ge(dsem[key[1]], val)
                        else:
                            ep, v = divmod(val, 100000)
                            eng_obj.wait_ge(esem[(key[1], ep)], v)
                    inst = op.fn(eng_obj)
                    if op.dma:
                        inst.then_inc(dsem[op.dsem], 16)
                    elif op.sig is not None:
                        ep = (op.sig - 1) // EP
                        inst.then_inc(esem[(op.eng, ep)], 1)
                if is_sp:
                    for i, v in enumerate(self.dfinal):
                        if v:
                            eng_obj.wait_ge(dsem[i], v)

            @block.sync
            def _(e):
                run(e, by_eng["sp"], True)

            @block.tensor
            def _(e):
                run(e, by_eng["pe"], False)

            @block.scalar
            def _(e):
                run(e, by_eng["act"], False)

            @block.vector
            def _(e):
                run(e, by_eng["dve"], False)

            @block.gpsimd
            def _(e):
                run(e, by_eng["pool"], False)
        return cnt

S = 2048
D = 1024
NT = S // 128
NTILES = 96
TS = 256
NSLOT = NTILES * TS
GM, GQ, GK, GA, BG, BD, WDW = 0, 8, 16, 24, 28, 36, 40
NV = 40 + 4 * 31
RG, RF, RLG, RLB, RBF, RBR, RBDW = 0, 1024, 2048, 2560, 3072, 3200, 3236
NR = 3748
IDF, LTRI, ONESF, THR, PCOL, CBASE, IOTA, CEPS, NCF = 0, 128, 256, 384, 480, 481, 485, 517, 525
IDB, NEGM, UST, ONEB, LTRIB, NCB = 0, 128, 256, 384, 512, 640


def _prod(s):
    r = 1
    for a in s:
        r *= a
    return r


class Arena:
    def __init__(self, nc, st, name, nbytes):
        self.t = st.enter_context(nc.sbuf_tensor(name, [128, nbytes // 4], F32))
        self.cap = nbytes
        self.off = 0

    def alloc(self, nbytes):
        off = (self.off + 63) // 64 * 64
        self.off = off + nbytes
        assert self.off <= self.cap, ("arena overflow", self.off, self.cap)
        return off

    def view(self, off, dtype, shape):
        n = _prod(shape)
        dsz = _DSZ[dtype]
        nb = (n * dsz + 3) // 4
        v = self.t[:, off // 4: off // 4 + nb]
        if dtype != F32:
            v = v.bitcast(dtype)
        v = v[:, 0:n]
        if len(shape) == 2:
            v = v.rearrange("p (a b) -> p a b", a=shape[0], b=shape[1])
        elif len(shape) == 3:
            v = v.rearrange("p (a b c) -> p a b c", a=shape[0], b=shape[1], c=shape[2])
        return v

    def new(self, dtype, shape):
        off = self.alloc(_prod(shape) * _DSZ[dtype])
        return self.view(off, dtype, shape)


import os as _os
BP = set(_os.environ.get('BPARTS', 'conv,convT,qk,mask,exp,pv,norm,tail,catT').split(','))


def build(nseq=4, debug=(), stop_after=None, n_moe_tiles=NTILES, skip_moe=False):
    nc = bass.Bass("TRN2", target_bir_lowering=False)
    NTOK = nseq * S
    NTT = NTOK // 128

    def din(name, shape, dt=F32):
        return nc.dram_tensor(name, shape, dt, kind="ExternalInput").ap()

    def scr(name, shape, dt):
        if name in debug:
            return nc.dram_tensor(name, shape, dt, kind="ExternalOutput").ap()
        return nc.dram_tensor(name, shape, dt).ap()

    x = din("x", [NTOK, D])
    mem = din("mem", [nseq * 256, D])
    w_in = din("w_in", [D, 2568])
    w_out = din("w_out", [D, D])
    w_mq = din("w_mq", [D, D])
    w_mkv = din("w_mkv", [D, 2 * D])
    w_mo = din("w_mo", [D, D])
    w_route = din("w_route", [128, 8 * 36])
    w_gate = din("w_gate", [32 * 128, 4096])
    w_up = din("w_up", [32 * 128, 4096])
    w_down = din("w_down", [32 * 512, 1024])
    vecs = din("vecs", [128, NV])
    rows = din("rows", [128, NR])
    constf = din("constf", [128, NCF])
    constb = din("constb", [128, NCB], BF16)
    consti = din("consti", [128, 64], I32)
    out = nc.dram_tensor("out", [NTOK, D], F32, kind="ExternalOutput").ap()

    w_in_bf = scr("w_in_bf", [D, 2568], BF16)
    w_out_bf = scr("w_out_bf", [D, D], BF16)
    w_mq_bf = scr("w_mq_bf", [D, D], BF16)
    w_mkv_bf = scr("w_mkv_bf", [D, 2 * D], BF16)
    w_mo_bf = scr("w_mo_bf", [D, D], BF16)
    x2s = scr("x2s", [NTOK, D], F32)
    h3s = scr("h3s", [NTOK, D], BF16)
    Ysc = scr("Ysc", [NSLOT, D], F32)
    s2t = scr("s2t", [NSLOT, 1], I32)
    dbg = {}
    for name, shape, dt in (("d_cat", [S, D], BF16), ("d_x1", [S, D], F32), ("d_lg", [NTOK, 36], F32),
                            ("d_q", [128, 4 * 2048], BF16), ("d_attn", [S, 512], F32), ("d_z", [128, 4 * 2080], BF16),
                            ("d_B", [128, 2048], F32), ("d_rt", [128, 64 * 8], F32), ("d_te", [128, 96], F32),
                            ("d_conv", [S, 512], F32)):
        if name in debug:
            dbg[name] = nc.dram_tensor(name, shape, dt, kind="ExternalOutput").ap()

    P = Prog(nc)
    st = ExitStack()
    A = Arena(nc, st, "arena", 210944)
    PS = [st.enter_context(nc.psum_tensor("ps%d" % i, [128, 512], F32)) for i in range(8)]

    def psf(i):
        return PS[i][:, :]

    def psb(i):
        return PS[i][:, :].bitcast(BF16)

    cf = A.new(F32, [NCF])
    cb = A.new(BF16, [NCB])
    ci = A.new(I32, [64])
    vv = A.new(F32, [NV])
    rA = A.new(F32, [1700])
    LG_, LB_, BF_, BR_, BDW_ = 0, 512, 1024, 1152, 1188
    A1 = A.new(BF16, [64, 32])
    A2 = A.new(BF16, [64, 32])
    G1 = A.new(F32, [64]); G2 = A.new(F32, [64]); P1 = A.new(F32, [64]); P2 = A.new(F32, [64])
    POS1 = A.new(I32, [64]); POS2 = A.new(I32, [64])
    base = A.new(F32, [32])
    wr_bf = A.new(BF16, [8, 36])
    v_ = None
    identf = cf[:, IDF:IDF + 128]
    identb = cb[:, IDB:IDB + 128]
    negm = cb[:, NEGM:NEGM + 128]
    eps1024 = cf[:, CEPS:CEPS + 1]
    eps512 = cf[:, CEPS + 1:CEPS + 2]
    one_c = cf[:, CEPS + 2:CEPS + 3]
    epsln = cf[:, CEPS + 3:CEPS + 4]
    P.dma(cf, constf); P.dma(cb, constb); P.dma(ci, consti); P.dma(vv, vecs)
    P.dma(rA, rows[:, RLG:RLG + 1700])
    P.memset(base, 0.0)
    P.tensor_scalar(vv[:, GM:GM + 24], vv[:, GM:GM + 24], 32.0, None, ALU.mult)
    P.tensor_scalar(vv[:, GA:GA + 4], vv[:, GA:GA + 4], float(np.sqrt(512.0)), None, ALU.mult)
    mark_persist = A.off

    Wreg_off = A.alloc(49152)
    Win = A.view(Wreg_off, BF16, [8, 2568])
    Woqm = A.view(Wreg_off, BF16, [3, 8, 1024])
    xT = A.new(BF16, [8, 2048])
    K2T = A.new(BF16, [8, 256])
    V2 = A.new(BF16, [2, 4, 257])
    Bt = A.new(F32, [8, 16, 16])
    nlf = A.new(F32, [16, 8]); incl = A.new(F32, [16, 8]); Cc = A.new(F32, [16, 8]); Cend = A.new(F32, [16, 8])
    U_off = A.alloc(66048 + 64)
    qT = A.view(U_off, BF16, [4, 2048])
    kT = A.view(U_off + 16384, BF16, [4, 2048])
    Wkv = A.view(U_off, BF16, [8, 2048])
    vS = A.view(U_off + 32768, BF16, [16, 8, 65])
    zT = A.view(U_off + 32768 + 16640, BF16, [4, 2080])
    memT = A.view(U_off + 32768 + 16640, BF16, [8, 256])
    T_off = A.alloc(22272)
    xin = [A.view(T_off + i * 4096, F32, [1024]) for i in range(2)]
    xn = [A.view(T_off + 8192 + i * 2048, BF16, [1024]) for i in range(2)]
    sig = [A.view(T_off + 12288 + i * 2048, F32, [512]) for i in range(2)]
    stA = A.view(T_off + 16384, F32, [16])
    PT = [A.view(T_off + i * 256, BF16, [128]) for i in range(6)]
    attn = A.view(T_off + 1536, F32, [512])
    cat = [A.view(T_off + 3584, BF16, [1024])] * 2
    cz = [A.view(T_off + 5632 + i * 2048, F32, [512]) for i in range(2)]
    stB = A.view(T_off + 9728, F32, [64])
    Dg = A.view(T_off + 9984, BF16, [31, 128])
    zoff = U_off + 32768 + 16640
    cvo = [A.view(T_off + 17920, BF16, [16, 128])] + [A.view(zoff + i * 4160, BF16, [16, 128]) for i in range(3)]
    o_ = U_off
    x1 = A.view(o_, F32, [2, 1024]); o_ += 8192
    xr = [A.view(o_ + i * 4096, F32, [1024]) for i in range(2)]; o_ += 8192
    xn2 = [A.view(o_ + i * 2048, BF16, [1024]) for i in range(2)]; o_ += 4096
    h2T = A.view(o_, BF16, [8, 256]); o_ += 4096
    q2T = A.view(o_, BF16, [8, 256]); o_ += 4096
    P2T = [A.view(o_ + i * 512, BF16, [256]) for i in range(8)]; o_ += 4096
    oo = A.view(o_, BF16, [2, 1024]); o_ += 4096
    oT = A.view(o_, BF16, [8, 256]); o_ += 4096
    x2t = [A.view(o_ + i * 4096, F32, [1024]) for i in range(2)]; o_ += 8192
    h3t = [A.view(o_ + i * 2048, BF16, [1024]) for i in range(2)]; o_ += 4096
    h3T = A.view(o_, BF16, [8, 128]); o_ += 2048
    rG = A.view(o_, F32, [1024]); o_ += 4096
    rt = A.view(o_, F32, [256]); o_ += 1024
    rtb = A.view(o_, BF16, [64]); o_ += 128
    stC = A.view(o_, F32, [32]); o_ += 128
    assert o_ <= U_off + 66048

    st0 = [A.view(U_off + i * 15488, F32, [2568]) for i in range(4)]
    st0b = [A.view(U_off + 10304 + i * 15488, BF16, [2568]) for i in range(4)]
    k = 0
    for (src, dst, ncol, gcol, nsc) in ((w_in, w_in_bf, 2568, GM, 8), (w_mkv, w_mkv_bf, 2048, GK, 8),
                                       (w_mq, w_mq_bf, 1024, GQ, 8), (w_out, w_out_bf, 1024, GA, 4),
                                       (w_mo, w_mo_bf, 1024, None, 0)):
        for c in range(8):
            sb_, sbb = st0[k % 4], st0b[k % 4]
            P.dma(sb_[:, 0:ncol], src[c * 128:(c + 1) * 128, :])
            if c < nsc:
                if k % 2 == 0:
                    P.tensor_scalar(sbb[:, 0:ncol], sb_[:, 0:ncol], vv[:, gcol + c:gcol + c + 1], None, ALU.mult)
                else:
                    P.activation(sbb[:, 0:ncol], sb_[:, 0:ncol], AF.Copy, scale=vv[:, gcol + c:gcol + c + 1])
            else:
                P.copy(sbb[:, 0:ncol], sb_[:, 0:ncol], eng="dve" if k % 2 == 0 else "act")
            P.dma(dst[c * 128:(c + 1) * 128, :], sbb[:, 0:ncol])
            k += 1
    P.dma(st0[0][:, 0:288], w_route)
    P.copy(wr_bf, st0[0][:, 0:288].rearrange("p (c n) -> p c n", c=8))

    if stop_after == "phase0":
        P.dma(out[0:128, :], st0[0][:, 0:1024])
        P.emit()
        return nc
    bank = [0]

    def nb():
        bank[0] = (bank[0] + 1) % 8
        return bank[0]

    def rstd_from_ss(dst, ss, epsc):
        P.activation(dst, ss, AF.Ln, bias=epsc, scale=1.0)
        P.activation(dst, dst, AF.Exp, scale=-0.5)


    def recip(o, i, eng="dve"):
        P.add(eng, lambda e: e.reciprocal(o, i), reads=[i], writes=[o])

    def route_tile(gi, pl, tt):
        lg = rt[:, 0:36]
        P.tensor_tensor(lg, pl, rA[:, BR_:BR_ + 36], ALU.add)
        if "d_lg" in dbg:
            P.dma(dbg["d_lg"][gi * 128:(gi + 1) * 128, :], lg)
        gmax, ngmax, gsum, gtop = rt[:, 40:41], rt[:, 41:42], rt[:, 42:43], rt[:, 43:44]
        ohg = rt[:, 44:48]
        P.reduce(gmax, lg[:, 0:4], ALU.max)
        P.tensor_scalar(ohg, lg[:, 0:4], gmax, None, ALU.is_equal)
        P.tensor_scalar(ngmax, gmax, -1.0, None, ALU.mult)
        P.activation(rt[:, 48:52], lg[:, 0:4], AF.Exp, bias=ngmax, scale=1.0, accum_out=gsum)
        recip(gtop, gsum)
        esel = rt[:, 52:60]
        P.tensor_scalar(esel, lg[:, 4:12], ohg[:, 0:1], None, ALU.mult)
        for g in range(1, 4):
            P.stt(esel, lg[:, 4 + 8 * g:12 + 8 * g], ohg[:, g:g + 1], esel, ALU.mult, ALU.add)
        top8 = rt[:, 60:68]
        P.add("dve", lambda e: e.max(top8, esel), reads=[esel], writes=[top8])
        oh1, oh2 = rt[:, 68:76], rt[:, 76:84]
        P.tensor_scalar(oh1, esel, top8[:, 0:1], None, ALU.is_equal)
        P.tensor_scalar(oh2, esel, top8[:, 1:2], None, ALU.is_equal)
        dd, w1 = rt[:, 84:85], rt[:, 85:86]
        P.tensor_tensor(dd, top8[:, 1:2], top8[:, 0:1], ALU.subtract)
        P.activation(dd, dd, AF.Exp)
        P.tensor_scalar(dd, dd, 1.0, None, ALU.add)
        recip(w1, dd)
        P.tensor_tensor(G1[:, gi:gi + 1], gtop, w1, ALU.mult)
        P.tensor_tensor(G2[:, gi:gi + 1], gtop, G1[:, gi:gi + 1], ALU.subtract)
        a1, a2 = rt[:, 96:128], rt[:, 128:160]
        for a_, oh in ((a1, oh1), (a2, oh2)):
            P.tensor_tensor(a_.rearrange("p (g e) -> p g e", g=4), ohg.unsqueeze(2).to_broadcast([128, 4, 8]),
                            oh.unsqueeze(1).to_broadcast([128, 4, 8]), ALU.mult)
        P.copy(A1[:, gi, :], a1)
        P.copy(A2[:, gi, :], a2)
        cmb = rtb[:, 0:32]
        P.tensor_tensor(cmb, a1, a2, ALU.add)
        pr = psf(7)[:, 384 + 64 * tt:448 + 64 * tt]
        P.matmul(pr[:, 0:32], cb[:, UST:UST + 128], cmb)
        P.matmul(pr[:, 32:64], cb[:, ONEB:ONEB + 128], cmb)
        rk, jk = rt[:, 160:192], rt[:, 192:224]
        P.tensor_tensor(rk, pr[:, 0:32], base, ALU.add)
        P.tensor_tensor(jk, a1, rk, ALU.mult)
        P.reduce(P1[:, gi:gi + 1], jk, ALU.add)
        P.tensor_tensor(jk, a2, rk, ALU.mult)
        P.reduce(P2[:, gi:gi + 1], jk, ALU.add)
        P.tensor_tensor(base, base, pr[:, 32:64], ALU.add)

    def sumsq(junk, src, ss):
        P.activation(junk, src, AF.Square, accum_out=ss)

    def transposeT(src_bf, dstT, col0, ev, b=None):
        if b is None:
            b = 6 + (nb() % 2)
        pv = psb(b)
        for c in range(8):
            P.transpose(pv[:, c * 128:(c + 1) * 128], src_bf[:, c * 128:(c + 1) * 128], identb)
        P.copy(dstT[:, :, col0:col0 + 128], pv.rearrange("p (c t) -> p c t", c=8), eng=ev)


    for b in range(nseq):
        tok0 = b * S
        for c in range(8):
            P.dma(Wkv[:, c, :], w_mkv_bf[c * 128:(c + 1) * 128, :])
        for mt in range(2):
            xi, xb = xin[mt], xn[mt]
            P.dma(xi, mem[b * 256 + mt * 128: b * 256 + (mt + 1) * 128, :])
            ss, rs = stA[:, 2 * mt:2 * mt + 1], stA[:, 2 * mt + 1:2 * mt + 2]
            sumsq(xb, xi, ss)
            rstd_from_ss(rs, ss, eps1024)
            P.tensor_scalar(xb, xi, rs, None, ALU.mult)
            transposeT(xb, memT, mt * 128, "dve")
        for cc in range(8):
            bk = nb() % 6
            pv = psf(bk)[:, 0:256]
            for c in range(8):
                P.matmul(pv, Wkv[:, c, cc * 128:(cc + 1) * 128], memT[:, c, :], start=(c == 0), stop=(c == 7))
            P.copy(K2T[:, cc, :], pv, eng="act" if cc % 2 else "dve")
        for mt in range(2):
            for nh in range(2):
                bk = nb() % 6
                pv = psf(bk)
                for c in range(8):
                    P.matmul(pv, memT[:, c, mt * 128:(mt + 1) * 128], Wkv[:, c, 1024 + nh * 512:1024 + (nh + 1) * 512],
                             start=(c == 0), stop=(c == 7))
                P.copy(V2[:, mt, 2 * nh:2 * nh + 2, 0:256], pv.rearrange("p (h d) -> p h d", h=2),
                       eng="act" if nh % 2 else "dve")
        P.memset(V2[:, :, :, 256:257], 1.0)
        if stop_after == "A0":
            P.dma(out[0:128, :], xin[0])
            P.emit()
            return nc
        for c in range(8):
            P.dma(Win[:, c, :], w_in_bf[c * 128:(c + 1) * 128, :])
        for i in range(NT):
            xi, xb = xin[i % 2], xn[i % 2]
            P.dma(xi, x[tok0 + i * 128: tok0 + (i + 1) * 128, :])
            ss, rs = stA[:, 4 + 2 * (i % 2):5 + 2 * (i % 2)], stA[:, 5 + 2 * (i % 2):6 + 2 * (i % 2)]
            sumsq(xb, xi, ss)
            rstd_from_ss(rs, ss, eps1024)
            P.tensor_scalar(xb, xi, rs, None, ALU.mult)
            transposeT(xb, xT, i * 128, "act" if i % 2 else "dve")
        if stop_after == "A1":
            P.dma(out[0:128, :], xin[0])
            P.emit()
            return nc
        P.memset(zT[:, :, 0:30], 0.0)
        P.memset(zT[:, :, 2078:2080], 0.0)
        P.memset(vS[:, :, :, 64:65], 1.0)
        OFF_K, OFF_V, OFF_F, OFF_C = 512, 1024, 1536, 1544
        for tb in range(4):
            tsl = slice(tb * 512, (tb + 1) * 512)
            for cc in range(8):
                bk = nb() % 6
                pv = psf(bk)
                for c in range(8):
                    P.matmul(pv, Win[:, c, cc * 128:(cc + 1) * 128], xT[:, c, tsl], start=(c == 0), stop=(c == 7))
                dst = qT[:, cc, tsl] if cc < 4 else kT[:, cc - 4, tsl]
                P.copy(dst, pv, eng="act" if cc % 2 else "dve")
            for i4 in range(4):
                bg = nb() % 6
                pg = psf(bg)
                for c in range(8):
                    P.matmul(pg, Win[:, c, OFF_C + 512 + i4 * 128:OFF_C + 512 + (i4 + 1) * 128], xT[:, c, tsl],
                             start=(c == 0), stop=(c == 7))
                sg = sig[i4 % 2]
                P.activation(sg, pg, AF.Sigmoid, bias=vv[:, BG + 4 + i4:BG + 5 + i4], scale=1.0)
                ba = nb() % 6
                pa = psf(ba)
                for c in range(8):
                    P.matmul(pa, Win[:, c, OFF_C + i4 * 128:OFF_C + (i4 + 1) * 128], xT[:, c, tsl],
                             start=(c == 0), stop=(c == 7))
                P.stt(zT[:, i4, 30 + tb * 512:30 + (tb + 1) * 512], pa, vv[:, BG + i4:BG + i4 + 1], sg, ALU.add, ALU.mult)
        pf_ = psf(6)
        for i in range(NT):
            bk = nb() % 6
            pv = psf(bk)
            for c in range(8):
                P.matmul(pv, xT[:, c, i * 128:(i + 1) * 128], Win[:, c, OFF_V:OFF_V + 512], start=(c == 0), stop=(c == 7))
            P.copy(vS[:, i, :, 0:64], pv.rearrange("p (h d) -> p h d", h=8), eng="act" if i % 2 else "dve")
            for c in range(8):
                P.matmul(pf_[:, i * 8:(i + 1) * 8], xT[:, c, i * 128:(i + 1) * 128], Win[:, c, OFF_F:OFF_F + 8],
                         start=(c == 0), stop=(c == 7))
        if stop_after == "A2":
            P.dma(out[0:128, :], xin[0])
            P.emit()
            return nc
        nlf2 = nlf.rearrange("p a b -> p (a b)")
        P.tensor_tensor(nlf2, pf_[:, 0:128], rA[:, BF_:BF_ + 128], ALU.add)
        P.activation(nlf2, nlf2, AF.Exp, scale=-1.0)
        P.activation(nlf2, nlf2, AF.Ln, bias=one_c, scale=1.0)
        if stop_after == "A3a":
            P.dma(out[0:128, :], xin[0])
            P.emit()
            return nc
        P.copy(incl[:, 0, :], nlf[:, 0, :])
        for i in range(1, NT):
            P.tensor_tensor(incl[:, i, :], incl[:, i - 1, :], nlf[:, i, :], ALU.add)
        if stop_after == "A3b":
            P.dma(out[0:128, :], xin[0])
            P.emit()
            return nc
        pc1 = psf(7)[:, 0:128]
        pc2 = psf(7)[:, 128:256]
        for (pdst, lmat, srcf) in ((pc1, cb[:, ONEB:ONEB + 128], incl.rearrange("p a b -> p (a b)")),
                                   (pc2, cb[:, LTRIB:LTRIB + 128], nlf2)):
            rres = cz[0][:, 0:128]
            for t3 in range(3):
                piece = xn[0][:, t3 * 128:(t3 + 1) * 128]
                P.copy(piece, srcf if t3 == 0 else rres)
                if t3 < 2:
                    P.tensor_tensor(rres, srcf if t3 == 0 else rres, piece, ALU.subtract)
                P.matmul(pdst, lmat, piece, start=(t3 == 0), stop=(t3 == 2))
        P.copy(Cend.rearrange("p a b -> p (a b)"), pc1)
        P.copy(Cc.rearrange("p a b -> p (a b)"), pc2)
        if stop_after == "A3c":
            P.dma(out[0:128, :], xin[0])
            P.emit()
            return nc
        P.tensor_tensor(Cc[:, 1:16, :], Cc[:, 1:16, :], Cend[:, 0:15, :], ALU.add)
        if stop_after == "A3d":
            P.dma(out[0:128, :], xin[0])
            P.emit()
            return nc
        for h in range(8):
            P.tensor_tensor(Bt[:, h, :, :], Cc[:, :, h:h + 1].to_broadcast([128, 16, 16]),
                            Cend[:, :, h].unsqueeze(1).to_broadcast([128, 16, 16]), ALU.subtract)
        if "d_q" in dbg:
            P.dma(dbg["d_q"], qT.rearrange("p a b -> p (a b)"))
        if "d_z" in dbg:
            P.dma(dbg["d_z"], zT.rearrange("p a b -> p (a b)"))
        if "d_B" in dbg:
            P.dma(dbg["d_B"], Bt.rearrange("p a b c -> p (a b c)"))
        if stop_after == "A3":
            P.dma(out[0:128, :], xin[0])
            P.emit()
            return nc
        if 'conv' in BP:
            for i4 in range(4):
                for kk in range(31):
                    P.tensor_scalar(Dg[:, kk, :], identb, vv[:, WDW + i4 * 31 + kk:WDW + i4 * 31 + kk + 1], None, ALU.mult)
                for j4 in range(4):
                    pcv = psf(nb() % 6)
                    for t4 in range(4):
                        c0 = (j4 * 4 + t4) * 128
                        for kk in range(31):
                            P.matmul(pcv[:, t4 * 128:(t4 + 1) * 128], zT[:, i4, c0 + kk:c0 + kk + 128], Dg[:, kk, :],
                                     start=(kk == 0), stop=(kk == 30))
                    P.tensor_tensor(cvo[i4][:, j4 * 4:(j4 + 1) * 4, :], pcv.rearrange("p (t c) -> p t c", t=4),
                                    rA[:, BDW_ + i4 * 128:BDW_ + (i4 + 1) * 128].unsqueeze(1).to_broadcast([128, 4, 128]),
                                    ALU.add)
        for wi, wsrc in enumerate((w_out_bf, w_mq_bf, w_mo_bf)):
            for c in range(8):
                P.dma(Woqm[:, wi, c, :], wsrc[c * 128:(c + 1) * 128, :])

        catT = xT
        LAG = 3

        for j in range(NT):
            accb = [psf(2), psf(3)]
            pairs = [(h, kt) for h in range(8) for kt in range(j + 1)]
            stslots = {}
            n_pairs = len(pairs)

            def emit_qk(n):
                h, kt = pairs[n]
                stv = psf((0, 1, 4, 5, 6)[n % 5])[:, 0:128]
                r0 = 0 if 'evenonly' in BP else (h % 2) * 64
                if 'qk' in BP:
                    P.matmul(stv, kT[r0:r0 + 64, h // 2, kt * 128:(kt + 1) * 128], qT[r0:r0 + 64, h // 2, j * 128:(j + 1) * 128],
                             start=True, stop=(kt != j or 'mask' not in BP))
                if kt == j and 'mask' in BP:
                    P.matmul(stv, identb, negm, start=False, stop=True)
                pt = PT[n % 6]
                if 'exp' in BP:
                    if 'expdve' in BP:
                        P.copy(pt, stv, eng='dve')
                    elif 'expcopy' in BP:
                        P.copy(pt, stv, eng='act')
                    elif 'nobias' in BP:
                        P.activation(pt, stv, AF.Exp, scale=0.125)
                    else:
                        P.activation(pt, stv, AF.Exp, bias=Bt[:, h, kt, j:j + 1], scale=0.125)

            def emit_pv(n):
                h, kt = pairs[n]
                pt = PT[n % 6]
                av = accb[h // 4][:, (h % 4) * 65:(h % 4) * 65 + 65]
                if 'pv' in BP:
                    P.matmul(av, pt, vS[:, kt, h, :], start=(kt == 0), stop=(kt == j))

            for n in range(n_pairs + LAG):
                if n < n_pairs:
                    emit_qk(n)
                if n >= LAG:
                    emit_pv(n - LAG)
            ct = cat[j % 2]
            rden = stB[:, 0:8]
            for hh in range(2):
                av = accb[hh][:, 0:260].rearrange("p (h d) -> p h d", h=4)
                P.add("dve", lambda e, o=rden[:, hh * 4:(hh + 1) * 4], i=av[:, :, 64]: e.reciprocal(o, i),
                      reads=[av[:, :, 64]], writes=[rden[:, hh * 4:(hh + 1) * 4]])
                P.tensor_tensor(attn[:, hh * 256:(hh + 1) * 256].rearrange("p (h d) -> p h d", h=4), av[:, :, 0:64],
                                rden[:, hh * 4:(hh + 1) * 4].unsqueeze(2).to_broadcast([128, 4, 64]), ALU.mult)
            ssa, rsa = stB[:, 8:9], stB[:, 9:10]
            sumsq(cz[0], attn, ssa)
            rstd_from_ss(rsa, ssa, eps512)
            P.tensor_scalar(ct[:, 0:512], attn, rsa, None, ALU.mult)
            if "d_attn" in dbg:
                P.dma(dbg["d_attn"][j * 128:(j + 1) * 128, :], attn)
            s1, nm, sq, rsd = stB[:, 10:11], stB[:, 11:12], stB[:, 12:13], stB[:, 13:14]
            xc, tmp = cz[0], cz[1]
            for i4 in range(4):
                P.copy(xc[:, i4 * 128:(i4 + 1) * 128], cvo[i4][:, j, :], eng="pool" if i4 % 2 else "dve")
            if "d_conv" in dbg:
                P.dma(dbg["d_conv"][j * 128:(j + 1) * 128, :], xc)
            P.reduce(s1, xc, ALU.add)
            P.tensor_scalar(nm, s1, -1.0 / 512.0, None, ALU.mult)
            P.tensor_scalar(xc, xc, nm, None, ALU.add)
            sumsq(tmp, xc, sq)
            P.activation(rsd, sq, AF.Ln, bias=epsln, scale=1.0 / 512.0)
            P.activation(rsd, rsd, AF.Exp, scale=-0.5)
            P.stt(xc, xc, rsd, rA[:, LG_:LG_ + 512], ALU.mult, ALU.mult)
            P.tensor_tensor(xc, xc, rA[:, LB_:LB_ + 512], ALU.add)
            P.activation(tmp, xc, AF.Exp, scale=-1.0)
            P.tensor_scalar(tmp, tmp, 1.0, None, ALU.add)
            P.add("dve", lambda e, o=tmp: e.reciprocal(o, o), reads=[tmp], writes=[tmp])
            P.tensor_tensor(ct[:, 512:1024], xc, tmp, ALU.mult)
            if "d_cat" in dbg:
                P.dma(dbg["d_cat"][j * 128:(j + 1) * 128, :], ct)
            transposeT(ct, catT, j * 128, "act" if j % 2 else "dve", b=7)

        if stop_after == "B":
            P.dma(out[0:128, :], xin[0])
            P.emit()
            return nc
        P.dma(rG, rows[:, RG:RG + 1024])
        P.tensor_scalar(rG, rG, 32.0, None, ALU.mult)
        Wout, Wmq, Wmo = Woqm[:, 0], Woqm[:, 1], Woqm[:, 2]
        for blk in range(NT // 2):
            for tt in range(2):
                i = blk * 2 + tt
                P.dma(xr[tt], x[tok0 + i * 128: tok0 + (i + 1) * 128, :])
                for nh in range(2):
                    pv = psf(nb() % 6)
                    for c in range(8):
                        P.matmul(pv, catT[:, c, i * 128:(i + 1) * 128], Wout[:, c, nh * 512:(nh + 1) * 512],
                                 start=(c == 0), stop=(c == 7))
                    P.tensor_tensor(x1[:, tt, nh * 512:(nh + 1) * 512], pv, xr[tt][:, nh * 512:(nh + 1) * 512], ALU.add)
                if "d_x1" in dbg:
                    P.dma(dbg["d_x1"][i * 128:(i + 1) * 128, :], x1[:, tt, :])
                ss, rs = stC[:, 2 * tt:2 * tt + 1], stC[:, 2 * tt + 1:2 * tt + 2]
                sumsq(xn2[tt], x1[:, tt, :], ss)
                rstd_from_ss(rs, ss, eps1024)
                P.tensor_scalar(xn2[tt], x1[:, tt, :], rs, None, ALU.mult)
                transposeT(xn2[tt], h2T, tt * 128, "act" if tt else "dve")
            for cc in range(8):
                pv = psf(nb() % 6)[:, 0:256]
                for c in range(8):
                    P.matmul(pv, Wmq[:, c, cc * 128:(cc + 1) * 128], h2T[:, c, :], start=(c == 0), stop=(c == 7))
                P.copy(q2T[:, cc, :], pv, eng="act" if cc % 2 else "dve")
            for h in range(4):
                for mt in range(2):
                    pv = psf(nb() % 6)[:, 0:256]
                    for k2 in range(2):
                        P.matmul(pv, K2T[:, 2 * h + k2, mt * 128:(mt + 1) * 128], q2T[:, 2 * h + k2, :],
                                 start=(k2 == 0), stop=(k2 == 1))
                    P.activation(P2T[(h % 2) * 2 + mt + 4 * (blk % 2)], pv, AF.Exp, scale=1.0 / 16.0)
                for tt in range(2):
                    pa = psf(nb() % 6)[:, 0:257]
                    for mt in range(2):
                        P.matmul(pa, P2T[(h % 2) * 2 + mt + 4 * (blk % 2)][:, tt * 128:(tt + 1) * 128], V2[:, mt, h, :],
                                 start=(mt == 0), stop=(mt == 1))
                    rd = stC[:, 8 + tt:9 + tt]
                    P.add("dve", lambda e, o=rd, i_=pa[:, 256:257]: e.reciprocal(o, i_), reads=[pa[:, 256:257]], writes=[rd])
                    P.tensor_scalar(oo[:, tt, h * 256:(h + 1) * 256], pa[:, 0:256], rd, None, ALU.mult)
            for tt in range(2):
                transposeT(oo[:, tt, :], oT, tt * 128, "act" if tt else "dve")
            for tt in range(2):
                i = blk * 2 + tt
                gi = b * NT + i
                xo = x2t[tt]
                for nh in range(2):
                    pv = psf(nb() % 6)
                    for c in range(8):
                        P.matmul(pv, oT[:, c, tt * 128:(tt + 1) * 128], Wmo[:, c, nh * 512:(nh + 1) * 512],
                                 start=(c == 0), stop=(c == 7))
                    P.tensor_tensor(xo[:, nh * 512:(nh + 1) * 512], pv, x1[:, tt, nh * 512:(nh + 1) * 512], ALU.add)
                P.dma(x2s[tok0 + i * 128: tok0 + (i + 1) * 128, :], xo)
                ss, rs = stC[:, 4 + 2 * tt:5 + 2 * tt], stC[:, 5 + 2 * tt:6 + 2 * tt]
                hb = h3t[tt]
                sumsq(hb, xo, ss)
                rstd_from_ss(rs, ss, eps1024)
                P.stt(hb.rearrange("t (c p) -> t p c", c=8), xo.rearrange("t (p c) -> t p c", c=8), rs,
                      rG.rearrange("t (p c) -> t p c", c=8), ALU.mult, ALU.mult)
                P.dma(h3s[tok0 + i * 128: tok0 + (i + 1) * 128, :], hb)
                transposeT(hb, h3T, 0, "act" if tt else "dve")
                pl = psf(7)[:, 256 + 64 * tt:256 + 64 * tt + 36]
                for c in range(8):
                    P.matmul(pl, h3T[:, c, :], wr_bf[:, c, :], start=(c == 0), stop=(c == 7))
                route_tile(gi, pl, tt)

    if "d_rt" in dbg:
        P.dma(dbg["d_rt"][:, 0:NTT], G1[:, 0:NTT]); P.dma(dbg["d_rt"][:, 64:64 + NTT], G2[:, 0:NTT])
        P.dma(dbg["d_rt"][:, 128:128 + NTT], P1[:, 0:NTT]); P.dma(dbg["d_rt"][:, 192:192 + NTT], P2[:, 0:NTT])
        P.dma(dbg["d_rt"][:, 256:288], base)
    if stop_after == "phase1":
        P.dma(out[0:128, :], x2t[0])
        P.emit()
        return nc

    A.off = mark_persist
    pcf = A.new(F32, [32]); pci = A.new(I32, [32]); offs = A.new(F32, [32]); ends = A.new(F32, [32])
    te = A.new(F32, [96]); gidx = A.new(I32, [96]); didx = A.new(I32, [96, 4]); tf = A.new(F32, [96])
    zero_i = A.new(I32, [192]); big = A.new(F32, [64, 32]); posf = A.new(F32, [64])
    P.tensor_scalar(pcf, base, 127.5, 1.0 / 256.0, ALU.add, ALU.mult)
    P.copy(pci, pcf)
    P.copy(pcf, pci)
    P.tensor_scalar(pcf, pcf, 256.0, None, ALU.mult)
    P.memset(offs[:, 0:1], 0.0)
    for e in range(1, 32):
        P.tensor_tensor(offs[:, e:e + 1], offs[:, e - 1:e], pcf[:, e - 1:e], ALU.add)
    P.tensor_tensor(ends, offs, pcf, ALU.add)
    P.memset(te, 0.0)
    for e in range(32):
        P.stt(te, cf[:, THR:THR + 96], ends[:, e:e + 1], te, ALU.is_ge, ALU.add)
    P.tensor_scalar(te, te, 31.0, None, ALU.min)
    if "d_te" in dbg:
        P.dma(dbg["d_te"], te)
    P.tensor_scalar(tf, te, 128.0, cf[:, PCOL:PCOL + 1], ALU.mult, ALU.add)
    P.copy(gidx, tf)
    for c in range(4):
        P.tensor_scalar(tf, te, 512.0, cf[:, CBASE + c:CBASE + c + 1], ALU.mult, ALU.add)
        P.copy(didx[:, :, c], tf)
    for (Ax, Px, POSx) in ((A1, P1, POS1), (A2, P2, POS2)):
        P.tensor_tensor(big[:, 0:NTT, :], Ax[:, 0:NTT, :], offs.unsqueeze(1).to_broadcast([128, NTT, 32]), ALU.mult)
        P.reduce(posf[:, 0:NTT], big[:, 0:NTT, :], ALU.add)
        P.tensor_tensor(posf[:, 0:NTT], posf[:, 0:NTT], Px[:, 0:NTT], ALU.add)
        P.copy(POSx[:, 0:NTT], posf[:, 0:NTT])
    P.memset(zero_i, 0)
    P.dma(s2t.rearrange("(p n) o -> p (n o)", p=128), zero_i)
    FAKE = "D:s2t_scatter"
    nsc = 0
    for gi in range(NTT):
        for POSx in (POS1, POS2):
            P.add("pool", lambda e, po=POSx[:, gi:gi + 1], ti=ci[:, gi:gi + 1]: e.indirect_dma_start(
                out=s2t, out_offset=bass.IndirectOffsetOnAxis(ap=po, axis=0), in_=ti, in_offset=None),
                reads=[POSx[:, gi:gi + 1], ci[:, gi:gi + 1], s2t], writes=[(FAKE, 0, 1, nsc, nsc + 1)], dma=True)
            nsc += 1

    mark_p2 = A.off
    wg = [A.new(BF16, [8, 512]) for _ in range(3)]
    wu = [A.new(BF16, [8, 512]) for _ in range(3)]
    wd = [A.new(BF16, [4, 1024]) for _ in range(3)]
    sidx = [A.new(I32, [2]) for _ in range(3)]
    hs = [A.new(BF16, [2, 1024]) for _ in range(2)]
    hTs = [A.new(BF16, [8, 256]) for _ in range(2)]
    sil = [A.new(F32, [256]) for _ in range(2)]
    hid = [A.new(BF16, [4, 256]) for _ in range(2)]
    yt = [A.new(F32, [1024]) for _ in range(3)]
    s2t_v = s2t.rearrange("(j g p) o -> j p (g o)", g=2, p=128)
    ny = 0
    for j in range(n_moe_tiles):
        k3 = j % 3
        for (wdst, wsrc) in ((wg[k3], w_gate), (wu[k3], w_up)):
            P.add("pool", lambda e, o=wdst.rearrange("p a b -> p (a b)"), s=wsrc, ix=gidx[:, j:j + 1]: e.indirect_dma_start(
                out=o, out_offset=None, in_=s, in_offset=bass.IndirectOffsetOnAxis(ap=ix, axis=0)),
                reads=[gidx[:, j:j + 1], wsrc], writes=[wdst], dma=True)
        for c in range(4):
            P.add("pool", lambda e, o=wd[k3][:, c, :], ix=didx[:, j, c:c + 1]: e.indirect_dma_start(
                out=o, out_offset=None, in_=w_down, in_offset=bass.IndirectOffsetOnAxis(ap=ix, axis=0)),
                reads=[didx[:, j, c:c + 1], w_down], writes=[wd[k3][:, c, :]], dma=True)
        P.add("sp", lambda e, o=sidx[k3], s=s2t_v[j]: e.dma_start(out=o, in_=s, allow_slow_non_contiguous=True),
              reads=[s2t_v[j], (FAKE, 0, 1, 0, 1 << 20)], writes=[sidx[k3]], dma=True)
        hsj, hTj, hidj = hs[j % 2], hTs[j % 2], hid[j % 2]
        for g in range(2):
            P.add("pool", lambda e, o=hsj[:, g, :], ix=sidx[k3][:, g:g + 1]: e.indirect_dma_start(
                out=o, out_offset=None, in_=h3s, in_offset=bass.IndirectOffsetOnAxis(ap=ix, axis=0)),
                reads=[sidx[k3][:, g:g + 1], h3s], writes=[hsj[:, g, :]], dma=True)
            transposeT(hsj[:, g, :], hTj, g * 128, "act" if g else "dve")
        for fc in range(4):
            pg = psf(nb() % 6)[:, 0:256]
            for c in range(8):
                P.matmul(pg, wg[k3][:, c, fc * 128:(fc + 1) * 128], hTj[:, c, :], start=(c == 0), stop=(c == 7))
            pu = psf(nb() % 6)[:, 0:256]
            for c in range(8):
                P.matmul(pu, wu[k3][:, c, fc * 128:(fc + 1) * 128], hTj[:, c, :], start=(c == 0), stop=(c == 7))
            sl = sil[fc % 2]
            P.activation(sl, pg, AF.Silu)
            P.tensor_tensor(hidj[:, fc, :], sl, pu, ALU.mult)
        for g in range(2):
            y_ = yt[ny % 3]
            ny += 1
            for nh in range(2):
                pv = psf(nb() % 6)
                for fc in range(4):
                    P.matmul(pv, hidj[:, fc, g * 128:(g + 1) * 128], wd[k3][:, fc, nh * 512:(nh + 1) * 512],
                             start=(fc == 0), stop=(fc == 3))
                P.copy(y_[:, nh * 512:(nh + 1) * 512], pv, eng="act" if nh else "dve")
            P.dma(Ysc[j * 256 + g * 128: j * 256 + (g + 1) * 128, :], y_)

    A.off = mark_p2
    fg = A.new(F32, [1024])
    NB4 = 4
    x2b = [A.new(F32, [1024]) for _ in range(NB4)]
    y1b = [A.new(F32, [1024]) for _ in range(NB4)]
    y2b = [A.new(F32, [1024]) for _ in range(NB4)]
    otb = [A.new(F32, [1024]) for _ in range(NB4)]
    st4 = A.new(F32, [2 * NB4])
    P.dma(fg, rows[:, RF:RF + 1024])
    P.tensor_scalar(fg, fg, 32.0, None, ALU.mult)
    for gi in range(NTT):
        k2 = gi % NB4
        P.dma(x2b[k2], x2s[gi * 128:(gi + 1) * 128, :])
        for (yb, POSx) in ((y1b[k2], POS1), (y2b[k2], POS2)):
            P.add("pool", lambda e, o=yb, ix=POSx[:, gi:gi + 1]: e.indirect_dma_start(
                out=o, out_offset=None, in_=Ysc, in_offset=bass.IndirectOffsetOnAxis(ap=ix, axis=0)),
                reads=[POSx[:, gi:gi + 1], Ysc], writes=[yb], dma=True)
        acc = x2b[k2]
        if skip_moe:
            pass
        else:
            P.stt(acc, y1b[k2], G1[:, gi:gi + 1], acc, ALU.mult, ALU.add)
            P.stt(acc, y2b[k2], G2[:, gi:gi + 1], acc, ALU.mult, ALU.add)
        ss, rs = st4[:, 2 * k2:2 * k2 + 1], st4[:, 2 * k2 + 1:2 * k2 + 2]
        sumsq(otb[k2], acc, ss)
        rstd_from_ss(rs, ss, eps1024)
        P.stt(otb[k2], acc, rs, fg, ALU.mult, ALU.mult)
        P.dma(out[gi * 128:(gi + 1) * 128, :], otb[k2])
    cnt = P.emit()
    st.close()
    return nc


def _consts():
    p = np.arange(128)
    cfm = np.zeros((128, NCF), np.float32)
    cfm[:, IDF:IDF + 128] = np.eye(128)
    cfm[:, LTRI:LTRI + 128] = (p[:, None] <= p[None, :])
    cfm[:, ONESF:ONESF + 128] = 1.0
    cfm[:, THR:THR + 96] = 256.0 * np.arange(96)[None, :]
    cfm[:, PCOL] = p
    cfm[:, CBASE:CBASE + 4] = np.arange(4)[None, :] * 128 + p[:, None]
    cfm[:, IOTA:IOTA + 32] = np.arange(32)[None, :]
    cfm[:, CEPS:CEPS + 4] = np.array([1024e-6, 512e-6, 1.0, 1e-6], np.float32)[None, :]
    cbm = np.zeros((128, NCB), np.float32)
    cbm[:, IDB:IDB + 128] = np.eye(128)
    cbm[:, NEGM:NEGM + 128] = np.where(p[:, None] > p[None, :], -30000.0, 0.0)
    cbm[:, UST:UST + 128] = (p[:, None] < p[None, :])
    cbm[:, ONEB:ONEB + 128] = 1.0
    cbm[:, LTRIB:LTRIB + 128] = (p[:, None] <= p[None, :])
    cim = (np.arange(64)[None, :] * 128 + p[:, None]).astype(np.int32)
    return cfm, cbm.astype(ml_dtypes.bfloat16), cim


def _host_inputs(core, nseq, x, mem, norm_mix_g, w_in, b_forget, b_glu, w_dw, b_dw, conv_ln_g, conv_ln_b,
                 attn_out_g, w_out, norm_mem_g, mem_norm_g, w_mq, w_mkv, w_mo, norm_ffn_g,
                 w_route_group, b_route_group, w_route_expert, b_route_expert, w_gate, w_up, w_down, final_g):
    f = lambda a: np.ascontiguousarray(np.asarray(a, dtype=np.float32))
    cfm, cbm, cim = _consts()
    vecs = np.zeros((128, NV), np.float32)
    vecs[:, GM:GM + 8] = f(norm_mix_g)[0].reshape(8, 128).T
    vecs[:, GQ:GQ + 8] = f(norm_mem_g)[0].reshape(8, 128).T
    vecs[:, GK:GK + 8] = f(mem_norm_g)[0].reshape(8, 128).T
    vecs[:, GA:GA + 4] = f(attn_out_g)[0].reshape(4, 128).T
    vecs[:, BG:BG + 8] = f(b_glu)[0].reshape(8, 128).T
    vecs[:, BD:BD + 4] = f(b_dw)[0].reshape(4, 128).T
    vecs[:, WDW:WDW + 124] = f(w_dw)[0].reshape(31, 4, 128).transpose(2, 1, 0).reshape(128, 124)
    rows = np.zeros((1, NR), np.float32)
    rows[0, RG:RG + 1024] = f(norm_ffn_g)[0]
    rows[0, RF:RF + 1024] = f(final_g)
    rows[0, RLG:RLG + 512] = f(conv_ln_g)[0]
    rows[0, RLB:RLB + 512] = f(conv_ln_b)[0]
    rows[0, RBF:RBF + 128] = np.tile(f(b_forget)[0], 16)
    rows[0, RBR:RBR + 4] = f(b_route_group)[0]
    rows[0, RBR + 4:RBR + 36] = f(b_route_expert)[0].reshape(32)
    rows[0, RBDW:RBDW + 512] = f(b_dw)[0]
    rows = np.ascontiguousarray(np.broadcast_to(rows, (128, NR)))
    wr = np.concatenate([f(w_route_group)[0], f(w_route_expert)[0].transpose(1, 0, 2).reshape(1024, 32)], axis=1)
    return {
        "x": f(x[core * nseq:(core + 1) * nseq]).reshape(nseq * S, D),
        "mem": f(mem[core * nseq:(core + 1) * nseq]).reshape(nseq * 256, D),
        "w_in": f(w_in)[0], "w_out": f(w_out)[0], "w_mq": f(w_mq)[0], "w_mkv": f(w_mkv)[0], "w_mo": f(w_mo)[0],
        "w_route": np.ascontiguousarray(wr.reshape(128, 8 * 36)),
        "w_gate": f(w_gate)[0].reshape(32 * 128, 4096), "w_up": f(w_up)[0].reshape(32 * 128, 4096),
        "w_down": f(w_down)[0].reshape(32 * 512, 1024),
        "vecs": vecs, "rows": rows, "constf": cfm, "constb": cbm, "consti": cim,
    }


_NC_CACHE = {}


def kernel(**inputs):
    n_cores = 8
    nseq = 4
    if "full" not in _NC_CACHE:
        _NC_CACHE["full"] = build(nseq=nseq)
    nc = _NC_CACHE["full"]
    in_maps = [_host_inputs(c, nseq, **inputs) for c in range(n_cores)]
    res = run_bass_kernel_spmd(nc, in_maps, core_ids=list(range(n_cores)))
    outs = [np.asarray(r["out"]).reshape(nseq, S, D) for r in res.results]
    return np.concatenate(outs, axis=0).astype(np.float32)
```

```python
import numpy as np
import ml_dtypes
from concourse.bass_utils import run_bass_kernel_spmd
from contextlib import ExitStack
import concourse.bass as bass
import concourse.mybir as mybir

F32 = mybir.dt.float32
BF16 = mybir.dt.bfloat16
I32 = mybir.dt.int32
ALU = mybir.AluOpType
AF = mybir.ActivationFunctionType
AX = mybir.AxisListType
_DSZ = {F32: 4, BF16: 2, I32: 4, mybir.dt.uint32: 4, mybir.dt.float16: 2, mybir.dt.uint8: 1,
        mybir.dt.int8: 1, mybir.dt.uint16: 2, mybir.dt.int16: 2}


def region(ap):
    dsz = _DSZ[ap.dtype]
    dims = ap.ap
    off = ap.offset
    sp = str(ap.space)
    if sp in ("SB", "PSUM"):
        shp = ap.tensor.shape
        rowlen = 1
        for s in list(shp)[1:]:
            rowlen *= s
        p0 = off // rowlen
        c0 = off % rowlen
        pstep, pcnt = dims[0]
        p1 = p0 + ((pcnt - 1) * pstep) // rowlen + 1 if pstep else p0 + 1
        ext = 0
        for st, cn in dims[1:]:
            ext += (cn - 1) * abs(st)
        if sp == "PSUM":
            return (sp + ":" + ap.tensor.name, 0, 128, 0, 1 << 20)
        return (sp + ":" + ap.tensor.name, p0, p1, c0 * dsz, (c0 + ext + 1) * dsz)
    else:
        ext = 0
        for st, cn in dims:
            ext += (cn - 1) * abs(st)
        return ("D:" + ap.tensor.name, 0, 1, off * dsz, (off + ext + 1) * dsz)


class Op:
    __slots__ = ("eng", "fn", "reads", "writes", "dma", "deps", "sig", "dsem", "dval",
                 "waits", "idx", "prewait")


class Prog:
    ENGS = ("pe", "act", "dve", "pool", "sp")
    EPOCH = 30000

    def __init__(self, nc, ndma_sems=56):
        self.nc = nc
        self.ops = []
        self.recs = {}
        self.ndma = ndma_sems
        self.extra_regions = {}

    def reg(self, e, val):
        d = self.__dict__.setdefault("_regs", {})
        if val not in d:
            d[val] = e.to_reg(val)
        return d[val]

    def add(self, eng, fn, reads=(), writes=(), dma=False):
        op = Op()
        op.eng = eng; op.fn = fn; op.dma = dma
        op.reads = [r if isinstance(r, tuple) else region(r) for r in reads]
        op.writes = [r if isinstance(r, tuple) else region(r) for r in writes]
        op.idx = len(self.ops)
        op.sig = None; op.dsem = None; op.dval = None; op.waits = []; op.prewait = None
        deps = set()
        for (sp, p0, p1, lo, hi) in op.reads:
            for r in self.recs.get(sp, ()):
                if r[5] and r[0] < p1 and p0 < r[1] and r[2] < hi and lo < r[3]:
                    deps.add(r[4])
        for (sp, p0, p1, lo, hi) in op.writes:
            lst = self.recs.get(sp)
            if lst is None:
                lst = self.recs[sp] = []
            keep = []
            for r in lst:
                if r[0] < p1 and p0 < r[1] and r[2] < hi and lo < r[3]:
                    deps.add(r[4])
                    if r[0] >= p0 and r[1] <= p1 and r[2] >= lo and r[3] <= hi:
                        continue
                keep.append(r)
            keep.append([p0, p1, lo, hi, op.idx, True])
            self.recs[sp] = keep
        for (sp, p0, p1, lo, hi) in op.reads:
            lst = self.recs.get(sp)
            if lst is None:
                lst = self.recs[sp] = []
            if not dma:
                for r in lst:
                    if (not r[5]) and r[0] == p0 and r[1] == p1 and r[2] == lo and r[3] == hi \
                            and (not self.ops[r[4]].dma) and self.ops[r[4]].eng == eng:
                        r[4] = op.idx
                        break
                else:
                    lst.append([p0, p1, lo, hi, op.idx, False])
            else:
                lst.append([p0, p1, lo, hi, op.idx, False])
        deps.discard(op.idx)
        op.deps = deps
        self.ops.append(op)
        return op

    def matmul(self, out, lhsT, rhs, start=True, stop=True, **kw):
        return self.add("pe", lambda e: e.matmul(out, lhsT, rhs, start=start, stop=stop, **kw),
                        reads=[lhsT, rhs], writes=[out])

    def transpose(self, out, in_, ident):
        return self.add("pe", lambda e: e.transpose(out, in_, ident), reads=[in_, ident], writes=[out])

    def activation(self, out, in_, func, bias=None, scale=None, accum_out=None, eng="act"):
        reads = [in_]
        kw = {}
        if bias is not None:
            kw["bias"] = bias
            if not isinstance(bias, (int, float)):
                reads.append(bias)
        if scale is not None:
            kw["scale"] = scale
            if not isinstance(scale, (int, float)):
                reads.append(scale)
        writes = [out]
        if accum_out is not None:
            kw["accum_out"] = accum_out
            writes.append(accum_out)
        return self.add(eng, lambda e: e.activation(out, in_, func, **kw), reads=reads, writes=writes)

    def tensor_scalar(self, out, in0, s1, s2, op0, op1=None, eng="dve", accum_out=None):
        reads = [in0]
        for s in (s1, s2):
            if s is not None and not isinstance(s, (int, float)):
                reads.append(s)
        kw = {}
        if op1 is not None:
            kw["op1"] = op1
        writes = [out]
        if accum_out is not None:
            kw["accum_out"] = accum_out
            writes.append(accum_out)
        return self.add(eng, lambda e: e.tensor_scalar(out, in0, s1, s2, op0, **kw), reads=reads, writes=writes)

    def tensor_tensor(self, out, in0, in1, op, eng="dve"):
        return self.add(eng, lambda e: e.tensor_tensor(out, in0, in1, op), reads=[in0, in1], writes=[out])

    def stt(self, out, in0, scalar, in1, op0, op1, eng="dve", accum_out=None):
        reads = [in0, in1]
        if not isinstance(scalar, (int, float)):
            reads.append(scalar)
        kw = {}
        writes = [out]
        if accum_out is not None:
            kw["accum_out"] = accum_out
            writes.append(accum_out)
        return self.add(eng, lambda e: e.scalar_tensor_tensor(out, in0, scalar, in1, op0, op1, **kw),
                        reads=reads, writes=writes)

    def copy(self, out, in_, eng="dve"):
        if eng == "act":
            return self.add(eng, lambda e: e.copy(out, in_), reads=[in_], writes=[out])
        return self.add(eng, lambda e: e.tensor_copy(out, in_), reads=[in_], writes=[out])

    def memset(self, out, val, eng="dve"):
        return self.add(eng, lambda e: e.memset(out, val), reads=[], writes=[out])

    def reduce(self, out, in_, op, axis=None, eng="dve"):
        ax = axis if axis is not None else AX.X
        return self.add(eng, lambda e: e.tensor_reduce(out, in_, ax, op), reads=[in_], writes=[out])

    def dma(self, out, in_, q="sp", **kw):
        return self.add(q, lambda e: e.dma_start(out=out, in_=in_, **kw), reads=[in_], writes=[out], dma=True)

    def finalize(self):
        ops = self.ops
        need = [False] * len(ops)
        for op in ops:
            for d in op.deps:
                p = ops[d]
                if p.dma:
                    continue
                if p.eng == op.eng and not op.dma:
                    if p.eng == "pe":
                        continue
                    continue_flag = True
                    for (sp, p0, p1, lo, hi) in op.reads:
                        for (sp2, q0, q1, lo2, hi2) in p.writes:
                            if sp == sp2 and q0 < p1 and p0 < q1 and lo2 < hi and lo < hi2:
                                continue_flag = False
                    if continue_flag:
                        continue
                need[d] = True
        cnt = {e: 0 for e in self.ENGS}
        for op in ops:
            if (not op.dma) and need[op.idx]:
                cnt[op.eng] += 1
                op.sig = cnt[op.eng]
        dcount = [0] * self.ndma
        k = 0
        for op in ops:
            if op.dma:
                s = k % self.ndma
                k += 1
                dcount[s] += 1
                op.dsem = s
                op.dval = 16 * dcount[s]
                op.prewait = (("dma", s), 16 * (dcount[s] - 1)) if dcount[s] > 1 else None
        self.dfinal = [16 * c for c in dcount]
        seen = {e: {} for e in self.ENGS}
        pos = {e: 0 for e in self.ENGS}
        opos = {}
        for op in ops:
            pos[op.eng] += 1
            opos[op.idx] = pos[op.eng]
        nw = 0
        for op in ops:
            sn = seen[op.eng]
            w = {}
            if op.prewait is not None:
                key, val = op.prewait
                if sn.get(key, 0) < val:
                    w[key] = val
            for d in op.deps:
                p = ops[d]
                if p.dma:
                    key, val = ("dma", p.dsem), p.dval
                else:
                    if p.sig is None:
                        continue
                    if p.eng == op.eng and not op.dma:
                        if p.eng == "pe":
                            continue
                        if opos[op.idx] - opos[p.idx] > 3:
                            continue
                    ep = (p.sig - 1) // self.EPOCH
                    key, val = ("eng", p.eng), ep * 100000 + (p.sig - 1) % self.EPOCH + 1
                if sn.get(key, 0) < val and w.get(key, 0) < val:
                    w[key] = val
            for key, val in w.items():
                sn[key] = val
            op.waits = list(w.items())
            nw += len(op.waits)
        self.n_waits = nw
        self.n_epochs = {e: (cnt[e] - 1) // self.EPOCH + 1 if cnt[e] else 1 for e in self.ENGS}
        return cnt

    def emit(self):
        nc = self.nc
        cnt = self.finalize()
        ops = self.ops
        with ExitStack() as st:
            esem = {}
            for e in self.ENGS:
                for ep in range(self.n_epochs[e]):
                    esem[(e, ep)] = st.enter_context(nc.semaphore("s_%s%d" % (e, ep)))
            dsem = [st.enter_context(nc.semaphore("d%d" % i)) for i in range(self.ndma)]
            block = st.enter_context(nc.Block())
            by_eng = {e: [op for op in ops if op.eng == e] for e in self.ENGS}
            EP = self.EPOCH

            def run(eng_obj, lst, is_sp):
                for op in lst:
                    for key, val in op.waits:
                        if key[0] == "dma":
                            eng_obj.wait_ge(dsem[key[1]], val)
                        else:
                            ep, v = divmod(val, 100000)
                            eng_obj.wait_ge(esem[(key[1], ep)], v)
                    inst = op.fn(eng_obj)
                    if op.dma:
                        inst.then_inc(dsem[op.dsem], 16)
                    elif op.sig is not None:
                        ep = (op.sig - 1) // EP
                        inst.then_inc(esem[(op.eng, ep)], 1)
                if is_sp:
                    for i, v in enumerate(self.dfinal):
                        if v:
                            eng_obj.wait_ge(dsem[i], v)

            @block.sync
            def _(e):
                run(e, by_eng["sp"], True)

            @block.tensor
            def _(e):
                run(e, by_eng["pe"], False)

            @block.scalar
            def _(e):
                run(e, by_eng["act"], False)

            @block.vector
            def _(e):
                run(e, by_eng["dve"], False)

            @block.gpsimd
            def _(e):
                run(e, by_eng["pool"], False)
        return cnt

S = 2048
D = 1024
NT = S // 128
NTILES = 96
TS = 256
NSLOT = NTILES * TS
GM, GQ, GK, GA, BG, BD, WDW = 0, 8, 16, 24, 28, 36, 40
NV = 40 + 4 * 31
RG, RF, RLG, RLB, RBF, RBR, RBDW = 0, 1024, 2048, 2560, 3072, 3200, 3236
NR = 3748
THR, PCOL, CBASE, IOTA, CEPS, NCF = 0, 96, 97, 101, 133, 141
IDB, NEGM, UST, ONEB, LTRIB, NCB = 0, 128, 256, 384, 512, 640


def _prod(s):
    r = 1
    for a in s:
        r *= a
    return r


class Arena:
    def __init__(self, nc, st, name, nbytes):
        self.t = st.enter_context(nc.sbuf_tensor(name, [128, nbytes // 4], F32))
        self.cap = nbytes
        self.off = 0

    def alloc(self, nbytes):
        off = (self.off + 63) // 64 * 64
        self.off = off + nbytes
        assert self.off <= self.cap, ("arena overflow", self.off, self.cap)
        return off

    def view(self, off, dtype, shape):
        n = _prod(shape)
        dsz = _DSZ[dtype]
        nb = (n * dsz + 3) // 4
        v = self.t[:, off // 4: off // 4 + nb]
        if dtype != F32:
            v = v.bitcast(dtype)
        v = v[:, 0:n]
        if len(shape) == 2:
            v = v.rearrange("p (a b) -> p a b", a=shape[0], b=shape[1])
        elif len(shape) == 3:
            v = v.rearrange("p (a b c) -> p a b c", a=shape[0], b=shape[1], c=shape[2])
        return v

    def new(self, dtype, shape):
        off = self.alloc(_prod(shape) * _DSZ[dtype])
        return self.view(off, dtype, shape)


import os as _os
BP = set(_os.environ.get('BPARTS', 'conv,convT,qk,mask,exp,pv,norm,tail,catT').split(','))


def build(nseq=4, debug=(), stop_after=None, n_moe_tiles=NTILES, skip_moe=False):
    nc = bass.Bass("TRN2", target_bir_lowering=False)
    NTOK = nseq * S
    NTT = NTOK // 128

    def din(name, shape, dt=F32):
        return nc.dram_tensor(name, shape, dt, kind="ExternalInput").ap()

    def scr(name, shape, dt):
        if name in debug:
            return nc.dram_tensor(name, shape, dt, kind="ExternalOutput").ap()
        return nc.dram_tensor(name, shape, dt).ap()

    x = din("x", [NTOK, D])
    mem = din("mem", [nseq * 256, D])
    w_in = din("w_in", [D, 2568])
    w_out = din("w_out", [D, D])
    w_mq = din("w_mq", [D, D])
    w_mkv = din("w_mkv", [D, 2 * D])
    w_mo = din("w_mo", [D, D])
    w_route = din("w_route", [128, 8 * 36])
    w_gate = din("w_gate", [32 * 128, 4096])
    w_up = din("w_up", [32 * 128, 4096])
    w_down = din("w_down", [32 * 512, 1024])
    vecs = din("vecs", [128, NV])
    rows = din("rows", [128, NR])
    constf = din("constf", [128, NCF])
    constb = din("constb", [128, NCB], BF16)
    consti = din("consti", [128, 64], I32)
    out = nc.dram_tensor("out", [NTOK, D], F32, kind="ExternalOutput").ap()

    w_in_bf = scr("w_in_bf", [D, 2568], BF16)
    w_out_bf = scr("w_out_bf", [D, D], BF16)
    w_mq_bf = scr("w_mq_bf", [D, D], BF16)
    w_mkv_bf = scr("w_mkv_bf", [D, 2 * D], BF16)
    w_mo_bf = scr("w_mo_bf", [D, D], BF16)
    x2s = scr("x2s", [NTOK, D], F32)
    h3s = scr("h3s", [NTOK, D], BF16)
    Ysc = scr("Ysc", [NSLOT, D], F32)
    s2t = scr("s2t", [NSLOT, 1], I32)
    dbg = {}
    for name, shape, dt in (("d_cat", [S, D], BF16), ("d_x1", [S, D], F32), ("d_lg", [NTOK, 36], F32),
                            ("d_q", [128, 4 * 2048], BF16), ("d_attn", [S, 512], F32), ("d_z", [128, 4 * 2080], BF16),
                            ("d_B", [128, 2048], F32), ("d_rt", [128, 64 * 8], F32), ("d_te", [128, 96], F32),
                            ("d_conv", [S, 512], F32)):
        if name in debug:
            dbg[name] = nc.dram_tensor(name, shape, dt, kind="ExternalOutput").ap()

    P = Prog(nc)
    st = ExitStack()
    A = Arena(nc, st, "arena", 210944)
    PS = [st.enter_context(nc.psum_tensor("ps%d" % i, [128, 512], F32)) for i in range(8)]

    def psf(i):
        return PS[i][:, :]

    def psb(i):
        return PS[i][:, :].bitcast(BF16)

    cf = A.new(F32, [NCF])
    cb = A.new(BF16, [NCB])
    ci = A.new(I32, [64])
    vv = A.new(F32, [NV])
    rA = A.new(F32, [1700])
    LG_, LB_, BF_, BR_, BDW_ = 0, 512, 1024, 1152, 1188
    A1 = A.new(BF16, [64, 32])
    A2 = A.new(BF16, [64, 32])
    G1 = A.new(F32, [64]); G2 = A.new(F32, [64]); P1 = A.new(F32, [64]); P2 = A.new(F32, [64])
    POS1 = A.new(I32, [64]); POS2 = A.new(I32, [64])
    base = A.new(F32, [32])
    wr_bf = A.new(BF16, [8, 36])
    v_ = None
    identb = cb[:, IDB:IDB + 128]
    negm = cb[:, NEGM:NEGM + 128]
    eps1024 = cf[:, CEPS:CEPS + 1]
    eps512 = cf[:, CEPS + 1:CEPS + 2]
    one_c = cf[:, CEPS + 2:CEPS + 3]
    epsln = cf[:, CEPS + 3:CEPS + 4]
    P.dma(cf, constf); P.dma(cb, constb); P.dma(ci, consti); P.dma(vv, vecs)
    P.dma(rA, rows[:, RLG:RLG + 1700])
    P.memset(base, 0.0)
    P.tensor_scalar(vv[:, GM:GM + 24], vv[:, GM:GM + 24], 32.0, None, ALU.mult)
    P.tensor_scalar(vv[:, GA:GA + 4], vv[:, GA:GA + 4], float(np.sqrt(512.0)), None, ALU.mult)
    mark_persist = A.off

    Wreg_off = A.alloc(49152)
    Win = A.view(Wreg_off, BF16, [8, 2568])
    Woqm = A.view(Wreg_off, BF16, [3, 8, 1024])
    xT = A.new(BF16, [8, 2048])
    K2T = A.new(BF16, [8, 256])
    V2 = A.new(BF16, [2, 4, 257])
    Bt = A.new(F32, [8, 16, 16])
    nlf = A.new(F32, [16, 8]); incl = A.new(F32, [16, 8]); Cc = A.new(F32, [16, 8]); Cend = A.new(F32, [16, 8])
    U_off = A.alloc(66048 + 64)
    qT = A.view(U_off, BF16, [4, 2048])
    kT = A.view(U_off + 16384, BF16, [4, 2048])
    Wkv = A.view(U_off, BF16, [8, 2048])
    vS = A.view(U_off + 32768, BF16, [16, 8, 65])
    zT = A.view(U_off + 32768 + 16640, BF16, [4, 2080])
    memT = A.view(U_off + 32768 + 16640, BF16, [8, 256])
    T_off = A.alloc(23808)
    xin = [A.view(T_off + i * 4096, F32, [1024]) for i in range(2)]
    xn = [A.view(T_off + 8192 + i * 2048, BF16, [1024]) for i in range(2)]
    sig = [A.view(T_off + 12288 + i * 2048, F32, [512]) for i in range(2)]
    stA = A.view(T_off + 16384, F32, [16])
    PT = [A.view(T_off + i * 256, BF16, [128]) for i in range(6)]
    attn = A.view(T_off + 1536, F32, [512])
    catA = A.view(T_off + 3584, BF16, [512])
    catC = [A.view(T_off + 4608 + i * 1024, BF16, [512]) for i in range(2)]
    cz = [A.view(T_off + 6656 + i * 2048, F32, [512]) for i in range(2)]
    stB = A.view(T_off + 10752, F32, [64])
    Dg = A.view(T_off + 11008, BF16, [31, 128])
    zoff = U_off + 32768 + 16640
    cvo = [A.view(T_off + 18944, BF16, [16, 128])] + [A.view(zoff + i * 4160, BF16, [16, 128]) for i in range(3)]
    o_ = U_off
    x1 = A.view(o_, F32, [2, 1024]); o_ += 8192
    xr = [A.view(o_ + i * 4096, F32, [1024]) for i in range(2)]; o_ += 8192
    xn2 = [A.view(o_ + i * 2048, BF16, [1024]) for i in range(2)]; o_ += 4096
    h2T = A.view(o_, BF16, [8, 256]); o_ += 4096
    q2T = A.view(o_, BF16, [8, 256]); o_ += 4096
    P2T = [A.view(o_ + i * 512, BF16, [256]) for i in range(8)]; o_ += 4096
    oo = A.view(o_, BF16, [2, 1024]); o_ += 4096
    oT = A.view(o_, BF16, [8, 256]); o_ += 4096
    x2t = [A.view(o_ + i * 4096, F32, [1024]) for i in range(2)]; o_ += 8192
    h3t = [A.view(o_ + i * 2048, BF16, [1024]) for i in range(2)]; o_ += 4096
    h3T = A.view(o_, BF16, [8, 128]); o_ += 2048
    rG = A.view(o_, F32, [1024]); o_ += 4096
    rt = A.view(o_, F32, [256]); o_ += 1024
    rtb = A.view(o_, BF16, [64]); o_ += 128
    stC = A.view(o_, F32, [32]); o_ += 128
    assert o_ <= U_off + 66048

    st0 = [A.view(U_off + i * 15488, F32, [2568]) for i in range(4)]
    st0b = [A.view(U_off + 10304 + i * 15488, BF16, [2568]) for i in range(4)]
    k = 0
    for (src, dst, ncol, gcol, nsc) in ((w_in, w_in_bf, 2568, GM, 8), (w_mkv, w_mkv_bf, 2048, GK, 8),
                                       (w_mq, w_mq_bf, 1024, GQ, 8), (w_out, w_out_bf, 1024, GA, 4),
                                       (w_mo, w_mo_bf, 1024, None, 0)):
        for c in range(8):
            sb_, sbb = st0[k % 4], st0b[k % 4]
            P.dma(sb_[:, 0:ncol], src[c * 128:(c + 1) * 128, :])
            if c < nsc:
                if k % 2 == 0:
                    P.tensor_scalar(sbb[:, 0:ncol], sb_[:, 0:ncol], vv[:, gcol + c:gcol + c + 1], None, ALU.mult)
                else:
                    P.activation(sbb[:, 0:ncol], sb_[:, 0:ncol], AF.Copy, scale=vv[:, gcol + c:gcol + c + 1])
            else:
                P.copy(sbb[:, 0:ncol], sb_[:, 0:ncol], eng="dve" if k % 2 == 0 else "act")
            P.dma(dst[c * 128:(c + 1) * 128, :], sbb[:, 0:ncol])
            k += 1
    P.dma(st0[0][:, 0:288], w_route)
    P.copy(wr_bf, st0[0][:, 0:288].rearrange("p (c n) -> p c n", c=8))

    if stop_after == "phase0":
        P.dma(out[0:128, :], st0[0][:, 0:1024])
        P.emit()
        return nc
    bank = [0]

    def nb():
        bank[0] = (bank[0] + 1) % 8
        return bank[0]

    def rstd_from_ss(dst, ss, epsc):
        P.activation(dst, ss, AF.Ln, bias=epsc, scale=1.0)
        P.activation(dst, dst, AF.Exp, scale=-0.5)


    def recip(o, i, eng="dve"):
        P.add(eng, lambda e: e.reciprocal(o, i), reads=[i], writes=[o])

    def route_tile(gi, pl, tt):
        lg = rt[:, 0:36]
        P.tensor_tensor(lg, pl, rA[:, BR_:BR_ + 36], ALU.add)
        if "d_lg" in dbg:
            P.dma(dbg["d_lg"][gi * 128:(gi + 1) * 128, :], lg)
        gmax, ngmax, gsum, gtop = rt[:, 40:41], rt[:, 41:42], rt[:, 42:43], rt[:, 43:44]
        ohg = rt[:, 44:48]
        P.reduce(gmax, lg[:, 0:4], ALU.max)
        P.tensor_scalar(ohg, lg[:, 0:4], gmax, None, ALU.is_equal)
        P.tensor_scalar(ngmax, gmax, -1.0, None, ALU.mult)
        P.activation(rt[:, 48:52], lg[:, 0:4], AF.Exp, bias=ngmax, scale=1.0, accum_out=gsum)
        recip(gtop, gsum)
        esel = rt[:, 52:60]
        P.tensor_scalar(esel, lg[:, 4:12], ohg[:, 0:1], None, ALU.mult)
        for g in range(1, 4):
            P.stt(esel, lg[:, 4 + 8 * g:12 + 8 * g], ohg[:, g:g + 1], esel, ALU.mult, ALU.add)
        top8 = rt[:, 60:68]
        P.add("dve", lambda e: e.max(top8, esel), reads=[esel], writes=[top8])
        oh1, oh2 = rt[:, 68:76], rt[:, 76:84]
        P.tensor_scalar(oh1, esel, top8[:, 0:1], None, ALU.is_equal)
        P.tensor_scalar(oh2, esel, top8[:, 1:2], None, ALU.is_equal)
        dd, w1 = rt[:, 84:85], rt[:, 85:86]
        P.tensor_tensor(dd, top8[:, 1:2], top8[:, 0:1], ALU.subtract)
        P.activation(dd, dd, AF.Exp)
        P.tensor_scalar(dd, dd, 1.0, None, ALU.add)
        recip(w1, dd)
        P.tensor_tensor(G1[:, gi:gi + 1], gtop, w1, ALU.mult)
        P.tensor_tensor(G2[:, gi:gi + 1], gtop, G1[:, gi:gi + 1], ALU.subtract)
        a1, a2 = rt[:, 96:128], rt[:, 128:160]
        for a_, oh in ((a1, oh1), (a2, oh2)):
            P.tensor_tensor(a_.rearrange("p (g e) -> p g e", g=4), ohg.unsqueeze(2).to_broadcast([128, 4, 8]),
                            oh.unsqueeze(1).to_broadcast([128, 4, 8]), ALU.mult)
        P.copy(A1[:, gi, :], a1)
        P.copy(A2[:, gi, :], a2)
        cmb = rtb[:, 0:32]
        P.tensor_tensor(cmb, a1, a2, ALU.add)
        pr = psf(7)[:, 384 + 64 * tt:448 + 64 * tt]
        P.matmul(pr[:, 0:32], cb[:, UST:UST + 128], cmb)
        P.matmul(pr[:, 32:64], cb[:, ONEB:ONEB + 128], cmb)
        rk, jk = rt[:, 160:192], rt[:, 192:224]
        P.tensor_tensor(rk, pr[:, 0:32], base, ALU.add)
        P.tensor_tensor(jk, a1, rk, ALU.mult)
        P.reduce(P1[:, gi:gi + 1], jk, ALU.add)
        P.tensor_tensor(jk, a2, rk, ALU.mult)
        P.reduce(P2[:, gi:gi + 1], jk, ALU.add)
        P.tensor_tensor(base, base, pr[:, 32:64], ALU.add)

    def sumsq(junk, src, ss):
        P.activation(junk, src, AF.Square, accum_out=ss)

    def transposeT(src_bf, dstT, col0, ev, b=None):
        if b is None:
            b = 6 + (nb() % 2)
        pv = psb(b)
        for c in range(8):
            P.transpose(pv[:, c * 128:(c + 1) * 128], src_bf[:, c * 128:(c + 1) * 128], identb)
        P.copy(dstT[:, :, col0:col0 + 128], pv.rearrange("p (c t) -> p c t", c=8), eng=ev)


    for b in range(nseq):
        tok0 = b * S
        for c in range(8):
            P.dma(Wkv[:, c, :], w_mkv_bf[c * 128:(c + 1) * 128, :])
        for mt in range(2):
            xi, xb = xin[mt], xn[mt]
            P.dma(xi, mem[b * 256 + mt * 128: b * 256 + (mt + 1) * 128, :])
            ss, rs = stA[:, 2 * mt:2 * mt + 1], stA[:, 2 * mt + 1:2 * mt + 2]
            sumsq(xb, xi, ss)
            rstd_from_ss(rs, ss, eps1024)
            P.tensor_scalar(xb, xi, rs, None, ALU.mult)
            transposeT(xb, memT, mt * 128, "dve")
        for cc in range(8):
            bk = nb() % 6
            pv = psf(bk)[:, 0:256]
            for c in range(8):
                P.matmul(pv, Wkv[:, c, cc * 128:(cc + 1) * 128], memT[:, c, :], start=(c == 0), stop=(c == 7))
            P.copy(K2T[:, cc, :], pv, eng="act" if cc % 2 else "dve")
        for mt in range(2):
            for nh in range(2):
                bk = nb() % 6
                pv = psf(bk)
                for c in range(8):
                    P.matmul(pv, memT[:, c, mt * 128:(mt + 1) * 128], Wkv[:, c, 1024 + nh * 512:1024 + (nh + 1) * 512],
                             start=(c == 0), stop=(c == 7))
                P.copy(V2[:, mt, 2 * nh:2 * nh + 2, 0:256], pv.rearrange("p (h d) -> p h d", h=2),
                       eng="act" if nh % 2 else "dve")
        P.memset(V2[:, :, :, 256:257], 1.0)
        if stop_after == "A0":
            P.dma(out[0:128, :], xin[0])
            P.emit()
            return nc
        for c in range(8):
            P.dma(Win[:, c, :], w_in_bf[c * 128:(c + 1) * 128, :])
        for i in range(NT):
            xi, xb = xin[i % 2], xn[i % 2]
            P.dma(xi, x[tok0 + i * 128: tok0 + (i + 1) * 128, :])
            ss, rs = stA[:, 4 + 2 * (i % 2):5 + 2 * (i % 2)], stA[:, 5 + 2 * (i % 2):6 + 2 * (i % 2)]
            sumsq(xb, xi, ss)
            rstd_from_ss(rs, ss, eps1024)
            P.tensor_scalar(xb, xi, rs, None, ALU.mult)
            transposeT(xb, xT, i * 128, "act" if i % 2 else "dve")
        if stop_after == "A1":
            P.dma(out[0:128, :], xin[0])
            P.emit()
            return nc
        P.memset(zT[:, :, 0:30], 0.0)
        P.memset(zT[:, :, 2078:2080], 0.0)
        P.memset(vS[:, :, :, 64:65], 1.0)
        OFF_K, OFF_V, OFF_F, OFF_C = 512, 1024, 1536, 1544
        for tb in range(4):
            tsl = slice(tb * 512, (tb + 1) * 512)
            for cc in range(8):
                bk = nb() % 6
                pv = psf(bk)
                for c in range(8):
                    P.matmul(pv, Win[:, c, cc * 128:(cc + 1) * 128], xT[:, c, tsl], start=(c == 0), stop=(c == 7))
                dst = qT[:, cc, tsl] if cc < 4 else kT[:, cc - 4, tsl]
                P.copy(dst, pv, eng="act" if cc % 2 else "dve")
            for i4 in range(4):
                bg = nb() % 6
                pg = psf(bg)
                for c in range(8):
                    P.matmul(pg, Win[:, c, OFF_C + 512 + i4 * 128:OFF_C + 512 + (i4 + 1) * 128], xT[:, c, tsl],
                             start=(c == 0), stop=(c == 7))
                sg = sig[i4 % 2]
                P.activation(sg, pg, AF.Sigmoid, bias=vv[:, BG + 4 + i4:BG + 5 + i4], scale=1.0)
                ba = nb() % 6
                pa = psf(ba)
                for c in range(8):
                    P.matmul(pa, Win[:, c, OFF_C + i4 * 128:OFF_C + (i4 + 1) * 128], xT[:, c, tsl],
                             start=(c == 0), stop=(c == 7))
                P.stt(zT[:, i4, 30 + tb * 512:30 + (tb + 1) * 512], pa, vv[:, BG + i4:BG + i4 + 1], sg, ALU.add, ALU.mult)
        pf_ = psf(6)
        for i in range(NT):
            bk = nb() % 6
            pv = psf(bk)
            for c in range(8):
                P.matmul(pv, xT[:, c, i * 128:(i + 1) * 128], Win[:, c, OFF_V:OFF_V + 512], start=(c == 0), stop=(c == 7))
            P.copy(vS[:, i, :, 0:64], pv.rearrange("p (h d) -> p h d", h=8), eng="act" if i % 2 else "dve")
            for c in range(8):
                P.matmul(pf_[:, i * 8:(i + 1) * 8], xT[:, c, i * 128:(i + 1) * 128], Win[:, c, OFF_F:OFF_F + 8],
                         start=(c == 0), stop=(c == 7))
        if stop_after == "A2":
            P.dma(out[0:128, :], xin[0])
            P.emit()
            return nc
        nlf2 = nlf.rearrange("p a b -> p (a b)")
        P.tensor_tensor(nlf2, pf_[:, 0:128], rA[:, BF_:BF_ + 128], ALU.add)
        P.activation(nlf2, nlf2, AF.Exp, scale=-1.0)
        P.activation(nlf2, nlf2, AF.Ln, bias=one_c, scale=1.0)
        if stop_after == "A3a":
            P.dma(out[0:128, :], xin[0])
            P.emit()
            return nc
        P.copy(incl[:, 0, :], nlf[:, 0, :])
        for i in range(1, NT):
            P.tensor_tensor(incl[:, i, :], incl[:, i - 1, :], nlf[:, i, :], ALU.add)
        if stop_after == "A3b":
            P.dma(out[0:128, :], xin[0])
            P.emit()
            return nc
        pc1 = psf(7)[:, 0:128]
        pc2 = psf(7)[:, 128:256]
        for (pdst, lmat, srcf) in ((pc1, cb[:, ONEB:ONEB + 128], incl.rearrange("p a b -> p (a b)")),
                                   (pc2, cb[:, LTRIB:LTRIB + 128], nlf2)):
            rres = cz[0][:, 0:128]
            for t3 in range(3):
                piece = xn[0][:, t3 * 128:(t3 + 1) * 128]
                P.copy(piece, srcf if t3 == 0 else rres)
                if t3 < 2:
                    P.tensor_tensor(rres, srcf if t3 == 0 else rres, piece, ALU.subtract)
                P.matmul(pdst, lmat, piece, start=(t3 == 0), stop=(t3 == 2))
        P.copy(Cend.rearrange("p a b -> p (a b)"), pc1)
        P.copy(Cc.rearrange("p a b -> p (a b)"), pc2)
        if stop_after == "A3c":
            P.dma(out[0:128, :], xin[0])
            P.emit()
            return nc
        P.tensor_tensor(Cc[:, 1:16, :], Cc[:, 1:16, :], Cend[:, 0:15, :], ALU.add)
        if stop_after == "A3d":
            P.dma(out[0:128, :], xin[0])
            P.emit()
            return nc
        for h in range(8):
            P.tensor_tensor(Bt[:, h, :, :], Cc[:, :, h:h + 1].to_broadcast([128, 16, 16]),
                            Cend[:, :, h].unsqueeze(1).to_broadcast([128, 16, 16]), ALU.subtract)
        if "d_q" in dbg:
            P.dma(dbg["d_q"], qT.rearrange("p a b -> p (a b)"))
        if "d_z" in dbg:
            P.dma(dbg["d_z"], zT.rearrange("p a b -> p (a b)"))
        if "d_B" in dbg:
            P.dma(dbg["d_B"], Bt.rearrange("p a b c -> p (a b c)"))
        if stop_after == "A3":
            P.dma(out[0:128, :], xin[0])
            P.emit()
            return nc
        if 'conv' in BP:
            for i4 in range(4):
                for kk in range(31):
                    P.tensor_scalar(Dg[:, kk, :], identb, vv[:, WDW + i4 * 31 + kk:WDW + i4 * 31 + kk + 1], None, ALU.mult)
                for j4 in range(4):
                    pcv = psf(nb() % 6)
                    for t4 in range(4):
                        c0 = (j4 * 4 + t4) * 128
                        for kk in range(31):
                            P.matmul(pcv[:, t4 * 128:(t4 + 1) * 128], zT[:, i4, c0 + kk:c0 + kk + 128], Dg[:, kk, :],
                                     start=(kk == 0), stop=(kk == 30))
                    P.tensor_tensor(cvo[i4][:, j4 * 4:(j4 + 1) * 4, :], pcv.rearrange("p (t c) -> p t c", t=4),
                                    rA[:, BDW_ + i4 * 128:BDW_ + (i4 + 1) * 128].unsqueeze(1).to_broadcast([128, 4, 128]),
                                    ALU.add)
        for wi, wsrc in enumerate((w_out_bf, w_mq_bf, w_mo_bf)):
            for c in range(8):
                P.dma(Woqm[:, wi, c, :], wsrc[c * 128:(c + 1) * 128, :])

        catT = xT
        LAG = 3

        def tail_conv(j):
            s1, nm, sq, rsd = stB[:, 10:11], stB[:, 11:12], stB[:, 12:13], stB[:, 13:14]
            xc, tmp = cz[0], cz[1]
            for i4 in range(4):
                P.copy(xc[:, i4 * 128:(i4 + 1) * 128], cvo[i4][:, j, :], eng="pool" if i4 % 2 else "dve")
            if "d_conv" in dbg:
                P.dma(dbg["d_conv"][j * 128:(j + 1) * 128, :], xc)
            P.reduce(s1, xc, ALU.add)
            P.tensor_scalar(nm, s1, -1.0 / 512.0, None, ALU.mult)
            P.tensor_scalar(xc, xc, nm, None, ALU.add)
            sumsq(tmp, xc, sq)
            P.activation(rsd, sq, AF.Ln, bias=epsln, scale=1.0 / 512.0)
            P.activation(rsd, rsd, AF.Exp, scale=-0.5)
            P.stt(xc, xc, rsd, rA[:, LG_:LG_ + 512], ALU.mult, ALU.mult)
            P.tensor_tensor(xc, xc, rA[:, LB_:LB_ + 512], ALU.add)
            P.activation(tmp, xc, AF.Exp, scale=-1.0)
            P.tensor_scalar(tmp, tmp, 1.0, None, ALU.add)
            P.add("dve", lambda e, o=tmp: e.reciprocal(o, o), reads=[tmp], writes=[tmp])
            P.tensor_tensor(catC[j % 2], xc, tmp, ALU.mult)

        def norm_attn(j, accb):
            rden = stB[:, 0:8]
            for hh in range(2):
                av = accb[hh][:, 0:260].rearrange("p (h d) -> p h d", h=4)
                P.add("dve", lambda e, o=rden[:, hh * 4:(hh + 1) * 4], i=av[:, :, 64]: e.reciprocal(o, i),
                      reads=[av[:, :, 64]], writes=[rden[:, hh * 4:(hh + 1) * 4]])
                P.tensor_tensor(attn[:, hh * 256:(hh + 1) * 256].rearrange("p (h d) -> p h d", h=4), av[:, :, 0:64],
                                rden[:, hh * 4:(hh + 1) * 4].unsqueeze(2).to_broadcast([128, 4, 64]), ALU.mult)
            if "d_attn" in dbg:
                P.dma(dbg["d_attn"][j * 128:(j + 1) * 128, :], attn)

        def tail_attn(j):
            ssa, rsa = stB[:, 8:9], stB[:, 9:10]
            sumsq(catA, attn, ssa)
            rstd_from_ss(rsa, ssa, eps512)
            P.tensor_scalar(catA, attn, rsa, None, ALU.mult)
            pv = psb(7)
            for c in range(4):
                P.transpose(pv[:, c * 128:(c + 1) * 128], catA[:, c * 128:(c + 1) * 128], identb)
            for c in range(4):
                P.transpose(pv[:, (4 + c) * 128:(5 + c) * 128], catC[j % 2][:, c * 128:(c + 1) * 128], identb)
            if "d_cat" in dbg:
                P.dma(dbg["d_cat"][j * 128:(j + 1) * 128, 0:512], catA)
                P.dma(dbg["d_cat"][j * 128:(j + 1) * 128, 512:1024], catC[j % 2])
            P.copy(catT[:, :, j * 128:(j + 1) * 128], pv.rearrange("p (c t) -> p c t", c=8), eng="act" if j % 2 else "dve")

        for j in range(NT):
            if j == 0:
                tail_conv(0)
                tail_conv(1)
            accb = [psf(2), psf(3)]
            pairs = [(h, kt) for h in range(8) for kt in range(j + 1)]
            stslots = {}
            n_pairs = len(pairs)

            def emit_qk(n):
                h, kt = pairs[n]
                stv = psf((0, 1, 4, 5, 6)[n % 5])[:, 0:128]
                r0 = 0 if 'evenonly' in BP else (h % 2) * 64
                if 'qk' in BP:
                    P.matmul(stv, kT[r0:r0 + 64, h // 2, kt * 128:(kt + 1) * 128], qT[r0:r0 + 64, h // 2, j * 128:(j + 1) * 128],
                             start=True, stop=(kt != j or 'mask' not in BP))
                if kt == j and 'mask' in BP:
                    P.matmul(stv, identb, negm, start=False, stop=True)
                pt = PT[n % 6]
                if 'exp' in BP:
                    if 'expdve' in BP:
                        P.copy(pt, stv, eng='dve')
                    elif 'expcopy' in BP:
                        P.copy(pt, stv, eng='act')
                    elif 'nobias' in BP:
                        P.activation(pt, stv, AF.Exp, scale=0.125)
                    else:
                        P.activation(pt, stv, AF.Exp, bias=Bt[:, h, kt, j:j + 1], scale=0.125)

            def emit_pv(n):
                h, kt = pairs[n]
                pt = PT[n % 6]
                av = accb[h // 4][:, (h % 4) * 65:(h % 4) * 65 + 65]
                if 'pv' in BP:
                    P.matmul(av, pt, vS[:, kt, h, :], start=(kt == 0), stop=(kt == j))

            for n in range(n_pairs + LAG):
                if j > 0 and n == min(6, n_pairs + LAG - 1):
                    tail_attn(j - 1)
                    if j + 1 < NT:
                        tail_conv(j + 1)
                if n < n_pairs:
                    emit_qk(n)
                if n >= LAG:
                    emit_pv(n - LAG)
            norm_attn(j, accb)
        tail_attn(NT - 1)

        if stop_after == "B":
            P.dma(out[0:128, :], xin[0])
            P.emit()
            return nc
        P.dma(rG, rows[:, RG:RG + 1024])
        P.tensor_scalar(rG, rG, 32.0, None, ALU.mult)
        Wout, Wmq, Wmo = Woqm[:, 0], Woqm[:, 1], Woqm[:, 2]
        for blk in range(NT // 2):
            for tt in range(2):
                i = blk * 2 + tt
                P.dma(xr[tt], x[tok0 + i * 128: tok0 + (i + 1) * 128, :])
                for nh in range(2):
                    pv = psf(nb() % 6)
                    for c in range(8):
                        P.matmul(pv, catT[:, c, i * 128:(i + 1) * 128], Wout[:, c, nh * 512:(nh + 1) * 512],
                                 start=(c == 0), stop=(c == 7))
                    P.tensor_tensor(x1[:, tt, nh * 512:(nh + 1) * 512], pv, xr[tt][:, nh * 512:(nh + 1) * 512], ALU.add)
                if "d_x1" in dbg:
                    P.dma(dbg["d_x1"][i * 128:(i + 1) * 128, :], x1[:, tt, :])
                ss, rs = stC[:, 2 * tt:2 * tt + 1], stC[:, 2 * tt + 1:2 * tt + 2]
                sumsq(xn2[tt], x1[:, tt, :], ss)
                rstd_from_ss(rs, ss, eps1024)
                P.tensor_scalar(xn2[tt], x1[:, tt, :], rs, None, ALU.mult)
                transposeT(xn2[tt], h2T, tt * 128, "act" if tt else "dve")
            for cc in range(8):
                pv = psf(nb() % 6)[:, 0:256]
                for c in range(8):
                    P.matmul(pv, Wmq[:, c, cc * 128:(cc + 1) * 128], h2T[:, c, :], start=(c == 0), stop=(c == 7))
                P.copy(q2T[:, cc, :], pv, eng="act" if cc % 2 else "dve")
            for h in range(4):
                for mt in range(2):
                    pv = psf(nb() % 6)[:, 0:256]
                    for k2 in range(2):
                        P.matmul(pv, K2T[:, 2 * h + k2, mt * 128:(mt + 1) * 128], q2T[:, 2 * h + k2, :],
                                 start=(k2 == 0), stop=(k2 == 1))
                    P.activation(P2T[(h % 2) * 2 + mt + 4 * (blk % 2)], pv, AF.Exp, scale=1.0 / 16.0)
                for tt in range(2):
                    pa = psf(nb() % 6)[:, 0:257]
                    for mt in range(2):
                        P.matmul(pa, P2T[(h % 2) * 2 + mt + 4 * (blk % 2)][:, tt * 128:(tt + 1) * 128], V2[:, mt, h, :],
                                 start=(mt == 0), stop=(mt == 1))
                    rd = stC[:, 8 + tt:9 + tt]
                    P.add("dve", lambda e, o=rd, i_=pa[:, 256:257]: e.reciprocal(o, i_), reads=[pa[:, 256:257]], writes=[rd])
                    P.tensor_scalar(oo[:, tt, h * 256:(h + 1) * 256], pa[:, 0:256], rd, None, ALU.mult)
            for tt in range(2):
                transposeT(oo[:, tt, :], oT, tt * 128, "act" if tt else "dve")
            for tt in range(2):
                i = blk * 2 + tt
                gi = b * NT + i
                xo = x2t[tt]
                for nh in range(2):
                    pv = psf(nb() % 6)
                    for c in range(8):
                        P.matmul(pv, oT[:, c, tt * 128:(tt + 1) * 128], Wmo[:, c, nh * 512:(nh + 1) * 512],
                                 start=(c == 0), stop=(c == 7))
                    P.tensor_tensor(xo[:, nh * 512:(nh + 1) * 512], pv, x1[:, tt, nh * 512:(nh + 1) * 512], ALU.add)
                P.dma(x2s[tok0 + i * 128: tok0 + (i + 1) * 128, :], xo)
                ss, rs = stC[:, 4 + 2 * tt:5 + 2 * tt], stC[:, 5 + 2 * tt:6 + 2 * tt]
                hb = h3t[tt]
                sumsq(hb, xo, ss)
                rstd_from_ss(rs, ss, eps1024)
                P.stt(hb.rearrange("t (c p) -> t p c", c=8), xo.rearrange("t (p c) -> t p c", c=8), rs,
                      rG.rearrange("t (p c) -> t p c", c=8), ALU.mult, ALU.mult)
                P.dma(h3s[tok0 + i * 128: tok0 + (i + 1) * 128, :], hb)
                transposeT(hb, h3T, 0, "act" if tt else "dve")
                pl = psf(7)[:, 256 + 64 * tt:256 + 64 * tt + 36]
                for c in range(8):
                    P.matmul(pl, h3T[:, c, :], wr_bf[:, c, :], start=(c == 0), stop=(c == 7))
                route_tile(gi, pl, tt)

    if "d_rt" in dbg:
        P.dma(dbg["d_rt"][:, 0:NTT], G1[:, 0:NTT]); P.dma(dbg["d_rt"][:, 64:64 + NTT], G2[:, 0:NTT])
        P.dma(dbg["d_rt"][:, 128:128 + NTT], P1[:, 0:NTT]); P.dma(dbg["d_rt"][:, 192:192 + NTT], P2[:, 0:NTT])
        P.dma(dbg["d_rt"][:, 256:288], base)
    if stop_after == "phase1":
        P.dma(out[0:128, :], x2t[0])
        P.emit()
        return nc

    A.off = mark_persist
    pcf = A.new(F32, [32]); pci = A.new(I32, [32]); offs = A.new(F32, [32]); ends = A.new(F32, [32])
    te = A.new(F32, [96]); gidx = A.new(I32, [96]); didx = A.new(I32, [96, 4]); tf = A.new(F32, [96])
    zero_i = A.new(I32, [192]); big = A.new(F32, [64, 32]); posf = A.new(F32, [64])
    P.tensor_scalar(pcf, base, 127.5, 1.0 / 256.0, ALU.add, ALU.mult)
    P.copy(pci, pcf)
    P.copy(pcf, pci)
    P.tensor_scalar(pcf, pcf, 256.0, None, ALU.mult)
    P.memset(offs[:, 0:1], 0.0)
    for e in range(1, 32):
        P.tensor_tensor(offs[:, e:e + 1], offs[:, e - 1:e], pcf[:, e - 1:e], ALU.add)
    P.tensor_tensor(ends, offs, pcf, ALU.add)
    P.memset(te, 0.0)
    for e in range(32):
        P.stt(te, cf[:, THR:THR + 96], ends[:, e:e + 1], te, ALU.is_ge, ALU.add)
    if "d_te" in dbg:
        P.dma(dbg["d_te"], te)
    P.tensor_scalar(tf, te, 128.0, cf[:, PCOL:PCOL + 1], ALU.mult, ALU.add)
    P.copy(gidx, tf)
    for c in range(4):
        P.tensor_scalar(tf, te, 512.0, cf[:, CBASE + c:CBASE + c + 1], ALU.mult, ALU.add)
        P.copy(didx[:, :, c], tf)
    for (Ax, Px, POSx) in ((A1, P1, POS1), (A2, P2, POS2)):
        P.tensor_tensor(big[:, 0:NTT, :], Ax[:, 0:NTT, :], offs.unsqueeze(1).to_broadcast([128, NTT, 32]), ALU.mult)
        P.reduce(posf[:, 0:NTT], big[:, 0:NTT, :], ALU.add)
        P.tensor_tensor(posf[:, 0:NTT], posf[:, 0:NTT], Px[:, 0:NTT], ALU.add)
        P.copy(POSx[:, 0:NTT], posf[:, 0:NTT])
    P.memset(zero_i, 0)
    P.dma(s2t.rearrange("(p n) o -> p (n o)", p=128), zero_i)
    FAKE = "D:s2t_scatter"
    nsc = 0
    for gi in range(NTT):
        for POSx in (POS1, POS2):
            P.add("pool", lambda e, po=POSx[:, gi:gi + 1], ti=ci[:, gi:gi + 1]: e.indirect_dma_start(
                out=s2t, out_offset=bass.IndirectOffsetOnAxis(ap=po, axis=0), in_=ti, in_offset=None),
                reads=[POSx[:, gi:gi + 1], ci[:, gi:gi + 1], s2t], writes=[(FAKE, 0, 1, nsc, nsc + 1)], dma=True)
            nsc += 1

    mark_p2 = A.off
    wg = [A.new(BF16, [8, 512]) for _ in range(3)]
    wu = [A.new(BF16, [8, 512]) for _ in range(3)]
    wd = [A.new(BF16, [4, 1024]) for _ in range(3)]
    sidx = [A.new(I32, [2]) for _ in range(3)]
    hs = [A.new(BF16, [2, 1024]) for _ in range(2)]
    hTs = [A.new(BF16, [8, 256]) for _ in range(2)]
    sil = [A.new(F32, [256]) for _ in range(2)]
    hid = [A.new(BF16, [4, 256]) for _ in range(2)]
    yt = [A.new(F32, [1024]) for _ in range(3)]
    s2t_v = s2t.rearrange("(j g p) o -> j p (g o)", g=2, p=128)
    ny = 0
    for j in range(n_moe_tiles):
        k3 = j % 3
        for (wdst, wsrc) in ((wg[k3], w_gate), (wu[k3], w_up)):
            P.add("pool", lambda e, o=wdst.rearrange("p a b -> p (a b)"), s=wsrc, ix=gidx[:, j:j + 1]: e.indirect_dma_start(
                out=o, out_offset=None, in_=s, in_offset=bass.IndirectOffsetOnAxis(ap=ix, axis=0),
                bounds_check=P.reg(e, 32 * 128 - 1), oob_is_err=False),
                reads=[gidx[:, j:j + 1], wsrc], writes=[wdst], dma=True)
        for c in range(4):
            P.add("pool", lambda e, o=wd[k3][:, c, :], ix=didx[:, j, c:c + 1]: e.indirect_dma_start(
                out=o, out_offset=None, in_=w_down, in_offset=bass.IndirectOffsetOnAxis(ap=ix, axis=0),
                bounds_check=P.reg(e, 32 * 512 - 1), oob_is_err=False),
                reads=[didx[:, j, c:c + 1], w_down], writes=[wd[k3][:, c, :]], dma=True)
        P.add("sp", lambda e, o=sidx[k3], s=s2t_v[j]: e.dma_start(out=o, in_=s, allow_slow_non_contiguous=True),
              reads=[s2t_v[j], (FAKE, 0, 1, 0, 1 << 20)], writes=[sidx[k3]], dma=True)
        hsj, hTj, hidj = hs[j % 2], hTs[j % 2], hid[j % 2]
        for g in range(2):
            P.add("pool", lambda e, o=hsj[:, g, :], ix=sidx[k3][:, g:g + 1]: e.indirect_dma_start(
                out=o, out_offset=None, in_=h3s, in_offset=bass.IndirectOffsetOnAxis(ap=ix, axis=0)),
                reads=[sidx[k3][:, g:g + 1], h3s], writes=[hsj[:, g, :]], dma=True)
            transposeT(hsj[:, g, :], hTj, g * 128, "act" if g else "dve")
        for fc in range(4):
            pg = psf(nb() % 6)[:, 0:256]
            for c in range(8):
                P.matmul(pg, wg[k3][:, c, fc * 128:(fc + 1) * 128], hTj[:, c, :], start=(c == 0), stop=(c == 7))
            pu = psf(nb() % 6)[:, 0:256]
            for c in range(8):
                P.matmul(pu, wu[k3][:, c, fc * 128:(fc + 1) * 128], hTj[:, c, :], start=(c == 0), stop=(c == 7))
            sl = sil[fc % 2]
            P.activation(sl, pg, AF.Silu)
            P.tensor_tensor(hidj[:, fc, :], sl, pu, ALU.mult)
        for g in range(2):
            y_ = yt[ny % 3]
            ny += 1
            for nh in range(2):
                pv = psf(nb() % 6)
                for fc in range(4):
                    P.matmul(pv, hidj[:, fc, g * 128:(g + 1) * 128], wd[k3][:, fc, nh * 512:(nh + 1) * 512],
                             start=(fc == 0), stop=(fc == 3))
                P.copy(y_[:, nh * 512:(nh + 1) * 512], pv, eng="act" if nh else "dve")
            P.dma(Ysc[j * 256 + g * 128: j * 256 + (g + 1) * 128, :], y_)

    A.off = mark_p2
    fg = A.new(F32, [1024])
    NB4 = 4
    x2b = [A.new(F32, [1024]) for _ in range(NB4)]
    y1b = [A.new(F32, [1024]) for _ in range(NB4)]
    y2b = [A.new(F32, [1024]) for _ in range(NB4)]
    otb = [A.new(F32, [1024]) for _ in range(NB4)]
    st4 = A.new(F32, [2 * NB4])
    P.dma(fg, rows[:, RF:RF + 1024])
    P.tensor_scalar(fg, fg, 32.0, None, ALU.mult)
    for gi in range(NTT):
        k2 = gi % NB4
        P.dma(x2b[k2], x2s[gi * 128:(gi + 1) * 128, :])
        for (yb, POSx) in ((y1b[k2], POS1), (y2b[k2], POS2)):
            P.add("pool", lambda e, o=yb, ix=POSx[:, gi:gi + 1]: e.indirect_dma_start(
                out=o, out_offset=None, in_=Ysc, in_offset=bass.IndirectOffsetOnAxis(ap=ix, axis=0)),
                reads=[POSx[:, gi:gi + 1], Ysc], writes=[yb], dma=True)
        acc = x2b[k2]
        if skip_moe:
            pass
        else:
            P.stt(acc, y1b[k2], G1[:, gi:gi + 1], acc, ALU.mult, ALU.add)
            P.stt(acc, y2b[k2], G2[:, gi:gi + 1], acc, ALU.mult, ALU.add)
        ss, rs = st4[:, 2 * k2:2 * k2 + 1], st4[:, 2 * k2 + 1:2 * k2 + 2]
        sumsq(otb[k2], acc, ss)
        rstd_from_ss(rs, ss, eps1024)
        P.stt(otb[k2], acc, rs, fg, ALU.mult, ALU.mult)
        P.dma(out[gi * 128:(gi + 1) * 128, :], otb[k2])
    cnt = P.emit()
    st.close()
    return nc


def _consts():
    p = np.arange(128)
    cfm = np.zeros((128, NCF), np.float32)
    cfm[:, THR:THR + 96] = 256.0 * np.arange(96)[None, :]
    cfm[:, PCOL] = p
    cfm[:, CBASE:CBASE + 4] = np.arange(4)[None, :] * 128 + p[:, None]
    cfm[:, IOTA:IOTA + 32] = np.arange(32)[None, :]
    cfm[:, CEPS:CEPS + 4] = np.array([1024e-6, 512e-6, 1.0, 1e-6], np.float32)[None, :]
    cbm = np.zeros((128, NCB), np.float32)
    cbm[:, IDB:IDB + 128] = np.eye(128)
    cbm[:, NEGM:NEGM + 128] = np.where(p[:, None] > p[None, :], -30000.0, 0.0)
    cbm[:, UST:UST + 128] = (p[:, None] < p[None, :])
    cbm[:, ONEB:ONEB + 128] = 1.0
    cbm[:, LTRIB:LTRIB + 128] = (p[:, None] <= p[None, :])
    cim = (np.arange(64)[None, :] * 128 + p[:, None]).astype(np.int32)
    return cfm, cbm.astype(ml_dtypes.bfloat16), cim


def _host_inputs(core, nseq, x, mem, norm_mix_g, w_in, b_forget, b_glu, w_dw, b_dw, conv_ln_g, conv_ln_b,
                 attn_out_g, w_out, norm_mem_g, mem_norm_g, w_mq, w_mkv, w_mo, norm_ffn_g,
                 w_route_group, b_route_group, w_route_expert, b_route_expert, w_gate, w_up, w_down, final_g):
    f = lambda a: np.ascontiguousarray(np.asarray(a, dtype=np.float32))
    cfm, cbm, cim = _consts()
    vecs = np.zeros((128, NV), np.float32)
    vecs[:, GM:GM + 8] = f(norm_mix_g)[0].reshape(8, 128).T
    vecs[:, GQ:GQ + 8] = f(norm_mem_g)[0].reshape(8, 128).T
    vecs[:, GK:GK + 8] = f(mem_norm_g)[0].reshape(8, 128).T
    vecs[:, GA:GA + 4] = f(attn_out_g)[0].reshape(4, 128).T
    vecs[:, BG:BG + 8] = f(b_glu)[0].reshape(8, 128).T
    vecs[:, BD:BD + 4] = f(b_dw)[0].reshape(4, 128).T
    vecs[:, WDW:WDW + 124] = f(w_dw)[0].reshape(31, 4, 128).transpose(2, 1, 0).reshape(128, 124)
    rows = np.zeros((1, NR), np.float32)
    rows[0, RG:RG + 1024] = f(norm_ffn_g)[0]
    rows[0, RF:RF + 1024] = f(final_g)
    rows[0, RLG:RLG + 512] = f(conv_ln_g)[0]
    rows[0, RLB:RLB + 512] = f(conv_ln_b)[0]
    rows[0, RBF:RBF + 128] = np.tile(f(b_forget)[0], 16)
    rows[0, RBR:RBR + 4] = f(b_route_group)[0]
    rows[0, RBR + 4:RBR + 36] = f(b_route_expert)[0].reshape(32)
    rows[0, RBDW:RBDW + 512] = f(b_dw)[0]
    rows = np.ascontiguousarray(np.broadcast_to(rows, (128, NR)))
    wr = np.concatenate([f(w_route_group)[0], f(w_route_expert)[0].transpose(1, 0, 2).reshape(1024, 32)], axis=1)
    return {
        "x": f(x[core * nseq:(core + 1) * nseq]).reshape(nseq * S, D),
        "mem": f(mem[core * nseq:(core + 1) * nseq]).reshape(nseq * 256, D),
        "w_in": f(w_in)[0], "w_out": f(w_out)[0], "w_mq": f(w_mq)[0], "w_mkv": f(w_mkv)[0], "w_mo": f(w_mo)[0],
        "w_route": np.ascontiguousarray(wr.reshape(128, 8 * 36)),
        "w_gate": f(w_gate)[0].reshape(32 * 128, 4096), "w_up": f(w_up)[0].reshape(32 * 128, 4096),
        "w_down": f(w_down)[0].reshape(32 * 512, 1024),
        "vecs": vecs, "rows": rows, "constf": cfm, "constb": cbm, "consti": cim,
    }


_NC_CACHE = {}


def kernel(**inputs):
    n_cores = 8
    nseq = 4
    if "full" not in _NC_CACHE:
        _NC_CACHE["full"] = build(nseq=nseq)
    nc = _NC_CACHE["full"]
    in_maps = [_host_inputs(c, nseq, **inputs) for c in range(n_cores)]
    res = run_bass_kernel_spmd(nc, in_maps, core_ids=list(range(n_cores)))
    outs = [np.asarray(r["out"]).reshape(nseq, S, D) for r in res.results]
    return np.concatenate(outs, axis=0).astype(np.float32)
```

```python
import numpy as np
import ml_dtypes
from concourse.bass_utils import run_bass_kernel_spmd
from contextlib import ExitStack
import concourse.bass as bass
import concourse.mybir as mybir

F32 = mybir.dt.float32
BF16 = mybir.dt.bfloat16
I32 = mybir.dt.int32
ALU = mybir.AluOpType
AF = mybir.ActivationFunctionType
AX = mybir.AxisListType
_DSZ = {F32: 4, BF16: 2, I32: 4, mybir.dt.uint32: 4, mybir.dt.float16: 2, mybir.dt.uint8: 1,
        mybir.dt.int8: 1, mybir.dt.uint16: 2, mybir.dt.int16: 2}


def region(ap):
    dsz = _DSZ[ap.dtype]
    dims = ap.ap
    off = ap.offset
    sp = str(ap.space)
    if sp in ("SB", "PSUM"):
        shp = ap.tensor.shape
        rowlen = 1
        for s in list(shp)[1:]:
            rowlen *= s
        p0 = off // rowlen
        c0 = off % rowlen
        pstep, pcnt = dims[0]
        p1 = p0 + ((pcnt - 1) * pstep) // rowlen + 1 if pstep else p0 + 1
        ext = 0
        for st, cn in dims[1:]:
            ext += (cn - 1) * abs(st)
        if sp == "PSUM":
            return (sp + ":" + ap.tensor.name, 0, 128, 0, 1 << 20)
        return (sp + ":" + ap.tensor.name, p0, p1, c0 * dsz, (c0 + ext + 1) * dsz)
    else:
        ext = 0
        for st, cn in dims:
            ext += (cn - 1) * abs(st)
        return ("D:" + ap.tensor.name, 0, 1, off * dsz, (off + ext + 1) * dsz)


class Op:
    __slots__ = ("eng", "fn", "reads", "writes", "dma", "deps", "sig", "dsem", "dval",
                 "waits", "idx", "prewait")


class Prog:
    ENGS = ("pe", "act", "dve", "pool", "sp")
    EPOCH = 30000

    def __init__(self, nc, ndma_sems=56):
        self.nc = nc
        self.ops = []
        self.recs = {}
        self.ndma = ndma_sems
        self.extra_regions = {}

    def reg(self, e, val):
        d = self.__dict__.setdefault("_regs", {})
        if val not in d:
            d[val] = e.to_reg(val)
        return d[val]

    def add(self, eng, fn, reads=(), writes=(), dma=False):
        op = Op()
        op.eng = eng; op.fn = fn; op.dma = dma
        op.reads = [r if isinstance(r, tuple) else region(r) for r in reads]
        op.writes = [r if isinstance(r, tuple) else region(r) for r in writes]
        op.idx = len(self.ops)
        op.sig = None; op.dsem = None; op.dval = None; op.waits = []; op.prewait = None
        deps = set()
        for (sp, p0, p1, lo, hi) in op.reads:
            for r in self.recs.get(sp, ()):
                if r[5] and r[0] < p1 and p0 < r[1] and r[2] < hi and lo < r[3]:
                    deps.add(r[4])
        for (sp, p0, p1, lo, hi) in op.writes:
            lst = self.recs.get(sp)
            if lst is None:
                lst = self.recs[sp] = []
            keep = []
            for r in lst:
                if r[0] < p1 and p0 < r[1] and r[2] < hi and lo < r[3]:
                    deps.add(r[4])
                    if r[0] >= p0 and r[1] <= p1 and r[2] >= lo and r[3] <= hi:
                        continue
                keep.append(r)
            keep.append([p0, p1, lo, hi, op.idx, True])
            self.recs[sp] = keep
        for (sp, p0, p1, lo, hi) in op.reads:
            lst = self.recs.get(sp)
            if lst is None:
                lst = self.recs[sp] = []
            if not dma:
                for r in lst:
                    if (not r[5]) and r[0] == p0 and r[1] == p1 and r[2] == lo and r[3] == hi \
                            and (not self.ops[r[4]].dma) and self.ops[r[4]].eng == eng:
                        r[4] = op.idx
                        break
                else:
                    lst.append([p0, p1, lo, hi, op.idx, False])
            else:
                lst.append([p0, p1, lo, hi, op.idx, False])
        deps.discard(op.idx)
        op.deps = deps
        self.ops.append(op)
        return op

    def matmul(self, out, lhsT, rhs, start=True, stop=True, **kw):
        return self.add("pe", lambda e: e.matmul(out, lhsT, rhs, start=start, stop=stop, **kw),
                        reads=[lhsT, rhs], writes=[out])

    def transpose(self, out, in_, ident):
        return self.add("pe", lambda e: e.transpose(out, in_, ident), reads=[in_, ident], writes=[out])

    def activation(self, out, in_, func, bias=None, scale=None, accum_out=None, eng="act"):
        reads = [in_]
        kw = {}
        if bias is not None:
            kw["bias"] = bias
            if not isinstance(bias, (int, float)):
                reads.append(bias)
        if scale is not None:
            kw["scale"] = scale
            if not isinstance(scale, (int, float)):
                reads.append(scale)
        writes = [out]
        if accum_out is not None:
            kw["accum_out"] = accum_out
            writes.append(accum_out)
        return self.add(eng, lambda e: e.activation(out, in_, func, **kw), reads=reads, writes=writes)

    def tensor_scalar(self, out, in0, s1, s2, op0, op1=None, eng="dve", accum_out=None):
        reads = [in0]
        for s in (s1, s2):
            if s is not None and not isinstance(s, (int, float)):
                reads.append(s)
        kw = {}
        if op1 is not None:
            kw["op1"] = op1
        writes = [out]
        if accum_out is not None:
            kw["accum_out"] = accum_out
            writes.append(accum_out)
        return self.add(eng, lambda e: e.tensor_scalar(out, in0, s1, s2, op0, **kw), reads=reads, writes=writes)

    def tensor_tensor(self, out, in0, in1, op, eng="dve"):
        return self.add(eng, lambda e: e.tensor_tensor(out, in0, in1, op), reads=[in0, in1], writes=[out])

    def stt(self, out, in0, scalar, in1, op0, op1, eng="dve", accum_out=None):
        reads = [in0, in1]
        if not isinstance(scalar, (int, float)):
            reads.append(scalar)
        kw = {}
        writes = [out]
        if accum_out is not None:
            kw["accum_out"] = accum_out
            writes.append(accum_out)
        return self.add(eng, lambda e: e.scalar_tensor_tensor(out, in0, scalar, in1, op0, op1, **kw),
                        reads=reads, writes=writes)

    def copy(self, out, in_, eng="dve"):
        if eng == "act":
            return self.add(eng, lambda e: e.copy(out, in_), reads=[in_], writes=[out])
        return self.add(eng, lambda e: e.tensor_copy(out, in_), reads=[in_], writes=[out])

    def memset(self, out, val, eng="dve"):
        return self.add(eng, lambda e: e.memset(out, val), reads=[], writes=[out])

    def reduce(self, out, in_, op, axis=None, eng="dve"):
        ax = axis if axis is not None else AX.X
        return self.add(eng, lambda e: e.tensor_reduce(out, in_, ax, op), reads=[in_], writes=[out])

    def dma(self, out, in_, q="sp", **kw):
        return self.add(q, lambda e: e.dma_start(out=out, in_=in_, **kw), reads=[in_], writes=[out], dma=True)

    def finalize(self):
        ops = self.ops
        need = [False] * len(ops)
        for op in ops:
            for d in op.deps:
                p = ops[d]
                if p.dma:
                    continue
                if p.eng == op.eng and not op.dma:
                    if p.eng == "pe":
                        continue
                    continue_flag = True
                    for (sp, p0, p1, lo, hi) in op.reads:
                        for (sp2, q0, q1, lo2, hi2) in p.writes:
                            if sp == sp2 and q0 < p1 and p0 < q1 and lo2 < hi and lo < hi2:
                                continue_flag = False
                    if continue_flag:
                        continue
                need[d] = True
        cnt = {e: 0 for e in self.ENGS}
        for op in ops:
            if (not op.dma) and need[op.idx]:
                cnt[op.eng] += 1
                op.sig = cnt[op.eng]
        dcount = [0] * self.ndma
        k = 0
        for op in ops:
            if op.dma:
                s = k % self.ndma
                k += 1
                dcount[s] += 1
                op.dsem = s
                op.dval = 16 * dcount[s]
                op.prewait = (("dma", s), 16 * (dcount[s] - 1)) if dcount[s] > 1 else None
        self.dfinal = [16 * c for c in dcount]
        seen = {e: {} for e in self.ENGS}
        pos = {e: 0 for e in self.ENGS}
        opos = {}
        for op in ops:
            pos[op.eng] += 1
            opos[op.idx] = pos[op.eng]
        nw = 0
        for op in ops:
            sn = seen[op.eng]
            w = {}
            if op.prewait is not None:
                key, val = op.prewait
                if sn.get(key, 0) < val:
                    w[key] = val
            for d in op.deps:
                p = ops[d]
                if p.dma:
                    key, val = ("dma", p.dsem), p.dval
                else:
                    if p.sig is None:
                        continue
                    if p.eng == op.eng and not op.dma:
                        if p.eng == "pe":
                            continue
                        if opos[op.idx] - opos[p.idx] > 3:
                            continue
                    ep = (p.sig - 1) // self.EPOCH
                    key, val = ("eng", p.eng), ep * 100000 + (p.sig - 1) % self.EPOCH + 1
                if sn.get(key, 0) < val and w.get(key, 0) < val:
                    w[key] = val
            for key, val in w.items():
                sn[key] = val
            op.waits = list(w.items())
            nw += len(op.waits)
        self.n_waits = nw
        self.n_epochs = {e: (cnt[e] - 1) // self.EPOCH + 1 if cnt[e] else 1 for e in self.ENGS}
        return cnt

    def emit(self):
        nc = self.nc
        cnt = self.finalize()
        ops = self.ops
        with ExitStack() as st:
            esem = {}
            for e in self.ENGS:
                for ep in range(self.n_epochs[e]):
                    esem[(e, ep)] = st.enter_context(nc.semaphore("s_%s%d" % (e, ep)))
            dsem = [st.enter_context(nc.semaphore("d%d" % i)) for i in range(self.ndma)]
            block = st.enter_context(nc.Block())
            by_eng = {e: [op for op in ops if op.eng == e] for e in self.ENGS}
            EP = self.EPOCH

            def run(eng_obj, lst, is_sp):
                for op in lst:
                    for key, val in op.waits:
                        if key[0] == "dma":
                            eng_obj.wait_ge(dsem[key[1]], val)
                        else:
                            ep, v = divmod(val, 100000)
                            eng_obj.wait_ge(esem[(key[1], ep)], v)
                    inst = op.fn(eng_obj)
                    if op.dma:
                        inst.then_inc(dsem[op.dsem], 16)
                    elif op.sig is not None:
                        ep = (op.sig - 1) // EP
                        inst.then_inc(esem[(op.eng, ep)], 1)
                if is_sp:
                    for i, v in enumerate(self.dfinal):
                        if v:
                            eng_obj.wait_ge(dsem[i], v)

            @block.sync
            def _(e):
                run(e, by_eng["sp"], True)

            @block.tensor
            def _(e):
                run(e, by_eng["pe"], False)

            @block.scalar
            def _(e):
                run(e, by_eng["act"], False)

            @block.vector
            def _(e):
                run(e, by_eng["dve"], False)

            @block.gpsimd
            def _(e):
                run(e, by_eng["pool"], False)
        return cnt

S = 2048
D = 1024
NT = S // 128
TS = 512
NTILES = 16384 // TS + 32
GT = TS // 128
NSLOT = NTILES * TS
GM, GQ, GK, GA, BG, BD, WDW = 0, 8, 16, 24, 28, 36, 40
NV = 40 + 4 * 31
RG, RF, RLG, RLB, RBF, RBR, RBDW = 0, 1024, 2048, 2560, 3072, 3200, 3236
NR = 3748
THR, PCOL, CBASE, IOTA, CEPS, NCF = 0, 96, 97, 101, 133, 141
IDB, NEGM, UST, ONEB, LTRIB, NCB = 0, 128, 256, 384, 512, 640


def _prod(s):
    r = 1
    for a in s:
        r *= a
    return r


class Arena:
    def __init__(self, nc, st, name, nbytes):
        self.t = st.enter_context(nc.sbuf_tensor(name, [128, nbytes // 4], F32))
        self.cap = nbytes
        self.off = 0

    def alloc(self, nbytes):
        off = (self.off + 63) // 64 * 64
        self.off = off + nbytes
        assert self.off <= self.cap, ("arena overflow", self.off, self.cap)
        return off

    def view(self, off, dtype, shape):
        n = _prod(shape)
        dsz = _DSZ[dtype]
        nb = (n * dsz + 3) // 4
        v = self.t[:, off // 4: off // 4 + nb]
        if dtype != F32:
            v = v.bitcast(dtype)
        v = v[:, 0:n]
        if len(shape) == 2:
            v = v.rearrange("p (a b) -> p a b", a=shape[0], b=shape[1])
        elif len(shape) == 3:
            v = v.rearrange("p (a b c) -> p a b c", a=shape[0], b=shape[1], c=shape[2])
        return v

    def new(self, dtype, shape):
        off = self.alloc(_prod(shape) * _DSZ[dtype])
        return self.view(off, dtype, shape)


import os as _os
BP = set(_os.environ.get('BPARTS', 'conv,convT,qk,mask,exp,pv,norm,tail,catT').split(','))


def build(nseq=4, debug=(), stop_after=None, n_moe_tiles=NTILES, skip_moe=False):
    nc = bass.Bass("TRN2", target_bir_lowering=False)
    NTOK = nseq * S
    NTT = NTOK // 128

    def din(name, shape, dt=F32):
        return nc.dram_tensor(name, shape, dt, kind="ExternalInput").ap()

    def scr(name, shape, dt):
        if name in debug:
            return nc.dram_tensor(name, shape, dt, kind="ExternalOutput").ap()
        return nc.dram_tensor(name, shape, dt).ap()

    x = din("x", [NTOK, D])
    mem = din("mem", [nseq * 256, D])
    w_in = din("w_in", [D, 2568])
    w_out = din("w_out", [D, D])
    w_mq = din("w_mq", [D, D])
    w_mkv = din("w_mkv", [D, 2 * D])
    w_mo = din("w_mo", [D, D])
    w_route = din("w_route", [128, 8 * 36])
    w_gate = din("w_gate", [32 * 128, 4096])
    w_up = din("w_up", [32 * 128, 4096])
    w_down = din("w_down", [32 * 512, 1024])
    vecs = din("vecs", [128, NV])
    rows = din("rows", [128, NR])
    constf = din("constf", [128, NCF])
    constb = din("constb", [128, NCB], BF16)
    consti = din("consti", [128, 64], I32)
    out = nc.dram_tensor("out", [NTOK, D], F32, kind="ExternalOutput").ap()

    w_in_bf = scr("w_in_bf", [D, 2568], BF16)
    w_out_bf = scr("w_out_bf", [D, D], BF16)
    w_mq_bf = scr("w_mq_bf", [D, D], BF16)
    w_mkv_bf = scr("w_mkv_bf", [D, 2 * D], BF16)
    w_mo_bf = scr("w_mo_bf", [D, D], BF16)
    x2s = scr("x2s", [NTOK, D], F32)
    h3s = scr("h3s", [NTOK, D], BF16)
    Ysc = scr("Ysc", [NSLOT, D], BF16)
    s2t = scr("s2t", [NSLOT, 1], I32)
    dbg = {}
    for name, shape, dt in (("d_cat", [S, D], BF16), ("d_x1", [S, D], F32), ("d_lg", [NTOK, 36], F32),
                            ("d_q", [128, 4 * 2048], BF16), ("d_attn", [S, 512], F32), ("d_z", [128, 4 * 2080], BF16),
                            ("d_B", [128, 2048], F32), ("d_rt", [128, 64 * 8], F32), ("d_te", [128, 96], F32),
                            ("d_conv", [S, 512], F32)):
        if name in debug:
            dbg[name] = nc.dram_tensor(name, shape, dt, kind="ExternalOutput").ap()

    P = Prog(nc)
    st = ExitStack()
    A = Arena(nc, st, "arena", 210944)
    PS = [st.enter_context(nc.psum_tensor("ps%d" % i, [128, 512], F32)) for i in range(8)]

    def psf(i):
        return PS[i][:, :]

    def psb(i):
        return PS[i][:, :].bitcast(BF16)

    cf = A.new(F32, [NCF])
    cb = A.new(BF16, [NCB])
    ci = A.new(I32, [64])
    vv = A.new(F32, [NV])
    rA = A.new(F32, [1700])
    LG_, LB_, BF_, BR_, BDW_ = 0, 512, 1024, 1152, 1188
    A1 = A.new(BF16, [64, 32])
    A2 = A.new(BF16, [64, 32])
    G1 = A.new(F32, [64]); G2 = A.new(F32, [64]); P1 = A.new(F32, [64]); P2 = A.new(F32, [64])
    POS1 = A.new(I32, [64]); POS2 = A.new(I32, [64])
    base = A.new(F32, [32])
    wr_bf = A.new(BF16, [8, 36])
    v_ = None
    identb = cb[:, IDB:IDB + 128]
    negm = cb[:, NEGM:NEGM + 128]
    eps1024 = cf[:, CEPS:CEPS + 1]
    eps512 = cf[:, CEPS + 1:CEPS + 2]
    one_c = cf[:, CEPS + 2:CEPS + 3]
    epsln = cf[:, CEPS + 3:CEPS + 4]
    P.dma(cf, constf); P.dma(cb, constb); P.dma(ci, consti); P.dma(vv, vecs)
    P.dma(rA, rows[:, RLG:RLG + 1700])
    P.memset(base, 0.0)
    P.tensor_scalar(vv[:, GM:GM + 24], vv[:, GM:GM + 24], 32.0, None, ALU.mult)
    P.tensor_scalar(vv[:, GA:GA + 4], vv[:, GA:GA + 4], float(np.sqrt(512.0)), None, ALU.mult)
    mark_persist = A.off

    Wreg_off = A.alloc(49152)
    Win = A.view(Wreg_off, BF16, [8, 2568])
    Woqm = A.view(Wreg_off, BF16, [3, 8, 1024])
    xT = A.new(BF16, [8, 2048])
    K2T = A.new(BF16, [8, 256])
    V2 = A.new(BF16, [2, 4, 257])
    Bt = A.new(F32, [8, 16, 16])
    nlf = A.new(F32, [16, 8]); incl = A.new(F32, [16, 8]); Cc = A.new(F32, [16, 8]); Cend = A.new(F32, [16, 8])
    U_off = A.alloc(66048 + 64)
    qT = A.view(U_off, BF16, [4, 2048])
    kT = A.view(U_off + 16384, BF16, [4, 2048])
    Wkv = A.view(U_off, BF16, [8, 2048])
    vS = A.view(U_off + 32768, BF16, [16, 8, 65])
    zT = A.view(U_off + 32768 + 16640, BF16, [4, 2080])
    memT = A.view(U_off + 32768 + 16640, BF16, [8, 256])
    T_off = A.alloc(23808)
    xin = [A.view(T_off + i * 4096, F32, [1024]) for i in range(2)]
    xn = [A.view(T_off + 8192 + i * 2048, BF16, [1024]) for i in range(2)]
    sig = [A.view(T_off + 12288 + i * 2048, F32, [512]) for i in range(2)]
    stA = A.view(T_off + 16384, F32, [16])
    PT = [A.view(T_off + i * 256, BF16, [128]) for i in range(6)]
    attn = A.view(T_off + 1536, F32, [512])
    catA = A.view(T_off + 3584, BF16, [512])
    catC = [A.view(T_off + 4608 + i * 1024, BF16, [512]) for i in range(2)]
    cz = [A.view(T_off + 6656 + i * 2048, F32, [512]) for i in range(2)]
    stB = A.view(T_off + 10752, F32, [64])
    Dg = A.view(T_off + 11008, BF16, [31, 128])
    zoff = U_off + 32768 + 16640
    cvo = [A.view(T_off + 18944, BF16, [16, 128])] + [A.view(zoff + i * 4160, BF16, [16, 128]) for i in range(3)]
    o_ = U_off
    x1 = A.view(o_, F32, [2, 1024]); o_ += 8192
    xr = [A.view(o_ + i * 4096, F32, [1024]) for i in range(2)]; o_ += 8192
    xn2 = [A.view(o_ + i * 2048, BF16, [1024]) for i in range(2)]; o_ += 4096
    h2T = A.view(o_, BF16, [8, 256]); o_ += 4096
    q2T = A.view(o_, BF16, [8, 256]); o_ += 4096
    P2T = [A.view(o_ + i * 512, BF16, [256]) for i in range(8)]; o_ += 4096
    oo = A.view(o_, BF16, [2, 1024]); o_ += 4096
    oT = A.view(o_, BF16, [8, 256]); o_ += 4096
    x2t = [A.view(o_ + i * 4096, F32, [1024]) for i in range(2)]; o_ += 8192
    h3t = [A.view(o_ + i * 2048, BF16, [1024]) for i in range(2)]; o_ += 4096
    h3T = A.view(o_, BF16, [8, 128]); o_ += 2048
    rG = A.view(o_, F32, [1024]); o_ += 4096
    rt = A.view(o_, F32, [256]); o_ += 1024
    rtb = A.view(o_, BF16, [64]); o_ += 128
    stC = A.view(o_, F32, [32]); o_ += 128
    assert o_ <= U_off + 66048
    x1b = [x1, A.view(T_off, F32, [2, 1024])]
    h2Tb = [h2T, A.view(T_off + 8192, BF16, [8, 256])]
    q2Tb = [q2T, A.view(T_off + 12288, BF16, [8, 256])]

    st0 = [A.view(U_off + i * 15488, F32, [2568]) for i in range(4)]
    st0b = [A.view(U_off + 10304 + i * 15488, BF16, [2568]) for i in range(4)]
    k = 0
    for (src, dst, ncol, gcol, nsc) in ((w_in, w_in_bf, 2568, GM, 8), (w_mkv, w_mkv_bf, 2048, GK, 8),
                                       (w_mq, w_mq_bf, 1024, GQ, 8), (w_out, w_out_bf, 1024, GA, 4),
                                       (w_mo, w_mo_bf, 1024, None, 0)):
        for c in range(8):
            sb_, sbb = st0[k % 4], st0b[k % 4]
            P.dma(sb_[:, 0:ncol], src[c * 128:(c + 1) * 128, :])
            if c < nsc:
                if k % 2 == 0:
                    P.tensor_scalar(sbb[:, 0:ncol], sb_[:, 0:ncol], vv[:, gcol + c:gcol + c + 1], None, ALU.mult)
                else:
                    P.activation(sbb[:, 0:ncol], sb_[:, 0:ncol], AF.Copy, scale=vv[:, gcol + c:gcol + c + 1])
            else:
                P.copy(sbb[:, 0:ncol], sb_[:, 0:ncol], eng="dve" if k % 2 == 0 else "act")
            P.dma(dst[c * 128:(c + 1) * 128, :], sbb[:, 0:ncol])
            k += 1
    P.dma(st0[0][:, 0:288], w_route)
    P.copy(wr_bf, st0[0][:, 0:288].rearrange("p (c n) -> p c n", c=8))

    if stop_after == "phase0":
        P.dma(out[0:128, :], st0[0][:, 0:1024])
        P.emit()
        return nc
    bank = [0]

    def nb():
        bank[0] = (bank[0] + 1) % 8
        return bank[0]

    def rstd_from_ss(dst, ss, epsc):
        P.activation(dst, ss, AF.Ln, bias=epsc, scale=1.0)
        P.activation(dst, dst, AF.Exp, scale=-0.5)


    def recip(o, i, eng="dve"):
        P.add(eng, lambda e: e.reciprocal(o, i), reads=[i], writes=[o])

    def route_tile(gi, pl, tt):
        lg = rt[:, 0:36]
        P.tensor_tensor(lg, pl, rA[:, BR_:BR_ + 36], ALU.add)
        if "d_lg" in dbg:
            P.dma(dbg["d_lg"][gi * 128:(gi + 1) * 128, :], lg)
        gmax, ngmax, gsum, gtop = rt[:, 40:41], rt[:, 41:42], rt[:, 42:43], rt[:, 43:44]
        ohg = rt[:, 44:48]
        P.reduce(gmax, lg[:, 0:4], ALU.max)
        P.tensor_scalar(ohg, lg[:, 0:4], gmax, None, ALU.is_equal)
        P.tensor_scalar(ngmax, gmax, -1.0, None, ALU.mult)
        P.activation(rt[:, 48:52], lg[:, 0:4], AF.Exp, bias=ngmax, scale=1.0, accum_out=gsum)
        recip(gtop, gsum)
        esel = rt[:, 52:60]
        P.tensor_scalar(esel, lg[:, 4:12], ohg[:, 0:1], None, ALU.mult)
        for g in range(1, 4):
            P.stt(esel, lg[:, 4 + 8 * g:12 + 8 * g], ohg[:, g:g + 1], esel, ALU.mult, ALU.add)
        top8 = rt[:, 60:68]
        P.add("dve", lambda e: e.max(top8, esel), reads=[esel], writes=[top8])
        oh1, oh2 = rt[:, 68:76], rt[:, 76:84]
        P.tensor_scalar(oh1, esel, top8[:, 0:1], None, ALU.is_equal)
        P.tensor_scalar(oh2, esel, top8[:, 1:2], None, ALU.is_equal)
        dd, w1 = rt[:, 84:85], rt[:, 85:86]
        P.tensor_tensor(dd, top8[:, 1:2], top8[:, 0:1], ALU.subtract)
        P.activation(dd, dd, AF.Exp)
        P.tensor_scalar(dd, dd, 1.0, None, ALU.add)
        recip(w1, dd)
        P.tensor_tensor(G1[:, gi:gi + 1], gtop, w1, ALU.mult)
        P.tensor_tensor(G2[:, gi:gi + 1], gtop, G1[:, gi:gi + 1], ALU.subtract)
        a1, a2 = rt[:, 96:128], rt[:, 128:160]
        for a_, oh in ((a1, oh1), (a2, oh2)):
            P.tensor_tensor(a_.rearrange("p (g e) -> p g e", g=4), ohg.unsqueeze(2).to_broadcast([128, 4, 8]),
                            oh.unsqueeze(1).to_broadcast([128, 4, 8]), ALU.mult)
        P.copy(A1[:, gi, :], a1)
        P.copy(A2[:, gi, :], a2)
        cmb = rtb[:, 0:32]
        P.tensor_tensor(cmb, a1, a2, ALU.add)
        pr = psf(7)[:, 384 + 64 * tt:448 + 64 * tt]
        P.matmul(pr[:, 0:32], cb[:, UST:UST + 128], cmb)
        P.matmul(pr[:, 32:64], cb[:, ONEB:ONEB + 128], cmb)
        rk, jk = rt[:, 160:192], rt[:, 192:224]
        P.tensor_tensor(rk, pr[:, 0:32], base, ALU.add)
        P.tensor_tensor(jk, a1, rk, ALU.mult)
        P.reduce(P1[:, gi:gi + 1], jk, ALU.add)
        P.tensor_tensor(jk, a2, rk, ALU.mult)
        P.reduce(P2[:, gi:gi + 1], jk, ALU.add)
        P.tensor_tensor(base, base, pr[:, 32:64], ALU.add)

    def sumsq(junk, src, ss):
        P.activation(junk, src, AF.Square, accum_out=ss)

    def transposeT(src_bf, dstT, col0, ev, b=None):
        if b is None:
            b = 6 + (nb() % 2)
        pv = psb(b)
        for c in range(8):
            P.transpose(pv[:, c * 128:(c + 1) * 128], src_bf[:, c * 128:(c + 1) * 128], identb)
        P.copy(dstT[:, :, col0:col0 + 128], pv.rearrange("p (c t) -> p c t", c=8), eng=ev)


    for b in range(nseq):
        tok0 = b * S
        for c in range(8):
            P.dma(Wkv[:, c, :], w_mkv_bf[c * 128:(c + 1) * 128, :])
        for mt in range(2):
            xi, xb = xin[mt], xn[mt]
            P.dma(xi, mem[b * 256 + mt * 128: b * 256 + (mt + 1) * 128, :])
            ss, rs = stA[:, 2 * mt:2 * mt + 1], stA[:, 2 * mt + 1:2 * mt + 2]
            sumsq(xb, xi, ss)
            rstd_from_ss(rs, ss, eps1024)
            P.tensor_scalar(xb, xi, rs, None, ALU.mult)
            transposeT(xb, memT, mt * 128, "dve")
        for cc in range(8):
            bk = nb() % 6
            pv = psf(bk)[:, 0:256]
            for c in range(8):
                P.matmul(pv, Wkv[:, c, cc * 128:(cc + 1) * 128], memT[:, c, :], start=(c == 0), stop=(c == 7))
            P.copy(K2T[:, cc, :], pv, eng="act" if cc % 2 else "dve")
        for mt in range(2):
            for nh in range(2):
                bk = nb() % 6
                pv = psf(bk)
                for c in range(8):
                    P.matmul(pv, memT[:, c, mt * 128:(mt + 1) * 128], Wkv[:, c, 1024 + nh * 512:1024 + (nh + 1) * 512],
                             start=(c == 0), stop=(c == 7))
                P.copy(V2[:, mt, 2 * nh:2 * nh + 2, 0:256], pv.rearrange("p (h d) -> p h d", h=2),
                       eng="act" if nh % 2 else "dve")
        P.memset(V2[:, :, :, 256:257], 1.0)
        if stop_after == "A0":
            P.dma(out[0:128, :], xin[0])
            P.emit()
            return nc
        for c in range(8):
            P.dma(Win[:, c, :], w_in_bf[c * 128:(c + 1) * 128, :])
        for i in range(NT):
            xi, xb = xin[i % 2], xn[i % 2]
            P.dma(xi, x[tok0 + i * 128: tok0 + (i + 1) * 128, :])
            ss, rs = stA[:, 4 + 2 * (i % 2):5 + 2 * (i % 2)], stA[:, 5 + 2 * (i % 2):6 + 2 * (i % 2)]
            sumsq(xb, xi, ss)
            rstd_from_ss(rs, ss, eps1024)
            P.tensor_scalar(xb, xi, rs, None, ALU.mult)
            transposeT(xb, xT, i * 128, "act" if i % 2 else "dve")
        if stop_after == "A1":
            P.dma(out[0:128, :], xin[0])
            P.emit()
            return nc
        P.memset(zT[:, :, 0:30], 0.0)
        P.memset(zT[:, :, 2078:2080], 0.0)
        P.memset(vS[:, :, :, 64:65], 1.0)
        OFF_K, OFF_V, OFF_F, OFF_C = 512, 1024, 1536, 1544
        for tb in range(4):
            tsl = slice(tb * 512, (tb + 1) * 512)
            for cc in range(8):
                bk = nb() % 6
                pv = psf(bk)
                for c in range(8):
                    P.matmul(pv, Win[:, c, cc * 128:(cc + 1) * 128], xT[:, c, tsl], start=(c == 0), stop=(c == 7))
                dst = qT[:, cc, tsl] if cc < 4 else kT[:, cc - 4, tsl]
                P.copy(dst, pv, eng="act" if cc % 2 else "dve")
            for i4 in range(4):
                bg = nb() % 6
                pg = psf(bg)
                for c in range(8):
                    P.matmul(pg, Win[:, c, OFF_C + 512 + i4 * 128:OFF_C + 512 + (i4 + 1) * 128], xT[:, c, tsl],
                             start=(c == 0), stop=(c == 7))
                sg = sig[i4 % 2]
                P.activation(sg, pg, AF.Sigmoid, bias=vv[:, BG + 4 + i4:BG + 5 + i4], scale=1.0)
                ba = nb() % 6
                pa = psf(ba)
                for c in range(8):
                    P.matmul(pa, Win[:, c, OFF_C + i4 * 128:OFF_C + (i4 + 1) * 128], xT[:, c, tsl],
                             start=(c == 0), stop=(c == 7))
                P.stt(zT[:, i4, 30 + tb * 512:30 + (tb + 1) * 512], pa, vv[:, BG + i4:BG + i4 + 1], sg, ALU.add, ALU.mult)
        pf_ = psf(6)
        for i in range(NT):
            bk = nb() % 6
            pv = psf(bk)
            for c in range(8):
                P.matmul(pv, xT[:, c, i * 128:(i + 1) * 128], Win[:, c, OFF_V:OFF_V + 512], start=(c == 0), stop=(c == 7))
            P.copy(vS[:, i, :, 0:64], pv.rearrange("p (h d) -> p h d", h=8), eng="act" if i % 2 else "dve")
            for c in range(8):
                P.matmul(pf_[:, i * 8:(i + 1) * 8], xT[:, c, i * 128:(i + 1) * 128], Win[:, c, OFF_F:OFF_F + 8],
                         start=(c == 0), stop=(c == 7))
        if stop_after == "A2":
            P.dma(out[0:128, :], xin[0])
            P.emit()
            return nc
        nlf2 = nlf.rearrange("p a b -> p (a b)")
        P.tensor_tensor(nlf2, pf_[:, 0:128], rA[:, BF_:BF_ + 128], ALU.add)
        P.activation(nlf2, nlf2, AF.Exp, scale=-1.0)
        P.activation(nlf2, nlf2, AF.Ln, bias=one_c, scale=1.0)
        if stop_after == "A3a":
            P.dma(out[0:128, :], xin[0])
            P.emit()
            return nc
        P.copy(incl[:, 0, :], nlf[:, 0, :])
        for i in range(1, NT):
            P.tensor_tensor(incl[:, i, :], incl[:, i - 1, :], nlf[:, i, :], ALU.add)
        if stop_after == "A3b":
            P.dma(out[0:128, :], xin[0])
            P.emit()
            return nc
        pc1 = psf(7)[:, 0:128]
        pc2 = psf(7)[:, 128:256]
        for (pdst, lmat, srcf) in ((pc1, cb[:, ONEB:ONEB + 128], incl.rearrange("p a b -> p (a b)")),
                                   (pc2, cb[:, LTRIB:LTRIB + 128], nlf2)):
            rres = cz[0][:, 0:128]
            for t3 in range(3):
                piece = xn[0][:, t3 * 128:(t3 + 1) * 128]
                P.copy(piece, srcf if t3 == 0 else rres)
                if t3 < 2:
                    P.tensor_tensor(rres, srcf if t3 == 0 else rres, piece, ALU.subtract)
                P.matmul(pdst, lmat, piece, start=(t3 == 0), stop=(t3 == 2))
        P.copy(Cend.rearrange("p a b -> p (a b)"), pc1)
        P.copy(Cc.rearrange("p a b -> p (a b)"), pc2)
        if stop_after == "A3c":
            P.dma(out[0:128, :], xin[0])
            P.emit()
            return nc
        P.tensor_tensor(Cc[:, 1:16, :], Cc[:, 1:16, :], Cend[:, 0:15, :], ALU.add)
        if stop_after == "A3d":
            P.dma(out[0:128, :], xin[0])
            P.emit()
            return nc
        for h in range(8):
            P.tensor_tensor(Bt[:, h, :, :], Cc[:, :, h:h + 1].to_broadcast([128, 16, 16]),
                            Cend[:, :, h].unsqueeze(1).to_broadcast([128, 16, 16]), ALU.subtract)
        if "d_q" in dbg:
            P.dma(dbg["d_q"], qT.rearrange("p a b -> p (a b)"))
        if "d_z" in dbg:
            P.dma(dbg["d_z"], zT.rearrange("p a b -> p (a b)"))
        if "d_B" in dbg:
            P.dma(dbg["d_B"], Bt.rearrange("p a b c -> p (a b c)"))
        if stop_after == "A3":
            P.dma(out[0:128, :], xin[0])
            P.emit()
            return nc
        if 'conv' in BP:
            for i4 in range(4):
                for kk in range(31):
                    P.tensor_scalar(Dg[:, kk, :], identb, vv[:, WDW + i4 * 31 + kk:WDW + i4 * 31 + kk + 1], None, ALU.mult)
                for j4 in range(4):
                    pcv = psf(nb() % 6)
                    for t4 in range(4):
                        c0 = (j4 * 4 + t4) * 128
                        for kk in range(31):
                            P.matmul(pcv[:, t4 * 128:(t4 + 1) * 128], zT[:, i4, c0 + kk:c0 + kk + 128], Dg[:, kk, :],
                                     start=(kk == 0), stop=(kk == 30))
                    P.tensor_tensor(cvo[i4][:, j4 * 4:(j4 + 1) * 4, :], pcv.rearrange("p (t c) -> p t c", t=4),
                                    rA[:, BDW_ + i4 * 128:BDW_ + (i4 + 1) * 128].unsqueeze(1).to_broadcast([128, 4, 128]),
                                    ALU.add)
        for wi, wsrc in enumerate((w_out_bf, w_mq_bf, w_mo_bf)):
            for c in range(8):
                P.dma(Woqm[:, wi, c, :], wsrc[c * 128:(c + 1) * 128, :])

        catT = xT
        LAG = 3

        def tail_conv(j):
            s1, nm, sq, rsd = stB[:, 10:11], stB[:, 11:12], stB[:, 12:13], stB[:, 13:14]
            xc, tmp = cz[0], cz[1]
            for i4 in range(4):
                P.copy(xc[:, i4 * 128:(i4 + 1) * 128], cvo[i4][:, j, :], eng="pool" if i4 % 2 else "dve")
            if "d_conv" in dbg:
                P.dma(dbg["d_conv"][j * 128:(j + 1) * 128, :], xc)
            P.reduce(s1, xc, ALU.add)
            P.tensor_scalar(nm, s1, -1.0 / 512.0, None, ALU.mult)
            P.tensor_scalar(xc, xc, nm, None, ALU.add)
            sumsq(tmp, xc, sq)
            P.activation(rsd, sq, AF.Ln, bias=epsln, scale=1.0 / 512.0)
            P.activation(rsd, rsd, AF.Exp, scale=-0.5)
            P.stt(xc, xc, rsd, rA[:, LG_:LG_ + 512], ALU.mult, ALU.mult)
            P.tensor_tensor(xc, xc, rA[:, LB_:LB_ + 512], ALU.add)
            P.activation(tmp, xc, AF.Exp, scale=-1.0)
            P.tensor_scalar(tmp, tmp, 1.0, None, ALU.add)
            P.add("dve", lambda e, o=tmp: e.reciprocal(o, o), reads=[tmp], writes=[tmp])
            P.tensor_tensor(catC[j % 2], xc, tmp, ALU.mult)

        def norm_attn(j, accb):
            rden = stB[:, 0:8]
            for hh in range(2):
                av = accb[hh][:, 0:260].rearrange("p (h d) -> p h d", h=4)
                P.add("dve", lambda e, o=rden[:, hh * 4:(hh + 1) * 4], i=av[:, :, 64]: e.reciprocal(o, i),
                      reads=[av[:, :, 64]], writes=[rden[:, hh * 4:(hh + 1) * 4]])
                P.tensor_tensor(attn[:, hh * 256:(hh + 1) * 256].rearrange("p (h d) -> p h d", h=4), av[:, :, 0:64],
                                rden[:, hh * 4:(hh + 1) * 4].unsqueeze(2).to_broadcast([128, 4, 64]), ALU.mult)
            if "d_attn" in dbg:
                P.dma(dbg["d_attn"][j * 128:(j + 1) * 128, :], attn)

        def tail_attn(j):
            ssa, rsa = stB[:, 8:9], stB[:, 9:10]
            sumsq(catA, attn, ssa)
            rstd_from_ss(rsa, ssa, eps512)
            P.tensor_scalar(catA, attn, rsa, None, ALU.mult)
            pv = psb(7)
            for c in range(4):
                P.transpose(pv[:, c * 128:(c + 1) * 128], catA[:, c * 128:(c + 1) * 128], identb)
            for c in range(4):
                P.transpose(pv[:, (4 + c) * 128:(5 + c) * 128], catC[j % 2][:, c * 128:(c + 1) * 128], identb)
            if "d_cat" in dbg:
                P.dma(dbg["d_cat"][j * 128:(j + 1) * 128, 0:512], catA)
                P.dma(dbg["d_cat"][j * 128:(j + 1) * 128, 512:1024], catC[j % 2])
            P.copy(catT[:, :, j * 128:(j + 1) * 128], pv.rearrange("p (c t) -> p c t", c=8), eng="act" if j % 2 else "dve")

        for j in range(NT):
            if j == 0:
                tail_conv(0)
                tail_conv(1)
            accb = [psf(2), psf(3)]
            pairs = [(h, kt) for h in range(8) for kt in range(j + 1)]
            stslots = {}
            n_pairs = len(pairs)

            def emit_qk(n):
                h, kt = pairs[n]
                stv = psf((0, 1, 4, 5, 6)[n % 5])[:, 0:128]
                r0 = 0 if 'evenonly' in BP else (h % 2) * 64
                if 'qk' in BP:
                    P.matmul(stv, kT[r0:r0 + 64, h // 2, kt * 128:(kt + 1) * 128], qT[r0:r0 + 64, h // 2, j * 128:(j + 1) * 128],
                             start=True, stop=(kt != j or 'mask' not in BP))
                if kt == j and 'mask' in BP:
                    P.matmul(stv, identb, negm, start=False, stop=True)
                pt = PT[n % 6]
                if 'exp' in BP:
                    if 'expdve' in BP:
                        P.copy(pt, stv, eng='dve')
                    elif 'expcopy' in BP:
                        P.copy(pt, stv, eng='act')
                    elif 'nobias' in BP:
                        P.activation(pt, stv, AF.Exp, scale=0.125)
                    else:
                        P.activation(pt, stv, AF.Exp, bias=Bt[:, h, kt, j:j + 1], scale=0.125)

            def emit_pv(n):
                h, kt = pairs[n]
                pt = PT[n % 6]
                av = accb[h // 4][:, (h % 4) * 65:(h % 4) * 65 + 65]
                if 'pv' in BP:
                    P.matmul(av, pt, vS[:, kt, h, :], start=(kt == 0), stop=(kt == j))

            for n in range(n_pairs + LAG):
                if j > 0 and n == min(6, n_pairs + LAG - 1):
                    tail_attn(j - 1)
                    if j + 1 < NT:
                        tail_conv(j + 1)
                if n < n_pairs:
                    emit_qk(n)
                if n >= LAG:
                    emit_pv(n - LAG)
            norm_attn(j, accb)
        tail_attn(NT - 1)

        if stop_after == "B":
            P.dma(out[0:128, :], xin[0])
            P.emit()
            return nc
        P.dma(rG, rows[:, RG:RG + 1024])
        P.tensor_scalar(rG, rG, 32.0, None, ALU.mult)
        Wout, Wmq, Wmo = Woqm[:, 0], Woqm[:, 1], Woqm[:, 2]

        def c_front(blk):
            pb = blk % 2
            for tt in range(2):
                i = blk * 2 + tt
                P.dma(xr[tt], x[tok0 + i * 128: tok0 + (i + 1) * 128, :])
                for nh in range(2):
                    pv = psf(nb() % 6)
                    for c in range(8):
                        P.matmul(pv, catT[:, c, i * 128:(i + 1) * 128], Wout[:, c, nh * 512:(nh + 1) * 512],
                                 start=(c == 0), stop=(c == 7))
                    P.tensor_tensor(x1b[pb][:, tt, nh * 512:(nh + 1) * 512], pv, xr[tt][:, nh * 512:(nh + 1) * 512], ALU.add)
                if "d_x1" in dbg:
                    P.dma(dbg["d_x1"][i * 128:(i + 1) * 128, :], x1b[pb][:, tt, :])
                yield
                ss, rs = stC[:, 2 * tt:2 * tt + 1], stC[:, 2 * tt + 1:2 * tt + 2]
                sumsq(xn2[tt], x1b[pb][:, tt, :], ss)
                rstd_from_ss(rs, ss, eps1024)
                P.tensor_scalar(xn2[tt], x1b[pb][:, tt, :], rs, None, ALU.mult)
                transposeT(xn2[tt], h2Tb[pb], tt * 128, "act" if tt else "dve")
                yield
            for cc in range(8):
                pv = psf(nb() % 6)[:, 0:256]
                for c in range(8):
                    P.matmul(pv, Wmq[:, c, cc * 128:(cc + 1) * 128], h2Tb[pb][:, c, :], start=(c == 0), stop=(c == 7))
                P.copy(q2Tb[pb][:, cc, :], pv, eng="act" if cc % 2 else "dve")
                if cc % 2:
                    yield

        def c_back(blk):
            pb = blk % 2

            def scores(h):
                for mt in range(2):
                    pv = psf(nb() % 6)[:, 0:256]
                    for k2 in range(2):
                        P.matmul(pv, K2T[:, 2 * h + k2, mt * 128:(mt + 1) * 128], q2Tb[pb][:, 2 * h + k2, :],
                                 start=(k2 == 0), stop=(k2 == 1))
                    P.activation(P2T[(h % 2) * 2 + mt + 4 * pb], pv, AF.Exp, scale=1.0 / 16.0)

            def pvh(h):
                for tt in range(2):
                    pa = psf(nb() % 6)[:, 0:257]
                    for mt in range(2):
                        P.matmul(pa, P2T[(h % 2) * 2 + mt + 4 * pb][:, tt * 128:(tt + 1) * 128], V2[:, mt, h, :],
                                 start=(mt == 0), stop=(mt == 1))
                    rd = stC[:, 8 + tt:9 + tt]
                    P.add("dve", lambda e, o=rd, i_=pa[:, 256:257]: e.reciprocal(o, i_), reads=[pa[:, 256:257]], writes=[rd])
                    P.tensor_scalar(oo[:, tt, h * 256:(h + 1) * 256], pa[:, 0:256], rd, None, ALU.mult)

            scores(0)
            yield
            scores(1)
            yield
            pvh(0)
            yield
            scores(2)
            pvh(1)
            yield
            scores(3)
            pvh(2)
            yield
            pvh(3)
            yield
            for tt in range(2):
                transposeT(oo[:, tt, :], oT, tt * 128, "act" if tt else "dve")
                yield
            for tt in range(2):
                i = blk * 2 + tt
                gi = b * NT + i
                xo = x2t[tt]
                for nh in range(2):
                    pv = psf(nb() % 6)
                    for c in range(8):
                        P.matmul(pv, oT[:, c, tt * 128:(tt + 1) * 128], Wmo[:, c, nh * 512:(nh + 1) * 512],
                                 start=(c == 0), stop=(c == 7))
                    P.tensor_tensor(xo[:, nh * 512:(nh + 1) * 512], pv, x1b[pb][:, tt, nh * 512:(nh + 1) * 512], ALU.add)
                P.dma(x2s[tok0 + i * 128: tok0 + (i + 1) * 128, :], xo)
                yield
                ss, rs = stC[:, 4 + 2 * tt:5 + 2 * tt], stC[:, 5 + 2 * tt:6 + 2 * tt]
                hb = h3t[tt]
                sumsq(hb, xo, ss)
                rstd_from_ss(rs, ss, eps1024)
                P.stt(hb.rearrange("t (c p) -> t p c", c=8), xo.rearrange("t (p c) -> t p c", c=8), rs,
                      rG.rearrange("t (p c) -> t p c", c=8), ALU.mult, ALU.mult)
                P.dma(h3s[tok0 + i * 128: tok0 + (i + 1) * 128, :], hb)
                transposeT(hb, h3T, 0, "act" if tt else "dve")
                yield
                pl = psf(7)[:, 256 + 64 * tt:256 + 64 * tt + 36]
                for c in range(8):
                    P.matmul(pl, h3T[:, c, :], wr_bf[:, c, :], start=(c == 0), stop=(c == 7))
                route_tile(gi, pl, tt)
                yield

        def interleave(*gens):
            gens = [g for g in gens if g is not None]
            while gens:
                for g in list(gens):
                    try:
                        next(g)
                    except StopIteration:
                        gens.remove(g)

        interleave(c_front(0))
        for blk in range(NT // 2):
            interleave(c_back(blk), c_front(blk + 1) if blk + 1 < NT // 2 else None)

    if "d_rt" in dbg:
        P.dma(dbg["d_rt"][:, 0:NTT], G1[:, 0:NTT]); P.dma(dbg["d_rt"][:, 64:64 + NTT], G2[:, 0:NTT])
        P.dma(dbg["d_rt"][:, 128:128 + NTT], P1[:, 0:NTT]); P.dma(dbg["d_rt"][:, 192:192 + NTT], P2[:, 0:NTT])
        P.dma(dbg["d_rt"][:, 256:288], base)
    if stop_after == "phase1":
        P.dma(out[0:128, :], x2t[0])
        P.emit()
        return nc

    A.off = mark_persist
    pcf = A.new(F32, [32]); pci = A.new(I32, [32]); offs = A.new(F32, [32]); ends = A.new(F32, [32])
    te = A.new(F32, [96]); gidx = A.new(I32, [96]); didx = A.new(I32, [96, 4]); tf = A.new(F32, [96])
    zero_i = A.new(I32, [NSLOT // 128]); big = A.new(F32, [64, 32]); posf = A.new(F32, [64])
    P.tensor_scalar(pcf, base, TS / 2 - 0.5, 1.0 / TS, ALU.add, ALU.mult)
    P.copy(pci, pcf)
    P.copy(pcf, pci)
    P.tensor_scalar(pcf, pcf, float(TS), None, ALU.mult)
    P.memset(offs[:, 0:1], 0.0)
    for e in range(1, 32):
        P.tensor_tensor(offs[:, e:e + 1], offs[:, e - 1:e], pcf[:, e - 1:e], ALU.add)
    P.tensor_tensor(ends, offs, pcf, ALU.add)
    P.memset(te, 0.0)
    for e in range(32):
        P.stt(te, cf[:, THR:THR + 96], ends[:, e:e + 1], te, ALU.is_ge, ALU.add)
    if "d_te" in dbg:
        P.dma(dbg["d_te"], te)
    P.tensor_scalar(tf, te, 128.0, cf[:, PCOL:PCOL + 1], ALU.mult, ALU.add)
    P.copy(gidx, tf)
    for c in range(4):
        P.tensor_scalar(tf, te, 512.0, cf[:, CBASE + c:CBASE + c + 1], ALU.mult, ALU.add)
        P.copy(didx[:, :, c], tf)
    for (Ax, Px, POSx) in ((A1, P1, POS1), (A2, P2, POS2)):
        P.tensor_tensor(big[:, 0:NTT, :], Ax[:, 0:NTT, :], offs.unsqueeze(1).to_broadcast([128, NTT, 32]), ALU.mult)
        P.reduce(posf[:, 0:NTT], big[:, 0:NTT, :], ALU.add)
        P.tensor_tensor(posf[:, 0:NTT], posf[:, 0:NTT], Px[:, 0:NTT], ALU.add)
        P.copy(POSx[:, 0:NTT], posf[:, 0:NTT])
    P.memset(zero_i, 0)
    P.dma(s2t.rearrange("(p n) o -> p (n o)", p=128), zero_i)
    FAKE = "D:s2t_scatter"
    nsc = 0
    for gi in range(NTT):
        for POSx in (POS1, POS2):
            P.add("pool", lambda e, po=POSx[:, gi:gi + 1], ti=ci[:, gi:gi + 1]: e.indirect_dma_start(
                out=s2t, out_offset=bass.IndirectOffsetOnAxis(ap=po, axis=0), in_=ti, in_offset=None),
                reads=[POSx[:, gi:gi + 1], ci[:, gi:gi + 1], s2t], writes=[(FAKE, 0, 1, nsc, nsc + 1)], dma=True)
            nsc += 1

    mark_p2 = A.off
    wg = [A.new(BF16, [8, 512]) for _ in range(3)]
    wu = [A.new(BF16, [8, 512]) for _ in range(3)]
    wd = [A.new(BF16, [4, 1024]) for _ in range(3)]
    sidx = [A.new(I32, [GT]) for _ in range(3)]
    hs = [A.new(BF16, [GT, 1024]) for _ in range(2)]
    hTs = [A.new(BF16, [8, TS]) for _ in range(2)]
    sil = [A.new(F32, [TS]) for _ in range(2)]
    hid = [A.new(BF16, [4, TS]) for _ in range(2)]
    yt = [A.new(BF16, [1024]) for _ in range(4)]
    s2t_v = s2t.rearrange("(j g p) o -> j p (g o)", g=GT, p=128)
    ny = 0
    for j in range(n_moe_tiles):
        k3 = j % 3
        for (wdst, wsrc) in ((wg[k3], w_gate), (wu[k3], w_up)):
            P.add("pool", lambda e, o=wdst.rearrange("p a b -> p (a b)"), s=wsrc, ix=gidx[:, j:j + 1]: e.indirect_dma_start(
                out=o, out_offset=None, in_=s, in_offset=bass.IndirectOffsetOnAxis(ap=ix, axis=0),
                bounds_check=P.reg(e, 32 * 128 - 1), oob_is_err=False),
                reads=[gidx[:, j:j + 1], wsrc], writes=[wdst], dma=True)
        for c in range(4):
            P.add("pool", lambda e, o=wd[k3][:, c, :], ix=didx[:, j, c:c + 1]: e.indirect_dma_start(
                out=o, out_offset=None, in_=w_down, in_offset=bass.IndirectOffsetOnAxis(ap=ix, axis=0),
                bounds_check=P.reg(e, 32 * 512 - 1), oob_is_err=False),
                reads=[didx[:, j, c:c + 1], w_down], writes=[wd[k3][:, c, :]], dma=True)
        P.add("sp", lambda e, o=sidx[k3], s=s2t_v[j]: e.dma_start(out=o, in_=s, allow_slow_non_contiguous=True),
              reads=[s2t_v[j], (FAKE, 0, 1, 0, 1 << 20)], writes=[sidx[k3]], dma=True)
        hsj, hTj, hidj = hs[j % 2], hTs[j % 2], hid[j % 2]
        for g in range(GT):
            P.add("pool", lambda e, o=hsj[:, g, :], ix=sidx[k3][:, g:g + 1]: e.indirect_dma_start(
                out=o, out_offset=None, in_=h3s, in_offset=bass.IndirectOffsetOnAxis(ap=ix, axis=0)),
                reads=[sidx[k3][:, g:g + 1], h3s], writes=[hsj[:, g, :]], dma=True)
            transposeT(hsj[:, g, :], hTj, g * 128, "act" if g else "dve")
        for fc in range(4):
            pg = psf(nb() % 6)[:, 0:TS]
            for c in range(8):
                P.matmul(pg, wg[k3][:, c, fc * 128:(fc + 1) * 128], hTj[:, c, :], start=(c == 0), stop=(c == 7))
            pu = psf(nb() % 6)[:, 0:TS]
            for c in range(8):
                P.matmul(pu, wu[k3][:, c, fc * 128:(fc + 1) * 128], hTj[:, c, :], start=(c == 0), stop=(c == 7))
            sl = sil[fc % 2]
            P.activation(sl, pg, AF.Silu)
            P.tensor_tensor(hidj[:, fc, :], sl, pu, ALU.mult)
        for g in range(GT):
            y_ = yt[ny % 4]
            ny += 1
            for nh in range(2):
                pv = psf(nb() % 6)
                for fc in range(4):
                    P.matmul(pv, hidj[:, fc, g * 128:(g + 1) * 128], wd[k3][:, fc, nh * 512:(nh + 1) * 512],
                             start=(fc == 0), stop=(fc == 3))
                P.copy(y_[:, nh * 512:(nh + 1) * 512], pv, eng="act" if nh else "dve")
            P.dma(Ysc[j * TS + g * 128: j * TS + (g + 1) * 128, :], y_)

    A.off = mark_p2
    fg = A.new(F32, [1024])
    NB4 = 4
    x2b = [A.new(F32, [1024]) for _ in range(NB4)]
    y1b = [A.new(BF16, [1024]) for _ in range(NB4)]
    y2b = [A.new(BF16, [1024]) for _ in range(NB4)]
    otb = [A.new(F32, [1024]) for _ in range(NB4)]
    st4 = A.new(F32, [2 * NB4])
    P.dma(fg, rows[:, RF:RF + 1024])
    P.tensor_scalar(fg, fg, 32.0, None, ALU.mult)
    for gi in range(NTT):
        k2 = gi % NB4
        P.dma(x2b[k2], x2s[gi * 128:(gi + 1) * 128, :])
        for (yb, POSx) in ((y1b[k2], POS1), (y2b[k2], POS2)):
            P.add("pool", lambda e, o=yb, ix=POSx[:, gi:gi + 1]: e.indirect_dma_start(
                out=o, out_offset=None, in_=Ysc, in_offset=bass.IndirectOffsetOnAxis(ap=ix, axis=0)),
                reads=[POSx[:, gi:gi + 1], Ysc], writes=[yb], dma=True)
        acc = x2b[k2]
        if skip_moe:
            pass
        else:
            P.stt(acc, y1b[k2], G1[:, gi:gi + 1], acc, ALU.mult, ALU.add)
            P.stt(acc, y2b[k2], G2[:, gi:gi + 1], acc, ALU.mult, ALU.add)
        ss, rs = st4[:, 2 * k2:2 * k2 + 1], st4[:, 2 * k2 + 1:2 * k2 + 2]
        sumsq(otb[k2], acc, ss)
        rstd_from_ss(rs, ss, eps1024)
        P.stt(otb[k2], acc, rs, fg, ALU.mult, ALU.mult)
        P.dma(out[gi * 128:(gi + 1) * 128, :], otb[k2])
    cnt = P.emit()
    st.close()
    return nc


def _consts():
    p = np.arange(128)
    cfm = np.zeros((128, NCF), np.float32)
    cfm[:, THR:THR + 96] = float(TS) * np.arange(96)[None, :]
    cfm[:, PCOL] = p
    cfm[:, CBASE:CBASE + 4] = np.arange(4)[None, :] * 128 + p[:, None]
    cfm[:, IOTA:IOTA + 32] = np.arange(32)[None, :]
    cfm[:, CEPS:CEPS + 4] = np.array([1024e-6, 512e-6, 1.0, 1e-6], np.float32)[None, :]
    cbm = np.zeros((128, NCB), np.float32)
    cbm[:, IDB:IDB + 128] = np.eye(128)
    cbm[:, NEGM:NEGM + 128] = np.where(p[:, None] > p[None, :], -30000.0, 0.0)
    cbm[:, UST:UST + 128] = (p[:, None] < p[None, :])
    cbm[:, ONEB:ONEB + 128] = 1.0
    cbm[:, LTRIB:LTRIB + 128] = (p[:, None] <= p[None, :])
    cim = (np.arange(64)[None, :] * 128 + p[:, None]).astype(np.int32)
    return cfm, cbm.astype(ml_dtypes.bfloat16), cim


def _host_inputs(core, nseq, x, mem, norm_mix_g, w_in, b_forget, b_glu, w_dw, b_dw, conv_ln_g, conv_ln_b,
                 attn_out_g, w_out, norm_mem_g, mem_norm_g, w_mq, w_mkv, w_mo, norm_ffn_g,
                 w_route_group, b_route_group, w_route_expert, b_route_expert, w_gate, w_up, w_down, final_g):
    f = lambda a: np.ascontiguousarray(np.asarray(a, dtype=np.float32))
    cfm, cbm, cim = _consts()
    vecs = np.zeros((128, NV), np.float32)
    vecs[:, GM:GM + 8] = f(norm_mix_g)[0].reshape(8, 128).T
    vecs[:, GQ:GQ + 8] = f(norm_mem_g)[0].reshape(8, 128).T
    vecs[:, GK:GK + 8] = f(mem_norm_g)[0].reshape(8, 128).T
    vecs[:, GA:GA + 4] = f(attn_out_g)[0].reshape(4, 128).T
    vecs[:, BG:BG + 8] = f(b_glu)[0].reshape(8, 128).T
    vecs[:, BD:BD + 4] = f(b_dw)[0].reshape(4, 128).T
    vecs[:, WDW:WDW + 124] = f(w_dw)[0].reshape(31, 4, 128).transpose(2, 1, 0).reshape(128, 124)
    rows = np.zeros((1, NR), np.float32)
    rows[0, RG:RG + 1024] = f(norm_ffn_g)[0]
    rows[0, RF:RF + 1024] = f(final_g)
    rows[0, RLG:RLG + 512] = f(conv_ln_g)[0]
    rows[0, RLB:RLB + 512] = f(conv_ln_b)[0]
    rows[0, RBF:RBF + 128] = np.tile(f(b_forget)[0], 16)
    rows[0, RBR:RBR + 4] = f(b_route_group)[0]
    rows[0, RBR + 4:RBR + 36] = f(b_route_expert)[0].reshape(32)
    rows[0, RBDW:RBDW + 512] = f(b_dw)[0]
    rows = np.ascontiguousarray(np.broadcast_to(rows, (128, NR)))
    wr = np.concatenate([f(w_route_group)[0], f(w_route_expert)[0].transpose(1, 0, 2).reshape(1024, 32)], axis=1)
    return {
        "x": f(x[core * nseq:(core + 1) * nseq]).reshape(nseq * S, D),
        "mem": f(mem[core * nseq:(core + 1) * nseq]).reshape(nseq * 256, D),
        "w_in": f(w_in)[0], "w_out": f(w_out)[0], "w_mq": f(w_mq)[0], "w_mkv": f(w_mkv)[0], "w_mo": f(w_mo)[0],
        "w_route": np.ascontiguousarray(wr.reshape(128, 8 * 36)),
        "w_gate": f(w_gate)[0].reshape(32 * 128, 4096), "w_up": f(w_up)[0].reshape(32 * 128, 4096),
        "w_down": f(w_down)[0].reshape(32 * 512, 1024),
        "vecs": vecs, "rows": rows, "constf": cfm, "constb": cbm, "consti": cim,
    }


_NC_CACHE = {}


def kernel(**inputs):
    n_cores = 8
    nseq = 4
    if "full" not in _NC_CACHE:
        _NC_CACHE["full"] = build(nseq=nseq)
    nc = _NC_CACHE["full"]
    in_maps = [_host_inputs(c, nseq, **inputs) for c in range(n_cores)]
    res = run_bass_kernel_spmd(nc, in_maps, core_ids=list(range(n_cores)))
    outs = [np.asarray(r["out"]).reshape(nseq, S, D) for r in res.results]
    return np.concatenate(outs, axis=0).astype(np.float32)
```

```python
import numpy as np
import ml_dtypes
from concourse.bass_utils import run_bass_kernel_spmd
from contextlib import ExitStack
import concourse.bass as bass
import concourse.mybir as mybir

F32 = mybir.dt.float32
BF16 = mybir.dt.bfloat16
I32 = mybir.dt.int32
ALU = mybir.AluOpType
AF = mybir.ActivationFunctionType
AX = mybir.AxisListType
_DSZ = {F32: 4, BF16: 2, I32: 4, mybir.dt.uint32: 4, mybir.dt.float16: 2, mybir.dt.uint8: 1,
        mybir.dt.int8: 1, mybir.dt.uint16: 2, mybir.dt.int16: 2}


def region(ap):
    dsz = _DSZ[ap.dtype]
    dims = ap.ap
    off = ap.offset
    sp = str(ap.space)
    if sp in ("SB", "PSUM"):
        shp = ap.tensor.shape
        rowlen = 1
        for s in list(shp)[1:]:
            rowlen *= s
        p0 = off // rowlen
        c0 = off % rowlen
        pstep, pcnt = dims[0]
        p1 = p0 + ((pcnt - 1) * pstep) // rowlen + 1 if pstep else p0 + 1
        ext = 0
        for st, cn in dims[1:]:
            ext += (cn - 1) * abs(st)
        if sp == "PSUM":
            return (sp + ":" + ap.tensor.name, 0, 128, 0, 1 << 20)
        return (sp + ":" + ap.tensor.name, p0, p1, c0 * dsz, (c0 + ext + 1) * dsz)
    else:
        ext = 0
        for st, cn in dims:
            ext += (cn - 1) * abs(st)
        return ("D:" + ap.tensor.name, 0, 1, off * dsz, (off + ext + 1) * dsz)


class Op:
    __slots__ = ("eng", "fn", "reads", "writes", "dma", "deps", "sig", "dsem", "dval",
                 "waits", "idx", "prewait")


class Prog:
    ENGS = ("pe", "act", "dve", "pool", "sp")
    EPOCH = 30000

    def __init__(self, nc, ndma_sems=56):
        self.nc = nc
        self.ops = []
        self.recs = {}
        self.ndma = ndma_sems
        self.extra_regions = {}

    def reg(self, e, val):
        d = self.__dict__.setdefault("_regs", {})
        if val not in d:
            d[val] = e.to_reg(val)
        return d[val]

    def add(self, eng, fn, reads=(), writes=(), dma=False):
        op = Op()
        op.eng = eng; op.fn = fn; op.dma = dma
        op.reads = [r if isinstance(r, tuple) else region(r) for r in reads]
        op.writes = [r if isinstance(r, tuple) else region(r) for r in writes]
        op.idx = len(self.ops)
        op.sig = None; op.dsem = None; op.dval = None; op.waits = []; op.prewait = None
        deps = set()
        for (sp, p0, p1, lo, hi) in op.reads:
            for r in self.recs.get(sp, ()):
                if r[5] and r[0] < p1 and p0 < r[1] and r[2] < hi and lo < r[3]:
                    deps.add(r[4])
        for (sp, p0, p1, lo, hi) in op.writes:
            lst = self.recs.get(sp)
            if lst is None:
                lst = self.recs[sp] = []
            keep = []
            for r in lst:
                if r[0] < p1 and p0 < r[1] and r[2] < hi and lo < r[3]:
                    deps.add(r[4])
                    if r[0] >= p0 and r[1] <= p1 and r[2] >= lo and r[3] <= hi:
                        continue
                keep.append(r)
            keep.append([p0, p1, lo, hi, op.idx, True])
            self.recs[sp] = keep
        for (sp, p0, p1, lo, hi) in op.reads:
            lst = self.recs.get(sp)
            if lst is None:
                lst = self.recs[sp] = []
            if not dma:
                for r in lst:
                    if (not r[5]) and r[0] == p0 and r[1] == p1 and r[2] == lo and r[3] == hi \
                            and (not self.ops[r[4]].dma) and self.ops[r[4]].eng == eng:
                        r[4] = op.idx
                        break
                else:
                    lst.append([p0, p1, lo, hi, op.idx, False])
            else:
                lst.append([p0, p1, lo, hi, op.idx, False])
        deps.discard(op.idx)
        op.deps = deps
        self.ops.append(op)
        return op

    def matmul(self, out, lhsT, rhs, start=True, stop=True, **kw):
        return self.add("pe", lambda e: e.matmul(out, lhsT, rhs, start=start, stop=stop, **kw),
                        reads=[lhsT, rhs], writes=[out])

    def transpose(self, out, in_, ident):
        return self.add("pe", lambda e: e.transpose(out, in_, ident), reads=[in_, ident], writes=[out])

    def activation(self, out, in_, func, bias=None, scale=None, accum_out=None, eng="act"):
        reads = [in_]
        kw = {}
        if bias is not None:
            kw["bias"] = bias
            if not isinstance(bias, (int, float)):
                reads.append(bias)
        if scale is not None:
            kw["scale"] = scale
            if not isinstance(scale, (int, float)):
                reads.append(scale)
        writes = [out]
        if accum_out is not None:
            kw["accum_out"] = accum_out
            writes.append(accum_out)
        return self.add(eng, lambda e: e.activation(out, in_, func, **kw), reads=reads, writes=writes)

    def tensor_scalar(self, out, in0, s1, s2, op0, op1=None, eng="dve", accum_out=None):
        reads = [in0]
        for s in (s1, s2):
            if s is not None and not isinstance(s, (int, float)):
                reads.append(s)
        kw = {}
        if op1 is not None:
            kw["op1"] = op1
        writes = [out]
        if accum_out is not None:
            kw["accum_out"] = accum_out
            writes.append(accum_out)
        return self.add(eng, lambda e: e.tensor_scalar(out, in0, s1, s2, op0, **kw), reads=reads, writes=writes)

    def tensor_tensor(self, out, in0, in1, op, eng="dve"):
        return self.add(eng, lambda e: e.tensor_tensor(out, in0, in1, op), reads=[in0, in1], writes=[out])

    def stt(self, out, in0, scalar, in1, op0, op1, eng="dve", accum_out=None):
        reads = [in0, in1]
        if not isinstance(scalar, (int, float)):
            reads.append(scalar)
        kw = {}
        writes = [out]
        if accum_out is not None:
            kw["accum_out"] = accum_out
            writes.append(accum_out)
        return self.add(eng, lambda e: e.scalar_tensor_tensor(out, in0, scalar, in1, op0, op1, **kw),
                        reads=reads, writes=writes)

    def copy(self, out, in_, eng="dve"):
        if eng == "act":
            return self.add(eng, lambda e: e.copy(out, in_), reads=[in_], writes=[out])
        return self.add(eng, lambda e: e.tensor_copy(out, in_), reads=[in_], writes=[out])

    def memset(self, out, val, eng="dve"):
        return self.add(eng, lambda e: e.memset(out, val), reads=[], writes=[out])

    def reduce(self, out, in_, op, axis=None, eng="dve"):
        ax = axis if axis is not None else AX.X
        return self.add(eng, lambda e: e.tensor_reduce(out, in_, ax, op), reads=[in_], writes=[out])

    def dma(self, out, in_, q="sp", **kw):
        return self.add(q, lambda e: e.dma_start(out=out, in_=in_, **kw), reads=[in_], writes=[out], dma=True)

    def finalize(self):
        ops = self.ops
        need = [False] * len(ops)
        for op in ops:
            for d in op.deps:
                p = ops[d]
                if p.dma:
                    continue
                if p.eng == op.eng and not op.dma:
                    if p.eng == "pe":
                        continue
                    continue_flag = True
                    for (sp, p0, p1, lo, hi) in op.reads:
                        for (sp2, q0, q1, lo2, hi2) in p.writes:
                            if sp == sp2 and q0 < p1 and p0 < q1 and lo2 < hi and lo < hi2:
                                continue_flag = False
                    if continue_flag:
                        continue
                need[d] = True
        cnt = {e: 0 for e in self.ENGS}
        for op in ops:
            if (not op.dma) and need[op.idx]:
                cnt[op.eng] += 1
                op.sig = cnt[op.eng]
        dcount = [0] * self.ndma
        k = 0
        for op in ops:
            if op.dma:
                s = k % self.ndma
                k += 1
                dcount[s] += 1
                op.dsem = s
                op.dval = 16 * dcount[s]
                op.prewait = (("dma", s), 16 * (dcount[s] - 1)) if dcount[s] > 1 else None
        self.dfinal = [16 * c for c in dcount]
        seen = {e: {} for e in self.ENGS}
        pos = {e: 0 for e in self.ENGS}
        opos = {}
        for op in ops:
            pos[op.eng] += 1
            opos[op.idx] = pos[op.eng]
        nw = 0
        for op in ops:
            sn = seen[op.eng]
            w = {}
            if op.prewait is not None:
                key, val = op.prewait
                if sn.get(key, 0) < val:
                    w[key] = val
            for d in op.deps:
                p = ops[d]
                if p.dma:
                    key, val = ("dma", p.dsem), p.dval
                else:
                    if p.sig is None:
                        continue
                    if p.eng == op.eng and not op.dma:
                        if p.eng == "pe":
                            continue
                        if opos[op.idx] - opos[p.idx] > 3:
                            continue
                    ep = (p.sig - 1) // self.EPOCH
                    key, val = ("eng", p.eng), ep * 100000 + (p.sig - 1) % self.EPOCH + 1
                if sn.get(key, 0) < val and w.get(key, 0) < val:
                    w[key] = val
            for key, val in w.items():
                sn[key] = val
            op.waits = list(w.items())
            nw += len(op.waits)
        self.n_waits = nw
        self.n_epochs = {e: (cnt[e] - 1) // self.EPOCH + 1 if cnt[e] else 1 for e in self.ENGS}
        return cnt

    def emit(self):
        nc = self.nc
        cnt = self.finalize()
        ops = self.ops
        with ExitStack() as st:
            esem = {}
            for e in self.ENGS:
                for ep in range(self.n_epochs[e]):
                    esem[(e, ep)] = st.enter_context(nc.semaphore("s_%s%d" % (e, ep)))
            dsem = [st.enter_context(nc.semaphore("d%d" % i)) for i in range(self.ndma)]
            block = st.enter_context(nc.Block())
            by_eng = {e: [op for op in ops if op.eng == e] for e in self.ENGS}
            EP = self.EPOCH

            def run(eng_obj, lst, is_sp):
                for op in lst:
                    for key, val in op.waits:
                        if key[0] == "dma":
                            eng_obj.wait_ge(dsem[key[1]], val)
                        else:
                            ep, v = divmod(val, 100000)
                            eng_obj.wait_ge(esem[(key[1], ep)], v)
                    inst = op.fn(eng_obj)
                    if op.dma:
                        inst.then_inc(dsem[op.dsem], 16)
                    elif op.sig is not None:
                        ep = (op.sig - 1) // EP
                        inst.then_inc(esem[(op.eng, ep)], 1)
                if is_sp:
                    for i, v in enumerate(self.dfinal):
                        if v:
                            eng_obj.wait_ge(dsem[i], v)

            @block.sync
            def _(e):
                run(e, by_eng["sp"], True)

            @block.tensor
            def _(e):
                run(e, by_eng["pe"], False)

            @block.scalar
            def _(e):
                run(e, by_eng["act"], False)

            @block.vector
            def _(e):
                run(e, by_eng["dve"], False)

            @block.gpsimd
            def _(e):
                run(e, by_eng["pool"], False)
        return cnt

S = 2048
D = 1024
NT = S // 128
TS = 512
NTILES = 16384 // TS + 32
GT = TS // 128
NSLOT = NTILES * TS
GM, GQ, GK, GA, BG, BD, WDW = 0, 8, 16, 24, 28, 36, 40
NV = 40 + 4 * 31
RG, RF, RLG, RLB, RBF, RBR, RBDW = 0, 1024, 2048, 2560, 3072, 3200, 3236
NR = 3748
THR, PCOL, CBASE, IOTA, CEPS, NCF = 0, 96, 97, 101, 133, 141
IDB, NEGM, UST, ONEB, LTRIB, NCB = 0, 128, 256, 384, 512, 640


def _prod(s):
    r = 1
    for a in s:
        r *= a
    return r


class Arena:
    def __init__(self, nc, st, name, nbytes):
        self.t = st.enter_context(nc.sbuf_tensor(name, [128, nbytes // 4], F32))
        self.cap = nbytes
        self.off = 0

    def alloc(self, nbytes):
        off = (self.off + 63) // 64 * 64
        self.off = off + nbytes
        assert self.off <= self.cap, ("arena overflow", self.off, self.cap)
        return off

    def view(self, off, dtype, shape):
        n = _prod(shape)
        dsz = _DSZ[dtype]
        nb = (n * dsz + 3) // 4
        v = self.t[:, off // 4: off // 4 + nb]
        if dtype != F32:
            v = v.bitcast(dtype)
        v = v[:, 0:n]
        if len(shape) == 2:
            v = v.rearrange("p (a b) -> p a b", a=shape[0], b=shape[1])
        elif len(shape) == 3:
            v = v.rearrange("p (a b c) -> p a b c", a=shape[0], b=shape[1], c=shape[2])
        return v

    def new(self, dtype, shape):
        off = self.alloc(_prod(shape) * _DSZ[dtype])
        return self.view(off, dtype, shape)


import os as _os
BP = set(_os.environ.get('BPARTS', 'conv,convT,qk,mask,exp,pv,norm,tail,catT').split(','))


def build(nseq=4, debug=(), stop_after=None, n_moe_tiles=NTILES, skip_moe=False):
    nc = bass.Bass("TRN2", target_bir_lowering=False)
    NTOK = nseq * S
    NTT = NTOK // 128

    def din(name, shape, dt=F32):
        return nc.dram_tensor(name, shape, dt, kind="ExternalInput").ap()

    def scr(name, shape, dt):
        if name in debug:
            return nc.dram_tensor(name, shape, dt, kind="ExternalOutput").ap()
        return nc.dram_tensor(name, shape, dt).ap()

    x = din("x", [NTOK, D])
    mem = din("mem", [nseq * 256, D])
    w_in = din("w_in", [D, 2568])
    w_out = din("w_out", [D, D])
    w_mq = din("w_mq", [D, D])
    w_mkv = din("w_mkv", [D, 2 * D])
    w_mo = din("w_mo", [D, D])
    w_route = din("w_route", [128, 8 * 36])
    w_gate = din("w_gate", [32 * 128, 4096])
    w_up = din("w_up", [32 * 128, 4096])
    w_down = din("w_down", [32 * 512, 1024])
    vecs = din("vecs", [128, NV])
    rows = din("rows", [128, NR])
    constf = din("constf", [128, NCF])
    constb = din("constb", [128, NCB], BF16)
    consti = din("consti", [128, 64], I32)
    out = nc.dram_tensor("out", [NTOK, D], F32, kind="ExternalOutput").ap()

    w_in_bf = scr("w_in_bf", [D, 2568], BF16)
    w_out_bf = scr("w_out_bf", [D, D], BF16)
    w_mq_bf = scr("w_mq_bf", [D, D], BF16)
    w_mkv_bf = scr("w_mkv_bf", [D, 2 * D], BF16)
    w_mo_bf = scr("w_mo_bf", [D, D], BF16)
    x2s = scr("x2s", [NTOK, D], F32)
    h3s = scr("h3s", [NTOK, D], BF16)
    Ysc = scr("Ysc", [NSLOT, D], BF16)
    s2t = scr("s2t", [NSLOT, 1], I32)
    dbg = {}
    for name, shape, dt in (("d_cat", [S, D], BF16), ("d_x1", [S, D], F32), ("d_lg", [NTOK, 36], F32),
                            ("d_q", [128, 4 * 2048], BF16), ("d_attn", [S, 512], F32), ("d_z", [128, 4 * 2080], BF16),
                            ("d_B", [128, 2048], F32), ("d_rt", [128, 64 * 8], F32), ("d_te", [128, 96], F32),
                            ("d_conv", [S, 512], F32)):
        if name in debug:
            dbg[name] = nc.dram_tensor(name, shape, dt, kind="ExternalOutput").ap()

    P = Prog(nc)
    st = ExitStack()
    A = Arena(nc, st, "arena", 210944)
    PS = [st.enter_context(nc.psum_tensor("ps%d" % i, [128, 512], F32)) for i in range(8)]

    def psf(i):
        return PS[i][:, :]

    def psb(i):
        return PS[i][:, :].bitcast(BF16)

    cf = A.new(F32, [NCF])
    cb = A.new(BF16, [NCB])
    ci = A.new(I32, [64])
    vv = A.new(F32, [NV])
    rA = A.new(F32, [1700])
    LG_, LB_, BF_, BR_, BDW_ = 0, 512, 1024, 1152, 1188
    LG = A.new(F32, [64, 36])
    G1 = A.new(F32, [64]); G2 = A.new(F32, [64]); P1 = A.new(F32, [64]); P2 = A.new(F32, [64])
    POS1 = A.new(I32, [64]); POS2 = A.new(I32, [64])
    base = A.new(F32, [32])
    wr_bf = A.new(BF16, [8, 36])
    v_ = None
    identb = cb[:, IDB:IDB + 128]
    negm = cb[:, NEGM:NEGM + 128]
    eps1024 = cf[:, CEPS:CEPS + 1]
    eps512 = cf[:, CEPS + 1:CEPS + 2]
    one_c = cf[:, CEPS + 2:CEPS + 3]
    epsln = cf[:, CEPS + 3:CEPS + 4]
    P.dma(cf, constf); P.dma(cb, constb); P.dma(ci, consti); P.dma(vv, vecs)
    P.dma(rA, rows[:, RLG:RLG + 1700])
    P.memset(base, 0.0)
    P.tensor_scalar(vv[:, GM:GM + 24], vv[:, GM:GM + 24], 32.0, None, ALU.mult)
    P.tensor_scalar(vv[:, GA:GA + 4], vv[:, GA:GA + 4], float(np.sqrt(512.0)), None, ALU.mult)
    mark_persist = A.off

    Wreg_off = A.alloc(49152)
    Win = A.view(Wreg_off, BF16, [8, 2568])
    Woqm = A.view(Wreg_off, BF16, [3, 8, 1024])
    xT = A.new(BF16, [8, 2048])
    K2T = A.new(BF16, [8, 256])
    V2 = A.new(BF16, [2, 4, 257])
    Bt = A.new(F32, [8, 16, 16])
    nlf = A.new(F32, [16, 8]); incl = A.new(F32, [16, 8]); Cc = A.new(F32, [16, 8]); Cend = A.new(F32, [16, 8])
    U_off = A.alloc(66048 + 64)
    qT = A.view(U_off, BF16, [4, 2048])
    kT = A.view(U_off + 16384, BF16, [4, 2048])
    Wkv = A.view(U_off, BF16, [8, 2048])
    vS = A.view(U_off + 32768, BF16, [16, 8, 65])
    zT = A.view(U_off + 32768 + 16640, BF16, [4, 2080])
    memT = A.view(U_off + 32768 + 16640, BF16, [8, 256])
    T_off = A.alloc(23040)
    xin = [A.view(T_off + i * 4096, F32, [1024]) for i in range(2)]
    xn = [A.view(T_off + 8192 + i * 2048, BF16, [1024]) for i in range(2)]
    sig = [A.view(T_off + 12288 + i * 2048, F32, [512]) for i in range(2)]
    stA = A.view(T_off + 16384, F32, [16])
    PT = [A.view(T_off + i * 256, BF16, [128]) for i in range(6)]
    attn = A.view(T_off + 1536, F32, [512])
    catA = A.view(T_off + 3584, BF16, [512])
    catC = [A.view(T_off + 4608 + i * 1024, BF16, [512]) for i in range(2)]
    cz = [A.view(T_off + 6656 + i * 2048, F32, [512]) for i in range(2)]
    stB = A.view(T_off + 10752, F32, [64])
    Dg = A.view(T_off + 11008, BF16, [31, 128])
    zoff = U_off + 32768 + 16640
    cvo = [A.view(T_off + 18944, BF16, [16, 128])] + [A.view(zoff + i * 4160, BF16, [16, 128]) for i in range(3)]
    o_ = U_off
    x1 = A.view(o_, F32, [2, 1024]); o_ += 8192
    xr = [A.view(o_ + i * 4096, F32, [1024]) for i in range(2)]; o_ += 8192
    xn2 = [A.view(o_ + i * 2048, BF16, [1024]) for i in range(2)]; o_ += 4096
    h2T = A.view(o_, BF16, [8, 256]); o_ += 4096
    q2T = A.view(o_, BF16, [8, 256]); o_ += 4096
    P2T = [A.view(o_ + i * 512, BF16, [256]) for i in range(8)]; o_ += 4096
    oo = A.view(o_, BF16, [2, 1024]); o_ += 4096
    oT = A.view(o_, BF16, [8, 256]); o_ += 4096
    x2t = [A.view(o_ + i * 4096, F32, [1024]) for i in range(2)]; o_ += 8192
    h3t = [A.view(o_ + i * 2048, BF16, [1024]) for i in range(2)]; o_ += 4096
    h3T = A.view(o_, BF16, [8, 128]); o_ += 2048
    rG = A.view(o_, F32, [1024]); o_ += 4096
    rt = A.view(o_, F32, [256]); o_ += 1024
    rtb = A.view(o_, BF16, [64]); o_ += 128
    stC = A.view(o_, F32, [32]); o_ += 128
    assert o_ <= U_off + 66048
    x1b = [x1, A.view(T_off, F32, [2, 1024])]
    h2Tb = [h2T, A.view(T_off + 8192, BF16, [8, 256])]
    q2Tb = [q2T, A.view(T_off + 12288, BF16, [8, 256])]

    st0 = [A.view(U_off + i * 15488, F32, [2568]) for i in range(4)]
    st0b = [A.view(U_off + 10304 + i * 15488, BF16, [2568]) for i in range(4)]
    k = 0
    for (src, dst, ncol, gcol, nsc) in ((w_in, w_in_bf, 2568, GM, 8), (w_mkv, w_mkv_bf, 2048, GK, 8),
                                       (w_mq, w_mq_bf, 1024, GQ, 8), (w_out, w_out_bf, 1024, GA, 4),
                                       (w_mo, w_mo_bf, 1024, None, 0)):
        for c in range(8):
            sb_, sbb = st0[k % 4], st0b[k % 4]
            P.dma(sb_[:, 0:ncol], src[c * 128:(c + 1) * 128, :])
            if c < nsc:
                if k % 2 == 0:
                    P.tensor_scalar(sbb[:, 0:ncol], sb_[:, 0:ncol], vv[:, gcol + c:gcol + c + 1], None, ALU.mult)
                else:
                    P.activation(sbb[:, 0:ncol], sb_[:, 0:ncol], AF.Copy, scale=vv[:, gcol + c:gcol + c + 1])
            else:
                P.copy(sbb[:, 0:ncol], sb_[:, 0:ncol], eng="dve" if k % 2 == 0 else "act")
            P.dma(dst[c * 128:(c + 1) * 128, :], sbb[:, 0:ncol])
            k += 1
    P.dma(st0[0][:, 0:288], w_route)
    P.copy(wr_bf, st0[0][:, 0:288].rearrange("p (c n) -> p c n", c=8))

    if stop_after == "phase0":
        P.dma(out[0:128, :], st0[0][:, 0:1024])
        P.emit()
        return nc
    bank = [0]

    def nb():
        bank[0] = (bank[0] + 1) % 8
        return bank[0]

    def rstd_from_ss(dst, ss, epsc):
        P.activation(dst, ss, AF.Ln, bias=epsc, scale=1.0)
        P.activation(dst, dst, AF.Exp, scale=-0.5)


    def recip(o, i, eng="dve"):
        P.add(eng, lambda e: e.reciprocal(o, i), reads=[i], writes=[o])

    def route_tile(gi, pl, tt):
        lg = rt[:, 0:36]
        P.tensor_tensor(lg, pl, rA[:, BR_:BR_ + 36], ALU.add)
        if "d_lg" in dbg:
            P.dma(dbg["d_lg"][gi * 128:(gi + 1) * 128, :], lg)
        gmax, ngmax, gsum, gtop = rt[:, 40:41], rt[:, 41:42], rt[:, 42:43], rt[:, 43:44]
        ohg = rt[:, 44:48]
        P.reduce(gmax, lg[:, 0:4], ALU.max)
        P.tensor_scalar(ohg, lg[:, 0:4], gmax, None, ALU.is_equal)
        P.tensor_scalar(ngmax, gmax, -1.0, None, ALU.mult)
        P.activation(rt[:, 48:52], lg[:, 0:4], AF.Exp, bias=ngmax, scale=1.0, accum_out=gsum)
        recip(gtop, gsum)
        esel = rt[:, 52:60]
        P.tensor_scalar(esel, lg[:, 4:12], ohg[:, 0:1], None, ALU.mult)
        for g in range(1, 4):
            P.stt(esel, lg[:, 4 + 8 * g:12 + 8 * g], ohg[:, g:g + 1], esel, ALU.mult, ALU.add)
        top8 = rt[:, 60:68]
        P.add("dve", lambda e: e.max(top8, esel), reads=[esel], writes=[top8])
        oh1, oh2 = rt[:, 68:76], rt[:, 76:84]
        P.tensor_scalar(oh1, esel, top8[:, 0:1], None, ALU.is_equal)
        P.tensor_scalar(oh2, esel, top8[:, 1:2], None, ALU.is_equal)
        dd, w1 = rt[:, 84:85], rt[:, 85:86]
        P.tensor_tensor(dd, top8[:, 1:2], top8[:, 0:1], ALU.subtract)
        P.activation(dd, dd, AF.Exp)
        P.tensor_scalar(dd, dd, 1.0, None, ALU.add)
        recip(w1, dd)
        P.tensor_tensor(G1[:, gi:gi + 1], gtop, w1, ALU.mult)
        P.tensor_tensor(G2[:, gi:gi + 1], gtop, G1[:, gi:gi + 1], ALU.subtract)
        a1, a2 = rt[:, 96:128], rt[:, 128:160]
        for a_, oh in ((a1, oh1), (a2, oh2)):
            P.tensor_tensor(a_.rearrange("p (g e) -> p g e", g=4), ohg.unsqueeze(2).to_broadcast([128, 4, 8]),
                            oh.unsqueeze(1).to_broadcast([128, 4, 8]), ALU.mult)
        P.copy(A1[:, gi, :], a1)
        P.copy(A2[:, gi, :], a2)
        cmb = rtb[:, 0:32]
        P.tensor_tensor(cmb, a1, a2, ALU.add)
        pr = psf(7)[:, 384 + 64 * tt:448 + 64 * tt]
        P.matmul(pr[:, 0:32], cb[:, UST:UST + 128], cmb)
        P.matmul(pr[:, 32:64], cb[:, ONEB:ONEB + 128], cmb)
        rk, jk = rt[:, 160:192], rt[:, 192:224]
        P.tensor_tensor(rk, pr[:, 0:32], base, ALU.add)
        P.tensor_tensor(jk, a1, rk, ALU.mult)
        P.reduce(P1[:, gi:gi + 1], jk, ALU.add)
        P.tensor_tensor(jk, a2, rk, ALU.mult)
        P.reduce(P2[:, gi:gi + 1], jk, ALU.add)
        P.tensor_tensor(base, base, pr[:, 32:64], ALU.add)

    def sumsq(junk, src, ss):
        P.activation(junk, src, AF.Square, accum_out=ss)

    def transposeT(src_bf, dstT, col0, ev, b=None):
        if b is None:
            b = 6 + (nb() % 2)
        pv = psb(b)
        for c in range(8):
            P.transpose(pv[:, c * 128:(c + 1) * 128], src_bf[:, c * 128:(c + 1) * 128], identb)
        P.copy(dstT[:, :, col0:col0 + 128], pv.rearrange("p (c t) -> p c t", c=8), eng=ev)


    for b in range(nseq):
        tok0 = b * S
        for c in range(8):
            P.dma(Wkv[:, c, :], w_mkv_bf[c * 128:(c + 1) * 128, :])
        for mt in range(2):
            xi, xb = xin[mt], xn[mt]
            P.dma(xi, mem[b * 256 + mt * 128: b * 256 + (mt + 1) * 128, :])
            ss, rs = stA[:, 2 * mt:2 * mt + 1], stA[:, 2 * mt + 1:2 * mt + 2]
            sumsq(xb, xi, ss)
            rstd_from_ss(rs, ss, eps1024)
            P.tensor_scalar(xb, xi, rs, None, ALU.mult)
            transposeT(xb, memT, mt * 128, "dve")
        for cc in range(8):
            bk = nb() % 6
            pv = psf(bk)[:, 0:256]
            for c in range(8):
                P.matmul(pv, Wkv[:, c, cc * 128:(cc + 1) * 128], memT[:, c, :], start=(c == 0), stop=(c == 7))
            P.copy(K2T[:, cc, :], pv, eng="act" if cc % 2 else "dve")
        for mt in range(2):
            for nh in range(2):
                bk = nb() % 6
                pv = psf(bk)
                for c in range(8):
                    P.matmul(pv, memT[:, c, mt * 128:(mt + 1) * 128], Wkv[:, c, 1024 + nh * 512:1024 + (nh + 1) * 512],
                             start=(c == 0), stop=(c == 7))
                P.copy(V2[:, mt, 2 * nh:2 * nh + 2, 0:256], pv.rearrange("p (h d) -> p h d", h=2),
                       eng="act" if nh % 2 else "dve")
        P.memset(V2[:, :, :, 256:257], 1.0)
        if stop_after == "A0":
            P.dma(out[0:128, :], xin[0])
            P.emit()
            return nc
        for c in range(8):
            P.dma(Win[:, c, :], w_in_bf[c * 128:(c + 1) * 128, :])
        for i in range(NT):
            xi, xb = xin[i % 2], xn[i % 2]
            P.dma(xi, x[tok0 + i * 128: tok0 + (i + 1) * 128, :])
            ss, rs = stA[:, 4 + 2 * (i % 2):5 + 2 * (i % 2)], stA[:, 5 + 2 * (i % 2):6 + 2 * (i % 2)]
            sumsq(xb, xi, ss)
            rstd_from_ss(rs, ss, eps1024)
            P.tensor_scalar(xb, xi, rs, None, ALU.mult)
            transposeT(xb, xT, i * 128, "act" if i % 2 else "dve")
        if stop_after == "A1":
            P.dma(out[0:128, :], xin[0])
            P.emit()
            return nc
        P.memset(zT[:, :, 0:30], 0.0)
        P.memset(zT[:, :, 2078:2080], 0.0)
        P.memset(vS[:, :, :, 64:65], 1.0)
        OFF_K, OFF_V, OFF_F, OFF_C = 512, 1024, 1536, 1544
        for tb in range(4):
            tsl = slice(tb * 512, (tb + 1) * 512)
            for cc in range(8):
                bk = nb() % 6
                pv = psf(bk)
                for c in range(8):
                    P.matmul(pv, Win[:, c, cc * 128:(cc + 1) * 128], xT[:, c, tsl], start=(c == 0), stop=(c == 7))
                dst = qT[:, cc, tsl] if cc < 4 else kT[:, cc - 4, tsl]
                P.copy(dst, pv, eng="act" if cc % 2 else "dve")
            for i4 in range(4):
                bg = nb() % 6
                pg = psf(bg)
                for c in range(8):
                    P.matmul(pg, Win[:, c, OFF_C + 512 + i4 * 128:OFF_C + 512 + (i4 + 1) * 128], xT[:, c, tsl],
                             start=(c == 0), stop=(c == 7))
                sg = sig[i4 % 2]
                P.activation(sg, pg, AF.Sigmoid, bias=vv[:, BG + 4 + i4:BG + 5 + i4], scale=1.0)
                ba = nb() % 6
                pa = psf(ba)
                for c in range(8):
                    P.matmul(pa, Win[:, c, OFF_C + i4 * 128:OFF_C + (i4 + 1) * 128], xT[:, c, tsl],
                             start=(c == 0), stop=(c == 7))
                P.stt(zT[:, i4, 30 + tb * 512:30 + (tb + 1) * 512], pa, vv[:, BG + i4:BG + i4 + 1], sg, ALU.add, ALU.mult)
        pf_ = psf(6)
        for i in range(NT):
            bk = nb() % 6
            pv = psf(bk)
            for c in range(8):
                P.matmul(pv, xT[:, c, i * 128:(i + 1) * 128], Win[:, c, OFF_V:OFF_V + 512], start=(c == 0), stop=(c == 7))
            P.copy(vS[:, i, :, 0:64], pv.rearrange("p (h d) -> p h d", h=8), eng="act" if i % 2 else "dve")
            for c in range(8):
                P.matmul(pf_[:, i * 8:(i + 1) * 8], xT[:, c, i * 128:(i + 1) * 128], Win[:, c, OFF_F:OFF_F + 8],
                         start=(c == 0), stop=(c == 7))
        if stop_after == "A2":
            P.dma(out[0:128, :], xin[0])
            P.emit()
            return nc
        nlf2 = nlf.rearrange("p a b -> p (a b)")
        P.tensor_tensor(nlf2, pf_[:, 0:128], rA[:, BF_:BF_ + 128], ALU.add)
        P.activation(nlf2, nlf2, AF.Exp, scale=-1.0)
        P.activation(nlf2, nlf2, AF.Ln, bias=one_c, scale=1.0)
        if stop_after == "A3a":
            P.dma(out[0:128, :], xin[0])
            P.emit()
            return nc
        P.copy(incl[:, 0, :], nlf[:, 0, :])
        for i in range(1, NT):
            P.tensor_tensor(incl[:, i, :], incl[:, i - 1, :], nlf[:, i, :], ALU.add)
        if stop_after == "A3b":
            P.dma(out[0:128, :], xin[0])
            P.emit()
            return nc
        pc1 = psf(7)[:, 0:128]
        pc2 = psf(7)[:, 128:256]
        for (pdst, lmat, srcf) in ((pc1, cb[:, ONEB:ONEB + 128], incl.rearrange("p a b -> p (a b)")),
                                   (pc2, cb[:, LTRIB:LTRIB + 128], nlf2)):
            rres = cz[0][:, 0:128]
            for t3 in range(3):
                piece = xn[0][:, t3 * 128:(t3 + 1) * 128]
                P.copy(piece, srcf if t3 == 0 else rres)
                if t3 < 2:
                    P.tensor_tensor(rres, srcf if t3 == 0 else rres, piece, ALU.subtract)
                P.matmul(pdst, lmat, piece, start=(t3 == 0), stop=(t3 == 2))
        P.copy(Cend.rearrange("p a b -> p (a b)"), pc1)
        P.copy(Cc.rearrange("p a b -> p (a b)"), pc2)
        if stop_after == "A3c":
            P.dma(out[0:128, :], xin[0])
            P.emit()
            return nc
        P.tensor_tensor(Cc[:, 1:16, :], Cc[:, 1:16, :], Cend[:, 0:15, :], ALU.add)
        if stop_after == "A3d":
            P.dma(out[0:128, :], xin[0])
            P.emit()
            return nc
        for h in range(8):
            P.tensor_tensor(Bt[:, h, :, :], Cc[:, :, h:h + 1].to_broadcast([128, 16, 16]),
                            Cend[:, :, h].unsqueeze(1).to_broadcast([128, 16, 16]), ALU.subtract)
        if "d_q" in dbg:
            P.dma(dbg["d_q"], qT.rearrange("p a b -> p (a b)"))
        if "d_z" in dbg:
            P.dma(dbg["d_z"], zT.rearrange("p a b -> p (a b)"))
        if "d_B" in dbg:
            P.dma(dbg["d_B"], Bt.rearrange("p a b c -> p (a b c)"))
        if stop_after == "A3":
            P.dma(out[0:128, :], xin[0])
            P.emit()
            return nc
        if 'conv' in BP:
            for i4 in range(4):
                for kk in range(31):
                    P.tensor_scalar(Dg[:, kk, :], identb, vv[:, WDW + i4 * 31 + kk:WDW + i4 * 31 + kk + 1], None, ALU.mult)
                for j4 in range(4):
                    pcv = psf(nb() % 6)
                    for t4 in range(4):
                        c0 = (j4 * 4 + t4) * 128
                        for kk in range(31):
                            P.matmul(pcv[:, t4 * 128:(t4 + 1) * 128], zT[:, i4, c0 + kk:c0 + kk + 128], Dg[:, kk, :],
                                     start=(kk == 0), stop=(kk == 30))
                    P.tensor_tensor(cvo[i4][:, j4 * 4:(j4 + 1) * 4, :], pcv.rearrange("p (t c) -> p t c", t=4),
                                    rA[:, BDW_ + i4 * 128:BDW_ + (i4 + 1) * 128].unsqueeze(1).to_broadcast([128, 4, 128]),
                                    ALU.add)
        for wi, wsrc in enumerate((w_out_bf, w_mq_bf, w_mo_bf)):
            for c in range(8):
                P.dma(Woqm[:, wi, c, :], wsrc[c * 128:(c + 1) * 128, :])

        catT = xT
        LAG = 3

        def tail_conv(j):
            s1, nm, sq, rsd = stB[:, 10:11], stB[:, 11:12], stB[:, 12:13], stB[:, 13:14]
            xc, tmp = cz[0], cz[1]
            for i4 in range(4):
                P.copy(xc[:, i4 * 128:(i4 + 1) * 128], cvo[i4][:, j, :], eng="pool" if i4 % 2 else "dve")
            if "d_conv" in dbg:
                P.dma(dbg["d_conv"][j * 128:(j + 1) * 128, :], xc)
            P.reduce(s1, xc, ALU.add)
            P.tensor_scalar(nm, s1, -1.0 / 512.0, None, ALU.mult)
            P.tensor_scalar(xc, xc, nm, None, ALU.add)
            sumsq(tmp, xc, sq)
            P.activation(rsd, sq, AF.Ln, bias=epsln, scale=1.0 / 512.0)
            P.activation(rsd, rsd, AF.Exp, scale=-0.5)
            P.stt(xc, xc, rsd, rA[:, LG_:LG_ + 512], ALU.mult, ALU.mult)
            P.tensor_tensor(xc, xc, rA[:, LB_:LB_ + 512], ALU.add)
            P.activation(tmp, xc, AF.Exp, scale=-1.0)
            P.tensor_scalar(tmp, tmp, 1.0, None, ALU.add)
            P.add("dve", lambda e, o=tmp: e.reciprocal(o, o), reads=[tmp], writes=[tmp])
            P.tensor_tensor(catC[j % 2], xc, tmp, ALU.mult)

        def norm_attn(j, accb):
            rden = stB[:, 0:8]
            for hh in range(2):
                av = accb[hh][:, 0:260].rearrange("p (h d) -> p h d", h=4)
                P.add("dve", lambda e, o=rden[:, hh * 4:(hh + 1) * 4], i=av[:, :, 64]: e.reciprocal(o, i),
                      reads=[av[:, :, 64]], writes=[rden[:, hh * 4:(hh + 1) * 4]])
                P.tensor_tensor(attn[:, hh * 256:(hh + 1) * 256].rearrange("p (h d) -> p h d", h=4), av[:, :, 0:64],
                                rden[:, hh * 4:(hh + 1) * 4].unsqueeze(2).to_broadcast([128, 4, 64]), ALU.mult)
            if "d_attn" in dbg:
                P.dma(dbg["d_attn"][j * 128:(j + 1) * 128, :], attn)

        def tail_attn(j):
            ssa, rsa = stB[:, 8:9], stB[:, 9:10]
            sumsq(catA, attn, ssa)
            rstd_from_ss(rsa, ssa, eps512)
            P.tensor_scalar(catA, attn, rsa, None, ALU.mult)
            pv = psb(7)
            for c in range(4):
                P.transpose(pv[:, c * 128:(c + 1) * 128], catA[:, c * 128:(c + 1) * 128], identb)
            for c in range(4):
                P.transpose(pv[:, (4 + c) * 128:(5 + c) * 128], catC[j % 2][:, c * 128:(c + 1) * 128], identb)
            if "d_cat" in dbg:
                P.dma(dbg["d_cat"][j * 128:(j + 1) * 128, 0:512], catA)
                P.dma(dbg["d_cat"][j * 128:(j + 1) * 128, 512:1024], catC[j % 2])
            P.copy(catT[:, :, j * 128:(j + 1) * 128], pv.rearrange("p (c t) -> p c t", c=8), eng="act" if j % 2 else "dve")

        for j in range(NT):
            if j == 0:
                tail_conv(0)
                tail_conv(1)
            accb = [psf(2), psf(3)]
            pairs = [(h, kt) for h in range(8) for kt in range(j + 1)]
            stslots = {}
            n_pairs = len(pairs)

            def emit_qk(n):
                h, kt = pairs[n]
                stv = psf((0, 1, 4, 5, 6)[n % 5])[:, 0:128]
                r0 = 0 if 'evenonly' in BP else (h % 2) * 64
                if 'qk' in BP:
                    P.matmul(stv, kT[r0:r0 + 64, h // 2, kt * 128:(kt + 1) * 128], qT[r0:r0 + 64, h // 2, j * 128:(j + 1) * 128],
                             start=True, stop=(kt != j or 'mask' not in BP))
                if kt == j and 'mask' in BP:
                    P.matmul(stv, identb, negm, start=False, stop=True)
                pt = PT[n % 6]
                if 'exp' in BP:
                    if 'expdve' in BP:
                        P.copy(pt, stv, eng='dve')
                    elif 'expcopy' in BP:
                        P.copy(pt, stv, eng='act')
                    elif 'nobias' in BP:
                        P.activation(pt, stv, AF.Exp, scale=0.125)
                    else:
                        P.activation(pt, stv, AF.Exp, bias=Bt[:, h, kt, j:j + 1], scale=0.125)

            def emit_pv(n):
                h, kt = pairs[n]
                pt = PT[n % 6]
                av = accb[h // 4][:, (h % 4) * 65:(h % 4) * 65 + 65]
                if 'pv' in BP:
                    P.matmul(av, pt, vS[:, kt, h, :], start=(kt == 0), stop=(kt == j))

            for n in range(n_pairs + LAG):
                if j > 0 and n == min(6, n_pairs + LAG - 1):
                    tail_attn(j - 1)
                    if j + 1 < NT:
                        tail_conv(j + 1)
                if n < n_pairs:
                    emit_qk(n)
                if n >= LAG:
                    emit_pv(n - LAG)
            norm_attn(j, accb)
        tail_attn(NT - 1)

        if stop_after == "B":
            P.dma(out[0:128, :], xin[0])
            P.emit()
            return nc
        P.dma(rG, rows[:, RG:RG + 1024])
        P.tensor_scalar(rG, rG, 32.0, None, ALU.mult)
        Wout, Wmq, Wmo = Woqm[:, 0], Woqm[:, 1], Woqm[:, 2]

        def c_front(blk):
            pb = blk % 2
            for tt in range(2):
                i = blk * 2 + tt
                P.dma(xr[tt], x[tok0 + i * 128: tok0 + (i + 1) * 128, :])
                for nh in range(2):
                    pv = psf(nb() % 6)
                    for c in range(8):
                        P.matmul(pv, catT[:, c, i * 128:(i + 1) * 128], Wout[:, c, nh * 512:(nh + 1) * 512],
                                 start=(c == 0), stop=(c == 7))
                    P.tensor_tensor(x1b[pb][:, tt, nh * 512:(nh + 1) * 512], pv, xr[tt][:, nh * 512:(nh + 1) * 512], ALU.add)
                if "d_x1" in dbg:
                    P.dma(dbg["d_x1"][i * 128:(i + 1) * 128, :], x1b[pb][:, tt, :])
                yield
                ss, rs = stC[:, 2 * tt:2 * tt + 1], stC[:, 2 * tt + 1:2 * tt + 2]
                sumsq(xn2[tt], x1b[pb][:, tt, :], ss)
                rstd_from_ss(rs, ss, eps1024)
                P.tensor_scalar(xn2[tt], x1b[pb][:, tt, :], rs, None, ALU.mult)
                transposeT(xn2[tt], h2Tb[pb], tt * 128, "act" if tt else "dve")
                yield
            for cc in range(8):
                pv = psf(nb() % 6)[:, 0:256]
                for c in range(8):
                    P.matmul(pv, Wmq[:, c, cc * 128:(cc + 1) * 128], h2Tb[pb][:, c, :], start=(c == 0), stop=(c == 7))
                P.copy(q2Tb[pb][:, cc, :], pv, eng="act" if cc % 2 else "dve")
                if cc % 2:
                    yield

        def c_back(blk):
            pb = blk % 2

            def scores(h):
                for mt in range(2):
                    pv = psf(nb() % 6)[:, 0:256]
                    for k2 in range(2):
                        P.matmul(pv, K2T[:, 2 * h + k2, mt * 128:(mt + 1) * 128], q2Tb[pb][:, 2 * h + k2, :],
                                 start=(k2 == 0), stop=(k2 == 1))
                    P.activation(P2T[(h % 2) * 2 + mt + 4 * pb], pv, AF.Exp, scale=1.0 / 16.0)

            def pvh(h):
                for tt in range(2):
                    pa = psf(nb() % 6)[:, 0:257]
                    for mt in range(2):
                        P.matmul(pa, P2T[(h % 2) * 2 + mt + 4 * pb][:, tt * 128:(tt + 1) * 128], V2[:, mt, h, :],
                                 start=(mt == 0), stop=(mt == 1))
                    rd = stC[:, 8 + tt:9 + tt]
                    P.add("dve", lambda e, o=rd, i_=pa[:, 256:257]: e.reciprocal(o, i_), reads=[pa[:, 256:257]], writes=[rd])
                    P.tensor_scalar(oo[:, tt, h * 256:(h + 1) * 256], pa[:, 0:256], rd, None, ALU.mult)

            scores(0)
            yield
            scores(1)
            yield
            pvh(0)
            yield
            scores(2)
            pvh(1)
            yield
            scores(3)
            pvh(2)
            yield
            pvh(3)
            yield
            for tt in range(2):
                transposeT(oo[:, tt, :], oT, tt * 128, "act" if tt else "dve")
                yield
            for tt in range(2):
                i = blk * 2 + tt
                gi = b * NT + i
                xo = x2t[tt]
                for nh in range(2):
                    pv = psf(nb() % 6)
                    for c in range(8):
                        P.matmul(pv, oT[:, c, tt * 128:(tt + 1) * 128], Wmo[:, c, nh * 512:(nh + 1) * 512],
                                 start=(c == 0), stop=(c == 7))
                    P.tensor_tensor(xo[:, nh * 512:(nh + 1) * 512], pv, x1b[pb][:, tt, nh * 512:(nh + 1) * 512], ALU.add)
                P.dma(x2s[tok0 + i * 128: tok0 + (i + 1) * 128, :], xo)
                yield
                ss, rs = stC[:, 4 + 2 * tt:5 + 2 * tt], stC[:, 5 + 2 * tt:6 + 2 * tt]
                hb = h3t[tt]
                sumsq(hb, xo, ss)
                rstd_from_ss(rs, ss, eps1024)
                P.stt(hb.rearrange("t (c p) -> t p c", c=8), xo.rearrange("t (p c) -> t p c", c=8), rs,
                      rG.rearrange("t (p c) -> t p c", c=8), ALU.mult, ALU.mult)
                P.dma(h3s[tok0 + i * 128: tok0 + (i + 1) * 128, :], hb)
                transposeT(hb, h3T, 0, "act" if tt else "dve")
                yield
                pl = psf(7)[:, 256 + 64 * tt:256 + 64 * tt + 36]
                for c in range(8):
                    P.matmul(pl, h3T[:, c, :], wr_bf[:, c, :], start=(c == 0), stop=(c == 7))
                P.copy(LG[:, gi, :], pl, eng="act")
                yield

        def interleave(*gens):
            gens = [g for g in gens if g is not None]
            while gens:
                for g in list(gens):
                    try:
                        next(g)
                    except StopIteration:
                        gens.remove(g)

        interleave(c_front(0))
        for blk in range(NT // 2):
            interleave(c_back(blk), c_front(blk + 1) if blk + 1 < NT // 2 else None)

    A.off = mark_persist
    gidx = A.new(I32, [96]); didx = A.new(I32, [96, 4])
    mark_p2 = A.off
    N_ = NTT
    lgb = A.new(F32, [64, 36]); gmax = A.new(F32, [64]); ohg = A.new(F32, [64, 4]); eg = A.new(F32, [64, 4])
    gsum = A.new(F32, [64]); gtop = A.new(F32, [64]); esel = A.new(F32, [64, 8]); tmp8 = A.new(F32, [64, 8])
    m1 = A.new(F32, [64]); m2 = A.new(F32, [64]); oh1 = A.new(F32, [64, 8]); oh2 = A.new(F32, [64, 8])
    dd = A.new(F32, [64]); w1 = A.new(F32, [64])
    a1f = A.new(F32, [64, 32]); a2f = A.new(F32, [64, 32]); cmb = A.new(BF16, [64, 32])
    rk = A.new(F32, [64, 32]); tot = A.new(F32, [64, 32]); bex = A.new(F32, [64, 32]); big = A.new(F32, [64, 32])

    def bc3(v, k):
        return v[:, 0:N_].unsqueeze(2).to_broadcast([128, N_, k])

    P.tensor_tensor(lgb[:, 0:N_, :], LG[:, 0:N_, :], rA[:, BR_:BR_ + 36].unsqueeze(1).to_broadcast([128, N_, 36]), ALU.add)
    if "d_lg" in dbg:
        for gi in range(N_):
            P.dma(dbg["d_lg"][gi * 128:(gi + 1) * 128, :], lgb[:, gi, :])
    P.reduce(gmax[:, 0:N_], lgb[:, 0:N_, 0:4], ALU.max)
    P.tensor_tensor(ohg[:, 0:N_, :], lgb[:, 0:N_, 0:4], bc3(gmax, 4), ALU.is_equal)
    P.tensor_tensor(eg[:, 0:N_, :], lgb[:, 0:N_, 0:4], bc3(gmax, 4), ALU.subtract)
    P.activation(eg[:, 0:N_, :], eg[:, 0:N_, :], AF.Exp)
    P.reduce(gsum[:, 0:N_], eg[:, 0:N_, :], ALU.add)
    recip(gtop[:, 0:N_], gsum[:, 0:N_])
    P.tensor_tensor(esel[:, 0:N_, :], lgb[:, 0:N_, 4:12], ohg[:, 0:N_, 0:1].to_broadcast([128, N_, 8]), ALU.mult)
    for g in range(1, 4):
        P.tensor_tensor(tmp8[:, 0:N_, :], lgb[:, 0:N_, 4 + 8 * g:12 + 8 * g], ohg[:, 0:N_, g:g + 1].to_broadcast([128, N_, 8]), ALU.mult)
        P.tensor_tensor(esel[:, 0:N_, :], esel[:, 0:N_, :], tmp8[:, 0:N_, :], ALU.add)
    P.reduce(m1[:, 0:N_], esel[:, 0:N_, :], ALU.max)
    P.tensor_tensor(oh1[:, 0:N_, :], esel[:, 0:N_, :], bc3(m1, 8), ALU.is_equal)
    P.stt(tmp8[:, 0:N_, :], oh1[:, 0:N_, :], -1e30, esel[:, 0:N_, :], ALU.mult, ALU.add)
    P.reduce(m2[:, 0:N_], tmp8[:, 0:N_, :], ALU.max)
    P.tensor_tensor(oh2[:, 0:N_, :], tmp8[:, 0:N_, :], bc3(m2, 8), ALU.is_equal)
    P.tensor_tensor(dd[:, 0:N_], m2[:, 0:N_], m1[:, 0:N_], ALU.subtract)
    P.activation(dd[:, 0:N_], dd[:, 0:N_], AF.Exp)
    P.tensor_scalar(dd[:, 0:N_], dd[:, 0:N_], 1.0, None, ALU.add)
    recip(w1[:, 0:N_], dd[:, 0:N_])
    P.tensor_tensor(G1[:, 0:N_], gtop[:, 0:N_], w1[:, 0:N_], ALU.mult)
    P.tensor_tensor(G2[:, 0:N_], gtop[:, 0:N_], G1[:, 0:N_], ALU.subtract)
    for a_, oh in ((a1f, oh1), (a2f, oh2)):
        P.tensor_tensor(a_[:, 0:N_, :].rearrange("p n (g e) -> p n g e", g=4),
                        ohg[:, 0:N_, :].unsqueeze(3).to_broadcast([128, N_, 4, 8]),
                        oh[:, 0:N_, :].unsqueeze(2).to_broadcast([128, N_, 4, 8]), ALU.mult)
    P.tensor_tensor(cmb[:, 0:N_, :], a1f[:, 0:N_, :], a2f[:, 0:N_, :], ALU.add)
    cmb2, rk2, tot2 = (v.rearrange("p n e -> p (n e)") for v in (cmb, rk, tot))
    for q0 in range(0, N_ * 32, 512):
        q1 = min(q0 + 512, N_ * 32)
        pr = psf(nb() % 6)[:, 0:q1 - q0]
        P.matmul(pr, cb[:, UST:UST + 128], cmb2[:, q0:q1])
        P.copy(rk2[:, q0:q1], pr)
        pt_ = psf(nb() % 6)[:, 0:q1 - q0]
        P.matmul(pt_, cb[:, ONEB:ONEB + 128], cmb2[:, q0:q1])
        P.copy(tot2[:, q0:q1], pt_, eng="act")
    P.memset(bex[:, 0, :], 0.0)
    for t_ in range(1, N_):
        P.tensor_tensor(bex[:, t_, :], bex[:, t_ - 1, :], tot[:, t_ - 1, :], ALU.add)
    P.tensor_tensor(base, bex[:, N_ - 1, :], tot[:, N_ - 1, :], ALU.add)
    P.tensor_tensor(rk[:, 0:N_, :], rk[:, 0:N_, :], bex[:, 0:N_, :], ALU.add)
    for (af, Px) in ((a1f, P1), (a2f, P2)):
        P.tensor_tensor(big[:, 0:N_, :], af[:, 0:N_, :], rk[:, 0:N_, :], ALU.mult)
        P.reduce(Px[:, 0:N_], big[:, 0:N_, :], ALU.add)
    if "d_rt" in dbg:
        P.dma(dbg["d_rt"][:, 0:NTT], G1[:, 0:NTT]); P.dma(dbg["d_rt"][:, 64:64 + NTT], G2[:, 0:NTT])
        P.dma(dbg["d_rt"][:, 128:128 + NTT], P1[:, 0:NTT]); P.dma(dbg["d_rt"][:, 192:192 + NTT], P2[:, 0:NTT])
        P.dma(dbg["d_rt"][:, 256:288], base)
    if stop_after == "phase1":
        P.dma(out[0:128, :], big[:, 0:32, :].rearrange("p a b -> p (a b)"))
        P.emit()
        return nc


    pcf = A.new(F32, [32]); pci = A.new(I32, [32]); offs = A.new(F32, [32]); ends = A.new(F32, [32])
    te = A.new(F32, [96]); tf = A.new(F32, [96])
    zero_i = A.new(I32, [NSLOT // 128]); posf = A.new(F32, [64])
    P.tensor_scalar(pcf, base, TS / 2 - 0.5, 1.0 / TS, ALU.add, ALU.mult)
    P.copy(pci, pcf)
    P.copy(pcf, pci)
    P.tensor_scalar(pcf, pcf, float(TS), None, ALU.mult)
    P.memset(offs[:, 0:1], 0.0)
    for e in range(1, 32):
        P.tensor_tensor(offs[:, e:e + 1], offs[:, e - 1:e], pcf[:, e - 1:e], ALU.add)
    P.tensor_tensor(ends, offs, pcf, ALU.add)
    P.memset(te, 0.0)
    for e in range(32):
        P.stt(te, cf[:, THR:THR + 96], ends[:, e:e + 1], te, ALU.is_ge, ALU.add)
    if "d_te" in dbg:
        P.dma(dbg["d_te"], te)
    P.tensor_scalar(tf, te, 128.0, cf[:, PCOL:PCOL + 1], ALU.mult, ALU.add)
    P.copy(gidx, tf)
    for c in range(4):
        P.tensor_scalar(tf, te, 512.0, cf[:, CBASE + c:CBASE + c + 1], ALU.mult, ALU.add)
        P.copy(didx[:, :, c], tf)
    for (Ax, Px, POSx) in ((a1f, P1, POS1), (a2f, P2, POS2)):
        P.tensor_tensor(big[:, 0:NTT, :], Ax[:, 0:NTT, :], offs.unsqueeze(1).to_broadcast([128, NTT, 32]), ALU.mult)
        P.reduce(posf[:, 0:NTT], big[:, 0:NTT, :], ALU.add)
        P.tensor_tensor(posf[:, 0:NTT], posf[:, 0:NTT], Px[:, 0:NTT], ALU.add)
        P.copy(POSx[:, 0:NTT], posf[:, 0:NTT])
    P.memset(zero_i, 0)
    P.dma(s2t.rearrange("(p n) o -> p (n o)", p=128), zero_i)
    FAKE = "D:s2t_scatter"
    nsc = 0
    for gi in range(NTT):
        for POSx in (POS1, POS2):
            P.add("pool", lambda e, po=POSx[:, gi:gi + 1], ti=ci[:, gi:gi + 1]: e.indirect_dma_start(
                out=s2t, out_offset=bass.IndirectOffsetOnAxis(ap=po, axis=0), in_=ti, in_offset=None),
                reads=[POSx[:, gi:gi + 1], ci[:, gi:gi + 1], s2t], writes=[(FAKE, 0, 1, nsc, nsc + 1)], dma=True)
            nsc += 1

    A.off = mark_p2
    wg = [A.new(BF16, [8, 512]) for _ in range(3)]
    wu = [A.new(BF16, [8, 512]) for _ in range(3)]
    wd = [A.new(BF16, [4, 1024]) for _ in range(3)]
    sidx = [A.new(I32, [GT]) for _ in range(3)]
    hs = [A.new(BF16, [GT, 1024]) for _ in range(2)]
    hTs = [A.new(BF16, [8, TS]) for _ in range(2)]
    sil = [A.new(F32, [TS]) for _ in range(2)]
    hid = [A.new(BF16, [4, TS]) for _ in range(2)]
    yt = [A.new(BF16, [1024]) for _ in range(4)]
    s2t_v = s2t.rearrange("(j g p) o -> j p (g o)", g=GT, p=128)
    def moe_loads(j):
        k3 = j % 3
        for (wdst, wsrc) in ((wg[k3], w_gate), (wu[k3], w_up)):
            P.add("pool", lambda e, o=wdst.rearrange("p a b -> p (a b)"), s=wsrc, ix=gidx[:, j:j + 1]: e.indirect_dma_start(
                out=o, out_offset=None, in_=s, in_offset=bass.IndirectOffsetOnAxis(ap=ix, axis=0),
                bounds_check=P.reg(e, 32 * 128 - 1), oob_is_err=False),
                reads=[gidx[:, j:j + 1], wsrc], writes=[wdst], dma=True)
        for c in range(4):
            P.add("pool", lambda e, o=wd[k3][:, c, :], ix=didx[:, j, c:c + 1]: e.indirect_dma_start(
                out=o, out_offset=None, in_=w_down, in_offset=bass.IndirectOffsetOnAxis(ap=ix, axis=0),
                bounds_check=P.reg(e, 32 * 512 - 1), oob_is_err=False),
                reads=[didx[:, j, c:c + 1], w_down], writes=[wd[k3][:, c, :]], dma=True)
        P.add("sp", lambda e, o=sidx[k3], s=s2t_v[j]: e.dma_start(out=o, in_=s, allow_slow_non_contiguous=True),
              reads=[s2t_v[j], (FAKE, 0, 1, 0, 1 << 20)], writes=[sidx[k3]], dma=True)
        hsj = hs[j % 2]
        for g in range(GT):
            P.add("pool", lambda e, o=hsj[:, g, :], ix=sidx[k3][:, g:g + 1]: e.indirect_dma_start(
                out=o, out_offset=None, in_=h3s, in_offset=bass.IndirectOffsetOnAxis(ap=ix, axis=0)),
                reads=[sidx[k3][:, g:g + 1], h3s], writes=[hsj[:, g, :]], dma=True)

    def moe_T(j):
        for g in range(GT):
            transposeT(hs[j % 2][:, g, :], hTs[j % 2], g * 128, "act" if g % 2 else "dve")

    def moe_GU(j):
        k3, hTj, hidj = j % 3, hTs[j % 2], hid[j % 2]
        for fc in range(4):
            pg = psf(nb() % 6)[:, 0:TS]
            for c in range(8):
                P.matmul(pg, wg[k3][:, c, fc * 128:(fc + 1) * 128], hTj[:, c, :], start=(c == 0), stop=(c == 7))
            pu = psf(nb() % 6)[:, 0:TS]
            for c in range(8):
                P.matmul(pu, wu[k3][:, c, fc * 128:(fc + 1) * 128], hTj[:, c, :], start=(c == 0), stop=(c == 7))
            sl = sil[fc % 2]
            P.activation(sl, pg, AF.Silu)
            P.tensor_tensor(hidj[:, fc, :], sl, pu, ALU.mult)

    ny = [0]

    def moe_D(j):
        k3, hidj = j % 3, hid[j % 2]
        for g in range(GT):
            y_ = yt[ny[0] % 4]
            ny[0] += 1
            for nh in range(2):
                pv = psf(nb() % 6)
                for fc in range(4):
                    P.matmul(pv, hidj[:, fc, g * 128:(g + 1) * 128], wd[k3][:, fc, nh * 512:(nh + 1) * 512],
                             start=(fc == 0), stop=(fc == 3))
                P.copy(y_[:, nh * 512:(nh + 1) * 512], pv, eng="act" if nh else "dve")
            P.dma(Ysc[j * TS + g * 128: j * TS + (g + 1) * 128, :], y_)

    if n_moe_tiles:
        moe_loads(0)
        moe_T(0)
    for j in range(n_moe_tiles):
        if j + 1 < n_moe_tiles:
            moe_loads(j + 1)
        moe_GU(j)
        if j + 1 < n_moe_tiles:
            moe_T(j + 1)
        moe_D(j)

    A.off = mark_p2
    fg = A.new(F32, [1024])
    NB4 = 4
    x2b = [A.new(F32, [1024]) for _ in range(NB4)]
    y1b = [A.new(BF16, [1024]) for _ in range(NB4)]
    y2b = [A.new(BF16, [1024]) for _ in range(NB4)]
    otb = [A.new(F32, [1024]) for _ in range(NB4)]
    st4 = A.new(F32, [2 * NB4])
    P.dma(fg, rows[:, RF:RF + 1024])
    P.tensor_scalar(fg, fg, 32.0, None, ALU.mult)
    for gi in range(NTT):
        k2 = gi % NB4
        P.dma(x2b[k2], x2s[gi * 128:(gi + 1) * 128, :])
        for (yb, POSx) in ((y1b[k2], POS1), (y2b[k2], POS2)):
            P.add("pool", lambda e, o=yb, ix=POSx[:, gi:gi + 1]: e.indirect_dma_start(
                out=o, out_offset=None, in_=Ysc, in_offset=bass.IndirectOffsetOnAxis(ap=ix, axis=0)),
                reads=[POSx[:, gi:gi + 1], Ysc], writes=[yb], dma=True)
        acc = x2b[k2]
        if skip_moe:
            pass
        else:
            P.stt(acc, y1b[k2], G1[:, gi:gi + 1], acc, ALU.mult, ALU.add)
            P.stt(acc, y2b[k2], G2[:, gi:gi + 1], acc, ALU.mult, ALU.add)
        ss, rs = st4[:, 2 * k2:2 * k2 + 1], st4[:, 2 * k2 + 1:2 * k2 + 2]
        sumsq(otb[k2], acc, ss)
        rstd_from_ss(rs, ss, eps1024)
        P.stt(otb[k2], acc, rs, fg, ALU.mult, ALU.mult)
        P.dma(out[gi * 128:(gi + 1) * 128, :], otb[k2])
    cnt = P.emit()
    st.close()
    return nc


def _consts():
    p = np.arange(128)
    cfm = np.zeros((128, NCF), np.float32)
    cfm[:, THR:THR + 96] = float(TS) * np.arange(96)[None, :]
    cfm[:, PCOL] = p
    cfm[:, CBASE:CBASE + 4] = np.arange(4)[None, :] * 128 + p[:, None]
    cfm[:, IOTA:IOTA + 32] = np.arange(32)[None, :]
    cfm[:, CEPS:CEPS + 4] = np.array([1024e-6, 512e-6, 1.0, 1e-6], np.float32)[None, :]
    cbm = np.zeros((128, NCB), np.float32)
    cbm[:, IDB:IDB + 128] = np.eye(128)
    cbm[:, NEGM:NEGM + 128] = np.where(p[:, None] > p[None, :], -30000.0, 0.0)
    cbm[:, UST:UST + 128] = (p[:, None] < p[None, :])
    cbm[:, ONEB:ONEB + 128] = 1.0
    cbm[:, LTRIB:LTRIB + 128] = (p[:, None] <= p[None, :])
    cim = (np.arange(64)[None, :] * 128 + p[:, None]).astype(np.int32)
    return cfm, cbm.astype(ml_dtypes.bfloat16), cim


def _host_inputs(core, nseq, x, mem, norm_mix_g, w_in, b_forget, b_glu, w_dw, b_dw, conv_ln_g, conv_ln_b,
                 attn_out_g, w_out, norm_mem_g, mem_norm_g, w_mq, w_mkv, w_mo, norm_ffn_g,
                 w_route_group, b_route_group, w_route_expert, b_route_expert, w_gate, w_up, w_down, final_g):
    f = lambda a: np.ascontiguousarray(np.asarray(a, dtype=np.float32))
    cfm, cbm, cim = _consts()
    vecs = np.zeros((128, NV), np.float32)
    vecs[:, GM:GM + 8] = f(norm_mix_g)[0].reshape(8, 128).T
    vecs[:, GQ:GQ + 8] = f(norm_mem_g)[0].reshape(8, 128).T
    vecs[:, GK:GK + 8] = f(mem_norm_g)[0].reshape(8, 128).T
    vecs[:, GA:GA + 4] = f(attn_out_g)[0].reshape(4, 128).T
    vecs[:, BG:BG + 8] = f(b_glu)[0].reshape(8, 128).T
    vecs[:, BD:BD + 4] = f(b_dw)[0].reshape(4, 128).T
    vecs[:, WDW:WDW + 124] = f(w_dw)[0].reshape(31, 4, 128).transpose(2, 1, 0).reshape(128, 124)
    rows = np.zeros((1, NR), np.float32)
    rows[0, RG:RG + 1024] = f(norm_ffn_g)[0]
    rows[0, RF:RF + 1024] = f(final_g)
    rows[0, RLG:RLG + 512] = f(conv_ln_g)[0]
    rows[0, RLB:RLB + 512] = f(conv_ln_b)[0]
    rows[0, RBF:RBF + 128] = np.tile(f(b_forget)[0], 16)
    rows[0, RBR:RBR + 4] = f(b_route_group)[0]
    rows[0, RBR + 4:RBR + 36] = f(b_route_expert)[0].reshape(32)
    rows[0, RBDW:RBDW + 512] = f(b_dw)[0]
    rows = np.ascontiguousarray(np.broadcast_to(rows, (128, NR)))
    wr = np.concatenate([f(w_route_group)[0], f(w_route_expert)[0].transpose(1, 0, 2).reshape(1024, 32)], axis=1)
    return {
        "x": f(x[core * nseq:(core + 1) * nseq]).reshape(nseq * S, D),
        "mem": f(mem[core * nseq:(core + 1) * nseq]).reshape(nseq * 256, D),
        "w_in": f(w_in)[0], "w_out": f(w_out)[0], "w_mq": f(w_mq)[0], "w_mkv": f(w_mkv)[0], "w_mo": f(w_mo)[0],
        "w_route": np.ascontiguousarray(wr.reshape(128, 8 * 36)),
        "w_gate": f(w_gate)[0].reshape(32 * 128, 4096), "w_up": f(w_up)[0].reshape(32 * 128, 4096),
        "w_down": f(w_down)[0].reshape(32 * 512, 1024),
        "vecs": vecs, "rows": rows, "constf": cfm, "constb": cbm, "consti": cim,
    }


_NC_CACHE = {}


def kernel(**inputs):
    n_cores = 8
    nseq = 4
    if "full" not in _NC_CACHE:
        _NC_CACHE["full"] = build(nseq=nseq)
    nc = _NC_CACHE["full"]
    in_maps = [_host_inputs(c, nseq, **inputs) for c in range(n_cores)]
    res = run_bass_kernel_spmd(nc, in_maps, core_ids=list(range(n_cores)))
    outs = [np.asarray(r["out"]).reshape(nseq, S, D) for r in res.results]
    return np.concatenate(outs, axis=0).astype(np.float32)
```

```python
import numpy as np
import ml_dtypes
from concourse.bass_utils import run_bass_kernel_spmd
from contextlib import ExitStack
import concourse.bass as bass
import concourse.mybir as mybir

F32 = mybir.dt.float32
BF16 = mybir.dt.bfloat16
I32 = mybir.dt.int32
ALU = mybir.AluOpType
AF = mybir.ActivationFunctionType
AX = mybir.AxisListType
_DSZ = {F32: 4, BF16: 2, I32: 4, mybir.dt.uint32: 4, mybir.dt.float16: 2, mybir.dt.uint8: 1,
        mybir.dt.int8: 1, mybir.dt.uint16: 2, mybir.dt.int16: 2}


def region(ap):
    dsz = _DSZ[ap.dtype]
    dims = ap.ap
    off = ap.offset
    sp = str(ap.space)
    if sp in ("SB", "PSUM"):
        shp = ap.tensor.shape
        rowlen = 1
        for s in list(shp)[1:]:
            rowlen *= s
        p0 = off // rowlen
        c0 = off % rowlen
        pstep, pcnt = dims[0]
        p1 = p0 + ((pcnt - 1) * pstep) // rowlen + 1 if pstep else p0 + 1
        ext = 0
        for st, cn in dims[1:]:
            ext += (cn - 1) * abs(st)
        if sp == "PSUM":
            return (sp + ":" + ap.tensor.name, 0, 128, 0, 1 << 20)
        return (sp + ":" + ap.tensor.name, p0, p1, c0 * dsz, (c0 + ext + 1) * dsz)
    else:
        ext = 0
        for st, cn in dims:
            ext += (cn - 1) * abs(st)
        return ("D:" + ap.tensor.name, 0, 1, off * dsz, (off + ext + 1) * dsz)


class Op:
    __slots__ = ("eng", "fn", "reads", "writes", "dma", "deps", "sig", "dsem", "dval",
                 "waits", "idx", "prewait")


class Prog:
    ENGS = ("pe", "act", "dve", "pool", "sp")
    EPOCH = 30000

    def __init__(self, nc, ndma_sems=56):
        self.nc = nc
        self.ops = []
        self.recs = {}
        self.ndma = ndma_sems
        self.extra_regions = {}

    def reg(self, e, val):
        d = self.__dict__.setdefault("_regs", {})
        if val not in d:
            d[val] = e.to_reg(val)
        return d[val]

    def add(self, eng, fn, reads=(), writes=(), dma=False):
        op = Op()
        op.eng = eng; op.fn = fn; op.dma = dma
        op.reads = [r if isinstance(r, tuple) else region(r) for r in reads]
        op.writes = [r if isinstance(r, tuple) else region(r) for r in writes]
        op.idx = len(self.ops)
        op.sig = None; op.dsem = None; op.dval = None; op.waits = []; op.prewait = None
        deps = set()
        for (sp, p0, p1, lo, hi) in op.reads:
            for r in self.recs.get(sp, ()):
                if r[5] and r[0] < p1 and p0 < r[1] and r[2] < hi and lo < r[3]:
                    deps.add(r[4])
        for (sp, p0, p1, lo, hi) in op.writes:
            lst = self.recs.get(sp)
            if lst is None:
                lst = self.recs[sp] = []
            keep = []
            for r in lst:
                if r[0] < p1 and p0 < r[1] and r[2] < hi and lo < r[3]:
                    deps.add(r[4])
                    if r[0] >= p0 and r[1] <= p1 and r[2] >= lo and r[3] <= hi:
                        continue
                keep.append(r)
            keep.append([p0, p1, lo, hi, op.idx, True])
            self.recs[sp] = keep
        for (sp, p0, p1, lo, hi) in op.reads:
            lst = self.recs.get(sp)
            if lst is None:
                lst = self.recs[sp] = []
            if not dma:
                for r in lst:
                    if (not r[5]) and r[0] == p0 and r[1] == p1 and r[2] == lo and r[3] == hi \
                            and (not self.ops[r[4]].dma) and self.ops[r[4]].eng == eng:
                        r[4] = op.idx
                        break
                else:
                    lst.append([p0, p1, lo, hi, op.idx, False])
            else:
                lst.append([p0, p1, lo, hi, op.idx, False])
        deps.discard(op.idx)
        op.deps = deps
        self.ops.append(op)
        return op

    def matmul(self, out, lhsT, rhs, start=True, stop=True, **kw):
        return self.add("pe", lambda e: e.matmul(out, lhsT, rhs, start=start, stop=stop, **kw),
                        reads=[lhsT, rhs], writes=[out])

    def transpose(self, out, in_, ident):
        return self.add("pe", lambda e: e.transpose(out, in_, ident), reads=[in_, ident], writes=[out])

    def activation(self, out, in_, func, bias=None, scale=None, accum_out=None, eng="act"):
        reads = [in_]
        kw = {}
        if bias is not None:
            kw["bias"] = bias
            if not isinstance(bias, (int, float)):
                reads.append(bias)
        if scale is not None:
            kw["scale"] = scale
            if not isinstance(scale, (int, float)):
                reads.append(scale)
        writes = [out]
        if accum_out is not None:
            kw["accum_out"] = accum_out
            writes.append(accum_out)
        return self.add(eng, lambda e: e.activation(out, in_, func, **kw), reads=reads, writes=writes)

    def tensor_scalar(self, out, in0, s1, s2, op0, op1=None, eng="dve", accum_out=None):
        reads = [in0]
        for s in (s1, s2):
            if s is not None and not isinstance(s, (int, float)):
                reads.append(s)
        kw = {}
        if op1 is not None:
            kw["op1"] = op1
        writes = [out]
        if accum_out is not None:
            kw["accum_out"] = accum_out
            writes.append(accum_out)
        return self.add(eng, lambda e: e.tensor_scalar(out, in0, s1, s2, op0, **kw), reads=reads, writes=writes)

    def tensor_tensor(self, out, in0, in1, op, eng="dve"):
        return self.add(eng, lambda e: e.tensor_tensor(out, in0, in1, op), reads=[in0, in1], writes=[out])

    def stt(self, out, in0, scalar, in1, op0, op1, eng="dve", accum_out=None):
        reads = [in0, in1]
        if not isinstance(scalar, (int, float)):
            reads.append(scalar)
        kw = {}
        writes = [out]
        if accum_out is not None:
            kw["accum_out"] = accum_out
            writes.append(accum_out)
        return self.add(eng, lambda e: e.scalar_tensor_tensor(out, in0, scalar, in1, op0, op1, **kw),
                        reads=reads, writes=writes)

    def copy(self, out, in_, eng="dve"):
        if eng == "act":
            return self.add(eng, lambda e: e.copy(out, in_), reads=[in_], writes=[out])
        return self.add(eng, lambda e: e.tensor_copy(out, in_), reads=[in_], writes=[out])

    def memset(self, out, val, eng="dve"):
        return self.add(eng, lambda e: e.memset(out, val), reads=[], writes=[out])

    def reduce(self, out, in_, op, axis=None, eng="dve"):
        ax = axis if axis is not None else AX.X
        return self.add(eng, lambda e: e.tensor_reduce(out, in_, ax, op), reads=[in_], writes=[out])

    def dma(self, out, in_, q="sp", **kw):
        return self.add(q, lambda e: e.dma_start(out=out, in_=in_, **kw), reads=[in_], writes=[out], dma=True)

    def finalize(self):
        ops = self.ops
        need = [False] * len(ops)
        for op in ops:
            for d in op.deps:
                p = ops[d]
                if p.dma:
                    continue
                if p.eng == op.eng and not op.dma:
                    if p.eng == "pe":
                        continue
                    continue_flag = True
                    for (sp, p0, p1, lo, hi) in op.reads:
                        for (sp2, q0, q1, lo2, hi2) in p.writes:
                            if sp == sp2 and q0 < p1 and p0 < q1 and lo2 < hi and lo < hi2:
                                continue_flag = False
                    if continue_flag:
                        continue
                need[d] = True
        cnt = {e: 0 for e in self.ENGS}
        for op in ops:
            if (not op.dma) and need[op.idx]:
                cnt[op.eng] += 1
                op.sig = cnt[op.eng]
        dcount = [0] * self.ndma
        k = 0
        for op in ops:
            if op.dma:
                s = k % self.ndma
                k += 1
                dcount[s] += 1
                op.dsem = s
                op.dval = 16 * dcount[s]
                op.prewait = (("dma", s), 16 * (dcount[s] - 1)) if dcount[s] > 1 else None
        self.dfinal = [16 * c for c in dcount]
        seen = {e: {} for e in self.ENGS}
        pos = {e: 0 for e in self.ENGS}
        opos = {}
        for op in ops:
            pos[op.eng] += 1
            opos[op.idx] = pos[op.eng]
        nw = 0
        for op in ops:
            sn = seen[op.eng]
            w = {}
            if op.prewait is not None:
                key, val = op.prewait
                if sn.get(key, 0) < val:
                    w[key] = val
            for d in op.deps:
                p = ops[d]
                if p.dma:
                    key, val = ("dma", p.dsem), p.dval
                else:
                    if p.sig is None:
                        continue
                    if p.eng == op.eng and not op.dma:
                        if p.eng == "pe":
                            continue
                        if opos[op.idx] - opos[p.idx] > 3:
                            continue
                    ep = (p.sig - 1) // self.EPOCH
                    key, val = ("eng", p.eng), ep * 100000 + (p.sig - 1) % self.EPOCH + 1
                if sn.get(key, 0) < val and w.get(key, 0) < val:
                    w[key] = val
            for key, val in w.items():
                sn[key] = val
            op.waits = list(w.items())
            nw += len(op.waits)
        self.n_waits = nw
        self.n_epochs = {e: (cnt[e] - 1) // self.EPOCH + 1 if cnt[e] else 1 for e in self.ENGS}
        return cnt

    def emit(self):
        nc = self.nc
        cnt = self.finalize()
        ops = self.ops
        with ExitStack() as st:
            esem = {}
            for e in self.ENGS:
                for ep in range(self.n_epochs[e]):
                    esem[(e, ep)] = st.enter_context(nc.semaphore("s_%s%d" % (e, ep)))
            dsem = [st.enter_context(nc.semaphore("d%d" % i)) for i in range(self.ndma)]
            block = st.enter_context(nc.Block())
            by_eng = {e: [op for op in ops if op.eng == e] for e in self.ENGS}
            EP = self.EPOCH

            def run(eng_obj, lst, is_sp):
                for op in lst:
                    for key, val in op.waits:
                        if key[0] == "dma":
                            eng_obj.wait_ge(dsem[key[1]], val)
                        else:
                            ep, v = divmod(val, 100000)
                            eng_obj.wait_ge(esem[(key[1], ep)], v)
                    inst = op.fn(eng_obj)
                    if op.dma:
                        inst.then_inc(dsem[op.dsem], 16)
                    elif op.sig is not None:
                        ep = (op.sig - 1) // EP
                        inst.then_inc(esem[(op.eng, ep)], 1)
                if is_sp:
                    for i, v in enumerate(self.dfinal):
                        if v:
                            eng_obj.wait_ge(dsem[i], v)

            @block.sync
            def _(e):
                run(e, by_eng["sp"], True)

            @block.tensor
            def _(e):
                run(e, by_eng["pe"], False)

            @block.scalar
            def _(e):
                run(e, by_eng["act"], False)

            @block.vector
            def _(e):
                run(e, by_eng["dve"], False)

            @block.gpsimd
            def _(e):
                run(e, by_eng["pool"], False)
        return cnt

S = 2048
D = 1024
NT = S // 128
TS = 512
NTILES = 16384 // TS + 32
GT = TS // 128
NSLOT = NTILES * TS
GM, GQ, GK, GA, BG, BD, WDW = 0, 8, 16, 24, 28, 36, 40
NV = 40 + 4 * 31
RG, RF, RLG, RLB, RBF, RBR, RBDW = 0, 1024, 2048, 2560, 3072, 3200, 3236
NR = 3748
THR, PCOL, CBASE, IOTA, CEPS, NCF = 0, 96, 97, 101, 133, 141
IDB, NEGM, UST, ONEB, LTRIB, NCB = 0, 128, 256, 384, 512, 640


def _prod(s):
    r = 1
    for a in s:
        r *= a
    return r


class Arena:
    def __init__(self, nc, st, name, nbytes):
        self.t = st.enter_context(nc.sbuf_tensor(name, [128, nbytes // 4], F32))
        self.cap = nbytes
        self.off = 0

    def alloc(self, nbytes):
        off = (self.off + 63) // 64 * 64
        self.off = off + nbytes
        assert self.off <= self.cap, ("arena overflow", self.off, self.cap)
        return off

    def view(self, off, dtype, shape):
        n = _prod(shape)
        dsz = _DSZ[dtype]
        nb = (n * dsz + 3) // 4
        v = self.t[:, off // 4: off // 4 + nb]
        if dtype != F32:
            v = v.bitcast(dtype)
        v = v[:, 0:n]
        if len(shape) == 2:
            v = v.rearrange("p (a b) -> p a b", a=shape[0], b=shape[1])
        elif len(shape) == 3:
            v = v.rearrange("p (a b c) -> p a b c", a=shape[0], b=shape[1], c=shape[2])
        return v

    def new(self, dtype, shape):
        off = self.alloc(_prod(shape) * _DSZ[dtype])
        return self.view(off, dtype, shape)


import os as _os
BP = set(_os.environ.get('BPARTS', 'conv,convT,qk,mask,exp,pv,norm,tail,catT').split(','))


def build(nseq=4, debug=(), stop_after=None, n_moe_tiles=NTILES, skip_moe=False):
    nc = bass.Bass("TRN2", target_bir_lowering=False)
    NTOK = nseq * S
    NTT = NTOK // 128

    def din(name, shape, dt=F32):
        return nc.dram_tensor(name, shape, dt, kind="ExternalInput").ap()

    def scr(name, shape, dt):
        if name in debug:
            return nc.dram_tensor(name, shape, dt, kind="ExternalOutput").ap()
        return nc.dram_tensor(name, shape, dt).ap()

    x = din("x", [NTOK, D])
    mem = din("mem", [nseq * 256, D])
    w_in = din("w_in", [D, 2568])
    w_out = din("w_out", [D, D])
    w_mq = din("w_mq", [D, D])
    w_mkv = din("w_mkv", [D, 2 * D])
    w_mo = din("w_mo", [D, D])
    w_route = din("w_route", [128, 8 * 36])
    w_gate = din("w_gate", [32 * 128, 4096])
    w_up = din("w_up", [32 * 128, 4096])
    w_down = din("w_down", [32 * 512, 1024])
    vecs = din("vecs", [128, NV])
    rows = din("rows", [128, NR])
    constf = din("constf", [128, NCF])
    constb = din("constb", [128, NCB], BF16)
    consti = din("consti", [128, 64], I32)
    out = nc.dram_tensor("out", [NTOK, D], F32, kind="ExternalOutput").ap()

    w_in_bf = scr("w_in_bf", [D, 2568], BF16)
    w_out_bf = scr("w_out_bf", [D, D], BF16)
    w_mq_bf = scr("w_mq_bf", [D, D], BF16)
    w_mkv_bf = scr("w_mkv_bf", [D, 2 * D], BF16)
    w_mo_bf = scr("w_mo_bf", [D, D], BF16)
    x2s = scr("x2s", [NTOK, D], F32)
    h3s = scr("h3s", [NTOK, D], BF16)
    Ysc = scr("Ysc", [NSLOT, D], BF16)
    s2t = scr("s2t", [NSLOT, 1], I32)
    dbg = {}
    for name, shape, dt in (("d_cat", [S, D], BF16), ("d_x1", [S, D], F32), ("d_lg", [NTOK, 36], F32),
                            ("d_q", [128, 4 * 2048], BF16), ("d_attn", [S, 512], F32), ("d_z", [128, 4 * 2080], BF16),
                            ("d_B", [128, 2048], F32), ("d_rt", [128, 64 * 8], F32), ("d_te", [128, 96], F32),
                            ("d_conv", [S, 512], F32)):
        if name in debug:
            dbg[name] = nc.dram_tensor(name, shape, dt, kind="ExternalOutput").ap()

    P = Prog(nc)
    st = ExitStack()
    A = Arena(nc, st, "arena", 210944)
    PS = [st.enter_context(nc.psum_tensor("ps%d" % i, [128, 512], F32)) for i in range(8)]

    def psf(i):
        return PS[i][:, :]

    def psb(i):
        return PS[i][:, :].bitcast(BF16)

    cf = A.new(F32, [NCF])
    cb = A.new(BF16, [NCB])
    ci = A.new(I32, [64])
    vv = A.new(F32, [NV])
    rA = A.new(F32, [1700])
    LG_, LB_, BF_, BR_, BDW_ = 0, 512, 1024, 1152, 1188
    LG = A.new(F32, [64, 36])
    G1 = A.new(F32, [64]); G2 = A.new(F32, [64]); P1 = A.new(F32, [64]); P2 = A.new(F32, [64])
    POS1 = A.new(I32, [64]); POS2 = A.new(I32, [64])
    base = A.new(F32, [32])
    wr_bf = A.new(BF16, [8, 36])
    v_ = None
    identb = cb[:, IDB:IDB + 128]
    negm = cb[:, NEGM:NEGM + 128]
    eps1024 = cf[:, CEPS:CEPS + 1]
    eps512 = cf[:, CEPS + 1:CEPS + 2]
    one_c = cf[:, CEPS + 2:CEPS + 3]
    epsln = cf[:, CEPS + 3:CEPS + 4]
    P.dma(cf, constf); P.dma(cb, constb); P.dma(ci, consti); P.dma(vv, vecs)
    P.dma(rA, rows[:, RLG:RLG + 1700])
    P.memset(base, 0.0)
    P.tensor_scalar(vv[:, GM:GM + 24], vv[:, GM:GM + 24], 32.0, None, ALU.mult)
    P.tensor_scalar(vv[:, GA:GA + 4], vv[:, GA:GA + 4], float(np.sqrt(512.0)), None, ALU.mult)
    mark_persist = A.off

    Wreg_off = A.alloc(49152)
    Win = A.view(Wreg_off, BF16, [8, 2568])
    Woqm = A.view(Wreg_off, BF16, [3, 8, 1024])
    xT = A.new(BF16, [8, 2048])
    K2T = A.new(BF16, [8, 256])
    V2 = A.new(BF16, [2, 4, 257])
    Bt = A.new(F32, [8, 16, 16])
    nlf = A.new(F32, [16, 8]); incl = A.new(F32, [16, 8]); Cc = A.new(F32, [16, 8]); Cend = A.new(F32, [16, 8])
    U_off = A.alloc(66048 + 64)
    qT = A.view(U_off, BF16, [4, 2048])
    kT = A.view(U_off + 16384, BF16, [4, 2048])
    Wkv = A.view(U_off, BF16, [8, 2048])
    vS = A.view(U_off + 32768, BF16, [16, 8, 65])
    zT = A.view(U_off + 32768 + 16640, BF16, [4, 2080])
    memT = A.view(U_off + 32768 + 16640, BF16, [8, 256])
    T_off = A.alloc(23040)
    xin = [A.view(T_off + i * 4096, F32, [1024]) for i in range(2)]
    xn = [A.view(T_off + 8192 + i * 2048, BF16, [1024]) for i in range(2)]
    sig = [A.view(T_off + 12288 + i * 2048, F32, [512]) for i in range(2)]
    stA = A.view(T_off + 16384, F32, [16])
    PT = [A.view(T_off + i * 256, BF16, [128]) for i in range(6)]
    attn = A.view(T_off + 1536, F32, [512])
    catA = A.view(T_off + 3584, BF16, [512])
    catC = [A.view(T_off + 4608 + i * 1024, BF16, [512]) for i in range(2)]
    cz = [A.view(T_off + 6656 + i * 2048, F32, [512]) for i in range(2)]
    stB = A.view(T_off + 10752, F32, [64])
    Dg = A.view(T_off + 11008, BF16, [31, 128])
    zoff = U_off + 32768 + 16640
    cvo = [A.view(T_off + 18944, BF16, [16, 128])] + [A.view(zoff + i * 4160, BF16, [16, 128]) for i in range(3)]
    o_ = U_off
    x1 = A.view(o_, F32, [2, 1024]); o_ += 8192
    xr = [A.view(o_ + i * 4096, F32, [1024]) for i in range(2)]; o_ += 8192
    xn2 = [A.view(o_ + i * 2048, BF16, [1024]) for i in range(2)]; o_ += 4096
    h2T = A.view(o_, BF16, [8, 256]); o_ += 4096
    q2T = A.view(o_, BF16, [8, 256]); o_ += 4096
    P2T = [A.view(o_ + i * 512, BF16, [256]) for i in range(8)]; o_ += 4096
    oo = A.view(o_, BF16, [2, 1024]); o_ += 4096
    oT = A.view(o_, BF16, [8, 256]); o_ += 4096
    x2t = [A.view(o_ + i * 4096, F32, [1024]) for i in range(2)]; o_ += 8192
    h3t = [A.view(o_ + i * 2048, BF16, [1024]) for i in range(2)]; o_ += 4096
    h3T = A.view(o_, BF16, [8, 128]); o_ += 2048
    rG = A.view(o_, F32, [1024]); o_ += 4096
    rt = A.view(o_, F32, [256]); o_ += 1024
    rtb = A.view(o_, BF16, [64]); o_ += 128
    stC = A.view(o_, F32, [32]); o_ += 128
    assert o_ <= U_off + 66048
    x1b = [x1, A.view(T_off, F32, [2, 1024])]
    h2Tb = [h2T, A.view(T_off + 8192, BF16, [8, 256])]
    q2Tb = [q2T, A.view(T_off + 12288, BF16, [8, 256])]

    st0 = [A.view(U_off + i * 15488, F32, [2568]) for i in range(4)]
    st0b = [A.view(U_off + 10304 + i * 15488, BF16, [2568]) for i in range(4)]
    jobs = []
    for (src, dst, ncol, gcol, nsc) in ((w_mkv, w_mkv_bf, 2048, GK, 8), (w_in, w_in_bf, 2568, GM, 8),
                                       (w_out, w_out_bf, 1024, GA, 4), (w_mq, w_mq_bf, 1024, GQ, 8),
                                       (w_mo, w_mo_bf, 1024, None, 0)):
        for c in range(8):
            jobs.append((src, dst, ncol, (gcol + c) if c < nsc else None, c))

    def p0_load(k):
        src, dst, ncol, g_, c = jobs[k]
        P.dma(st0[k % 4][:, 0:ncol], src[c * 128:(c + 1) * 128, :])

    for k in range(3):
        p0_load(k)
    for k in range(len(jobs)):
        if k + 3 < len(jobs):
            p0_load(k + 3)
        src, dst, ncol, g_, c = jobs[k]
        sb_, sbb = st0[k % 4], st0b[k % 4]
        if g_ is not None:
            if k % 2 == 0:
                P.tensor_scalar(sbb[:, 0:ncol], sb_[:, 0:ncol], vv[:, g_:g_ + 1], None, ALU.mult)
            else:
                P.activation(sbb[:, 0:ncol], sb_[:, 0:ncol], AF.Copy, scale=vv[:, g_:g_ + 1])
        else:
            P.copy(sbb[:, 0:ncol], sb_[:, 0:ncol], eng="dve" if k % 2 == 0 else "act")
        P.dma(dst[c * 128:(c + 1) * 128, :], sbb[:, 0:ncol])
    P.dma(st0[0][:, 0:288], w_route)
    P.copy(wr_bf, st0[0][:, 0:288].rearrange("p (c n) -> p c n", c=8))

    if stop_after == "phase0":
        P.dma(out[0:128, :], st0[0][:, 0:1024])
        P.emit()
        return nc
    bank = [0]

    def nb():
        bank[0] = (bank[0] + 1) % 8
        return bank[0]

    def rstd_from_ss(dst, ss, epsc):
        P.activation(dst, ss, AF.Ln, bias=epsc, scale=1.0)
        P.activation(dst, dst, AF.Exp, scale=-0.5)


    def recip(o, i, eng="dve"):
        P.add(eng, lambda e: e.reciprocal(o, i), reads=[i], writes=[o])

    def route_tile(gi, pl, tt):
        lg = rt[:, 0:36]
        P.tensor_tensor(lg, pl, rA[:, BR_:BR_ + 36], ALU.add)
        if "d_lg" in dbg:
            P.dma(dbg["d_lg"][gi * 128:(gi + 1) * 128, :], lg)
        gmax, ngmax, gsum, gtop = rt[:, 40:41], rt[:, 41:42], rt[:, 42:43], rt[:, 43:44]
        ohg = rt[:, 44:48]
        P.reduce(gmax, lg[:, 0:4], ALU.max)
        P.tensor_scalar(ohg, lg[:, 0:4], gmax, None, ALU.is_equal)
        P.tensor_scalar(ngmax, gmax, -1.0, None, ALU.mult)
        P.activation(rt[:, 48:52], lg[:, 0:4], AF.Exp, bias=ngmax, scale=1.0, accum_out=gsum)
        recip(gtop, gsum)
        esel = rt[:, 52:60]
        P.tensor_scalar(esel, lg[:, 4:12], ohg[:, 0:1], None, ALU.mult)
        for g in range(1, 4):
            P.stt(esel, lg[:, 4 + 8 * g:12 + 8 * g], ohg[:, g:g + 1], esel, ALU.mult, ALU.add)
        top8 = rt[:, 60:68]
        P.add("dve", lambda e: e.max(top8, esel), reads=[esel], writes=[top8])
        oh1, oh2 = rt[:, 68:76], rt[:, 76:84]
        P.tensor_scalar(oh1, esel, top8[:, 0:1], None, ALU.is_equal)
        P.tensor_scalar(oh2, esel, top8[:, 1:2], None, ALU.is_equal)
        dd, w1 = rt[:, 84:85], rt[:, 85:86]
        P.tensor_tensor(dd, top8[:, 1:2], top8[:, 0:1], ALU.subtract)
        P.activation(dd, dd, AF.Exp)
        P.tensor_scalar(dd, dd, 1.0, None, ALU.add)
        recip(w1, dd)
        P.tensor_tensor(G1[:, gi:gi + 1], gtop, w1, ALU.mult)
        P.tensor_tensor(G2[:, gi:gi + 1], gtop, G1[:, gi:gi + 1], ALU.subtract)
        a1, a2 = rt[:, 96:128], rt[:, 128:160]
        for a_, oh in ((a1, oh1), (a2, oh2)):
            P.tensor_tensor(a_.rearrange("p (g e) -> p g e", g=4), ohg.unsqueeze(2).to_broadcast([128, 4, 8]),
                            oh.unsqueeze(1).to_broadcast([128, 4, 8]), ALU.mult)
        P.copy(A1[:, gi, :], a1)
        P.copy(A2[:, gi, :], a2)
        cmb = rtb[:, 0:32]
        P.tensor_tensor(cmb, a1, a2, ALU.add)
        pr = psf(7)[:, 384 + 64 * tt:448 + 64 * tt]
        P.matmul(pr[:, 0:32], cb[:, UST:UST + 128], cmb)
        P.matmul(pr[:, 32:64], cb[:, ONEB:ONEB + 128], cmb)
        rk, jk = rt[:, 160:192], rt[:, 192:224]
        P.tensor_tensor(rk, pr[:, 0:32], base, ALU.add)
        P.tensor_tensor(jk, a1, rk, ALU.mult)
        P.reduce(P1[:, gi:gi + 1], jk, ALU.add)
        P.tensor_tensor(jk, a2, rk, ALU.mult)
        P.reduce(P2[:, gi:gi + 1], jk, ALU.add)
        P.tensor_tensor(base, base, pr[:, 32:64], ALU.add)

    def sumsq(junk, src, ss):
        P.activation(junk, src, AF.Square, accum_out=ss)

    def transposeT(src_bf, dstT, col0, ev, b=None):
        if b is None:
            b = 6 + (nb() % 2)
        pv = psb(b)
        for c in range(8):
            P.transpose(pv[:, c * 128:(c + 1) * 128], src_bf[:, c * 128:(c + 1) * 128], identb)
        P.copy(dstT[:, :, col0:col0 + 128], pv.rearrange("p (c t) -> p c t", c=8), eng=ev)


    for b in range(nseq):
        tok0 = b * S
        for c in range(8):
            P.dma(Wkv[:, c, :], w_mkv_bf[c * 128:(c + 1) * 128, :])
        for mt in range(2):
            xi, xb = xin[mt], xn[mt]
            P.dma(xi, mem[b * 256 + mt * 128: b * 256 + (mt + 1) * 128, :])
            ss, rs = stA[:, 2 * mt:2 * mt + 1], stA[:, 2 * mt + 1:2 * mt + 2]
            sumsq(xb, xi, ss)
            rstd_from_ss(rs, ss, eps1024)
            P.tensor_scalar(xb, xi, rs, None, ALU.mult)
            transposeT(xb, memT, mt * 128, "dve")
        for cc in range(8):
            bk = nb() % 6
            pv = psf(bk)[:, 0:256]
            for c in range(8):
                P.matmul(pv, Wkv[:, c, cc * 128:(cc + 1) * 128], memT[:, c, :], start=(c == 0), stop=(c == 7))
            P.copy(K2T[:, cc, :], pv, eng="act" if cc % 2 else "dve")
        for mt in range(2):
            for nh in range(2):
                bk = nb() % 6
                pv = psf(bk)
                for c in range(8):
                    P.matmul(pv, memT[:, c, mt * 128:(mt + 1) * 128], Wkv[:, c, 1024 + nh * 512:1024 + (nh + 1) * 512],
                             start=(c == 0), stop=(c == 7))
                P.copy(V2[:, mt, 2 * nh:2 * nh + 2, 0:256], pv.rearrange("p (h d) -> p h d", h=2),
                       eng="act" if nh % 2 else "dve")
        P.memset(V2[:, :, :, 256:257], 1.0)
        if stop_after == "A0":
            P.dma(out[0:128, :], xin[0])
            P.emit()
            return nc
        for c in range(8):
            P.dma(Win[:, c, :], w_in_bf[c * 128:(c + 1) * 128, :])
        for i in range(NT):
            xi, xb = xin[i % 2], xn[i % 2]
            P.dma(xi, x[tok0 + i * 128: tok0 + (i + 1) * 128, :])
            ss, rs = stA[:, 4 + 2 * (i % 2):5 + 2 * (i % 2)], stA[:, 5 + 2 * (i % 2):6 + 2 * (i % 2)]
            sumsq(xb, xi, ss)
            rstd_from_ss(rs, ss, eps1024)
            P.tensor_scalar(xb, xi, rs, None, ALU.mult)
            transposeT(xb, xT, i * 128, "act" if i % 2 else "dve")
        if stop_after == "A1":
            P.dma(out[0:128, :], xin[0])
            P.emit()
            return nc
        P.memset(zT[:, :, 0:30], 0.0)
        P.memset(zT[:, :, 2078:2080], 0.0)
        P.memset(vS[:, :, :, 64:65], 1.0)
        OFF_K, OFF_V, OFF_F, OFF_C = 512, 1024, 1536, 1544
        for tb in range(4):
            tsl = slice(tb * 512, (tb + 1) * 512)
            for cc in range(8):
                bk = nb() % 6
                pv = psf(bk)
                for c in range(8):
                    P.matmul(pv, Win[:, c, cc * 128:(cc + 1) * 128], xT[:, c, tsl], start=(c == 0), stop=(c == 7))
                dst = qT[:, cc, tsl] if cc < 4 else kT[:, cc - 4, tsl]
                P.copy(dst, pv, eng="act" if cc % 2 else "dve")
            for i4 in range(4):
                bg = nb() % 6
                pg = psf(bg)
                for c in range(8):
                    P.matmul(pg, Win[:, c, OFF_C + 512 + i4 * 128:OFF_C + 512 + (i4 + 1) * 128], xT[:, c, tsl],
                             start=(c == 0), stop=(c == 7))
                sg = sig[i4 % 2]
                P.activation(sg, pg, AF.Sigmoid, bias=vv[:, BG + 4 + i4:BG + 5 + i4], scale=1.0)
                ba = nb() % 6
                pa = psf(ba)
                for c in range(8):
                    P.matmul(pa, Win[:, c, OFF_C + i4 * 128:OFF_C + (i4 + 1) * 128], xT[:, c, tsl],
                             start=(c == 0), stop=(c == 7))
                P.stt(zT[:, i4, 30 + tb * 512:30 + (tb + 1) * 512], pa, vv[:, BG + i4:BG + i4 + 1], sg, ALU.add, ALU.mult)
        pf_ = psf(6)
        for i in range(NT):
            bk = nb() % 6
            pv = psf(bk)
            for c in range(8):
                P.matmul(pv, xT[:, c, i * 128:(i + 1) * 128], Win[:, c, OFF_V:OFF_V + 512], start=(c == 0), stop=(c == 7))
            P.copy(vS[:, i, :, 0:64], pv.rearrange("p (h d) -> p h d", h=8), eng="act" if i % 2 else "dve")
            for c in range(8):
                P.matmul(pf_[:, i * 8:(i + 1) * 8], xT[:, c, i * 128:(i + 1) * 128], Win[:, c, OFF_F:OFF_F + 8],
                         start=(c == 0), stop=(c == 7))
        if stop_after == "A2":
            P.dma(out[0:128, :], xin[0])
            P.emit()
            return nc
        nlf2 = nlf.rearrange("p a b -> p (a b)")
        P.tensor_tensor(nlf2, pf_[:, 0:128], rA[:, BF_:BF_ + 128], ALU.add)
        P.activation(nlf2, nlf2, AF.Exp, scale=-1.0)
        P.activation(nlf2, nlf2, AF.Ln, bias=one_c, scale=1.0)
        if stop_after == "A3a":
            P.dma(out[0:128, :], xin[0])
            P.emit()
            return nc
        P.copy(incl[:, 0, :], nlf[:, 0, :])
        for i in range(1, NT):
            P.tensor_tensor(incl[:, i, :], incl[:, i - 1, :], nlf[:, i, :], ALU.add)
        if stop_after == "A3b":
            P.dma(out[0:128, :], xin[0])
            P.emit()
            return nc
        pc1 = psf(7)[:, 0:128]
        pc2 = psf(7)[:, 128:256]
        for (pdst, lmat, srcf) in ((pc1, cb[:, ONEB:ONEB + 128], incl.rearrange("p a b -> p (a b)")),
                                   (pc2, cb[:, LTRIB:LTRIB + 128], nlf2)):
            rres = cz[0][:, 0:128]
            for t3 in range(3):
                piece = xn[0][:, t3 * 128:(t3 + 1) * 128]
                P.copy(piece, srcf if t3 == 0 else rres)
                if t3 < 2:
                    P.tensor_tensor(rres, srcf if t3 == 0 else rres, piece, ALU.subtract)
                P.matmul(pdst, lmat, piece, start=(t3 == 0), stop=(t3 == 2))
        P.copy(Cend.rearrange("p a b -> p (a b)"), pc1)
        P.copy(Cc.rearrange("p a b -> p (a b)"), pc2)
        if stop_after == "A3c":
            P.dma(out[0:128, :], xin[0])
            P.emit()
            return nc
        P.tensor_tensor(Cc[:, 1:16, :], Cc[:, 1:16, :], Cend[:, 0:15, :], ALU.add)
        if stop_after == "A3d":
            P.dma(out[0:128, :], xin[0])
            P.emit()
            return nc
        for h in range(8):
            P.tensor_tensor(Bt[:, h, :, :], Cc[:, :, h:h + 1].to_broadcast([128, 16, 16]),
                            Cend[:, :, h].unsqueeze(1).to_broadcast([128, 16, 16]), ALU.subtract)
        if "d_q" in dbg:
            P.dma(dbg["d_q"], qT.rearrange("p a b -> p (a b)"))
        if "d_z" in dbg:
            P.dma(dbg["d_z"], zT.rearrange("p a b -> p (a b)"))
        if "d_B" in dbg:
            P.dma(dbg["d_B"], Bt.rearrange("p a b c -> p (a b c)"))
        if stop_after == "A3":
            P.dma(out[0:128, :], xin[0])
            P.emit()
            return nc
        if 'conv' in BP:
            for i4 in range(4):
                for kk in range(31):
                    P.tensor_scalar(Dg[:, kk, :], identb, vv[:, WDW + i4 * 31 + kk:WDW + i4 * 31 + kk + 1], None, ALU.mult)
                for j4 in range(4):
                    pcv = psf(nb() % 6)
                    for t4 in range(4):
                        c0 = (j4 * 4 + t4) * 128
                        for kk in range(31):
                            P.matmul(pcv[:, t4 * 128:(t4 + 1) * 128], zT[:, i4, c0 + kk:c0 + kk + 128], Dg[:, kk, :],
                                     start=(kk == 0), stop=(kk == 30))
                    P.tensor_tensor(cvo[i4][:, j4 * 4:(j4 + 1) * 4, :], pcv.rearrange("p (t c) -> p t c", t=4),
                                    rA[:, BDW_ + i4 * 128:BDW_ + (i4 + 1) * 128].unsqueeze(1).to_broadcast([128, 4, 128]),
                                    ALU.add)
        for wi, wsrc in enumerate((w_out_bf, w_mq_bf, w_mo_bf)):
            for c in range(8):
                P.dma(Woqm[:, wi, c, :], wsrc[c * 128:(c + 1) * 128, :])

        catT = xT
        LAG = 3

        def tail_conv(j):
            s1, nm, sq, rsd = stB[:, 10:11], stB[:, 11:12], stB[:, 12:13], stB[:, 13:14]
            xc, tmp = cz[0], cz[1]
            for i4 in range(4):
                P.copy(xc[:, i4 * 128:(i4 + 1) * 128], cvo[i4][:, j, :], eng="pool" if i4 % 2 else "dve")
            if "d_conv" in dbg:
                P.dma(dbg["d_conv"][j * 128:(j + 1) * 128, :], xc)
            P.reduce(s1, xc, ALU.add)
            P.tensor_scalar(nm, s1, -1.0 / 512.0, None, ALU.mult)
            P.tensor_scalar(xc, xc, nm, None, ALU.add)
            sumsq(tmp, xc, sq)
            P.activation(rsd, sq, AF.Ln, bias=epsln, scale=1.0 / 512.0)
            P.activation(rsd, rsd, AF.Exp, scale=-0.5)
            P.stt(xc, xc, rsd, rA[:, LG_:LG_ + 512], ALU.mult, ALU.mult)
            P.tensor_tensor(xc, xc, rA[:, LB_:LB_ + 512], ALU.add)
            P.activation(tmp, xc, AF.Exp, scale=-1.0)
            P.tensor_scalar(tmp, tmp, 1.0, None, ALU.add)
            P.add("dve", lambda e, o=tmp: e.reciprocal(o, o), reads=[tmp], writes=[tmp])
            P.tensor_tensor(catC[j % 2], xc, tmp, ALU.mult)

        def norm_attn(j, accb):
            rden = stB[:, 0:8]
            for hh in range(2):
                av = accb[hh][:, 0:260].rearrange("p (h d) -> p h d", h=4)
                P.add("dve", lambda e, o=rden[:, hh * 4:(hh + 1) * 4], i=av[:, :, 64]: e.reciprocal(o, i),
                      reads=[av[:, :, 64]], writes=[rden[:, hh * 4:(hh + 1) * 4]])
                P.tensor_tensor(attn[:, hh * 256:(hh + 1) * 256].rearrange("p (h d) -> p h d", h=4), av[:, :, 0:64],
                                rden[:, hh * 4:(hh + 1) * 4].unsqueeze(2).to_broadcast([128, 4, 64]), ALU.mult)
            if "d_attn" in dbg:
                P.dma(dbg["d_attn"][j * 128:(j + 1) * 128, :], attn)

        def tail_attn(j):
            ssa, rsa = stB[:, 8:9], stB[:, 9:10]
            sumsq(catA, attn, ssa)
            rstd_from_ss(rsa, ssa, eps512)
            P.tensor_scalar(catA, attn, rsa, None, ALU.mult)
            pv = psb(7)
            for c in range(4):
                P.transpose(pv[:, c * 128:(c + 1) * 128], catA[:, c * 128:(c + 1) * 128], identb)
            for c in range(4):
                P.transpose(pv[:, (4 + c) * 128:(5 + c) * 128], catC[j % 2][:, c * 128:(c + 1) * 128], identb)
            if "d_cat" in dbg:
                P.dma(dbg["d_cat"][j * 128:(j + 1) * 128, 0:512], catA)
                P.dma(dbg["d_cat"][j * 128:(j + 1) * 128, 512:1024], catC[j % 2])
            P.copy(catT[:, :, j * 128:(j + 1) * 128], pv.rearrange("p (c t) -> p c t", c=8), eng="act" if j % 2 else "dve")

        for j in range(NT):
            if j == 0:
                tail_conv(0)
                tail_conv(1)
            accb = [psf(2), psf(3)]
            pairs = [(h, kt) for h in range(8) for kt in range(j + 1)]
            stslots = {}
            n_pairs = len(pairs)

            def emit_qk(n):
                h, kt = pairs[n]
                stv = psf((0, 1, 4, 5, 6)[n % 5])[:, 0:128]
                r0 = 0 if 'evenonly' in BP else (h % 2) * 64
                if 'qk' in BP:
                    P.matmul(stv, kT[r0:r0 + 64, h // 2, kt * 128:(kt + 1) * 128], qT[r0:r0 + 64, h // 2, j * 128:(j + 1) * 128],
                             start=True, stop=(kt != j or 'mask' not in BP))
                if kt == j and 'mask' in BP:
                    P.matmul(stv, identb, negm, start=False, stop=True)
                pt = PT[n % 6]
                if 'exp' in BP:
                    if 'expdve' in BP:
                        P.copy(pt, stv, eng='dve')
                    elif 'expcopy' in BP:
                        P.copy(pt, stv, eng='act')
                    elif 'nobias' in BP:
                        P.activation(pt, stv, AF.Exp, scale=0.125)
                    else:
                        P.activation(pt, stv, AF.Exp, bias=Bt[:, h, kt, j:j + 1], scale=0.125)

            def emit_pv(n):
                h, kt = pairs[n]
                pt = PT[n % 6]
                av = accb[h // 4][:, (h % 4) * 65:(h % 4) * 65 + 65]
                if 'pv' in BP:
                    P.matmul(av, pt, vS[:, kt, h, :], start=(kt == 0), stop=(kt == j))

            for n in range(n_pairs + LAG):
                if j > 0 and n == min(6, n_pairs + LAG - 1):
                    tail_attn(j - 1)
                    if j + 1 < NT:
                        tail_conv(j + 1)
                if n < n_pairs:
                    emit_qk(n)
                if n >= LAG:
                    emit_pv(n - LAG)
            norm_attn(j, accb)
        tail_attn(NT - 1)

        if stop_after == "B":
            P.dma(out[0:128, :], xin[0])
            P.emit()
            return nc
        P.dma(rG, rows[:, RG:RG + 1024])
        P.tensor_scalar(rG, rG, 32.0, None, ALU.mult)
        Wout, Wmq, Wmo = Woqm[:, 0], Woqm[:, 1], Woqm[:, 2]

        def c_front(blk):
            pb = blk % 2
            for tt in range(2):
                i = blk * 2 + tt
                P.dma(xr[tt], x[tok0 + i * 128: tok0 + (i + 1) * 128, :])
                for nh in range(2):
                    pv = psf(nb() % 6)
                    for c in range(8):
                        P.matmul(pv, catT[:, c, i * 128:(i + 1) * 128], Wout[:, c, nh * 512:(nh + 1) * 512],
                                 start=(c == 0), stop=(c == 7))
                    P.tensor_tensor(x1b[pb][:, tt, nh * 512:(nh + 1) * 512], pv, xr[tt][:, nh * 512:(nh + 1) * 512], ALU.add)
                if "d_x1" in dbg:
                    P.dma(dbg["d_x1"][i * 128:(i + 1) * 128, :], x1b[pb][:, tt, :])
                yield
                ss, rs = stC[:, 2 * tt:2 * tt + 1], stC[:, 2 * tt + 1:2 * tt + 2]
                sumsq(xn2[tt], x1b[pb][:, tt, :], ss)
                rstd_from_ss(rs, ss, eps1024)
                P.tensor_scalar(xn2[tt], x1b[pb][:, tt, :], rs, None, ALU.mult)
                transposeT(xn2[tt], h2Tb[pb], tt * 128, "act" if tt else "dve")
                yield
            for cc in range(8):
                pv = psf(nb() % 6)[:, 0:256]
                for c in range(8):
                    P.matmul(pv, Wmq[:, c, cc * 128:(cc + 1) * 128], h2Tb[pb][:, c, :], start=(c == 0), stop=(c == 7))
                P.copy(q2Tb[pb][:, cc, :], pv, eng="act" if cc % 2 else "dve")
                if cc % 2:
                    yield

        def c_back(blk):
            pb = blk % 2

            def scores(h):
                for mt in range(2):
                    pv = psf(nb() % 6)[:, 0:256]
                    for k2 in range(2):
                        P.matmul(pv, K2T[:, 2 * h + k2, mt * 128:(mt + 1) * 128], q2Tb[pb][:, 2 * h + k2, :],
                                 start=(k2 == 0), stop=(k2 == 1))
                    P.activation(P2T[(h % 2) * 2 + mt + 4 * pb], pv, AF.Exp, scale=1.0 / 16.0)

            def pvh(h):
                for tt in range(2):
                    pa = psf(nb() % 6)[:, 0:257]
                    for mt in range(2):
                        P.matmul(pa, P2T[(h % 2) * 2 + mt + 4 * pb][:, tt * 128:(tt + 1) * 128], V2[:, mt, h, :],
                                 start=(mt == 0), stop=(mt == 1))
                    rd = stC[:, 8 + tt:9 + tt]
                    P.add("dve", lambda e, o=rd, i_=pa[:, 256:257]: e.reciprocal(o, i_), reads=[pa[:, 256:257]], writes=[rd])
                    P.tensor_scalar(oo[:, tt, h * 256:(h + 1) * 256], pa[:, 0:256], rd, None, ALU.mult)

            scores(0)
            yield
            scores(1)
            yield
            pvh(0)
            yield
            scores(2)
            pvh(1)
            yield
            scores(3)
            pvh(2)
            yield
            pvh(3)
            yield
            for tt in range(2):
                transposeT(oo[:, tt, :], oT, tt * 128, "act" if tt else "dve")
                yield
            for tt in range(2):
                i = blk * 2 + tt
                gi = b * NT + i
                xo = x2t[tt]
                for nh in range(2):
                    pv = psf(nb() % 6)
                    for c in range(8):
                        P.matmul(pv, oT[:, c, tt * 128:(tt + 1) * 128], Wmo[:, c, nh * 512:(nh + 1) * 512],
                                 start=(c == 0), stop=(c == 7))
                    P.tensor_tensor(xo[:, nh * 512:(nh + 1) * 512], pv, x1b[pb][:, tt, nh * 512:(nh + 1) * 512], ALU.add)
                P.dma(x2s[tok0 + i * 128: tok0 + (i + 1) * 128, :], xo, q="pool")
                yield
                ss, rs = stC[:, 4 + 2 * tt:5 + 2 * tt], stC[:, 5 + 2 * tt:6 + 2 * tt]
                hb = h3t[tt]
                sumsq(hb, xo, ss)
                rstd_from_ss(rs, ss, eps1024)
                P.stt(hb.rearrange("t (c p) -> t p c", c=8), xo.rearrange("t (p c) -> t p c", c=8), rs,
                      rG.rearrange("t (p c) -> t p c", c=8), ALU.mult, ALU.mult)
                P.dma(h3s[tok0 + i * 128: tok0 + (i + 1) * 128, :], hb, q="pool")
                transposeT(hb, h3T, 0, "act" if tt else "dve")
                yield
                pl = psf(7)[:, 256 + 64 * tt:256 + 64 * tt + 36]
                for c in range(8):
                    P.matmul(pl, h3T[:, c, :], wr_bf[:, c, :], start=(c == 0), stop=(c == 7))
                P.copy(LG[:, gi, :], pl, eng="act")
                yield

        def interleave(*gens):
            gens = [g for g in gens if g is not None]
            while gens:
                for g in list(gens):
                    try:
                        next(g)
                    except StopIteration:
                        gens.remove(g)

        interleave(c_front(0))
        for blk in range(NT // 2):
            interleave(c_back(blk), c_front(blk + 1) if blk + 1 < NT // 2 else None)

    A.off = mark_persist
    gidx = A.new(I32, [96]); didx = A.new(I32, [96, 4])
    mark_p2 = A.off
    N_ = NTT
    lgb = A.new(F32, [64, 36]); gmax = A.new(F32, [64]); ohg = A.new(F32, [64, 4]); eg = A.new(F32, [64, 4])
    gsum = A.new(F32, [64]); gtop = A.new(F32, [64]); esel = A.new(F32, [64, 8]); tmp8 = A.new(F32, [64, 8])
    m1 = A.new(F32, [64]); m2 = A.new(F32, [64]); oh1 = A.new(F32, [64, 8]); oh2 = A.new(F32, [64, 8])
    dd = A.new(F32, [64]); w1 = A.new(F32, [64])
    a1f = A.new(F32, [64, 32]); a2f = A.new(F32, [64, 32]); cmb = A.new(BF16, [64, 32])
    rk = A.new(F32, [64, 32]); tot = A.new(F32, [64, 32]); bex = A.new(F32, [64, 32]); big = A.new(F32, [64, 32])

    def bc3(v, k):
        return v[:, 0:N_].unsqueeze(2).to_broadcast([128, N_, k])

    P.tensor_tensor(lgb[:, 0:N_, :], LG[:, 0:N_, :], rA[:, BR_:BR_ + 36].unsqueeze(1).to_broadcast([128, N_, 36]), ALU.add)
    if "d_lg" in dbg:
        for gi in range(N_):
            P.dma(dbg["d_lg"][gi * 128:(gi + 1) * 128, :], lgb[:, gi, :])
    P.reduce(gmax[:, 0:N_], lgb[:, 0:N_, 0:4], ALU.max)
    P.tensor_tensor(ohg[:, 0:N_, :], lgb[:, 0:N_, 0:4], bc3(gmax, 4), ALU.is_equal)
    P.tensor_tensor(eg[:, 0:N_, :], lgb[:, 0:N_, 0:4], bc3(gmax, 4), ALU.subtract)
    P.activation(eg[:, 0:N_, :], eg[:, 0:N_, :], AF.Exp)
    P.reduce(gsum[:, 0:N_], eg[:, 0:N_, :], ALU.add)
    recip(gtop[:, 0:N_], gsum[:, 0:N_])
    P.tensor_tensor(esel[:, 0:N_, :], lgb[:, 0:N_, 4:12], ohg[:, 0:N_, 0:1].to_broadcast([128, N_, 8]), ALU.mult)
    for g in range(1, 4):
        P.tensor_tensor(tmp8[:, 0:N_, :], lgb[:, 0:N_, 4 + 8 * g:12 + 8 * g], ohg[:, 0:N_, g:g + 1].to_broadcast([128, N_, 8]), ALU.mult)
        P.tensor_tensor(esel[:, 0:N_, :], esel[:, 0:N_, :], tmp8[:, 0:N_, :], ALU.add)
    P.reduce(m1[:, 0:N_], esel[:, 0:N_, :], ALU.max)
    P.tensor_tensor(oh1[:, 0:N_, :], esel[:, 0:N_, :], bc3(m1, 8), ALU.is_equal)
    P.stt(tmp8[:, 0:N_, :], oh1[:, 0:N_, :], -1e30, esel[:, 0:N_, :], ALU.mult, ALU.add)
    P.reduce(m2[:, 0:N_], tmp8[:, 0:N_, :], ALU.max)
    P.tensor_tensor(oh2[:, 0:N_, :], tmp8[:, 0:N_, :], bc3(m2, 8), ALU.is_equal)
    P.tensor_tensor(dd[:, 0:N_], m2[:, 0:N_], m1[:, 0:N_], ALU.subtract)
    P.activation(dd[:, 0:N_], dd[:, 0:N_], AF.Exp)
    P.tensor_scalar(dd[:, 0:N_], dd[:, 0:N_], 1.0, None, ALU.add)
    recip(w1[:, 0:N_], dd[:, 0:N_])
    P.tensor_tensor(G1[:, 0:N_], gtop[:, 0:N_], w1[:, 0:N_], ALU.mult)
    P.tensor_tensor(G2[:, 0:N_], gtop[:, 0:N_], G1[:, 0:N_], ALU.subtract)
    for a_, oh in ((a1f, oh1), (a2f, oh2)):
        P.tensor_tensor(a_[:, 0:N_, :].rearrange("p n (g e) -> p n g e", g=4),
                        ohg[:, 0:N_, :].unsqueeze(3).to_broadcast([128, N_, 4, 8]),
                        oh[:, 0:N_, :].unsqueeze(2).to_broadcast([128, N_, 4, 8]), ALU.mult)
    P.tensor_tensor(cmb[:, 0:N_, :], a1f[:, 0:N_, :], a2f[:, 0:N_, :], ALU.add)
    cmb2, rk2, tot2 = (v.rearrange("p n e -> p (n e)") for v in (cmb, rk, tot))
    for q0 in range(0, N_ * 32, 512):
        q1 = min(q0 + 512, N_ * 32)
        pr = psf(nb() % 6)[:, 0:q1 - q0]
        P.matmul(pr, cb[:, UST:UST + 128], cmb2[:, q0:q1])
        P.copy(rk2[:, q0:q1], pr)
        pt_ = psf(nb() % 6)[:, 0:q1 - q0]
        P.matmul(pt_, cb[:, ONEB:ONEB + 128], cmb2[:, q0:q1])
        P.copy(tot2[:, q0:q1], pt_, eng="act")
    P.memset(bex[:, 0, :], 0.0)
    for t_ in range(1, N_):
        P.tensor_tensor(bex[:, t_, :], bex[:, t_ - 1, :], tot[:, t_ - 1, :], ALU.add)
    P.tensor_tensor(base, bex[:, N_ - 1, :], tot[:, N_ - 1, :], ALU.add)
    P.tensor_tensor(rk[:, 0:N_, :], rk[:, 0:N_, :], bex[:, 0:N_, :], ALU.add)
    for (af, Px) in ((a1f, P1), (a2f, P2)):
        P.tensor_tensor(big[:, 0:N_, :], af[:, 0:N_, :], rk[:, 0:N_, :], ALU.mult)
        P.reduce(Px[:, 0:N_], big[:, 0:N_, :], ALU.add)
    if "d_rt" in dbg:
        P.dma(dbg["d_rt"][:, 0:NTT], G1[:, 0:NTT]); P.dma(dbg["d_rt"][:, 64:64 + NTT], G2[:, 0:NTT])
        P.dma(dbg["d_rt"][:, 128:128 + NTT], P1[:, 0:NTT]); P.dma(dbg["d_rt"][:, 192:192 + NTT], P2[:, 0:NTT])
        P.dma(dbg["d_rt"][:, 256:288], base)
    if stop_after == "phase1":
        P.dma(out[0:128, :], big[:, 0:32, :].rearrange("p a b -> p (a b)"))
        P.emit()
        return nc


    pcf = A.new(F32, [32]); pci = A.new(I32, [32]); offs = A.new(F32, [32]); ends = A.new(F32, [32])
    te = A.new(F32, [96]); tf = A.new(F32, [96])
    zero_i = A.new(I32, [NSLOT // 128]); posf = A.new(F32, [64])
    P.tensor_scalar(pcf, base, TS / 2 - 0.5, 1.0 / TS, ALU.add, ALU.mult)
    P.copy(pci, pcf)
    P.copy(pcf, pci)
    P.tensor_scalar(pcf, pcf, float(TS), None, ALU.mult)
    P.memset(offs[:, 0:1], 0.0)
    for e in range(1, 32):
        P.tensor_tensor(offs[:, e:e + 1], offs[:, e - 1:e], pcf[:, e - 1:e], ALU.add)
    P.tensor_tensor(ends, offs, pcf, ALU.add)
    P.memset(te, 0.0)
    for e in range(32):
        P.stt(te, cf[:, THR:THR + 96], ends[:, e:e + 1], te, ALU.is_ge, ALU.add)
    if "d_te" in dbg:
        P.dma(dbg["d_te"], te)
    P.tensor_scalar(tf, te, 128.0, cf[:, PCOL:PCOL + 1], ALU.mult, ALU.add)
    P.copy(gidx, tf)
    for c in range(4):
        P.tensor_scalar(tf, te, 512.0, cf[:, CBASE + c:CBASE + c + 1], ALU.mult, ALU.add)
        P.copy(didx[:, :, c], tf)
    for (Ax, Px, POSx) in ((a1f, P1, POS1), (a2f, P2, POS2)):
        P.tensor_tensor(big[:, 0:NTT, :], Ax[:, 0:NTT, :], offs.unsqueeze(1).to_broadcast([128, NTT, 32]), ALU.mult)
        P.reduce(posf[:, 0:NTT], big[:, 0:NTT, :], ALU.add)
        P.tensor_tensor(posf[:, 0:NTT], posf[:, 0:NTT], Px[:, 0:NTT], ALU.add)
        P.copy(POSx[:, 0:NTT], posf[:, 0:NTT])
    P.memset(zero_i, 0)
    P.dma(s2t.rearrange("(p n) o -> p (n o)", p=128), zero_i)
    FAKE = "D:s2t_scatter"
    nsc = 0
    for gi in range(NTT):
        for POSx in (POS1, POS2):
            P.add("pool", lambda e, po=POSx[:, gi:gi + 1], ti=ci[:, gi:gi + 1]: e.indirect_dma_start(
                out=s2t, out_offset=bass.IndirectOffsetOnAxis(ap=po, axis=0), in_=ti, in_offset=None),
                reads=[POSx[:, gi:gi + 1], ci[:, gi:gi + 1], s2t], writes=[(FAKE, 0, 1, nsc, nsc + 1)], dma=True)
            nsc += 1

    A.off = mark_p2
    wg = [A.new(BF16, [8, 512]) for _ in range(3)]
    wu = [A.new(BF16, [8, 512]) for _ in range(3)]
    wd = [A.new(BF16, [4, 1024]) for _ in range(3)]
    sidx = [A.new(I32, [GT]) for _ in range(3)]
    hs = [A.new(BF16, [GT, 1024]) for _ in range(2)]
    hTs = [A.new(BF16, [8, TS]) for _ in range(2)]
    sil = [A.new(F32, [TS]) for _ in range(2)]
    hid = [A.new(BF16, [4, TS]) for _ in range(2)]
    yt = [A.new(BF16, [1024]) for _ in range(4)]
    s2t_v = s2t.rearrange("(j g p) o -> j p (g o)", g=GT, p=128)
    def moe_loads(j):
        k3 = j % 3
        for (wdst, wsrc) in ((wg[k3], w_gate), (wu[k3], w_up)):
            P.add("pool", lambda e, o=wdst.rearrange("p a b -> p (a b)"), s=wsrc, ix=gidx[:, j:j + 1]: e.indirect_dma_start(
                out=o, out_offset=None, in_=s, in_offset=bass.IndirectOffsetOnAxis(ap=ix, axis=0),
                bounds_check=P.reg(e, 32 * 128 - 1), oob_is_err=False),
                reads=[gidx[:, j:j + 1], wsrc], writes=[wdst], dma=True)
        for c in range(4):
            P.add("pool", lambda e, o=wd[k3][:, c, :], ix=didx[:, j, c:c + 1]: e.indirect_dma_start(
                out=o, out_offset=None, in_=w_down, in_offset=bass.IndirectOffsetOnAxis(ap=ix, axis=0),
                bounds_check=P.reg(e, 32 * 512 - 1), oob_is_err=False),
                reads=[didx[:, j, c:c + 1], w_down], writes=[wd[k3][:, c, :]], dma=True)
        P.add("sp", lambda e, o=sidx[k3], s=s2t_v[j]: e.dma_start(out=o, in_=s, allow_slow_non_contiguous=True),
              reads=[s2t_v[j], (FAKE, 0, 1, 0, 1 << 20)], writes=[sidx[k3]], dma=True)
        hsj = hs[j % 2]
        for g in range(GT):
            P.add("pool", lambda e, o=hsj[:, g, :], ix=sidx[k3][:, g:g + 1]: e.indirect_dma_start(
                out=o, out_offset=None, in_=h3s, in_offset=bass.IndirectOffsetOnAxis(ap=ix, axis=0)),
                reads=[sidx[k3][:, g:g + 1], h3s], writes=[hsj[:, g, :]], dma=True)

    def moe_T(j):
        for g in range(GT):
            transposeT(hs[j % 2][:, g, :], hTs[j % 2], g * 128, "act" if g % 2 else "dve")

    def moe_GU(j):
        k3, hTj, hidj = j % 3, hTs[j % 2], hid[j % 2]
        for fc in range(4):
            pg = psf(nb() % 6)[:, 0:TS]
            for c in range(8):
                P.matmul(pg, wg[k3][:, c, fc * 128:(fc + 1) * 128], hTj[:, c, :], start=(c == 0), stop=(c == 7))
            pu = psf(nb() % 6)[:, 0:TS]
            for c in range(8):
                P.matmul(pu, wu[k3][:, c, fc * 128:(fc + 1) * 128], hTj[:, c, :], start=(c == 0), stop=(c == 7))
            sl = sil[fc % 2]
            P.activation(sl, pg, AF.Silu)
            P.tensor_tensor(hidj[:, fc, :], sl, pu, ALU.mult)

    ny = [0]

    def moe_D(j):
        k3, hidj = j % 3, hid[j % 2]
        for g in range(GT):
            y_ = yt[ny[0] % 4]
            ny[0] += 1
            for nh in range(2):
                pv = psf(nb() % 6)
                for fc in range(4):
                    P.matmul(pv, hidj[:, fc, g * 128:(g + 1) * 128], wd[k3][:, fc, nh * 512:(nh + 1) * 512],
                             start=(fc == 0), stop=(fc == 3))
                P.copy(y_[:, nh * 512:(nh + 1) * 512], pv, eng="act" if nh else "dve")
            P.dma(Ysc[j * TS + g * 128: j * TS + (g + 1) * 128, :], y_)

    if n_moe_tiles:
        moe_loads(0)
        moe_T(0)
    for j in range(n_moe_tiles):
        if j + 1 < n_moe_tiles:
            moe_loads(j + 1)
        moe_GU(j)
        if j + 1 < n_moe_tiles:
            moe_T(j + 1)
        moe_D(j)

    A.off = mark_p2
    fg = A.new(F32, [1024])
    NB4 = 4
    x2b = [A.new(F32, [1024]) for _ in range(NB4)]
    y1b = [A.new(BF16, [1024]) for _ in range(NB4)]
    y2b = [A.new(BF16, [1024]) for _ in range(NB4)]
    otb = [A.new(F32, [1024]) for _ in range(NB4)]
    st4 = A.new(F32, [2 * NB4])
    P.dma(fg, rows[:, RF:RF + 1024])
    P.tensor_scalar(fg, fg, 32.0, None, ALU.mult)
    def p4_loads(gi):
        k2 = gi % NB4
        P.dma(x2b[k2], x2s[gi * 128:(gi + 1) * 128, :])
        for (yb, POSx) in ((y1b[k2], POS1), (y2b[k2], POS2)):
            P.add("pool", lambda e, o=yb, ix=POSx[:, gi:gi + 1]: e.indirect_dma_start(
                out=o, out_offset=None, in_=Ysc, in_offset=bass.IndirectOffsetOnAxis(ap=ix, axis=0)),
                reads=[POSx[:, gi:gi + 1], Ysc], writes=[yb], dma=True)

    PF = NB4 - 1
    for gi in range(min(PF, NTT)):
        p4_loads(gi)
    for gi in range(NTT):
        if gi + PF < NTT:
            p4_loads(gi + PF)
        k2 = gi % NB4
        acc = x2b[k2]
        if not skip_moe:
            P.stt(acc, y1b[k2], G1[:, gi:gi + 1], acc, ALU.mult, ALU.add)
            P.stt(acc, y2b[k2], G2[:, gi:gi + 1], acc, ALU.mult, ALU.add)
        ss, rs = st4[:, 2 * k2:2 * k2 + 1], st4[:, 2 * k2 + 1:2 * k2 + 2]
        sumsq(otb[k2], acc, ss)
        rstd_from_ss(rs, ss, eps1024)
        P.stt(otb[k2], acc, rs, fg, ALU.mult, ALU.mult)
        P.dma(out[gi * 128:(gi + 1) * 128, :], otb[k2])
    cnt = P.emit()
    st.close()
    return nc


def _consts():
    p = np.arange(128)
    cfm = np.zeros((128, NCF), np.float32)
    cfm[:, THR:THR + 96] = float(TS) * np.arange(96)[None, :]
    cfm[:, PCOL] = p
    cfm[:, CBASE:CBASE + 4] = np.arange(4)[None, :] * 128 + p[:, None]
    cfm[:, IOTA:IOTA + 32] = np.arange(32)[None, :]
    cfm[:, CEPS:CEPS + 4] = np.array([1024e-6, 512e-6, 1.0, 1e-6], np.float32)[None, :]
    cbm = np.zeros((128, NCB), np.float32)
    cbm[:, IDB:IDB + 128] = np.eye(128)
    cbm[:, NEGM:NEGM + 128] = np.where(p[:, None] > p[None, :], -30000.0, 0.0)
    cbm[:, UST:UST + 128] = (p[:, None] < p[None, :])
    cbm[:, ONEB:ONEB + 128] = 1.0
    cbm[:, LTRIB:LTRIB + 128] = (p[:, None] <= p[None, :])
    cim = (np.arange(64)[None, :] * 128 + p[:, None]).astype(np.int32)
    return cfm, cbm.astype(ml_dtypes.bfloat16), cim


def _host_inputs(core, nseq, x, mem, norm_mix_g, w_in, b_forget, b_glu, w_dw, b_dw, conv_ln_g, conv_ln_b,
                 attn_out_g, w_out, norm_mem_g, mem_norm_g, w_mq, w_mkv, w_mo, norm_ffn_g,
                 w_route_group, b_route_group, w_route_expert, b_route_expert, w_gate, w_up, w_down, final_g):
    f = lambda a: np.ascontiguousarray(np.asarray(a, dtype=np.float32))
    cfm, cbm, cim = _consts()
    vecs = np.zeros((128, NV), np.float32)
    vecs[:, GM:GM + 8] = f(norm_mix_g)[0].reshape(8, 128).T
    vecs[:, GQ:GQ + 8] = f(norm_mem_g)[0].reshape(8, 128).T
    vecs[:, GK:GK + 8] = f(mem_norm_g)[0].reshape(8, 128).T
    vecs[:, GA:GA + 4] = f(attn_out_g)[0].reshape(4, 128).T
    vecs[:, BG:BG + 8] = f(b_glu)[0].reshape(8, 128).T
    vecs[:, BD:BD + 4] = f(b_dw)[0].reshape(4, 128).T
    vecs[:, WDW:WDW + 124] = f(w_dw)[0].reshape(31, 4, 128).transpose(2, 1, 0).reshape(128, 124)
    rows = np.zeros((1, NR), np.float32)
    rows[0, RG:RG + 1024] = f(norm_ffn_g)[0]
    rows[0, RF:RF + 1024] = f(final_g)
    rows[0, RLG:RLG + 512] = f(conv_ln_g)[0]
    rows[0, RLB:RLB + 512] = f(conv_ln_b)[0]
    rows[0, RBF:RBF + 128] = np.tile(f(b_forget)[0], 16)
    rows[0, RBR:RBR + 4] = f(b_route_group)[0]
    rows[0, RBR + 4:RBR + 36] = f(b_route_expert)[0].reshape(32)
    rows[0, RBDW:RBDW + 512] = f(b_dw)[0]
    rows = np.ascontiguousarray(np.broadcast_to(rows, (128, NR)))
    wr = np.concatenate([f(w_route_group)[0], f(w_route_expert)[0].transpose(1, 0, 2).reshape(1024, 32)], axis=1)
    return {
        "x": f(x[core * nseq:(core + 1) * nseq]).reshape(nseq * S, D),
        "mem": f(mem[core * nseq:(core + 1) * nseq]).reshape(nseq * 256, D),
        "w_in": f(w_in)[0], "w_out": f(w_out)[0], "w_mq": f(w_mq)[0], "w_mkv": f(w_mkv)[0], "w_mo": f(w_mo)[0],
        "w_route": np.ascontiguousarray(wr.reshape(128, 8 * 36)),
        "w_gate": f(w_gate)[0].reshape(32 * 128, 4096), "w_up": f(w_up)[0].reshape(32 * 128, 4096),
        "w_down": f(w_down)[0].reshape(32 * 512, 1024),
        "vecs": vecs, "rows": rows, "constf": cfm, "constb": cbm, "consti": cim,
    }


_NC_CACHE = {}


def kernel(**inputs):
    n_cores = 8
    nseq = 4
    if "full" not in _NC_CACHE:
        _NC_CACHE["full"] = build(nseq=nseq)
    nc = _NC_CACHE["full"]
    in_maps = [_host_inputs(c, nseq, **inputs) for c in range(n_cores)]
    res = run_bass_kernel_spmd(nc, in_maps, core_ids=list(range(n_cores)))
    outs = [np.asarray(r["out"]).reshape(nseq, S, D) for r in res.results]
    return np.concatenate(outs, axis=0).astype(np.float32)
```

```python
import numpy as np
import ml_dtypes
from concourse.bass_utils import run_bass_kernel_spmd
from contextlib import ExitStack
import concourse.bass as bass
import concourse.mybir as mybir

F32 = mybir.dt.float32
BF16 = mybir.dt.bfloat16
I32 = mybir.dt.int32
ALU = mybir.AluOpType
AF = mybir.ActivationFunctionType
AX = mybir.AxisListType
_DSZ = {F32: 4, BF16: 2, I32: 4, mybir.dt.uint32: 4, mybir.dt.float16: 2, mybir.dt.uint8: 1,
        mybir.dt.int8: 1, mybir.dt.uint16: 2, mybir.dt.int16: 2}


def region(ap):
    dsz = _DSZ[ap.dtype]
    dims = ap.ap
    off = ap.offset
    sp = str(ap.space)
    if sp in ("SB", "PSUM"):
        shp = ap.tensor.shape
        rowlen = 1
        for s in list(shp)[1:]:
            rowlen *= s
        p0 = off // rowlen
        c0 = off % rowlen
        pstep, pcnt = dims[0]
        p1 = p0 + ((pcnt - 1) * pstep) // rowlen + 1 if pstep else p0 + 1
        ext = 0
        for st, cn in dims[1:]:
            ext += (cn - 1) * abs(st)
        if sp == "PSUM":
            return (sp + ":" + ap.tensor.name, 0, 128, 0, 1 << 20)
        return (sp + ":" + ap.tensor.name, p0, p1, c0 * dsz, (c0 + ext + 1) * dsz)
    else:
        ext = 0
        for st, cn in dims:
            ext += (cn - 1) * abs(st)
        return ("D:" + ap.tensor.name, 0, 1, off * dsz, (off + ext + 1) * dsz)


class Op:
    __slots__ = ("eng", "fn", "reads", "writes", "dma", "deps", "sig", "dsem", "dval",
                 "waits", "idx", "prewait")


class Prog:
    ENGS = ("pe", "act", "dve", "pool", "sp")
    EPOCH = 30000

    def __init__(self, nc, ndma_sems=56):
        self.nc = nc
        self.ops = []
        self.recs = {}
        self.ndma = ndma_sems
        self.extra_regions = {}

    def reg(self, e, val):
        d = self.__dict__.setdefault("_regs", {})
        if val not in d:
            d[val] = e.to_reg(val)
        return d[val]

    def add(self, eng, fn, reads=(), writes=(), dma=False):
        op = Op()
        op.eng = eng; op.fn = fn; op.dma = dma
        op.reads = [r if isinstance(r, tuple) else region(r) for r in reads]
        op.writes = [r if isinstance(r, tuple) else region(r) for r in writes]
        op.idx = len(self.ops)
        op.sig = None; op.dsem = None; op.dval = None; op.waits = []; op.prewait = None
        deps = set()
        for (sp, p0, p1, lo, hi) in op.reads:
            for r in self.recs.get(sp, ()):
                if r[5] and r[0] < p1 and p0 < r[1] and r[2] < hi and lo < r[3]:
                    deps.add(r[4])
        for (sp, p0, p1, lo, hi) in op.writes:
            lst = self.recs.get(sp)
            if lst is None:
                lst = self.recs[sp] = []
            keep = []
            for r in lst:
                if r[0] < p1 and p0 < r[1] and r[2] < hi and lo < r[3]:
                    deps.add(r[4])
                    if r[0] >= p0 and r[1] <= p1 and r[2] >= lo and r[3] <= hi:
                        continue
                keep.append(r)
            keep.append([p0, p1, lo, hi, op.idx, True])
            self.recs[sp] = keep
        for (sp, p0, p1, lo, hi) in op.reads:
            lst = self.recs.get(sp)
            if lst is None:
                lst = self.recs[sp] = []
            if not dma:
                for r in lst:
                    if (not r[5]) and r[0] == p0 and r[1] == p1 and r[2] == lo and r[3] == hi \
                            and (not self.ops[r[4]].dma) and self.ops[r[4]].eng == eng:
                        r[4] = op.idx
                        break
                else:
                    lst.append([p0, p1, lo, hi, op.idx, False])
            else:
                lst.append([p0, p1, lo, hi, op.idx, False])
        deps.discard(op.idx)
        op.deps = deps
        self.ops.append(op)
        return op

    def matmul(self, out, lhsT, rhs, start=True, stop=True, **kw):
        return self.add("pe", lambda e: e.matmul(out, lhsT, rhs, start=start, stop=stop, **kw),
                        reads=[lhsT, rhs], writes=[out])

    def transpose(self, out, in_, ident):
        return self.add("pe", lambda e: e.transpose(out, in_, ident), reads=[in_, ident], writes=[out])

    def activation(self, out, in_, func, bias=None, scale=None, accum_out=None, eng="act"):
        reads = [in_]
        kw = {}
        if bias is not None:
            kw["bias"] = bias
            if not isinstance(bias, (int, float)):
                reads.append(bias)
        if scale is not None:
            kw["scale"] = scale
            if not isinstance(scale, (int, float)):
                reads.append(scale)
        writes = [out]
        if accum_out is not None:
            kw["accum_out"] = accum_out
            writes.append(accum_out)
        return self.add(eng, lambda e: e.activation(out, in_, func, **kw), reads=reads, writes=writes)

    def tensor_scalar(self, out, in0, s1, s2, op0, op1=None, eng="dve", accum_out=None):
        reads = [in0]
        for s in (s1, s2):
            if s is not None and not isinstance(s, (int, float)):
                reads.append(s)
        kw = {}
        if op1 is not None:
            kw["op1"] = op1
        writes = [out]
        if accum_out is not None:
            kw["accum_out"] = accum_out
            writes.append(accum_out)
        return self.add(eng, lambda e: e.tensor_scalar(out, in0, s1, s2, op0, **kw), reads=reads, writes=writes)

    def tensor_tensor(self, out, in0, in1, op, eng="dve"):
        return self.add(eng, lambda e: e.tensor_tensor(out, in0, in1, op), reads=[in0, in1], writes=[out])

    def stt(self, out, in0, scalar, in1, op0, op1, eng="dve", accum_out=None):
        reads = [in0, in1]
        if not isinstance(scalar, (int, float)):
            reads.append(scalar)
        kw = {}
        writes = [out]
        if accum_out is not None:
            kw["accum_out"] = accum_out
            writes.append(accum_out)
        return self.add(eng, lambda e: e.scalar_tensor_tensor(out, in0, scalar, in1, op0, op1, **kw),
                        reads=reads, writes=writes)

    def copy(self, out, in_, eng="dve"):
        if eng == "act":
            return self.add(eng, lambda e: e.copy(out, in_), reads=[in_], writes=[out])
        return self.add(eng, lambda e: e.tensor_copy(out, in_), reads=[in_], writes=[out])

    def memset(self, out, val, eng="dve"):
        return self.add(eng, lambda e: e.memset(out, val), reads=[], writes=[out])

    def reduce(self, out, in_, op, axis=None, eng="dve"):
        ax = axis if axis is not None else AX.X
        return self.add(eng, lambda e: e.tensor_reduce(out, in_, ax, op), reads=[in_], writes=[out])

    def dma(self, out, in_, q="sp", **kw):
        return self.add(q, lambda e: e.dma_start(out=out, in_=in_, **kw), reads=[in_], writes=[out], dma=True)

    def finalize(self):
        ops = self.ops
        need = [False] * len(ops)
        for op in ops:
            for d in op.deps:
                p = ops[d]
                if p.dma:
                    continue
                if p.eng == op.eng and not op.dma:
                    if p.eng == "pe":
                        continue
                    continue_flag = True
                    for (sp, p0, p1, lo, hi) in op.reads:
                        for (sp2, q0, q1, lo2, hi2) in p.writes:
                            if sp == sp2 and q0 < p1 and p0 < q1 and lo2 < hi and lo < hi2:
                                continue_flag = False
                    if continue_flag:
                        continue
                need[d] = True
        cnt = {e: 0 for e in self.ENGS}
        for op in ops:
            if (not op.dma) and need[op.idx]:
                cnt[op.eng] += 1
                op.sig = cnt[op.eng]
        dcount = [0] * self.ndma
        k = 0
        for op in ops:
            if op.dma:
                s = k % self.ndma
                k += 1
                dcount[s] += 1
                op.dsem = s
                op.dval = 16 * dcount[s]
                op.prewait = (("dma", s), 16 * (dcount[s] - 1)) if dcount[s] > 1 else None
        self.dfinal = [16 * c for c in dcount]
        seen = {e: {} for e in self.ENGS}
        pos = {e: 0 for e in self.ENGS}
        opos = {}
        for op in ops:
            pos[op.eng] += 1
            opos[op.idx] = pos[op.eng]
        nw = 0
        for op in ops:
            sn = seen[op.eng]
            w = {}
            if op.prewait is not None:
                key, val = op.prewait
                if sn.get(key, 0) < val:
                    w[key] = val
            for d in op.deps:
                p = ops[d]
                if p.dma:
                    key, val = ("dma", p.dsem), p.dval
                else:
                    if p.sig is None:
                        continue
                    if p.eng == op.eng and not op.dma:
                        if p.eng == "pe":
                            continue
                        if opos[op.idx] - opos[p.idx] > 3:
                            continue
                    ep = (p.sig - 1) // self.EPOCH
                    key, val = ("eng", p.eng), ep * 100000 + (p.sig - 1) % self.EPOCH + 1
                if sn.get(key, 0) < val and w.get(key, 0) < val:
                    w[key] = val
            for key, val in w.items():
                sn[key] = val
            op.waits = list(w.items())
            nw += len(op.waits)
        self.n_waits = nw
        self.n_epochs = {e: (cnt[e] - 1) // self.EPOCH + 1 if cnt[e] else 1 for e in self.ENGS}
        return cnt

    def emit(self):
        nc = self.nc
        cnt = self.finalize()
        ops = self.ops
        with ExitStack() as st:
            esem = {}
            for e in self.ENGS:
                for ep in range(self.n_epochs[e]):
                    esem[(e, ep)] = st.enter_context(nc.semaphore("s_%s%d" % (e, ep)))
            dsem = [st.enter_context(nc.semaphore("d%d" % i)) for i in range(self.ndma)]
            block = st.enter_context(nc.Block())
            by_eng = {e: [op for op in ops if op.eng == e] for e in self.ENGS}
            EP = self.EPOCH

            def run(eng_obj, lst, is_sp):
                for op in lst:
                    for key, val in op.waits:
                        if key[0] == "dma":
                            eng_obj.wait_ge(dsem[key[1]], val)
                        else:
                            ep, v = divmod(val, 100000)
                            eng_obj.wait_ge(esem[(key[1], ep)], v)
                    inst = op.fn(eng_obj)
                    if op.dma:
                        inst.then_inc(dsem[op.dsem], 16)
                    elif op.sig is not None:
                        ep = (op.sig - 1) // EP
                        inst.then_inc(esem[(op.eng, ep)], 1)
                if is_sp:
                    for i, v in enumerate(self.dfinal):
                        if v:
                            eng_obj.wait_ge(dsem[i], v)

            @block.sync
            def _(e):
                run(e, by_eng["sp"], True)

            @block.tensor
            def _(e):
                run(e, by_eng["pe"], False)

            @block.scalar
            def _(e):
                run(e, by_eng["act"], False)

            @block.vector
            def _(e):
                run(e, by_eng["dve"], False)

            @block.gpsimd
            def _(e):
                run(e, by_eng["pool"], False)
        return cnt

S = 2048
D = 1024
NT = S // 128
TS = 512
NTILES = 16384 // TS + 32
GT = TS // 128
NSLOT = NTILES * TS
GM, GQ, GK, GA, BG, BD, WDW = 0, 8, 16, 24, 28, 36, 40
NV = 40 + 4 * 31
RG, RF, RLG, RLB, RBF, RBR, RBDW = 0, 1024, 2048, 2560, 3072, 3200, 3236
NR = 3748
THR, PCOL, CBASE, IOTA, CEPS, NCF = 0, 96, 97, 101, 133, 141
IDB, NEGM, UST, ONEB, LTRIB, NCB = 0, 128, 256, 384, 512, 640


def _prod(s):
    r = 1
    for a in s:
        r *= a
    return r


class Arena:
    def __init__(self, nc, st, name, nbytes):
        self.t = st.enter_context(nc.sbuf_tensor(name, [128, nbytes // 4], F32))
        self.cap = nbytes
        self.off = 0

    def alloc(self, nbytes):
        off = (self.off + 63) // 64 * 64
        self.off = off + nbytes
        assert self.off <= self.cap, ("arena overflow", self.off, self.cap)
        return off

    def view(self, off, dtype, shape):
        n = _prod(shape)
        dsz = _DSZ[dtype]
        nb = (n * dsz + 3) // 4
        v = self.t[:, off // 4: off // 4 + nb]
        if dtype != F32:
            v = v.bitcast(dtype)
        v = v[:, 0:n]
        if len(shape) == 2:
            v = v.rearrange("p (a b) -> p a b", a=shape[0], b=shape[1])
        elif len(shape) == 3:
            v = v.rearrange("p (a b c) -> p a b c", a=shape[0], b=shape[1], c=shape[2])
        return v

    def new(self, dtype, shape):
        off = self.alloc(_prod(shape) * _DSZ[dtype])
        return self.view(off, dtype, shape)


import os as _os
BP = set(_os.environ.get('BPARTS', 'conv,convT,qk,mask,exp,pv,norm,tail,catT').split(','))


def build(nseq=4, debug=(), stop_after=None, n_moe_tiles=NTILES, skip_moe=False):
    nc = bass.Bass("TRN2", target_bir_lowering=False)
    NTOK = nseq * S
    NTT = NTOK // 128

    def din(name, shape, dt=F32):
        return nc.dram_tensor(name, shape, dt, kind="ExternalInput").ap()

    def scr(name, shape, dt):
        if name in debug:
            return nc.dram_tensor(name, shape, dt, kind="ExternalOutput").ap()
        return nc.dram_tensor(name, shape, dt).ap()

    x = din("x", [NTOK, D])
    mem = din("mem", [nseq * 256, D])
    w_in = din("w_in", [D, 2568])
    w_out = din("w_out", [D, D])
    w_mq = din("w_mq", [D, D])
    w_mkv = din("w_mkv", [D, 2 * D])
    w_mo = din("w_mo", [D, D])
    w_route = din("w_route", [128, 8 * 36])
    w_gate = din("w_gate", [32 * 128, 4096])
    w_up = din("w_up", [32 * 128, 4096])
    w_down = din("w_down", [32 * 512, 1024])
    vecs = din("vecs", [128, NV])
    rows = din("rows", [128, NR])
    constf = din("constf", [128, NCF])
    constb = din("constb", [128, NCB], BF16)
    consti = din("consti", [128, 64], I32)
    out = nc.dram_tensor("out", [NTOK, D], F32, kind="ExternalOutput").ap()

    w_in_bf = scr("w_in_bf", [D, 2568], BF16)
    w_out_bf = scr("w_out_bf", [D, D], BF16)
    w_mq_bf = scr("w_mq_bf", [D, D], BF16)
    w_mkv_bf = scr("w_mkv_bf", [D, 2 * D], BF16)
    w_mo_bf = scr("w_mo_bf", [D, D], BF16)
    x2s = scr("x2s", [NTOK, D], F32)
    h3s = scr("h3s", [NTOK, D], BF16)
    Ysc = scr("Ysc", [NSLOT, D], BF16)
    s2t = scr("s2t", [NSLOT, 1], I32)
    dbg = {}
    for name, shape, dt in (("d_cat", [S, D], BF16), ("d_x1", [S, D], F32), ("d_lg", [NTOK, 36], F32),
                            ("d_q", [128, 4 * 2048], BF16), ("d_attn", [S, 512], F32), ("d_z", [128, 4 * 2080], BF16),
                            ("d_B", [128, 2048], F32), ("d_rt", [128, 64 * 8], F32), ("d_te", [128, 96], F32),
                            ("d_conv", [S, 512], F32)):
        if name in debug:
            dbg[name] = nc.dram_tensor(name, shape, dt, kind="ExternalOutput").ap()

    P = Prog(nc)
    st = ExitStack()
    A = Arena(nc, st, "arena", 210944)
    PS = [st.enter_context(nc.psum_tensor("ps%d" % i, [128, 512], F32)) for i in range(8)]

    def psf(i):
        return PS[i][:, :]

    def psb(i):
        return PS[i][:, :].bitcast(BF16)

    cf = A.new(F32, [NCF])
    cb = A.new(BF16, [NCB])
    ci = A.new(I32, [64])
    vv = A.new(F32, [NV])
    rA = A.new(F32, [1700])
    LG_, LB_, BF_, BR_, BDW_ = 0, 512, 1024, 1152, 1188
    LG = A.new(F32, [64, 36])
    G1 = A.new(F32, [64]); G2 = A.new(F32, [64]); P1 = A.new(F32, [64]); P2 = A.new(F32, [64])
    POS1 = A.new(I32, [64]); POS2 = A.new(I32, [64])
    base = A.new(F32, [32])
    wr_bf = A.new(BF16, [8, 36])
    v_ = None
    identb = cb[:, IDB:IDB + 128]
    negm = cb[:, NEGM:NEGM + 128]
    eps1024 = cf[:, CEPS:CEPS + 1]
    eps512 = cf[:, CEPS + 1:CEPS + 2]
    one_c = cf[:, CEPS + 2:CEPS + 3]
    epsln = cf[:, CEPS + 3:CEPS + 4]
    P.dma(cf, constf); P.dma(cb, constb); P.dma(ci, consti); P.dma(vv, vecs)
    P.dma(rA, rows[:, RLG:RLG + 1700])
    P.memset(base, 0.0)
    P.tensor_scalar(vv[:, GM:GM + 24], vv[:, GM:GM + 24], 32.0, None, ALU.mult)
    P.tensor_scalar(vv[:, GA:GA + 4], vv[:, GA:GA + 4], float(np.sqrt(512.0)), None, ALU.mult)
    mark_persist = A.off

    Wreg_off = A.alloc(49152)
    Win = A.view(Wreg_off, BF16, [8, 2568])
    Woqm = A.view(Wreg_off, BF16, [3, 8, 1024])
    xT = A.new(BF16, [8, 2048])
    K2T = A.new(BF16, [8, 256])
    V2 = A.new(BF16, [2, 4, 257])
    Bt = A.new(F32, [8, 16, 16])
    nlf = A.new(F32, [16, 8]); incl = A.new(F32, [16, 8]); Cc = A.new(F32, [16, 8]); Cend = A.new(F32, [16, 8])
    U_off = A.alloc(66048 + 64)
    qT = A.view(U_off, BF16, [4, 2048])
    kT = A.view(U_off + 16384, BF16, [4, 2048])
    Wkv = A.view(U_off, BF16, [8, 2048])
    vS = A.view(U_off + 32768, BF16, [16, 8, 65])
    zT = A.view(U_off + 32768 + 16640, BF16, [4, 2080])
    memT = A.view(U_off + 32768 + 16640, BF16, [8, 256])
    T_off = A.alloc(23040)
    xin = [A.view(T_off + i * 4096, F32, [1024]) for i in range(2)]
    xn = [A.view(T_off + 8192 + i * 2048, BF16, [1024]) for i in range(2)]
    sig = [A.view(T_off + 12288 + i * 2048, F32, [512]) for i in range(2)]
    stA = A.view(T_off + 16384, F32, [16])
    PT = [A.view(T_off + i * 256, BF16, [128]) for i in range(6)]
    attn = A.view(T_off + 1536, F32, [512])
    catA = A.view(T_off + 3584, BF16, [512])
    catC = [A.view(T_off + 4608 + i * 1024, BF16, [512]) for i in range(2)]
    cz = [A.view(T_off + 6656 + i * 2048, F32, [512]) for i in range(2)]
    stB = A.view(T_off + 10752, F32, [64])
    Dg = A.view(T_off + 11008, BF16, [31, 128])
    zoff = U_off + 32768 + 16640
    cvo = [A.view(T_off + 18944, BF16, [16, 128])] + [A.view(zoff + i * 4160, BF16, [16, 128]) for i in range(3)]
    o_ = U_off
    x1 = A.view(o_, F32, [2, 1024]); o_ += 8192
    xr = [A.view(o_ + i * 4096, F32, [1024]) for i in range(2)]; o_ += 8192
    xn2 = [A.view(o_ + i * 2048, BF16, [1024]) for i in range(2)]; o_ += 4096
    h2T = A.view(o_, BF16, [8, 256]); o_ += 4096
    q2T = A.view(o_, BF16, [8, 256]); o_ += 4096
    P2T = [A.view(o_ + i * 512, BF16, [256]) for i in range(8)]; o_ += 4096
    oo = A.view(o_, BF16, [2, 1024]); o_ += 4096
    oT = A.view(o_, BF16, [8, 256]); o_ += 4096
    x2t = [A.view(o_ + i * 4096, F32, [1024]) for i in range(2)]; o_ += 8192
    h3t = [A.view(o_ + i * 2048, BF16, [1024]) for i in range(2)]; o_ += 4096
    h3T = A.view(o_, BF16, [8, 128]); o_ += 2048
    rG = A.view(o_, F32, [1024]); o_ += 4096
    rt = A.view(o_, F32, [256]); o_ += 1024
    rtb = A.view(o_, BF16, [64]); o_ += 128
    stC = A.view(o_, F32, [32]); o_ += 128
    assert o_ <= U_off + 66048
    x1b = [x1, A.view(T_off, F32, [2, 1024])]
    h2Tb = [h2T, A.view(T_off + 8192, BF16, [8, 256])]
    q2Tb = [q2T, A.view(T_off + 12288, BF16, [8, 256])]

    st0 = [A.view(U_off + i * 15488, F32, [2568]) for i in range(4)]
    st0b = [A.view(U_off + 10304 + i * 15488, BF16, [2568]) for i in range(4)]
    jobs = []
    for (src, dst, ncol, gcol, nsc) in ((w_mkv, w_mkv_bf, 2048, GK, 8), (w_in, w_in_bf, 2568, GM, 8),
                                       (w_out, w_out_bf, 1024, GA, 4), (w_mq, w_mq_bf, 1024, GQ, 8),
                                       (w_mo, w_mo_bf, 1024, None, 0)):
        for c in range(8):
            jobs.append((src, dst, ncol, (gcol + c) if c < nsc else None, c))

    def p0_load(k):
        src, dst, ncol, g_, c = jobs[k]
        P.dma(st0[k % 4][:, 0:ncol], src[c * 128:(c + 1) * 128, :])

    for k in range(3):
        p0_load(k)
    for k in range(len(jobs)):
        if k + 3 < len(jobs):
            p0_load(k + 3)
        src, dst, ncol, g_, c = jobs[k]
        sb_, sbb = st0[k % 4], st0b[k % 4]
        if g_ is not None:
            if k % 2 == 0:
                P.tensor_scalar(sbb[:, 0:ncol], sb_[:, 0:ncol], vv[:, g_:g_ + 1], None, ALU.mult)
            else:
                P.activation(sbb[:, 0:ncol], sb_[:, 0:ncol], AF.Copy, scale=vv[:, g_:g_ + 1])
        else:
            P.copy(sbb[:, 0:ncol], sb_[:, 0:ncol], eng="dve" if k % 2 == 0 else "act")
        P.dma(dst[c * 128:(c + 1) * 128, :], sbb[:, 0:ncol])
    P.dma(st0[0][:, 0:288], w_route)
    P.copy(wr_bf, st0[0][:, 0:288].rearrange("p (c n) -> p c n", c=8))

    if stop_after == "phase0":
        P.dma(out[0:128, :], st0[0][:, 0:1024])
        P.emit()
        return nc
    bank = [0]

    def nb():
        bank[0] = (bank[0] + 1) % 8
        return bank[0]

    def rstd_from_ss(dst, ss, epsc):
        P.activation(dst, ss, AF.Ln, bias=epsc, scale=1.0)
        P.activation(dst, dst, AF.Exp, scale=-0.5)


    def recip(o, i, eng="dve"):
        P.add(eng, lambda e: e.reciprocal(o, i), reads=[i], writes=[o])

    def route_tile(gi, pl, tt):
        lg = rt[:, 0:36]
        P.tensor_tensor(lg, pl, rA[:, BR_:BR_ + 36], ALU.add)
        if "d_lg" in dbg:
            P.dma(dbg["d_lg"][gi * 128:(gi + 1) * 128, :], lg)
        gmax, ngmax, gsum, gtop = rt[:, 40:41], rt[:, 41:42], rt[:, 42:43], rt[:, 43:44]
        ohg = rt[:, 44:48]
        P.reduce(gmax, lg[:, 0:4], ALU.max)
        P.tensor_scalar(ohg, lg[:, 0:4], gmax, None, ALU.is_equal)
        P.tensor_scalar(ngmax, gmax, -1.0, None, ALU.mult)
        P.activation(rt[:, 48:52], lg[:, 0:4], AF.Exp, bias=ngmax, scale=1.0, accum_out=gsum)
        recip(gtop, gsum)
        esel = rt[:, 52:60]
        P.tensor_scalar(esel, lg[:, 4:12], ohg[:, 0:1], None, ALU.mult)
        for g in range(1, 4):
            P.stt(esel, lg[:, 4 + 8 * g:12 + 8 * g], ohg[:, g:g + 1], esel, ALU.mult, ALU.add)
        top8 = rt[:, 60:68]
        P.add("dve", lambda e: e.max(top8, esel), reads=[esel], writes=[top8])
        oh1, oh2 = rt[:, 68:76], rt[:, 76:84]
        P.tensor_scalar(oh1, esel, top8[:, 0:1], None, ALU.is_equal)
        P.tensor_scalar(oh2, esel, top8[:, 1:2], None, ALU.is_equal)
        dd, w1 = rt[:, 84:85], rt[:, 85:86]
        P.tensor_tensor(dd, top8[:, 1:2], top8[:, 0:1], ALU.subtract)
        P.activation(dd, dd, AF.Exp)
        P.tensor_scalar(dd, dd, 1.0, None, ALU.add)
        recip(w1, dd)
        P.tensor_tensor(G1[:, gi:gi + 1], gtop, w1, ALU.mult)
        P.tensor_tensor(G2[:, gi:gi + 1], gtop, G1[:, gi:gi + 1], ALU.subtract)
        a1, a2 = rt[:, 96:128], rt[:, 128:160]
        for a_, oh in ((a1, oh1), (a2, oh2)):
            P.tensor_tensor(a_.rearrange("p (g e) -> p g e", g=4), ohg.unsqueeze(2).to_broadcast([128, 4, 8]),
                            oh.unsqueeze(1).to_broadcast([128, 4, 8]), ALU.mult)
        P.copy(A1[:, gi, :], a1)
        P.copy(A2[:, gi, :], a2)
        cmb = rtb[:, 0:32]
        P.tensor_tensor(cmb, a1, a2, ALU.add)
        pr = psf(7)[:, 384 + 64 * tt:448 + 64 * tt]
        P.matmul(pr[:, 0:32], cb[:, UST:UST + 128], cmb)
        P.matmul(pr[:, 32:64], cb[:, ONEB:ONEB + 128], cmb)
        rk, jk = rt[:, 160:192], rt[:, 192:224]
        P.tensor_tensor(rk, pr[:, 0:32], base, ALU.add)
        P.tensor_tensor(jk, a1, rk, ALU.mult)
        P.reduce(P1[:, gi:gi + 1], jk, ALU.add)
        P.tensor_tensor(jk, a2, rk, ALU.mult)
        P.reduce(P2[:, gi:gi + 1], jk, ALU.add)
        P.tensor_tensor(base, base, pr[:, 32:64], ALU.add)

    def sumsq(junk, src, ss):
        P.activation(junk, src, AF.Square, accum_out=ss)

    def transposeT(src_bf, dstT, col0, ev, b=None):
        if b is None:
            b = 6 + (nb() % 2)
        pv = psb(b)
        for c in range(8):
            P.transpose(pv[:, c * 128:(c + 1) * 128], src_bf[:, c * 128:(c + 1) * 128], identb)
        P.copy(dstT[:, :, col0:col0 + 128], pv.rearrange("p (c t) -> p c t", c=8), eng=ev)


    for b in range(nseq):
        tok0 = b * S
        for c in range(8):
            P.dma(Wkv[:, c, :], w_mkv_bf[c * 128:(c + 1) * 128, :])
        for mt in range(2):
            xi, xb = xin[mt], xn[mt]
            P.dma(xi, mem[b * 256 + mt * 128: b * 256 + (mt + 1) * 128, :])
            ss, rs = stA[:, 2 * mt:2 * mt + 1], stA[:, 2 * mt + 1:2 * mt + 2]
            sumsq(xb, xi, ss)
            rstd_from_ss(rs, ss, eps1024)
            P.tensor_scalar(xb, xi, rs, None, ALU.mult)
            transposeT(xb, memT, mt * 128, "dve")
        for cc in range(8):
            bk = nb() % 6
            pv = psf(bk)[:, 0:256]
            for c in range(8):
                P.matmul(pv, Wkv[:, c, cc * 128:(cc + 1) * 128], memT[:, c, :], start=(c == 0), stop=(c == 7))
            P.copy(K2T[:, cc, :], pv, eng="act" if cc % 2 else "dve")
        for mt in range(2):
            for nh in range(2):
                bk = nb() % 6
                pv = psf(bk)
                for c in range(8):
                    P.matmul(pv, memT[:, c, mt * 128:(mt + 1) * 128], Wkv[:, c, 1024 + nh * 512:1024 + (nh + 1) * 512],
                             start=(c == 0), stop=(c == 7))
                P.copy(V2[:, mt, 2 * nh:2 * nh + 2, 0:256], pv.rearrange("p (h d) -> p h d", h=2),
                       eng="act" if nh % 2 else "dve")
        P.memset(V2[:, :, :, 256:257], 1.0)
        if stop_after == "A0":
            P.dma(out[0:128, :], xin[0])
            P.emit()
            return nc
        for c in range(8):
            P.dma(Win[:, c, :], w_in_bf[c * 128:(c + 1) * 128, :])
        for i in range(NT):
            xi, xb = xin[i % 2], xn[i % 2]
            P.dma(xi, x[tok0 + i * 128: tok0 + (i + 1) * 128, :])
            ss, rs = stA[:, 4 + 2 * (i % 2):5 + 2 * (i % 2)], stA[:, 5 + 2 * (i % 2):6 + 2 * (i % 2)]
            sumsq(xb, xi, ss)
            rstd_from_ss(rs, ss, eps1024)
            P.tensor_scalar(xb, xi, rs, None, ALU.mult)
            transposeT(xb, xT, i * 128, "act" if i % 2 else "dve")
        if stop_after == "A1":
            P.dma(out[0:128, :], xin[0])
            P.emit()
            return nc
        P.memset(zT[:, :, 0:30], 0.0)
        P.memset(zT[:, :, 2078:2080], 0.0)
        P.memset(vS[:, :, :, 64:65], 1.0)
        OFF_K, OFF_V, OFF_F, OFF_C = 512, 1024, 1536, 1544
        for tb in range(4):
            tsl = slice(tb * 512, (tb + 1) * 512)
            for cc in range(8):
                bk = nb() % 6
                pv = psf(bk)
                for c in range(8):
                    P.matmul(pv, Win[:, c, cc * 128:(cc + 1) * 128], xT[:, c, tsl], start=(c == 0), stop=(c == 7))
                dst = qT[:, cc, tsl] if cc < 4 else kT[:, cc - 4, tsl]
                P.copy(dst, pv, eng="act" if cc % 2 else "dve")
            for i4 in range(4):
                bg = nb() % 6
                pg = psf(bg)
                for c in range(8):
                    P.matmul(pg, Win[:, c, OFF_C + 512 + i4 * 128:OFF_C + 512 + (i4 + 1) * 128], xT[:, c, tsl],
                             start=(c == 0), stop=(c == 7))
                sg = sig[i4 % 2]
                P.activation(sg, pg, AF.Sigmoid, bias=vv[:, BG + 4 + i4:BG + 5 + i4], scale=1.0)
                ba = nb() % 6
                pa = psf(ba)
                for c in range(8):
                    P.matmul(pa, Win[:, c, OFF_C + i4 * 128:OFF_C + (i4 + 1) * 128], xT[:, c, tsl],
                             start=(c == 0), stop=(c == 7))
                P.stt(zT[:, i4, 30 + tb * 512:30 + (tb + 1) * 512], pa, vv[:, BG + i4:BG + i4 + 1], sg, ALU.add, ALU.mult)
        pf_ = psf(6)
        for i in range(NT):
            bk = nb() % 6
            pv = psf(bk)
            for c in range(8):
                P.matmul(pv, xT[:, c, i * 128:(i + 1) * 128], Win[:, c, OFF_V:OFF_V + 512], start=(c == 0), stop=(c == 7))
            P.copy(vS[:, i, :, 0:64], pv.rearrange("p (h d) -> p h d", h=8), eng="act" if i % 2 else "dve")
            for c in range(8):
                P.matmul(pf_[:, i * 8:(i + 1) * 8], xT[:, c, i * 128:(i + 1) * 128], Win[:, c, OFF_F:OFF_F + 8],
                         start=(c == 0), stop=(c == 7))
        if stop_after == "A2":
            P.dma(out[0:128, :], xin[0])
            P.emit()
            return nc
        nlf2 = nlf.rearrange("p a b -> p (a b)")
        P.tensor_tensor(nlf2, pf_[:, 0:128], rA[:, BF_:BF_ + 128], ALU.add)
        P.activation(nlf2, nlf2, AF.Exp, scale=-1.0)
        P.activation(nlf2, nlf2, AF.Ln, bias=one_c, scale=1.0)
        if stop_after == "A3a":
            P.dma(out[0:128, :], xin[0])
            P.emit()
            return nc
        P.copy(incl[:, 0, :], nlf[:, 0, :])
        for i in range(1, NT):
            P.tensor_tensor(incl[:, i, :], incl[:, i - 1, :], nlf[:, i, :], ALU.add)
        if stop_after == "A3b":
            P.dma(out[0:128, :], xin[0])
            P.emit()
            return nc
        pc1 = psf(7)[:, 0:128]
        pc2 = psf(7)[:, 128:256]
        for (pdst, lmat, srcf) in ((pc1, cb[:, ONEB:ONEB + 128], incl.rearrange("p a b -> p (a b)")),
                                   (pc2, cb[:, LTRIB:LTRIB + 128], nlf2)):
            rres = cz[0][:, 0:128]
            for t3 in range(3):
                piece = xn[0][:, t3 * 128:(t3 + 1) * 128]
                P.copy(piece, srcf if t3 == 0 else rres)
                if t3 < 2:
                    P.tensor_tensor(rres, srcf if t3 == 0 else rres, piece, ALU.subtract)
                P.matmul(pdst, lmat, piece, start=(t3 == 0), stop=(t3 == 2))
        P.copy(Cend.rearrange("p a b -> p (a b)"), pc1)
        P.copy(Cc.rearrange("p a b -> p (a b)"), pc2)
        if stop_after == "A3c":
            P.dma(out[0:128, :], xin[0])
            P.emit()
            return nc
        P.tensor_tensor(Cc[:, 1:16, :], Cc[:, 1:16, :], Cend[:, 0:15, :], ALU.add)
        if stop_after == "A3d":
            P.dma(out[0:128, :], xin[0])
            P.emit()
            return nc
        for h in range(8):
            P.tensor_tensor(Bt[:, h, :, :], Cc[:, :, h:h + 1].to_broadcast([128, 16, 16]),
                            Cend[:, :, h].unsqueeze(1).to_broadcast([128, 16, 16]), ALU.subtract)
        if "d_q" in dbg:
            P.dma(dbg["d_q"], qT.rearrange("p a b -> p (a b)"))
        if "d_z" in dbg:
            P.dma(dbg["d_z"], zT.rearrange("p a b -> p (a b)"))
        if "d_B" in dbg:
            P.dma(dbg["d_B"], Bt.rearrange("p a b c -> p (a b c)"))
        if stop_after == "A3":
            P.dma(out[0:128, :], xin[0])
            P.emit()
            return nc
        if 'conv' in BP:
            for i4 in range(4):
                for kk in range(31):
                    P.tensor_scalar(Dg[:, kk, :], identb, vv[:, WDW + i4 * 31 + kk:WDW + i4 * 31 + kk + 1], None, ALU.mult)
                for j4 in range(4):
                    pcv = psf(nb() % 6)
                    for t4 in range(4):
                        c0 = (j4 * 4 + t4) * 128
                        for kk in range(31):
                            P.matmul(pcv[:, t4 * 128:(t4 + 1) * 128], zT[:, i4, c0 + kk:c0 + kk + 128], Dg[:, kk, :],
                                     start=(kk == 0), stop=(kk == 30))
                    P.tensor_tensor(cvo[i4][:, j4 * 4:(j4 + 1) * 4, :], pcv.rearrange("p (t c) -> p t c", t=4),
                                    rA[:, BDW_ + i4 * 128:BDW_ + (i4 + 1) * 128].unsqueeze(1).to_broadcast([128, 4, 128]),
                                    ALU.add)
        for wi, wsrc in enumerate((w_out_bf, w_mq_bf, w_mo_bf)):
            for c in range(8):
                P.dma(Woqm[:, wi, c, :], wsrc[c * 128:(c + 1) * 128, :])

        catT = xT
        LAG = 3

        def tail_conv(j):
            s1, nm, sq, rsd = stB[:, 10:11], stB[:, 11:12], stB[:, 12:13], stB[:, 13:14]
            xc, tmp = cz[0], cz[1]
            for i4 in range(4):
                P.copy(xc[:, i4 * 128:(i4 + 1) * 128], cvo[i4][:, j, :], eng="pool" if i4 % 2 else "dve")
            if "d_conv" in dbg:
                P.dma(dbg["d_conv"][j * 128:(j + 1) * 128, :], xc)
            P.reduce(s1, xc, ALU.add)
            P.tensor_scalar(nm, s1, -1.0 / 512.0, None, ALU.mult)
            P.tensor_scalar(xc, xc, nm, None, ALU.add)
            sumsq(tmp, xc, sq)
            P.activation(rsd, sq, AF.Ln, bias=epsln, scale=1.0 / 512.0)
            P.activation(rsd, rsd, AF.Exp, scale=-0.5)
            P.stt(xc, xc, rsd, rA[:, LG_:LG_ + 512], ALU.mult, ALU.mult)
            P.tensor_tensor(xc, xc, rA[:, LB_:LB_ + 512], ALU.add)
            P.activation(tmp, xc, AF.Exp, scale=-1.0)
            P.tensor_scalar(tmp, tmp, 1.0, None, ALU.add)
            P.add("dve", lambda e, o=tmp: e.reciprocal(o, o), reads=[tmp], writes=[tmp])
            P.tensor_tensor(catC[j % 2], xc, tmp, ALU.mult)

        def norm_attn(j, accb):
            rden = stB[:, 0:8]
            for hh in range(2):
                av = accb[hh][:, 0:260].rearrange("p (h d) -> p h d", h=4)
                P.add("dve", lambda e, o=rden[:, hh * 4:(hh + 1) * 4], i=av[:, :, 64]: e.reciprocal(o, i),
                      reads=[av[:, :, 64]], writes=[rden[:, hh * 4:(hh + 1) * 4]])
                P.tensor_tensor(attn[:, hh * 256:(hh + 1) * 256].rearrange("p (h d) -> p h d", h=4), av[:, :, 0:64],
                                rden[:, hh * 4:(hh + 1) * 4].unsqueeze(2).to_broadcast([128, 4, 64]), ALU.mult)
            if "d_attn" in dbg:
                P.dma(dbg["d_attn"][j * 128:(j + 1) * 128, :], attn)

        def tail_attn(j):
            ssa, rsa = stB[:, 8:9], stB[:, 9:10]
            sumsq(catA, attn, ssa)
            rstd_from_ss(rsa, ssa, eps512)
            P.tensor_scalar(catA, attn, rsa, None, ALU.mult)
            pv = psb(7)
            for c in range(4):
                P.transpose(pv[:, c * 128:(c + 1) * 128], catA[:, c * 128:(c + 1) * 128], identb)
            for c in range(4):
                P.transpose(pv[:, (4 + c) * 128:(5 + c) * 128], catC[j % 2][:, c * 128:(c + 1) * 128], identb)
            if "d_cat" in dbg:
                P.dma(dbg["d_cat"][j * 128:(j + 1) * 128, 0:512], catA)
                P.dma(dbg["d_cat"][j * 128:(j + 1) * 128, 512:1024], catC[j % 2])
            P.copy(catT[:, :, j * 128:(j + 1) * 128], pv.rearrange("p (c t) -> p c t", c=8), eng="act" if j % 2 else "dve")

        for j in range(NT):
            if j == 0:
                tail_conv(0)
                tail_conv(1)
            accb = [psf(2), psf(3)]
            pairs = [(h, kt) for h in range(8) for kt in range(j + 1)]
            stslots = {}
            n_pairs = len(pairs)

            def emit_qk(n):
                h, kt = pairs[n]
                stv = psf((0, 1, 4, 5, 6)[n % 5])[:, 0:128]
                r0 = 0 if 'evenonly' in BP else (h % 2) * 64
                if 'qk' in BP:
                    P.matmul(stv, kT[r0:r0 + 64, h // 2, kt * 128:(kt + 1) * 128], qT[r0:r0 + 64, h // 2, j * 128:(j + 1) * 128],
                             start=True, stop=(kt != j or 'mask' not in BP))
                if kt == j and 'mask' in BP:
                    P.matmul(stv, identb, negm, start=False, stop=True)
                pt = PT[n % 6]
                if 'exp' in BP:
                    if 'expdve' in BP:
                        P.copy(pt, stv, eng='dve')
                    elif 'expcopy' in BP:
                        P.copy(pt, stv, eng='act')
                    elif 'nobias' in BP:
                        P.activation(pt, stv, AF.Exp, scale=0.125)
                    else:
                        P.activation(pt, stv, AF.Exp, bias=Bt[:, h, kt, j:j + 1], scale=0.125)

            def emit_pv(n):
                h, kt = pairs[n]
                pt = PT[n % 6]
                av = accb[h // 4][:, (h % 4) * 65:(h % 4) * 65 + 65]
                if 'pv' in BP:
                    P.matmul(av, pt, vS[:, kt, h, :], start=(kt == 0), stop=(kt == j))

            for n in range(n_pairs + LAG):
                if j > 0 and n == min(6, n_pairs + LAG - 1):
                    tail_attn(j - 1)
                    if j + 1 < NT:
                        tail_conv(j + 1)
                if n < n_pairs:
                    emit_qk(n)
                if n >= LAG:
                    emit_pv(n - LAG)
            norm_attn(j, accb)
        tail_attn(NT - 1)

        if stop_after == "B":
            P.dma(out[0:128, :], xin[0])
            P.emit()
            return nc
        P.dma(rG, rows[:, RG:RG + 1024])
        P.tensor_scalar(rG, rG, 32.0, None, ALU.mult)
        Wout, Wmq, Wmo = Woqm[:, 0], Woqm[:, 1], Woqm[:, 2]

        def c_front(blk):
            pb = blk % 2
            for tt in range(2):
                i = blk * 2 + tt
                P.dma(xr[tt], x[tok0 + i * 128: tok0 + (i + 1) * 128, :])
                for nh in range(2):
                    pv = psf(nb() % 6)
                    for c in range(8):
                        P.matmul(pv, catT[:, c, i * 128:(i + 1) * 128], Wout[:, c, nh * 512:(nh + 1) * 512],
                                 start=(c == 0), stop=(c == 7))
                    P.tensor_tensor(x1b[pb][:, tt, nh * 512:(nh + 1) * 512], pv, xr[tt][:, nh * 512:(nh + 1) * 512], ALU.add)
                if "d_x1" in dbg:
                    P.dma(dbg["d_x1"][i * 128:(i + 1) * 128, :], x1b[pb][:, tt, :])
                yield
                ss, rs = stC[:, 2 * tt:2 * tt + 1], stC[:, 2 * tt + 1:2 * tt + 2]
                sumsq(xn2[tt], x1b[pb][:, tt, :], ss)
                rstd_from_ss(rs, ss, eps1024)
                P.tensor_scalar(xn2[tt], x1b[pb][:, tt, :], rs, None, ALU.mult)
                transposeT(xn2[tt], h2Tb[pb], tt * 128, "act" if tt else "dve")
                yield
            for cc in range(8):
                pv = psf(nb() % 6)[:, 0:256]
                for c in range(8):
                    P.matmul(pv, Wmq[:, c, cc * 128:(cc + 1) * 128], h2Tb[pb][:, c, :], start=(c == 0), stop=(c == 7))
                P.copy(q2Tb[pb][:, cc, :], pv, eng="act" if cc % 2 else "dve")
                if cc % 2:
                    yield

        def c_back(blk):
            pb = blk % 2

            def scores(h):
                for mt in range(2):
                    pv = psf(nb() % 6)[:, 0:256]
                    for k2 in range(2):
                        P.matmul(pv, K2T[:, 2 * h + k2, mt * 128:(mt + 1) * 128], q2Tb[pb][:, 2 * h + k2, :],
                                 start=(k2 == 0), stop=(k2 == 1))
                    P.activation(P2T[(h % 2) * 2 + mt + 4 * pb], pv, AF.Exp, scale=1.0 / 16.0)

            def pvh(h):
                for tt in range(2):
                    pa = psf(nb() % 6)[:, 0:257]
                    for mt in range(2):
                        P.matmul(pa, P2T[(h % 2) * 2 + mt + 4 * pb][:, tt * 128:(tt + 1) * 128], V2[:, mt, h, :],
                                 start=(mt == 0), stop=(mt == 1))
                    rd = stC[:, 8 + tt:9 + tt]
                    P.add("dve", lambda e, o=rd, i_=pa[:, 256:257]: e.reciprocal(o, i_), reads=[pa[:, 256:257]], writes=[rd])
                    P.tensor_scalar(oo[:, tt, h * 256:(h + 1) * 256], pa[:, 0:256], rd, None, ALU.mult)

            scores(0)
            yield
            scores(1)
            yield
            pvh(0)
            yield
            scores(2)
            pvh(1)
            yield
            scores(3)
            pvh(2)
            yield
            pvh(3)
            yield
            for tt in range(2):
                transposeT(oo[:, tt, :], oT, tt * 128, "act" if tt else "dve")
                yield
            for tt in range(2):
                i = blk * 2 + tt
                gi = b * NT + i
                xo = x2t[tt]
                for nh in range(2):
                    pv = psf(nb() % 6)
                    for c in range(8):
                        P.matmul(pv, oT[:, c, tt * 128:(tt + 1) * 128], Wmo[:, c, nh * 512:(nh + 1) * 512],
                                 start=(c == 0), stop=(c == 7))
                    P.tensor_tensor(xo[:, nh * 512:(nh + 1) * 512], pv, x1b[pb][:, tt, nh * 512:(nh + 1) * 512], ALU.add)
                P.dma(x2s[tok0 + i * 128: tok0 + (i + 1) * 128, :], xo, q="pool")
                yield
                ss, rs = stC[:, 4 + 2 * tt:5 + 2 * tt], stC[:, 5 + 2 * tt:6 + 2 * tt]
                hb = h3t[tt]
                sumsq(hb, xo, ss)
                rstd_from_ss(rs, ss, eps1024)
                P.stt(hb.rearrange("t (c p) -> t p c", c=8), xo.rearrange("t (p c) -> t p c", c=8), rs,
                      rG.rearrange("t (p c) -> t p c", c=8), ALU.mult, ALU.mult)
                P.dma(h3s[tok0 + i * 128: tok0 + (i + 1) * 128, :], hb, q="pool")
                transposeT(hb, h3T, 0, "act" if tt else "dve")
                yield
                pl = psf(7)[:, 256 + 64 * tt:256 + 64 * tt + 36]
                for c in range(8):
                    P.matmul(pl, h3T[:, c, :], wr_bf[:, c, :], start=(c == 0), stop=(c == 7))
                P.copy(LG[:, gi, :], pl, eng="act")
                yield

        def interleave(*gens):
            gens = [g for g in gens if g is not None]
            while gens:
                for g in list(gens):
                    try:
                        next(g)
                    except StopIteration:
                        gens.remove(g)

        interleave(c_front(0))
        for blk in range(NT // 2):
            interleave(c_back(blk), c_front(blk + 1) if blk + 1 < NT // 2 else None)

    A.off = mark_persist
    gidx = A.new(I32, [96]); didx = A.new(I32, [96, 4])
    mark_p2 = A.off
    N_ = NTT
    lgb = A.new(F32, [64, 36]); gmax = A.new(F32, [64]); ohg = A.new(F32, [64, 4]); eg = A.new(F32, [64, 4])
    gsum = A.new(F32, [64]); gtop = A.new(F32, [64]); esel = A.new(F32, [64, 8]); tmp8 = A.new(F32, [64, 8])
    m1 = A.new(F32, [64]); m2 = A.new(F32, [64]); oh1 = A.new(F32, [64, 8]); oh2 = A.new(F32, [64, 8])
    dd = A.new(F32, [64]); w1 = A.new(F32, [64])
    a1f = A.new(F32, [64, 32]); a2f = A.new(F32, [64, 32]); cmb = A.new(BF16, [64, 32])
    rk = A.new(F32, [64, 32]); tot = A.new(F32, [64, 32]); bex = A.new(F32, [64, 32]); big = A.new(F32, [64, 32])

    def bc3(v, k):
        return v[:, 0:N_].unsqueeze(2).to_broadcast([128, N_, k])

    P.tensor_tensor(lgb[:, 0:N_, :], LG[:, 0:N_, :], rA[:, BR_:BR_ + 36].unsqueeze(1).to_broadcast([128, N_, 36]), ALU.add)
    if "d_lg" in dbg:
        for gi in range(N_):
            P.dma(dbg["d_lg"][gi * 128:(gi + 1) * 128, :], lgb[:, gi, :])
    P.reduce(gmax[:, 0:N_], lgb[:, 0:N_, 0:4], ALU.max)
    P.tensor_tensor(ohg[:, 0:N_, :], lgb[:, 0:N_, 0:4], bc3(gmax, 4), ALU.is_equal)
    P.tensor_tensor(eg[:, 0:N_, :], lgb[:, 0:N_, 0:4], bc3(gmax, 4), ALU.subtract)
    P.activation(eg[:, 0:N_, :], eg[:, 0:N_, :], AF.Exp)
    P.reduce(gsum[:, 0:N_], eg[:, 0:N_, :], ALU.add)
    recip(gtop[:, 0:N_], gsum[:, 0:N_])
    P.tensor_tensor(esel[:, 0:N_, :], lgb[:, 0:N_, 4:12], ohg[:, 0:N_, 0:1].to_broadcast([128, N_, 8]), ALU.mult)
    for g in range(1, 4):
        P.tensor_tensor(tmp8[:, 0:N_, :], lgb[:, 0:N_, 4 + 8 * g:12 + 8 * g], ohg[:, 0:N_, g:g + 1].to_broadcast([128, N_, 8]), ALU.mult)
        P.tensor_tensor(esel[:, 0:N_, :], esel[:, 0:N_, :], tmp8[:, 0:N_, :], ALU.add)
    P.reduce(m1[:, 0:N_], esel[:, 0:N_, :], ALU.max)
    P.tensor_tensor(oh1[:, 0:N_, :], esel[:, 0:N_, :], bc3(m1, 8), ALU.is_equal)
    P.stt(tmp8[:, 0:N_, :], oh1[:, 0:N_, :], -1e30, esel[:, 0:N_, :], ALU.mult, ALU.add)
    P.reduce(m2[:, 0:N_], tmp8[:, 0:N_, :], ALU.max)
    P.tensor_tensor(oh2[:, 0:N_, :], tmp8[:, 0:N_, :], bc3(m2, 8), ALU.is_equal)
    P.tensor_tensor(dd[:, 0:N_], m2[:, 0:N_], m1[:, 0:N_], ALU.subtract)
    P.activation(dd[:, 0:N_], dd[:, 0:N_], AF.Exp)
    P.tensor_scalar(dd[:, 0:N_], dd[:, 0:N_], 1.0, None, ALU.add)
    recip(w1[:, 0:N_], dd[:, 0:N_])
    P.tensor_tensor(G1[:, 0:N_], gtop[:, 0:N_], w1[:, 0:N_], ALU.mult)
    P.tensor_tensor(G2[:, 0:N_], gtop[:, 0:N_], G1[:, 0:N_], ALU.subtract)
    for a_, oh in ((a1f, oh1), (a2f, oh2)):
        P.tensor_tensor(a_[:, 0:N_, :].rearrange("p n (g e) -> p n g e", g=4),
                        ohg[:, 0:N_, :].unsqueeze(3).to_broadcast([128, N_, 4, 8]),
                        oh[:, 0:N_, :].unsqueeze(2).to_broadcast([128, N_, 4, 8]), ALU.mult)
    P.tensor_tensor(cmb[:, 0:N_, :], a1f[:, 0:N_, :], a2f[:, 0:N_, :], ALU.add)
    cmb2, rk2, tot2 = (v.rearrange("p n e -> p (n e)") for v in (cmb, rk, tot))
    for q0 in range(0, N_ * 32, 512):
        q1 = min(q0 + 512, N_ * 32)
        pr = psf(nb() % 6)[:, 0:q1 - q0]
        P.matmul(pr, cb[:, UST:UST + 128], cmb2[:, q0:q1])
        P.copy(rk2[:, q0:q1], pr)
        pt_ = psf(nb() % 6)[:, 0:q1 - q0]
        P.matmul(pt_, cb[:, ONEB:ONEB + 128], cmb2[:, q0:q1])
        P.copy(tot2[:, q0:q1], pt_, eng="act")
    P.memset(bex[:, 0, :], 0.0)
    for t_ in range(1, N_):
        P.tensor_tensor(bex[:, t_, :], bex[:, t_ - 1, :], tot[:, t_ - 1, :], ALU.add)
    P.tensor_tensor(base, bex[:, N_ - 1, :], tot[:, N_ - 1, :], ALU.add)
    P.tensor_tensor(rk[:, 0:N_, :], rk[:, 0:N_, :], bex[:, 0:N_, :], ALU.add)
    for (af, Px) in ((a1f, P1), (a2f, P2)):
        P.tensor_tensor(big[:, 0:N_, :], af[:, 0:N_, :], rk[:, 0:N_, :], ALU.mult)
        P.reduce(Px[:, 0:N_], big[:, 0:N_, :], ALU.add)
    if "d_rt" in dbg:
        P.dma(dbg["d_rt"][:, 0:NTT], G1[:, 0:NTT]); P.dma(dbg["d_rt"][:, 64:64 + NTT], G2[:, 0:NTT])
        P.dma(dbg["d_rt"][:, 128:128 + NTT], P1[:, 0:NTT]); P.dma(dbg["d_rt"][:, 192:192 + NTT], P2[:, 0:NTT])
        P.dma(dbg["d_rt"][:, 256:288], base)
    if stop_after == "phase1":
        P.dma(out[0:128, :], big[:, 0:32, :].rearrange("p a b -> p (a b)"))
        P.emit()
        return nc


    pcf = A.new(F32, [32]); pci = A.new(I32, [32]); offs = A.new(F32, [32]); ends = A.new(F32, [32])
    te = A.new(F32, [96]); tf = A.new(F32, [96])
    zero_i = A.new(I32, [NSLOT // 128]); posf = A.new(F32, [64])
    P.tensor_scalar(pcf, base, TS / 2 - 0.5, 1.0 / TS, ALU.add, ALU.mult)
    P.copy(pci, pcf)
    P.copy(pcf, pci)
    P.tensor_scalar(pcf, pcf, float(TS), None, ALU.mult)
    P.memset(offs[:, 0:1], 0.0)
    for e in range(1, 32):
        P.tensor_tensor(offs[:, e:e + 1], offs[:, e - 1:e], pcf[:, e - 1:e], ALU.add)
    P.tensor_tensor(ends, offs, pcf, ALU.add)
    P.memset(te, 0.0)
    for e in range(32):
        P.stt(te, cf[:, THR:THR + 96], ends[:, e:e + 1], te, ALU.is_ge, ALU.add)
    if "d_te" in dbg:
        P.dma(dbg["d_te"], te)
    P.tensor_scalar(tf, te, 128.0, cf[:, PCOL:PCOL + 1], ALU.mult, ALU.add)
    P.copy(gidx, tf)
    for c in range(4):
        P.tensor_scalar(tf, te, 512.0, cf[:, CBASE + c:CBASE + c + 1], ALU.mult, ALU.add)
        P.copy(didx[:, :, c], tf)
    for (Ax, Px, POSx) in ((a1f, P1, POS1), (a2f, P2, POS2)):
        P.tensor_tensor(big[:, 0:NTT, :], Ax[:, 0:NTT, :], offs.unsqueeze(1).to_broadcast([128, NTT, 32]), ALU.mult)
        P.reduce(posf[:, 0:NTT], big[:, 0:NTT, :], ALU.add)
        P.tensor_tensor(posf[:, 0:NTT], posf[:, 0:NTT], Px[:, 0:NTT], ALU.add)
        P.copy(POSx[:, 0:NTT], posf[:, 0:NTT])
    P.memset(zero_i, 0)
    P.dma(s2t.rearrange("(p n) o -> p (n o)", p=128), zero_i)
    FAKE = "D:s2t_scatter"

    def scatter_slots():
        nsc = 0
        for gi in range(NTT):
            for POSx in (POS1, POS2):
                P.add("pool", lambda e, po=POSx[:, gi:gi + 1], ti=ci[:, gi:gi + 1]: e.indirect_dma_start(
                    out=s2t, out_offset=bass.IndirectOffsetOnAxis(ap=po, axis=0), in_=ti, in_offset=None),
                    reads=[POSx[:, gi:gi + 1], ci[:, gi:gi + 1], s2t], writes=[(FAKE, 0, 1, nsc, nsc + 1)], dma=True)
                nsc += 1

    A.off = mark_p2
    wg = [A.new(BF16, [8, 512]) for _ in range(3)]
    wu = [A.new(BF16, [8, 512]) for _ in range(3)]
    wd = [A.new(BF16, [4, 1024]) for _ in range(3)]
    sidx = [A.new(I32, [GT]) for _ in range(3)]
    hs = [A.new(BF16, [GT, 1024]) for _ in range(2)]
    hTs = [A.new(BF16, [8, TS]) for _ in range(2)]
    sil = [A.new(F32, [TS]) for _ in range(2)]
    hid = [A.new(BF16, [4, TS]) for _ in range(2)]
    yt = [A.new(BF16, [1024]) for _ in range(4)]
    s2t_v = s2t.rearrange("(j g p) o -> j p (g o)", g=GT, p=128)
    def moe_w(j):
        k3 = j % 3
        for (wdst, wsrc) in ((wg[k3], w_gate), (wu[k3], w_up)):
            P.add("pool", lambda e, o=wdst.rearrange("p a b -> p (a b)"), s=wsrc, ix=gidx[:, j:j + 1]: e.indirect_dma_start(
                out=o, out_offset=None, in_=s, in_offset=bass.IndirectOffsetOnAxis(ap=ix, axis=0),
                bounds_check=P.reg(e, 32 * 128 - 1), oob_is_err=False),
                reads=[gidx[:, j:j + 1], wsrc], writes=[wdst], dma=True)
        for c in range(4):
            P.add("pool", lambda e, o=wd[k3][:, c, :], ix=didx[:, j, c:c + 1]: e.indirect_dma_start(
                out=o, out_offset=None, in_=w_down, in_offset=bass.IndirectOffsetOnAxis(ap=ix, axis=0),
                bounds_check=P.reg(e, 32 * 512 - 1), oob_is_err=False),
                reads=[didx[:, j, c:c + 1], w_down], writes=[wd[k3][:, c, :]], dma=True)

    def moe_ix(j):
        k3 = j % 3
        P.add("sp", lambda e, o=sidx[k3], s=s2t_v[j]: e.dma_start(out=o, in_=s, allow_slow_non_contiguous=True),
              reads=[s2t_v[j], (FAKE, 0, 1, 0, 1 << 20)], writes=[sidx[k3]], dma=True)
        hsj = hs[j % 2]
        for g in range(GT):
            P.add("pool", lambda e, o=hsj[:, g, :], ix=sidx[k3][:, g:g + 1]: e.indirect_dma_start(
                out=o, out_offset=None, in_=h3s, in_offset=bass.IndirectOffsetOnAxis(ap=ix, axis=0)),
                reads=[sidx[k3][:, g:g + 1], h3s], writes=[hsj[:, g, :]], dma=True)

    def moe_T(j):
        for g in range(GT):
            transposeT(hs[j % 2][:, g, :], hTs[j % 2], g * 128, "act" if g % 2 else "dve")

    def moe_GU(j):
        k3, hTj, hidj = j % 3, hTs[j % 2], hid[j % 2]
        for fc in range(4):
            pg = psf(nb() % 6)[:, 0:TS]
            for c in range(8):
                P.matmul(pg, wg[k3][:, c, fc * 128:(fc + 1) * 128], hTj[:, c, :], start=(c == 0), stop=(c == 7))
            pu = psf(nb() % 6)[:, 0:TS]
            for c in range(8):
                P.matmul(pu, wu[k3][:, c, fc * 128:(fc + 1) * 128], hTj[:, c, :], start=(c == 0), stop=(c == 7))
            sl = sil[fc % 2]
            P.activation(sl, pg, AF.Silu)
            P.tensor_tensor(hidj[:, fc, :], sl, pu, ALU.mult)

    ny = [0]

    def moe_D(j):
        k3, hidj = j % 3, hid[j % 2]
        for g in range(GT):
            y_ = yt[ny[0] % 4]
            ny[0] += 1
            for nh in range(2):
                pv = psf(nb() % 6)
                for fc in range(4):
                    P.matmul(pv, hidj[:, fc, g * 128:(g + 1) * 128], wd[k3][:, fc, nh * 512:(nh + 1) * 512],
                             start=(fc == 0), stop=(fc == 3))
                P.copy(y_[:, nh * 512:(nh + 1) * 512], pv, eng="act" if nh else "dve")
            P.dma(Ysc[j * TS + g * 128: j * TS + (g + 1) * 128, :], y_)

    for j in range(min(3, n_moe_tiles)):
        moe_w(j)
    scatter_slots()
    if n_moe_tiles:
        moe_ix(0)
        moe_T(0)
    for j in range(n_moe_tiles):
        if j + 1 < n_moe_tiles:
            if j + 1 >= 3:
                moe_w(j + 1)
            moe_ix(j + 1)
        moe_GU(j)
        if j + 1 < n_moe_tiles:
            moe_T(j + 1)
        moe_D(j)

    A.off = mark_p2
    fg = A.new(F32, [1024])
    NB4 = 4
    x2b = [A.new(F32, [1024]) for _ in range(NB4)]
    y1b = [A.new(BF16, [1024]) for _ in range(NB4)]
    y2b = [A.new(BF16, [1024]) for _ in range(NB4)]
    otb = [A.new(F32, [1024]) for _ in range(NB4)]
    st4 = A.new(F32, [2 * NB4])
    P.dma(fg, rows[:, RF:RF + 1024])
    P.tensor_scalar(fg, fg, 32.0, None, ALU.mult)
    def p4_loads(gi):
        k2 = gi % NB4
        P.dma(x2b[k2], x2s[gi * 128:(gi + 1) * 128, :])
        for (yb, POSx) in ((y1b[k2], POS1), (y2b[k2], POS2)):
            P.add("pool", lambda e, o=yb, ix=POSx[:, gi:gi + 1]: e.indirect_dma_start(
                out=o, out_offset=None, in_=Ysc, in_offset=bass.IndirectOffsetOnAxis(ap=ix, axis=0)),
                reads=[POSx[:, gi:gi + 1], Ysc], writes=[yb], dma=True)

    PF = NB4 - 1
    for gi in range(min(PF, NTT)):
        p4_loads(gi)
    for gi in range(NTT):
        if gi + PF < NTT:
            p4_loads(gi + PF)
        k2 = gi % NB4
        acc = x2b[k2]
        if not skip_moe:
            P.stt(acc, y1b[k2], G1[:, gi:gi + 1], acc, ALU.mult, ALU.add)
            P.stt(acc, y2b[k2], G2[:, gi:gi + 1], acc, ALU.mult, ALU.add)
        ss, rs = st4[:, 2 * k2:2 * k2 + 1], st4[:, 2 * k2 + 1:2 * k2 + 2]
        sumsq(otb[k2], acc, ss)
        rstd_from_ss(rs, ss, eps1024)
        P.stt(otb[k2], acc, rs, fg, ALU.mult, ALU.mult)
        P.dma(out[gi * 128:(gi + 1) * 128, :], otb[k2])
    cnt = P.emit()
    st.close()
    return nc


def _consts():
    p = np.arange(128)
    cfm = np.zeros((128, NCF), np.float32)
    cfm[:, THR:THR + 96] = float(TS) * np.arange(96)[None, :]
    cfm[:, PCOL] = p
    cfm[:, CBASE:CBASE + 4] = np.arange(4)[None, :] * 128 + p[:, None]
    cfm[:, IOTA:IOTA + 32] = np.arange(32)[None, :]
    cfm[:, CEPS:CEPS + 4] = np.array([1024e-6, 512e-6, 1.0, 1e-6], np.float32)[None, :]
    cbm = np.zeros((128, NCB), np.float32)
    cbm[:, IDB:IDB + 128] = np.eye(128)
    cbm[:, NEGM:NEGM + 128] = np.where(p[:, None] > p[None, :], -30000.0, 0.0)
    cbm[:, UST:UST + 128] = (p[:, None] < p[None, :])
    cbm[:, ONEB:ONEB + 128] = 1.0
    cbm[:, LTRIB:LTRIB + 128] = (p[:, None] <= p[None, :])
    cim = (np.arange(64)[None, :] * 128 + p[:, None]).astype(np.int32)
    return cfm, cbm.astype(ml_dtypes.bfloat16), cim


def _host_inputs(core, nseq, x, mem, norm_mix_g, w_in, b_forget, b_glu, w_dw, b_dw, conv_ln_g, conv_ln_b,
                 attn_out_g, w_out, norm_mem_g, mem_norm_g, w_mq, w_mkv, w_mo, norm_ffn_g,
                 w_route_group, b_route_group, w_route_expert, b_route_expert, w_gate, w_up, w_down, final_g):
    f = lambda a: np.ascontiguousarray(np.asarray(a, dtype=np.float32))
    cfm, cbm, cim = _consts()
    vecs = np.zeros((128, NV), np.float32)
    vecs[:, GM:GM + 8] = f(norm_mix_g)[0].reshape(8, 128).T
    vecs[:, GQ:GQ + 8] = f(norm_mem_g)[0].reshape(8, 128).T
    vecs[:, GK:GK + 8] = f(mem_norm_g)[0].reshape(8, 128).T
    vecs[:, GA:GA + 4] = f(attn_out_g)[0].reshape(4, 128).T
    vecs[:, BG:BG + 8] = f(b_glu)[0].reshape(8, 128).T
    vecs[:, BD:BD + 4] = f(b_dw)[0].reshape(4, 128).T
    vecs[:, WDW:WDW + 124] = f(w_dw)[0].reshape(31, 4, 128).transpose(2, 1, 0).reshape(128, 124)
    rows = np.zeros((1, NR), np.float32)
    rows[0, RG:RG + 1024] = f(norm_ffn_g)[0]
    rows[0, RF:RF + 1024] = f(final_g)
    rows[0, RLG:RLG + 512] = f(conv_ln_g)[0]
    rows[0, RLB:RLB + 512] = f(conv_ln_b)[0]
    rows[0, RBF:RBF + 128] = np.tile(f(b_forget)[0], 16)
    rows[0, RBR:RBR + 4] = f(b_route_group)[0]
    rows[0, RBR + 4:RBR + 36] = f(b_route_expert)[0].reshape(32)
    rows[0, RBDW:RBDW + 512] = f(b_dw)[0]
    rows = np.ascontiguousarray(np.broadcast_to(rows, (128, NR)))
    wr = np.concatenate([f(w_route_group)[0], f(w_route_expert)[0].transpose(1, 0, 2).reshape(1024, 32)], axis=1)
    return {
        "x": f(x[core * nseq:(core + 1) * nseq]).reshape(nseq * S, D),
        "mem": f(mem[core * nseq:(core + 1) * nseq]).reshape(nseq * 256, D),
        "w_in": f(w_in)[0], "w_out": f(w_out)[0], "w_mq": f(w_mq)[0], "w_mkv": f(w_mkv)[0], "w_mo": f(w_mo)[0],
        "w_route": np.ascontiguousarray(wr.reshape(128, 8 * 36)),
        "w_gate": f(w_gate)[0].reshape(32 * 128, 4096), "w_up": f(w_up)[0].reshape(32 * 128, 4096),
        "w_down": f(w_down)[0].reshape(32 * 512, 1024),
        "vecs": vecs, "rows": rows, "constf": cfm, "constb": cbm, "consti": cim,
    }


_NC_CACHE = {}


def kernel(**inputs):
    n_cores = 8
    nseq = 4
    if "full" not in _NC_CACHE:
        _NC_CACHE["full"] = build(nseq=nseq)
    nc = _NC_CACHE["full"]
    in_maps = [_host_inputs(c, nseq, **inputs) for c in range(n_cores)]
    res = run_bass_kernel_spmd(nc, in_maps, core_ids=list(range(n_cores)))
    outs = [np.asarray(r["out"]).reshape(nseq, S, D) for r in res.results]
    return np.concatenate(outs, axis=0).astype(np.float32)
```

```python
import numpy as np
import ml_dtypes
from concourse.bass_utils import run_bass_kernel_spmd
from contextlib import ExitStack
import concourse.bass as bass
import concourse.mybir as mybir

F32 = mybir.dt.float32
BF16 = mybir.dt.bfloat16
I32 = mybir.dt.int32
ALU = mybir.AluOpType
AF = mybir.ActivationFunctionType
AX = mybir.AxisListType
_DSZ = {F32: 4, BF16: 2, I32: 4, mybir.dt.uint32: 4, mybir.dt.float16: 2, mybir.dt.uint8: 1,
        mybir.dt.int8: 1, mybir.dt.uint16: 2, mybir.dt.int16: 2}


def region(ap):
    dsz = _DSZ[ap.dtype]
    dims = ap.ap
    off = ap.offset
    sp = str(ap.space)
    if sp in ("SB", "PSUM"):
        shp = ap.tensor.shape
        rowlen = 1
        for s in list(shp)[1:]:
            rowlen *= s
        p0 = off // rowlen
        c0 = off % rowlen
        pstep, pcnt = dims[0]
        p1 = p0 + ((pcnt - 1) * pstep) // rowlen + 1 if pstep else p0 + 1
        ext = 0
        for st, cn in dims[1:]:
            ext += (cn - 1) * abs(st)
        if sp == "PSUM":
            return (sp + ":" + ap.tensor.name, 0, 128, 0, 1 << 20)
        return (sp + ":" + ap.tensor.name, p0, p1, c0 * dsz, (c0 + ext + 1) * dsz)
    else:
        ext = 0
        for st, cn in dims:
            ext += (cn - 1) * abs(st)
        return ("D:" + ap.tensor.name, 0, 1, off * dsz, (off + ext + 1) * dsz)


class Op:
    __slots__ = ("eng", "fn", "reads", "writes", "dma", "deps", "sig", "dsem", "dval",
                 "waits", "idx", "prewait")


class Prog:
    ENGS = ("pe", "act", "dve", "pool", "sp")
    EPOCH = 30000

    def __init__(self, nc, ndma_sems=56):
        self.nc = nc
        self.ops = []
        self.recs = {}
        self.ndma = ndma_sems
        self.extra_regions = {}

    def reg(self, e, val):
        d = self.__dict__.setdefault("_regs", {})
        if val not in d:
            d[val] = e.to_reg(val)
        return d[val]

    def add(self, eng, fn, reads=(), writes=(), dma=False):
        op = Op()
        op.eng = eng; op.fn = fn; op.dma = dma
        op.reads = [r if isinstance(r, tuple) else region(r) for r in reads]
        op.writes = [r if isinstance(r, tuple) else region(r) for r in writes]
        op.idx = len(self.ops)
        op.sig = None; op.dsem = None; op.dval = None; op.waits = []; op.prewait = None
        deps = set()
        for (sp, p0, p1, lo, hi) in op.reads:
            for r in self.recs.get(sp, ()):
                if r[5] and r[0] < p1 and p0 < r[1] and r[2] < hi and lo < r[3]:
                    deps.add(r[4])
        for (sp, p0, p1, lo, hi) in op.writes:
            lst = self.recs.get(sp)
            if lst is None:
                lst = self.recs[sp] = []
            keep = []
            for r in lst:
                if r[0] < p1 and p0 < r[1] and r[2] < hi and lo < r[3]:
                    deps.add(r[4])
                    if r[0] >= p0 and r[1] <= p1 and r[2] >= lo and r[3] <= hi:
                        continue
                keep.append(r)
            keep.append([p0, p1, lo, hi, op.idx, True])
            self.recs[sp] = keep
        for (sp, p0, p1, lo, hi) in op.reads:
            lst = self.recs.get(sp)
            if lst is None:
                lst = self.recs[sp] = []
            if not dma:
                for r in lst:
                    if (not r[5]) and r[0] == p0 and r[1] == p1 and r[2] == lo and r[3] == hi \
                            and (not self.ops[r[4]].dma) and self.ops[r[4]].eng == eng:
                        r[4] = op.idx
                        break
                else:
                    lst.append([p0, p1, lo, hi, op.idx, False])
            else:
                lst.append([p0, p1, lo, hi, op.idx, False])
        deps.discard(op.idx)
        op.deps = deps
        self.ops.append(op)
        return op

    def matmul(self, out, lhsT, rhs, start=True, stop=True, **kw):
        return self.add("pe", lambda e: e.matmul(out, lhsT, rhs, start=start, stop=stop, **kw),
                        reads=[lhsT, rhs], writes=[out])

    def transpose(self, out, in_, ident):
        return self.add("pe", lambda e: e.transpose(out, in_, ident), reads=[in_, ident], writes=[out])

    def activation(self, out, in_, func, bias=None, scale=None, accum_out=None, eng="act"):
        reads = [in_]
        kw = {}
        if bias is not None:
            kw["bias"] = bias
            if not isinstance(bias, (int, float)):
                reads.append(bias)
        if scale is not None:
            kw["scale"] = scale
            if not isinstance(scale, (int, float)):
                reads.append(scale)
        writes = [out]
        if accum_out is not None:
            kw["accum_out"] = accum_out
            writes.append(accum_out)
        return self.add(eng, lambda e: e.activation(out, in_, func, **kw), reads=reads, writes=writes)

    def tensor_scalar(self, out, in0, s1, s2, op0, op1=None, eng="dve", accum_out=None):
        reads = [in0]
        for s in (s1, s2):
            if s is not None and not isinstance(s, (int, float)):
                reads.append(s)
        kw = {}
        if op1 is not None:
            kw["op1"] = op1
        writes = [out]
        if accum_out is not None:
            kw["accum_out"] = accum_out
            writes.append(accum_out)
        return self.add(eng, lambda e: e.tensor_scalar(out, in0, s1, s2, op0, **kw), reads=reads, writes=writes)

    def tensor_tensor(self, out, in0, in1, op, eng="dve"):
        return self.add(eng, lambda e: e.tensor_tensor(out, in0, in1, op), reads=[in0, in1], writes=[out])

    def stt(self, out, in0, scalar, in1, op0, op1, eng="dve", accum_out=None):
        reads = [in0, in1]
        if not isinstance(scalar, (int, float)):
            reads.append(scalar)
        kw = {}
        writes = [out]
        if accum_out is not None:
            kw["accum_out"] = accum_out
            writes.append(accum_out)
        return self.add(eng, lambda e: e.scalar_tensor_tensor(out, in0, scalar, in1, op0, op1, **kw),
                        reads=reads, writes=writes)

    def copy(self, out, in_, eng="dve"):
        if eng == "act":
            return self.add(eng, lambda e: e.copy(out, in_), reads=[in_], writes=[out])
        return self.add(eng, lambda e: e.tensor_copy(out, in_), reads=[in_], writes=[out])

    def memset(self, out, val, eng="dve"):
        return self.add(eng, lambda e: e.memset(out, val), reads=[], writes=[out])

    def reduce(self, out, in_, op, axis=None, eng="dve"):
        ax = axis if axis is not None else AX.X
        return self.add(eng, lambda e: e.tensor_reduce(out, in_, ax, op), reads=[in_], writes=[out])

    def dma(self, out, in_, q="sp", **kw):
        return self.add(q, lambda e: e.dma_start(out=out, in_=in_, **kw), reads=[in_], writes=[out], dma=True)

    def finalize(self):
        ops = self.ops
        need = [False] * len(ops)
        for op in ops:
            for d in op.deps:
                p = ops[d]
                if p.dma:
                    continue
                if p.eng == op.eng and not op.dma:
                    if p.eng == "pe":
                        continue
                    continue_flag = True
                    for (sp, p0, p1, lo, hi) in op.reads:
                        for (sp2, q0, q1, lo2, hi2) in p.writes:
                            if sp == sp2 and q0 < p1 and p0 < q1 and lo2 < hi and lo < hi2:
                                continue_flag = False
                    if continue_flag:
                        continue
                need[d] = True
        cnt = {e: 0 for e in self.ENGS}
        for op in ops:
            if (not op.dma) and need[op.idx]:
                cnt[op.eng] += 1
                op.sig = cnt[op.eng]
        dcount = [0] * self.ndma
        k = 0
        for op in ops:
            if op.dma:
                s = k % self.ndma
                k += 1
                dcount[s] += 1
                op.dsem = s
                op.dval = 16 * dcount[s]
                op.prewait = (("dma", s), 16 * (dcount[s] - 1)) if dcount[s] > 1 else None
        self.dfinal = [16 * c for c in dcount]
        seen = {e: {} for e in self.ENGS}
        pos = {e: 0 for e in self.ENGS}
        opos = {}
        for op in ops:
            pos[op.eng] += 1
            opos[op.idx] = pos[op.eng]
        nw = 0
        for op in ops:
            sn = seen[op.eng]
            w = {}
            if op.prewait is not None:
                key, val = op.prewait
                if sn.get(key, 0) < val:
                    w[key] = val
            for d in op.deps:
                p = ops[d]
                if p.dma:
                    key, val = ("dma", p.dsem), p.dval
                else:
                    if p.sig is None:
                        continue
                    if p.eng == op.eng and not op.dma:
                        if p.eng == "pe":
                            continue
                        if opos[op.idx] - opos[p.idx] > 3:
                            continue
                    ep = (p.sig - 1) // self.EPOCH
                    key, val = ("eng", p.eng), ep * 100000 + (p.sig - 1) % self.EPOCH + 1
                if sn.get(key, 0) < val and w.get(key, 0) < val:
                    w[key] = val
            for key, val in w.items():
                sn[key] = val
            op.waits = list(w.items())
            nw += len(op.waits)
        self.n_waits = nw
        self.n_epochs = {e: (cnt[e] - 1) // self.EPOCH + 1 if cnt[e] else 1 for e in self.ENGS}
        return cnt

    def emit(self):
        nc = self.nc
        cnt = self.finalize()
        ops = self.ops
        with ExitStack() as st:
            esem = {}
            for e in self.ENGS:
                for ep in range(self.n_epochs[e]):
                    esem[(e, ep)] = st.enter_context(nc.semaphore("s_%s%d" % (e, ep)))
            dsem = [st.enter_context(nc.semaphore("d%d" % i)) for i in range(self.ndma)]
            block = st.enter_context(nc.Block())
            by_eng = {e: [op for op in ops if op.eng == e] for e in self.ENGS}
            EP = self.EPOCH

            def run(eng_obj, lst, is_sp):
                for op in lst:
                    for key, val in op.waits:
                        if key[0] == "dma":
                            eng_obj.wait_ge(dsem[key[1]], val)
                        else:
                            ep, v = divmod(val, 100000)
                            eng_obj.wait_ge(esem[(key[1], ep)], v)
                    inst = op.fn(eng_obj)
                    if op.dma:
                        inst.then_inc(dsem[op.dsem], 16)
                    elif op.sig is not None:
                        ep = (op.sig - 1) // EP
                        inst.then_inc(esem[(op.eng, ep)], 1)
                if is_sp:
                    for i, v in enumerate(self.dfinal):
                        if v:
                            eng_obj.wait_ge(dsem[i], v)

            @block.sync
            def _(e):
                run(e, by_eng["sp"], True)

            @block.tensor
            def _(e):
                run(e, by_eng["pe"], False)

            @block.scalar
            def _(e):
                run(e, by_eng["act"], False)

            @block.vector
            def _(e):
                run(e, by_eng["dve"], False)

            @block.gpsimd
            def _(e):
                run(e, by_eng["pool"], False)
        return cnt

S = 2048
D = 1024
NT = S // 128
TS = 512
NTILES = 16384 // TS + 32
GT = TS // 128
NSLOT = NTILES * TS
GM, GQ, GK, GA, BG, BD, WDW = 0, 8, 16, 24, 28, 36, 40
NV = 40 + 4 * 31
RG, RF, RLG, RLB, RBF, RBR, RBDW = 0, 1024, 2048, 2560, 3072, 3200, 3236
NR = 3748
THR, PCOL, CBASE, IOTA, CEPS, NCF = 0, 96, 97, 101, 133, 141
IDB, NEGM, UST, ONEB, LTRIB, NCB = 0, 128, 256, 384, 512, 640


def _prod(s):
    r = 1
    for a in s:
        r *= a
    return r


class Arena:
    def __init__(self, nc, st, name, nbytes):
        self.t = st.enter_context(nc.sbuf_tensor(name, [128, nbytes // 4], F32))
        self.cap = nbytes
        self.off = 0

    def alloc(self, nbytes):
        off = (self.off + 63) // 64 * 64
        self.off = off + nbytes
        assert self.off <= self.cap, ("arena overflow", self.off, self.cap)
        return off

    def view(self, off, dtype, shape):
        n = _prod(shape)
        dsz = _DSZ[dtype]
        nb = (n * dsz + 3) // 4
        v = self.t[:, off // 4: off // 4 + nb]
        if dtype != F32:
            v = v.bitcast(dtype)
        v = v[:, 0:n]
        if len(shape) == 2:
            v = v.rearrange("p (a b) -> p a b", a=shape[0], b=shape[1])
        elif len(shape) == 3:
            v = v.rearrange("p (a b c) -> p a b c", a=shape[0], b=shape[1], c=shape[2])
        return v

    def new(self, dtype, shape):
        off = self.alloc(_prod(shape) * _DSZ[dtype])
        return self.view(off, dtype, shape)


import os as _os
BP = set(_os.environ.get('BPARTS', 'conv,convT,qk,mask,exp,pv,norm,tail,catT').split(','))


def build(nseq=4, debug=(), stop_after=None, n_moe_tiles=NTILES, skip_moe=False):
    nc = bass.Bass("TRN2", target_bir_lowering=False)
    NTOK = nseq * S
    NTT = NTOK // 128

    def din(name, shape, dt=F32):
        return nc.dram_tensor(name, shape, dt, kind="ExternalInput").ap()

    def scr(name, shape, dt):
        if name in debug:
            return nc.dram_tensor(name, shape, dt, kind="ExternalOutput").ap()
        return nc.dram_tensor(name, shape, dt).ap()

    x = din("x", [NTOK, D])
    mem = din("mem", [nseq * 256, D])
    w_in = din("w_in", [D, 2568])
    w_out = din("w_out", [D, D])
    w_mq = din("w_mq", [D, D])
    w_mkv = din("w_mkv", [D, 2 * D])
    w_mo = din("w_mo", [D, D])
    w_route = din("w_route", [128, 8 * 36])
    w_gate = din("w_gate", [32 * 128, 4096])
    w_up = din("w_up", [32 * 128, 4096])
    w_down = din("w_down", [32 * 512, 1024])
    vecs = din("vecs", [128, NV])
    rows = din("rows", [128, NR])
    constf = din("constf", [128, NCF])
    constb = din("constb", [128, NCB], BF16)
    consti = din("consti", [128, 64], I32)
    out = nc.dram_tensor("out", [NTOK, D], F32, kind="ExternalOutput").ap()

    w_in_bf = scr("w_in_bf", [D, 2568], BF16)
    w_out_bf = scr("w_out_bf", [D, D], BF16)
    w_mq_bf = scr("w_mq_bf", [D, D], BF16)
    w_mkv_bf = scr("w_mkv_bf", [D, 2 * D], BF16)
    w_mo_bf = scr("w_mo_bf", [D, D], BF16)
    x2s = scr("x2s", [NTOK, D], F32)
    h3s = scr("h3s", [NTOK, D], BF16)
    Ysc = scr("Ysc", [NSLOT, D], BF16)
    s2t = scr("s2t", [NSLOT, 1], I32)
    dbg = {}
    for name, shape, dt in (("d_cat", [S, D], BF16), ("d_x1", [S, D], F32), ("d_lg", [NTOK, 36], F32),
                            ("d_q", [128, 4 * 2048], BF16), ("d_attn", [S, 512], F32), ("d_z", [128, 4 * 2080], BF16),
                            ("d_B", [128, 2048], F32), ("d_rt", [128, 64 * 8], F32), ("d_te", [128, 96], F32),
                            ("d_conv", [S, 512], F32)):
        if name in debug:
            dbg[name] = nc.dram_tensor(name, shape, dt, kind="ExternalOutput").ap()

    P = Prog(nc)
    st = ExitStack()
    A = Arena(nc, st, "arena", 210944)
    PS = [st.enter_context(nc.psum_tensor("ps%d" % i, [128, 512], F32)) for i in range(8)]

    def psf(i):
        return PS[i][:, :]

    def psb(i):
        return PS[i][:, :].bitcast(BF16)

    cf = A.new(F32, [NCF])
    cb = A.new(BF16, [NCB])
    ci = A.new(I32, [64])
    vv = A.new(F32, [NV])
    rA = A.new(F32, [1700])
    LG_, LB_, BF_, BR_, BDW_ = 0, 512, 1024, 1152, 1188
    LG = A.new(F32, [64, 36])
    G1 = A.new(F32, [64]); G2 = A.new(F32, [64]); P1 = A.new(F32, [64]); P2 = A.new(F32, [64])
    POS1 = A.new(I32, [64]); POS2 = A.new(I32, [64])
    base = A.new(F32, [32])
    wr_bf = A.new(BF16, [8, 36])
    v_ = None
    identb = cb[:, IDB:IDB + 128]
    negm = cb[:, NEGM:NEGM + 128]
    eps1024 = cf[:, CEPS:CEPS + 1]
    eps512 = cf[:, CEPS + 1:CEPS + 2]
    one_c = cf[:, CEPS + 2:CEPS + 3]
    epsln = cf[:, CEPS + 3:CEPS + 4]
    P.dma(cf, constf); P.dma(cb, constb); P.dma(ci, consti); P.dma(vv, vecs)
    P.dma(rA, rows[:, RLG:RLG + 1700])
    P.memset(base, 0.0)
    P.tensor_scalar(vv[:, GM:GM + 24], vv[:, GM:GM + 24], 32.0, None, ALU.mult)
    P.tensor_scalar(vv[:, GA:GA + 4], vv[:, GA:GA + 4], float(np.sqrt(512.0)), None, ALU.mult)
    mark_persist = A.off

    Wreg_off = A.alloc(49152)
    Win = A.view(Wreg_off, BF16, [8, 2568])
    Woqm = A.view(Wreg_off, BF16, [3, 8, 1024])
    xT = A.new(BF16, [8, 2048])
    K2T = A.new(BF16, [8, 256])
    V2 = A.new(BF16, [2, 4, 257])
    Bt = A.new(F32, [8, 16, 16])
    nlf = A.new(F32, [16, 8]); incl = A.new(F32, [16, 8]); Cc = A.new(F32, [16, 8]); Cend = A.new(F32, [16, 8])
    U_off = A.alloc(66048 + 64)
    qT = A.view(U_off, BF16, [4, 2048])
    kT = A.view(U_off + 16384, BF16, [4, 2048])
    Wkv = A.view(U_off, BF16, [8, 2048])
    vS = A.view(U_off + 32768, BF16, [16, 8, 65])
    zT = A.view(U_off + 32768 + 16640, BF16, [4, 2080])
    memT = A.view(U_off + 32768 + 16640, BF16, [8, 256])
    T_off = A.alloc(23040)
    xin = [A.view(T_off + i * 4096, F32, [1024]) for i in range(2)]
    xn = [A.view(T_off + 8192 + i * 2048, BF16, [1024]) for i in range(2)]
    sig = [A.view(T_off + 12288 + i * 2048, F32, [512]) for i in range(2)]
    stA = A.view(T_off + 16384, F32, [16])
    PT = [A.view(T_off + i * 256, BF16, [128]) for i in range(6)]
    attn = A.view(T_off + 1536, F32, [512])
    catA = A.view(T_off + 3584, BF16, [512])
    catC = [A.view(T_off + 4608 + i * 1024, BF16, [512]) for i in range(2)]
    cz = [A.view(T_off + 6656 + i * 2048, F32, [512]) for i in range(2)]
    stB = A.view(T_off + 10752, F32, [64])
    Dg = A.view(T_off + 11008, BF16, [31, 128])
    zoff = U_off + 32768 + 16640
    cvo = [A.view(T_off + 18944, BF16, [16, 128])] + [A.view(zoff + i * 4160, BF16, [16, 128]) for i in range(3)]
    o_ = U_off
    x1 = A.view(o_, F32, [2, 1024]); o_ += 8192
    xr = [A.view(o_ + i * 4096, F32, [1024]) for i in range(2)]; o_ += 8192
    xn2 = [A.view(o_ + i * 2048, BF16, [1024]) for i in range(2)]; o_ += 4096
    h2T = A.view(o_, BF16, [8, 256]); o_ += 4096
    q2T = A.view(o_, BF16, [8, 256]); o_ += 4096
    P2T = [A.view(o_ + i * 512, BF16, [256]) for i in range(8)]; o_ += 4096
    oo = A.view(o_, BF16, [2, 1024]); o_ += 4096
    oT = A.view(o_, BF16, [8, 256]); o_ += 4096
    x2t = [A.view(o_ + i * 4096, F32, [1024]) for i in range(2)]; o_ += 8192
    h3t = [A.view(o_ + i * 2048, BF16, [1024]) for i in range(2)]; o_ += 4096
    h3T = A.view(o_, BF16, [8, 128]); o_ += 2048
    rG = A.view(o_, F32, [1024]); o_ += 4096
    rt = A.view(o_, F32, [256]); o_ += 1024
    rtb = A.view(o_, BF16, [64]); o_ += 128
    stC = A.view(o_, F32, [32]); o_ += 128
    assert o_ <= U_off + 66048
    x1b = [x1, A.view(T_off, F32, [2, 1024])]
    h2Tb = [h2T, A.view(T_off + 8192, BF16, [8, 256])]
    q2Tb = [q2T, A.view(T_off + 12288, BF16, [8, 256])]

    st0 = [A.view(U_off + i * 15488, F32, [2568]) for i in range(4)]
    st0b = [A.view(U_off + 10304 + i * 15488, BF16, [2568]) for i in range(4)]
    jobs = []
    for (src, dst, ncol, gcol, nsc) in ((w_mkv, w_mkv_bf, 2048, GK, 8), (w_in, w_in_bf, 2568, GM, 8),
                                       (w_out, w_out_bf, 1024, GA, 4), (w_mq, w_mq_bf, 1024, GQ, 8),
                                       (w_mo, w_mo_bf, 1024, None, 0)):
        for c in range(8):
            jobs.append((src, dst, ncol, (gcol + c) if c < nsc else None, c))

    def p0_load(k):
        src, dst, ncol, g_, c = jobs[k]
        P.dma(st0[k % 4][:, 0:ncol], src[c * 128:(c + 1) * 128, :])

    for k in range(3):
        p0_load(k)
    for k in range(len(jobs)):
        if k + 3 < len(jobs):
            p0_load(k + 3)
        src, dst, ncol, g_, c = jobs[k]
        sb_, sbb = st0[k % 4], st0b[k % 4]
        if g_ is not None:
            if k % 2 == 0:
                P.tensor_scalar(sbb[:, 0:ncol], sb_[:, 0:ncol], vv[:, g_:g_ + 1], None, ALU.mult)
            else:
                P.activation(sbb[:, 0:ncol], sb_[:, 0:ncol], AF.Copy, scale=vv[:, g_:g_ + 1])
        else:
            P.copy(sbb[:, 0:ncol], sb_[:, 0:ncol], eng="dve" if k % 2 == 0 else "act")
        P.dma(dst[c * 128:(c + 1) * 128, :], sbb[:, 0:ncol])
    P.dma(st0[0][:, 0:288], w_route)
    P.copy(wr_bf, st0[0][:, 0:288].rearrange("p (c n) -> p c n", c=8))

    if stop_after == "phase0":
        P.dma(out[0:128, :], st0[0][:, 0:1024])
        P.emit()
        return nc
    bank = [0]

    def nb():
        bank[0] = (bank[0] + 1) % 8
        return bank[0]

    def rstd_from_ss(dst, ss, epsc):
        P.activation(dst, ss, AF.Ln, bias=epsc, scale=1.0)
        P.activation(dst, dst, AF.Exp, scale=-0.5)


    def recip(o, i, eng="dve"):
        P.add(eng, lambda e: e.reciprocal(o, i), reads=[i], writes=[o])

    def route_tile(gi, pl, tt):
        lg = rt[:, 0:36]
        P.tensor_tensor(lg, pl, rA[:, BR_:BR_ + 36], ALU.add)
        if "d_lg" in dbg:
            P.dma(dbg["d_lg"][gi * 128:(gi + 1) * 128, :], lg)
        gmax, ngmax, gsum, gtop = rt[:, 40:41], rt[:, 41:42], rt[:, 42:43], rt[:, 43:44]
        ohg = rt[:, 44:48]
        P.reduce(gmax, lg[:, 0:4], ALU.max)
        P.tensor_scalar(ohg, lg[:, 0:4], gmax, None, ALU.is_equal)
        P.tensor_scalar(ngmax, gmax, -1.0, None, ALU.mult)
        P.activation(rt[:, 48:52], lg[:, 0:4], AF.Exp, bias=ngmax, scale=1.0, accum_out=gsum)
        recip(gtop, gsum)
        esel = rt[:, 52:60]
        P.tensor_scalar(esel, lg[:, 4:12], ohg[:, 0:1], None, ALU.mult)
        for g in range(1, 4):
            P.stt(esel, lg[:, 4 + 8 * g:12 + 8 * g], ohg[:, g:g + 1], esel, ALU.mult, ALU.add)
        top8 = rt[:, 60:68]
        P.add("dve", lambda e: e.max(top8, esel), reads=[esel], writes=[top8])
        oh1, oh2 = rt[:, 68:76], rt[:, 76:84]
        P.tensor_scalar(oh1, esel, top8[:, 0:1], None, ALU.is_equal)
        P.tensor_scalar(oh2, esel, top8[:, 1:2], None, ALU.is_equal)
        dd, w1 = rt[:, 84:85], rt[:, 85:86]
        P.tensor_tensor(dd, top8[:, 1:2], top8[:, 0:1], ALU.subtract)
        P.activation(dd, dd, AF.Exp)
        P.tensor_scalar(dd, dd, 1.0, None, ALU.add)
        recip(w1, dd)
        P.tensor_tensor(G1[:, gi:gi + 1], gtop, w1, ALU.mult)
        P.tensor_tensor(G2[:, gi:gi + 1], gtop, G1[:, gi:gi + 1], ALU.subtract)
        a1, a2 = rt[:, 96:128], rt[:, 128:160]
        for a_, oh in ((a1, oh1), (a2, oh2)):
            P.tensor_tensor(a_.rearrange("p (g e) -> p g e", g=4), ohg.unsqueeze(2).to_broadcast([128, 4, 8]),
                            oh.unsqueeze(1).to_broadcast([128, 4, 8]), ALU.mult)
        P.copy(A1[:, gi, :], a1)
        P.copy(A2[:, gi, :], a2)
        cmb = rtb[:, 0:32]
        P.tensor_tensor(cmb, a1, a2, ALU.add)
        pr = psf(7)[:, 384 + 64 * tt:448 + 64 * tt]
        P.matmul(pr[:, 0:32], cb[:, UST:UST + 128], cmb)
        P.matmul(pr[:, 32:64], cb[:, ONEB:ONEB + 128], cmb)
        rk, jk = rt[:, 160:192], rt[:, 192:224]
        P.tensor_tensor(rk, pr[:, 0:32], base, ALU.add)
        P.tensor_tensor(jk, a1, rk, ALU.mult)
        P.reduce(P1[:, gi:gi + 1], jk, ALU.add)
        P.tensor_tensor(jk, a2, rk, ALU.mult)
        P.reduce(P2[:, gi:gi + 1], jk, ALU.add)
        P.tensor_tensor(base, base, pr[:, 32:64], ALU.add)

    def sumsq(junk, src, ss):
        P.activation(junk, src, AF.Square, accum_out=ss)

    def transposeT(src_bf, dstT, col0, ev, b=None):
        if b is None:
            b = 6 + (nb() % 2)
        pv = psb(b)
        for c in range(8):
            P.transpose(pv[:, c * 128:(c + 1) * 128], src_bf[:, c * 128:(c + 1) * 128], identb)
        P.copy(dstT[:, :, col0:col0 + 128], pv.rearrange("p (c t) -> p c t", c=8), eng=ev)


    for b in range(nseq):
        tok0 = b * S
        for c in range(8):
            P.dma(Wkv[:, c, :], w_mkv_bf[c * 128:(c + 1) * 128, :])
        for mt in range(2):
            xi, xb = xin[mt], xn[mt]
            P.dma(xi, mem[b * 256 + mt * 128: b * 256 + (mt + 1) * 128, :])
            ss, rs = stA[:, 2 * mt:2 * mt + 1], stA[:, 2 * mt + 1:2 * mt + 2]
            sumsq(xb, xi, ss)
            rstd_from_ss(rs, ss, eps1024)
            P.tensor_scalar(xb, xi, rs, None, ALU.mult)
            transposeT(xb, memT, mt * 128, "dve")
        for cc in range(8):
            bk = nb() % 6
            pv = psf(bk)[:, 0:256]
            for c in range(8):
                P.matmul(pv, Wkv[:, c, cc * 128:(cc + 1) * 128], memT[:, c, :], start=(c == 0), stop=(c == 7))
            P.copy(K2T[:, cc, :], pv, eng="act" if cc % 2 else "dve")
        for mt in range(2):
            for nh in range(2):
                bk = nb() % 6
                pv = psf(bk)
                for c in range(8):
                    P.matmul(pv, memT[:, c, mt * 128:(mt + 1) * 128], Wkv[:, c, 1024 + nh * 512:1024 + (nh + 1) * 512],
                             start=(c == 0), stop=(c == 7))
                P.copy(V2[:, mt, 2 * nh:2 * nh + 2, 0:256], pv.rearrange("p (h d) -> p h d", h=2),
                       eng="act" if nh % 2 else "dve")
        P.memset(V2[:, :, :, 256:257], 1.0)
        if stop_after == "A0":
            P.dma(out[0:128, :], xin[0])
            P.emit()
            return nc
        for c in range(8):
            P.dma(Win[:, c, :], w_in_bf[c * 128:(c + 1) * 128, :])
        for i in range(NT):
            xi, xb = xin[i % 2], xn[i % 2]
            P.dma(xi, x[tok0 + i * 128: tok0 + (i + 1) * 128, :])
            ss, rs = stA[:, 4 + 2 * (i % 2):5 + 2 * (i % 2)], stA[:, 5 + 2 * (i % 2):6 + 2 * (i % 2)]
            sumsq(xb, xi, ss)
            rstd_from_ss(rs, ss, eps1024)
            P.tensor_scalar(xb, xi, rs, None, ALU.mult)
            transposeT(xb, xT, i * 128, "act" if i % 2 else "dve")
        if stop_after == "A1":
            P.dma(out[0:128, :], xin[0])
            P.emit()
            return nc
        P.memset(zT[:, :, 0:30], 0.0)
        P.memset(zT[:, :, 2078:2080], 0.0)
        P.memset(vS[:, :, :, 64:65], 1.0)
        OFF_K, OFF_V, OFF_F, OFF_C = 512, 1024, 1536, 1544
        for tb in range(4):
            tsl = slice(tb * 512, (tb + 1) * 512)
            for cc in range(8):
                bk = nb() % 6
                pv = psf(bk)
                for c in range(8):
                    P.matmul(pv, Win[:, c, cc * 128:(cc + 1) * 128], xT[:, c, tsl], start=(c == 0), stop=(c == 7))
                dst = qT[:, cc, tsl] if cc < 4 else kT[:, cc - 4, tsl]
                P.copy(dst, pv, eng="act" if cc % 2 else "dve")
            for i4 in range(4):
                bg = nb() % 6
                pg = psf(bg)
                for c in range(8):
                    P.matmul(pg, Win[:, c, OFF_C + 512 + i4 * 128:OFF_C + 512 + (i4 + 1) * 128], xT[:, c, tsl],
                             start=(c == 0), stop=(c == 7))
                sg = sig[i4 % 2]
                P.activation(sg, pg, AF.Sigmoid, bias=vv[:, BG + 4 + i4:BG + 5 + i4], scale=1.0)
                ba = nb() % 6
                pa = psf(ba)
                for c in range(8):
                    P.matmul(pa, Win[:, c, OFF_C + i4 * 128:OFF_C + (i4 + 1) * 128], xT[:, c, tsl],
                             start=(c == 0), stop=(c == 7))
                P.stt(zT[:, i4, 30 + tb * 512:30 + (tb + 1) * 512], pa, vv[:, BG + i4:BG + i4 + 1], sg, ALU.add, ALU.mult)
        pf_ = psf(6)
        for i in range(NT):
            bk = nb() % 6
            pv = psf(bk)
            for c in range(8):
                P.matmul(pv, xT[:, c, i * 128:(i + 1) * 128], Win[:, c, OFF_V:OFF_V + 512], start=(c == 0), stop=(c == 7))
            P.copy(vS[:, i, :, 0:64], pv.rearrange("p (h d) -> p h d", h=8), eng="act" if i % 2 else "dve")
            for c in range(8):
                P.matmul(pf_[:, i * 8:(i + 1) * 8], xT[:, c, i * 128:(i + 1) * 128], Win[:, c, OFF_F:OFF_F + 8],
                         start=(c == 0), stop=(c == 7))
        if stop_after == "A2":
            P.dma(out[0:128, :], xin[0])
            P.emit()
            return nc
        nlf2 = nlf.rearrange("p a b -> p (a b)")
        P.tensor_tensor(nlf2, pf_[:, 0:128], rA[:, BF_:BF_ + 128], ALU.add)
        P.activation(nlf2, nlf2, AF.Exp, scale=-1.0)
        P.activation(nlf2, nlf2, AF.Ln, bias=one_c, scale=1.0)
        if stop_after == "A3a":
            P.dma(out[0:128, :], xin[0])
            P.emit()
            return nc
        P.copy(incl[:, 0, :], nlf[:, 0, :])
        for i in range(1, NT):
            P.tensor_tensor(incl[:, i, :], incl[:, i - 1, :], nlf[:, i, :], ALU.add)
        if stop_after == "A3b":
            P.dma(out[0:128, :], xin[0])
            P.emit()
            return nc
        pc1 = psf(7)[:, 0:128]
        pc2 = psf(7)[:, 128:256]
        for (pdst, lmat, srcf) in ((pc1, cb[:, ONEB:ONEB + 128], incl.rearrange("p a b -> p (a b)")),
                                   (pc2, cb[:, LTRIB:LTRIB + 128], nlf2)):
            rres = cz[0][:, 0:128]
            for t3 in range(3):
                piece = xn[0][:, t3 * 128:(t3 + 1) * 128]
                P.copy(piece, srcf if t3 == 0 else rres)
                if t3 < 2:
                    P.tensor_tensor(rres, srcf if t3 == 0 else rres, piece, ALU.subtract)
                P.matmul(pdst, lmat, piece, start=(t3 == 0), stop=(t3 == 2))
        P.copy(Cend.rearrange("p a b -> p (a b)"), pc1)
        P.copy(Cc.rearrange("p a b -> p (a b)"), pc2)
        if stop_after == "A3c":
            P.dma(out[0:128, :], xin[0])
            P.emit()
            return nc
        P.tensor_tensor(Cc[:, 1:16, :], Cc[:, 1:16, :], Cend[:, 0:15, :], ALU.add)
        if stop_after == "A3d":
            P.dma(out[0:128, :], xin[0])
            P.emit()
            return nc
        for h in range(8):
            P.tensor_tensor(Bt[:, h, :, :], Cc[:, :, h:h + 1].to_broadcast([128, 16, 16]),
                            Cend[:, :, h].unsqueeze(1).to_broadcast([128, 16, 16]), ALU.subtract)
        if "d_q" in dbg:
            P.dma(dbg["d_q"], qT.rearrange("p a b -> p (a b)"))
        if "d_z" in dbg:
            P.dma(dbg["d_z"], zT.rearrange("p a b -> p (a b)"))
        if "d_B" in dbg:
            P.dma(dbg["d_B"], Bt.rearrange("p a b c -> p (a b c)"))
        if stop_after == "A3":
            P.dma(out[0:128, :], xin[0])
            P.emit()
            return nc
        if 'conv' in BP:
            for i4 in range(4):
                for kk in range(31):
                    P.tensor_scalar(Dg[:, kk, :], identb, vv[:, WDW + i4 * 31 + kk:WDW + i4 * 31 + kk + 1], None, ALU.mult)
                for j4 in range(4):
                    pcv = psf(nb() % 6)
                    for t4 in range(4):
                        c0 = (j4 * 4 + t4) * 128
                        for kk in range(31):
                            P.matmul(pcv[:, t4 * 128:(t4 + 1) * 128], zT[:, i4, c0 + kk:c0 + kk + 128], Dg[:, kk, :],
                                     start=(kk == 0), stop=(kk == 30))
                    P.tensor_tensor(cvo[i4][:, j4 * 4:(j4 + 1) * 4, :], pcv.rearrange("p (t c) -> p t c", t=4),
                                    rA[:, BDW_ + i4 * 128:BDW_ + (i4 + 1) * 128].unsqueeze(1).to_broadcast([128, 4, 128]),
                                    ALU.add)
        for wi, wsrc in enumerate((w_out_bf, w_mq_bf, w_mo_bf)):
            for c in range(8):
                P.dma(Woqm[:, wi, c, :], wsrc[c * 128:(c + 1) * 128, :])

        catT = xT
        LAG = 3

        def tail_conv(j):
            s1, nm, sq, rsd = stB[:, 10:11], stB[:, 11:12], stB[:, 12:13], stB[:, 13:14]
            xc, tmp = cz[0], cz[1]
            for i4 in range(4):
                P.copy(xc[:, i4 * 128:(i4 + 1) * 128], cvo[i4][:, j, :], eng="pool" if i4 % 2 else "dve")
            if "d_conv" in dbg:
                P.dma(dbg["d_conv"][j * 128:(j + 1) * 128, :], xc)
            P.reduce(s1, xc, ALU.add)
            P.tensor_scalar(nm, s1, -1.0 / 512.0, None, ALU.mult)
            P.tensor_scalar(xc, xc, nm, None, ALU.add)
            sumsq(tmp, xc, sq)
            P.activation(rsd, sq, AF.Ln, bias=epsln, scale=1.0 / 512.0)
            P.activation(rsd, rsd, AF.Exp, scale=-0.5)
            P.stt(xc, xc, rsd, rA[:, LG_:LG_ + 512], ALU.mult, ALU.mult)
            P.tensor_tensor(xc, xc, rA[:, LB_:LB_ + 512], ALU.add)
            P.activation(tmp, xc, AF.Exp, scale=-1.0)
            P.tensor_scalar(tmp, tmp, 1.0, None, ALU.add)
            P.add("dve", lambda e, o=tmp: e.reciprocal(o, o), reads=[tmp], writes=[tmp])
            P.tensor_tensor(catC[j % 2], xc, tmp, ALU.mult)

        def norm_attn(j, accb):
            rden = stB[:, 0:8]
            for hh in range(2):
                av = accb[hh][:, 0:260].rearrange("p (h d) -> p h d", h=4)
                P.add("dve", lambda e, o=rden[:, hh * 4:(hh + 1) * 4], i=av[:, :, 64]: e.reciprocal(o, i),
                      reads=[av[:, :, 64]], writes=[rden[:, hh * 4:(hh + 1) * 4]])
                P.tensor_tensor(attn[:, hh * 256:(hh + 1) * 256].rearrange("p (h d) -> p h d", h=4), av[:, :, 0:64],
                                rden[:, hh * 4:(hh + 1) * 4].unsqueeze(2).to_broadcast([128, 4, 64]), ALU.mult)
            if "d_attn" in dbg:
                P.dma(dbg["d_attn"][j * 128:(j + 1) * 128, :], attn)

        def tail_attn(j):
            ssa, rsa = stB[:, 8:9], stB[:, 9:10]
            sumsq(catA, attn, ssa)
            rstd_from_ss(rsa, ssa, eps512)
            P.tensor_scalar(catA, attn, rsa, None, ALU.mult)
            pv = psb(7)
            for c in range(4):
                P.transpose(pv[:, c * 128:(c + 1) * 128], catA[:, c * 128:(c + 1) * 128], identb)
            for c in range(4):
                P.transpose(pv[:, (4 + c) * 128:(5 + c) * 128], catC[j % 2][:, c * 128:(c + 1) * 128], identb)
            if "d_cat" in dbg:
                P.dma(dbg["d_cat"][j * 128:(j + 1) * 128, 0:512], catA)
                P.dma(dbg["d_cat"][j * 128:(j + 1) * 128, 512:1024], catC[j % 2])
            P.copy(catT[:, :, j * 128:(j + 1) * 128], pv.rearrange("p (c t) -> p c t", c=8), eng="act" if j % 2 else "dve")

        for j in range(NT):
            if j == 0:
                tail_conv(0)
                tail_conv(1)
            accb = [psf(2), psf(3)]
            pairs = [(h, kt) for h in range(8) for kt in range(j + 1)]
            stslots = {}
            n_pairs = len(pairs)

            def emit_qk(n):
                h, kt = pairs[n]
                stv = psf((0, 1, 4, 5, 6)[n % 5])[:, 0:128]
                r0 = 0 if 'evenonly' in BP else (h % 2) * 64
                if 'qk' in BP:
                    P.matmul(stv, kT[r0:r0 + 64, h // 2, kt * 128:(kt + 1) * 128], qT[r0:r0 + 64, h // 2, j * 128:(j + 1) * 128],
                             start=True, stop=(kt != j or 'mask' not in BP))
                if kt == j and 'mask' in BP:
                    P.matmul(stv, identb, negm, start=False, stop=True)
                pt = PT[n % 6]
                if 'exp' in BP:
                    if 'expdve' in BP:
                        P.copy(pt, stv, eng='dve')
                    elif 'expcopy' in BP:
                        P.copy(pt, stv, eng='act')
                    elif 'nobias' in BP:
                        P.activation(pt, stv, AF.Exp, scale=0.125)
                    else:
                        P.activation(pt, stv, AF.Exp, bias=Bt[:, h, kt, j:j + 1], scale=0.125)

            def emit_pv(n):
                h, kt = pairs[n]
                pt = PT[n % 6]
                av = accb[h // 4][:, (h % 4) * 65:(h % 4) * 65 + 65]
                if 'pv' in BP:
                    P.matmul(av, pt, vS[:, kt, h, :], start=(kt == 0), stop=(kt == j))

            for n in range(n_pairs + LAG):
                if j > 0 and n == min(6, n_pairs + LAG - 1):
                    tail_attn(j - 1)
                    if j + 1 < NT:
                        tail_conv(j + 1)
                if n < n_pairs:
                    emit_qk(n)
                if n >= LAG:
                    emit_pv(n - LAG)
            norm_attn(j, accb)
        tail_attn(NT - 1)

        if stop_after == "B":
            P.dma(out[0:128, :], xin[0])
            P.emit()
            return nc
        P.dma(rG, rows[:, RG:RG + 1024])
        P.tensor_scalar(rG, rG, 32.0, None, ALU.mult)
        Wout, Wmq, Wmo = Woqm[:, 0], Woqm[:, 1], Woqm[:, 2]

        def c_front(blk):
            pb = blk % 2
            for tt in range(2):
                i = blk * 2 + tt
                P.dma(xr[tt], x[tok0 + i * 128: tok0 + (i + 1) * 128, :])
                for nh in range(2):
                    pv = psf(nb() % 6)
                    for c in range(8):
                        P.matmul(pv, catT[:, c, i * 128:(i + 1) * 128], Wout[:, c, nh * 512:(nh + 1) * 512],
                                 start=(c == 0), stop=(c == 7))
                    P.tensor_tensor(x1b[pb][:, tt, nh * 512:(nh + 1) * 512], pv, xr[tt][:, nh * 512:(nh + 1) * 512], ALU.add)
                if "d_x1" in dbg:
                    P.dma(dbg["d_x1"][i * 128:(i + 1) * 128, :], x1b[pb][:, tt, :])
                yield
                ss, rs = stC[:, 2 * tt:2 * tt + 1], stC[:, 2 * tt + 1:2 * tt + 2]
                sumsq(xn2[tt], x1b[pb][:, tt, :], ss)
                rstd_from_ss(rs, ss, eps1024)
                P.tensor_scalar(xn2[tt], x1b[pb][:, tt, :], rs, None, ALU.mult)
                transposeT(xn2[tt], h2Tb[pb], tt * 128, "act" if tt else "dve")
                yield
            for cc in range(8):
                pv = psf(nb() % 6)[:, 0:256]
                for c in range(8):
                    P.matmul(pv, Wmq[:, c, cc * 128:(cc + 1) * 128], h2Tb[pb][:, c, :], start=(c == 0), stop=(c == 7))
                P.copy(q2Tb[pb][:, cc, :], pv, eng="act" if cc % 2 else "dve")
                if cc % 2:
                    yield

        def c_back(blk):
            pb = blk % 2

            def scores(h):
                for mt in range(2):
                    pv = psf(nb() % 6)[:, 0:256]
                    for k2 in range(2):
                        P.matmul(pv, K2T[:, 2 * h + k2, mt * 128:(mt + 1) * 128], q2Tb[pb][:, 2 * h + k2, :],
                                 start=(k2 == 0), stop=(k2 == 1))
                    P.activation(P2T[(h % 2) * 2 + mt + 4 * pb], pv, AF.Exp, scale=1.0 / 16.0)

            def pvh(h):
                for tt in range(2):
                    pa = psf(nb() % 6)[:, 0:257]
                    for mt in range(2):
                        P.matmul(pa, P2T[(h % 2) * 2 + mt + 4 * pb][:, tt * 128:(tt + 1) * 128], V2[:, mt, h, :],
                                 start=(mt == 0), stop=(mt == 1))
                    rd = stC[:, 8 + tt:9 + tt]
                    P.add("dve", lambda e, o=rd, i_=pa[:, 256:257]: e.reciprocal(o, i_), reads=[pa[:, 256:257]], writes=[rd])
                    P.tensor_scalar(oo[:, tt, h * 256:(h + 1) * 256], pa[:, 0:256], rd, None, ALU.mult)

            scores(0)
            yield
            scores(1)
            yield
            pvh(0)
            yield
            scores(2)
            pvh(1)
            yield
            scores(3)
            pvh(2)
            yield
            pvh(3)
            yield
            for tt in range(2):
                transposeT(oo[:, tt, :], oT, tt * 128, "act" if tt else "dve")
                yield
            for tt in range(2):
                i = blk * 2 + tt
                gi = b * NT + i
                xo = x2t[tt]
                for nh in range(2):
                    pv = psf(nb() % 6)
                    for c in range(8):
                        P.matmul(pv, oT[:, c, tt * 128:(tt + 1) * 128], Wmo[:, c, nh * 512:(nh + 1) * 512],
                                 start=(c == 0), stop=(c == 7))
                    P.tensor_tensor(xo[:, nh * 512:(nh + 1) * 512], pv, x1b[pb][:, tt, nh * 512:(nh + 1) * 512], ALU.add)
                P.dma(x2s[tok0 + i * 128: tok0 + (i + 1) * 128, :], xo, q="pool")
                yield
                ss, rs = stC[:, 4 + 2 * tt:5 + 2 * tt], stC[:, 5 + 2 * tt:6 + 2 * tt]
                hb = h3t[tt]
                sumsq(hb, xo, ss)
                rstd_from_ss(rs, ss, eps1024)
                P.stt(hb.rearrange("t (c p) -> t p c", c=8), xo.rearrange("t (p c) -> t p c", c=8), rs,
                      rG.rearrange("t (p c) -> t p c", c=8), ALU.mult, ALU.mult)
                P.dma(h3s[tok0 + i * 128: tok0 + (i + 1) * 128, :], hb, q="pool")
                transposeT(hb, h3T, 0, "act" if tt else "dve")
                yield
                pl = psf(7)[:, 256 + 64 * tt:256 + 64 * tt + 36]
                for c in range(8):
                    P.matmul(pl, h3T[:, c, :], wr_bf[:, c, :], start=(c == 0), stop=(c == 7))
                P.copy(LG[:, gi, :], pl, eng="act")
                yield

        def interleave(*gens):
            gens = [g for g in gens if g is not None]
            while gens:
                for g in list(gens):
                    try:
                        next(g)
                    except StopIteration:
                        gens.remove(g)

        interleave(c_front(0))
        for blk in range(NT // 2):
            interleave(c_back(blk), c_front(blk + 1) if blk + 1 < NT // 2 else None)

    A.off = mark_persist
    gidx = A.new(I32, [96]); didx = A.new(I32, [96, 4])
    mark_p2 = A.off
    N_ = NTT
    lgb = A.new(F32, [64, 36]); gmax = A.new(F32, [64]); ohg = A.new(F32, [64, 4]); eg = A.new(F32, [64, 4])
    gsum = A.new(F32, [64]); gtop = A.new(F32, [64]); esel = A.new(F32, [64, 8]); tmp8 = A.new(F32, [64, 8])
    m1 = A.new(F32, [64]); m2 = A.new(F32, [64]); oh1 = A.new(F32, [64, 8]); oh2 = A.new(F32, [64, 8])
    dd = A.new(F32, [64]); w1 = A.new(F32, [64])
    a1f = A.new(F32, [64, 32]); a2f = A.new(F32, [64, 32]); cmb = A.new(BF16, [64, 32])
    rk = A.new(F32, [64, 32]); tot = A.new(F32, [64, 32]); bex = A.new(F32, [64, 32]); big = A.new(F32, [64, 32])

    def bc3(v, k):
        return v[:, 0:N_].unsqueeze(2).to_broadcast([128, N_, k])

    P.tensor_tensor(lgb[:, 0:N_, :], LG[:, 0:N_, :], rA[:, BR_:BR_ + 36].unsqueeze(1).to_broadcast([128, N_, 36]), ALU.add)
    if "d_lg" in dbg:
        for gi in range(N_):
            P.dma(dbg["d_lg"][gi * 128:(gi + 1) * 128, :], lgb[:, gi, :])
    P.reduce(gmax[:, 0:N_], lgb[:, 0:N_, 0:4], ALU.max)
    P.tensor_tensor(ohg[:, 0:N_, :], lgb[:, 0:N_, 0:4], bc3(gmax, 4), ALU.is_equal)
    P.tensor_tensor(eg[:, 0:N_, :], lgb[:, 0:N_, 0:4], bc3(gmax, 4), ALU.subtract)
    P.activation(eg[:, 0:N_, :], eg[:, 0:N_, :], AF.Exp)
    P.reduce(gsum[:, 0:N_], eg[:, 0:N_, :], ALU.add)
    recip(gtop[:, 0:N_], gsum[:, 0:N_])
    P.tensor_tensor(esel[:, 0:N_, :], lgb[:, 0:N_, 4:12], ohg[:, 0:N_, 0:1].to_broadcast([128, N_, 8]), ALU.mult)
    for g in range(1, 4):
        P.tensor_tensor(tmp8[:, 0:N_, :], lgb[:, 0:N_, 4 + 8 * g:12 + 8 * g], ohg[:, 0:N_, g:g + 1].to_broadcast([128, N_, 8]), ALU.mult)
        P.tensor_tensor(esel[:, 0:N_, :], esel[:, 0:N_, :], tmp8[:, 0:N_, :], ALU.add)
    P.reduce(m1[:, 0:N_], esel[:, 0:N_, :], ALU.max)
    P.tensor_tensor(oh1[:, 0:N_, :], esel[:, 0:N_, :], bc3(m1, 8), ALU.is_equal)
    P.stt(tmp8[:, 0:N_, :], oh1[:, 0:N_, :], -1e30, esel[:, 0:N_, :], ALU.mult, ALU.add)
    P.reduce(m2[:, 0:N_], tmp8[:, 0:N_, :], ALU.max)
    P.tensor_tensor(oh2[:, 0:N_, :], tmp8[:, 0:N_, :], bc3(m2, 8), ALU.is_equal)
    P.tensor_tensor(dd[:, 0:N_], m2[:, 0:N_], m1[:, 0:N_], ALU.subtract)
    P.activation(dd[:, 0:N_], dd[:, 0:N_], AF.Exp)
    P.tensor_scalar(dd[:, 0:N_], dd[:, 0:N_], 1.0, None, ALU.add)
    recip(w1[:, 0:N_], dd[:, 0:N_])
    P.tensor_tensor(G1[:, 0:N_], gtop[:, 0:N_], w1[:, 0:N_], ALU.mult)
    P.tensor_tensor(G2[:, 0:N_], gtop[:, 0:N_], G1[:, 0:N_], ALU.subtract)
    for a_, oh in ((a1f, oh1), (a2f, oh2)):
        P.tensor_tensor(a_[:, 0:N_, :].rearrange("p n (g e) -> p n g e", g=4),
                        ohg[:, 0:N_, :].unsqueeze(3).to_broadcast([128, N_, 4, 8]),
                        oh[:, 0:N_, :].unsqueeze(2).to_broadcast([128, N_, 4, 8]), ALU.mult)
    P.tensor_tensor(cmb[:, 0:N_, :], a1f[:, 0:N_, :], a2f[:, 0:N_, :], ALU.add)
    cmb2, rk2, tot2 = (v.rearrange("p n e -> p (n e)") for v in (cmb, rk, tot))
    for q0 in range(0, N_ * 32, 512):
        q1 = min(q0 + 512, N_ * 32)
        pr = psf(nb() % 6)[:, 0:q1 - q0]
        P.matmul(pr, cb[:, UST:UST + 128], cmb2[:, q0:q1])
        P.copy(rk2[:, q0:q1], pr)
        pt_ = psf(nb() % 6)[:, 0:q1 - q0]
        P.matmul(pt_, cb[:, ONEB:ONEB + 128], cmb2[:, q0:q1])
        P.copy(tot2[:, q0:q1], pt_, eng="act")
    P.memset(bex[:, 0, :], 0.0)
    for t_ in range(1, N_):
        P.tensor_tensor(bex[:, t_, :], bex[:, t_ - 1, :], tot[:, t_ - 1, :], ALU.add)
    P.tensor_tensor(base, bex[:, N_ - 1, :], tot[:, N_ - 1, :], ALU.add)
    P.tensor_tensor(rk[:, 0:N_, :], rk[:, 0:N_, :], bex[:, 0:N_, :], ALU.add)
    for (af, Px) in ((a1f, P1), (a2f, P2)):
        P.tensor_tensor(big[:, 0:N_, :], af[:, 0:N_, :], rk[:, 0:N_, :], ALU.mult)
        P.reduce(Px[:, 0:N_], big[:, 0:N_, :], ALU.add)
    if "d_rt" in dbg:
        P.dma(dbg["d_rt"][:, 0:NTT], G1[:, 0:NTT]); P.dma(dbg["d_rt"][:, 64:64 + NTT], G2[:, 0:NTT])
        P.dma(dbg["d_rt"][:, 128:128 + NTT], P1[:, 0:NTT]); P.dma(dbg["d_rt"][:, 192:192 + NTT], P2[:, 0:NTT])
        P.dma(dbg["d_rt"][:, 256:288], base)
    if stop_after == "phase1":
        P.dma(out[0:128, :], big[:, 0:32, :].rearrange("p a b -> p (a b)"))
        P.emit()
        return nc


    pcf = A.new(F32, [32]); pci = A.new(I32, [32]); offs = A.new(F32, [32]); ends = A.new(F32, [32])
    te = A.new(F32, [96]); tf = A.new(F32, [96])
    zero_i = A.new(I32, [NSLOT // 128]); posf = A.new(F32, [64])
    P.tensor_scalar(pcf, base, TS / 2 - 0.5, 1.0 / TS, ALU.add, ALU.mult)
    P.copy(pci, pcf)
    P.copy(pcf, pci)
    P.tensor_scalar(pcf, pcf, float(TS), None, ALU.mult)
    P.memset(offs[:, 0:1], 0.0)
    for e in range(1, 32):
        P.tensor_tensor(offs[:, e:e + 1], offs[:, e - 1:e], pcf[:, e - 1:e], ALU.add)
    P.tensor_tensor(ends, offs, pcf, ALU.add)
    P.memset(te, 0.0)
    for e in range(32):
        P.stt(te, cf[:, THR:THR + 96], ends[:, e:e + 1], te, ALU.is_ge, ALU.add)
    if "d_te" in dbg:
        P.dma(dbg["d_te"], te)
    P.tensor_scalar(tf, te, 128.0, cf[:, PCOL:PCOL + 1], ALU.mult, ALU.add)
    P.copy(gidx, tf)
    for c in range(4):
        P.tensor_scalar(tf, te, 512.0, cf[:, CBASE + c:CBASE + c + 1], ALU.mult, ALU.add)
        P.copy(didx[:, :, c], tf)
    for (Ax, Px, POSx) in ((a1f, P1, POS1), (a2f, P2, POS2)):
        P.tensor_tensor(big[:, 0:NTT, :], Ax[:, 0:NTT, :], offs.unsqueeze(1).to_broadcast([128, NTT, 32]), ALU.mult)
        P.reduce(posf[:, 0:NTT], big[:, 0:NTT, :], ALU.add)
        P.tensor_tensor(posf[:, 0:NTT], posf[:, 0:NTT], Px[:, 0:NTT], ALU.add)
        P.copy(POSx[:, 0:NTT], posf[:, 0:NTT])
    P.memset(zero_i, 0)
    P.dma(s2t.rearrange("(p n) o -> p (n o)", p=128), zero_i)
    FAKE = "D:s2t_scatter"

    def scatter_slots():
        nsc = 0
        for gi in range(NTT):
            for POSx in (POS1, POS2):
                P.add("pool", lambda e, po=POSx[:, gi:gi + 1], ti=ci[:, gi:gi + 1]: e.indirect_dma_start(
                    out=s2t, out_offset=bass.IndirectOffsetOnAxis(ap=po, axis=0), in_=ti, in_offset=None),
                    reads=[POSx[:, gi:gi + 1], ci[:, gi:gi + 1], s2t], writes=[(FAKE, 0, 1, nsc, nsc + 1)], dma=True)
                nsc += 1

    A.off = mark_p2
    wg = [A.new(BF16, [8, 512]) for _ in range(3)]
    wu = [A.new(BF16, [8, 512]) for _ in range(3)]
    wd = [A.new(BF16, [4, 1024]) for _ in range(3)]
    sidx = [A.new(I32, [GT]) for _ in range(3)]
    hs = [A.new(BF16, [GT, 1024]) for _ in range(2)]
    hTs = [A.new(BF16, [8, TS]) for _ in range(2)]
    sil = [A.new(F32, [TS]) for _ in range(2)]
    hid = [A.new(BF16, [4, TS]) for _ in range(2)]
    yt = [A.new(BF16, [1024]) for _ in range(4)]
    s2t_v = s2t.rearrange("(j g p) o -> j p (g o)", g=GT, p=128)
    def moe_w(j):
        k3 = j % 3
        for (wdst, wsrc) in ((wg[k3], w_gate), (wu[k3], w_up)):
            P.add("pool", lambda e, o=wdst.rearrange("p a b -> p (a b)"), s=wsrc, ix=gidx[:, j:j + 1]: e.indirect_dma_start(
                out=o, out_offset=None, in_=s, in_offset=bass.IndirectOffsetOnAxis(ap=ix, axis=0),
                bounds_check=P.reg(e, 32 * 128 - 1), oob_is_err=False),
                reads=[gidx[:, j:j + 1], wsrc], writes=[wdst], dma=True)
        for c in range(4):
            P.add("pool", lambda e, o=wd[k3][:, c, :], ix=didx[:, j, c:c + 1]: e.indirect_dma_start(
                out=o, out_offset=None, in_=w_down, in_offset=bass.IndirectOffsetOnAxis(ap=ix, axis=0),
                bounds_check=P.reg(e, 32 * 512 - 1), oob_is_err=False),
                reads=[didx[:, j, c:c + 1], w_down], writes=[wd[k3][:, c, :]], dma=True)

    def moe_ix(j):
        k3 = j % 3
        P.add("sp", lambda e, o=sidx[k3], s=s2t_v[j]: e.dma_start(out=o, in_=s, allow_slow_non_contiguous=True),
              reads=[s2t_v[j], (FAKE, 0, 1, 0, 1 << 20)], writes=[sidx[k3]], dma=True)
        hsj = hs[j % 2]
        for g in range(GT):
            P.add("pool", lambda e, o=hsj[:, g, :], ix=sidx[k3][:, g:g + 1]: e.indirect_dma_start(
                out=o, out_offset=None, in_=h3s, in_offset=bass.IndirectOffsetOnAxis(ap=ix, axis=0)),
                reads=[sidx[k3][:, g:g + 1], h3s], writes=[hsj[:, g, :]], dma=True)

    def moe_T(j):
        for g in range(GT):
            transposeT(hs[j % 2][:, g, :], hTs[j % 2], g * 128, "act" if g % 2 else "dve")

    def moe_GU(j):
        k3, hTj, hidj = j % 3, hTs[j % 2], hid[j % 2]
        for fc in range(4):
            pg = psf(nb() % 6)[:, 0:TS]
            for c in range(8):
                P.matmul(pg, wg[k3][:, c, fc * 128:(fc + 1) * 128], hTj[:, c, :], start=(c == 0), stop=(c == 7))
            pu = psf(nb() % 6)[:, 0:TS]
            for c in range(8):
                P.matmul(pu, wu[k3][:, c, fc * 128:(fc + 1) * 128], hTj[:, c, :], start=(c == 0), stop=(c == 7))
            sl = sil[fc % 2]
            P.activation(sl, pg, AF.Silu)
            P.tensor_tensor(hidj[:, fc, :], sl, pu, ALU.mult)

    ny = [0]

    def moe_D(j):
        k3, hidj = j % 3, hid[j % 2]
        for g in range(GT):
            y_ = yt[ny[0] % 4]
            ny[0] += 1
            for nh in range(2):
                pv = psf(nb() % 6)
                for fc in range(4):
                    P.matmul(pv, hidj[:, fc, g * 128:(g + 1) * 128], wd[k3][:, fc, nh * 512:(nh + 1) * 512],
                             start=(fc == 0), stop=(fc == 3))
                P.copy(y_[:, nh * 512:(nh + 1) * 512], pv, eng="act" if nh else "dve")
            P.dma(Ysc[j * TS + g * 128: j * TS + (g + 1) * 128, :], y_)

    for j in range(min(3, n_moe_tiles)):
        moe_w(j)
    scatter_slots()
    if n_moe_tiles:
        moe_ix(0)
        moe_T(0)
    for j in range(n_moe_tiles):
        if 3 <= j + 2 < n_moe_tiles:
            moe_w(j + 2)
        if j + 1 < n_moe_tiles:
            moe_ix(j + 1)
        moe_GU(j)
        if j + 1 < n_moe_tiles:
            moe_T(j + 1)
        moe_D(j)

    A.off = mark_p2
    fg = A.new(F32, [1024])
    NB4 = 4
    x2b = [A.new(F32, [1024]) for _ in range(NB4)]
    y1b = [A.new(BF16, [1024]) for _ in range(NB4)]
    y2b = [A.new(BF16, [1024]) for _ in range(NB4)]
    otb = [A.new(F32, [1024]) for _ in range(NB4)]
    st4 = A.new(F32, [2 * NB4])
    P.dma(fg, rows[:, RF:RF + 1024])
    P.tensor_scalar(fg, fg, 32.0, None, ALU.mult)
    def p4_loads(gi):
        k2 = gi % NB4
        P.dma(x2b[k2], x2s[gi * 128:(gi + 1) * 128, :])
        for (yb, POSx) in ((y1b[k2], POS1), (y2b[k2], POS2)):
            P.add("pool", lambda e, o=yb, ix=POSx[:, gi:gi + 1]: e.indirect_dma_start(
                out=o, out_offset=None, in_=Ysc, in_offset=bass.IndirectOffsetOnAxis(ap=ix, axis=0)),
                reads=[POSx[:, gi:gi + 1], Ysc], writes=[yb], dma=True)

    PF = NB4 - 1
    for gi in range(min(PF, NTT)):
        p4_loads(gi)
    for gi in range(NTT):
        if gi + PF < NTT:
            p4_loads(gi + PF)
        k2 = gi % NB4
        acc = x2b[k2]
        if not skip_moe:
            P.stt(acc, y1b[k2], G1[:, gi:gi + 1], acc, ALU.mult, ALU.add)
            P.stt(acc, y2b[k2], G2[:, gi:gi + 1], acc, ALU.mult, ALU.add)
        ss, rs = st4[:, 2 * k2:2 * k2 + 1], st4[:, 2 * k2 + 1:2 * k2 + 2]
        sumsq(otb[k2], acc, ss)
        rstd_from_ss(rs, ss, eps1024)
        P.stt(otb[k2], acc, rs, fg, ALU.mult, ALU.mult)
        P.dma(out[gi * 128:(gi + 1) * 128, :], otb[k2])
    cnt = P.emit()
    st.close()
    return nc


def _consts():
    p = np.arange(128)
    cfm = np.zeros((128, NCF), np.float32)
    cfm[:, THR:THR + 96] = float(TS) * np.arange(96)[None, :]
    cfm[:, PCOL] = p
    cfm[:, CBASE:CBASE + 4] = np.arange(4)[None, :] * 128 + p[:, None]
    cfm[:, IOTA:IOTA + 32] = np.arange(32)[None, :]
    cfm[:, CEPS:CEPS + 4] = np.array([1024e-6, 512e-6, 1.0, 1e-6], np.float32)[None, :]
    cbm = np.zeros((128, NCB), np.float32)
    cbm[:, IDB:IDB + 128] = np.eye(128)
    cbm[:, NEGM:NEGM + 128] = np.where(p[:, None] > p[None, :], -30000.0, 0.0)
    cbm[:, UST:UST + 128] = (p[:, None] < p[None, :])
    cbm[:, ONEB:ONEB + 128] = 1.0
    cbm[:, LTRIB:LTRIB + 128] = (p[:, None] <= p[None, :])
    cim = (np.arange(64)[None, :] * 128 + p[:, None]).astype(np.int32)
    return cfm, cbm.astype(ml_dtypes.bfloat16), cim


def _host_inputs(core, nseq, x, mem, norm_mix_g, w_in, b_forget, b_glu, w_dw, b_dw, conv_ln_g, conv_ln_b,
                 attn_out_g, w_out, norm_mem_g, mem_norm_g, w_mq, w_mkv, w_mo, norm_ffn_g,
                 w_route_group, b_route_group, w_route_expert, b_route_expert, w_gate, w_up, w_down, final_g):
    f = lambda a: np.ascontiguousarray(np.asarray(a, dtype=np.float32))
    cfm, cbm, cim = _consts()
    vecs = np.zeros((128, NV), np.float32)
    vecs[:, GM:GM + 8] = f(norm_mix_g)[0].reshape(8, 128).T
    vecs[:, GQ:GQ + 8] = f(norm_mem_g)[0].reshape(8, 128).T
    vecs[:, GK:GK + 8] = f(mem_norm_g)[0].reshape(8, 128).T
    vecs[:, GA:GA + 4] = f(attn_out_g)[0].reshape(4, 128).T
    vecs[:, BG:BG + 8] = f(b_glu)[0].reshape(8, 128).T
    vecs[:, BD:BD + 4] = f(b_dw)[0].reshape(4, 128).T
    vecs[:, WDW:WDW + 124] = f(w_dw)[0].reshape(31, 4, 128).transpose(2, 1, 0).reshape(128, 124)
    rows = np.zeros((1, NR), np.float32)
    rows[0, RG:RG + 1024] = f(norm_ffn_g)[0]
    rows[0, RF:RF + 1024] = f(final_g)
    rows[0, RLG:RLG + 512] = f(conv_ln_g)[0]
    rows[0, RLB:RLB + 512] = f(conv_ln_b)[0]
    rows[0, RBF:RBF + 128] = np.tile(f(b_forget)[0], 16)
    rows[0, RBR:RBR + 4] = f(b_route_group)[0]
    rows[0, RBR + 4:RBR + 36] = f(b_route_expert)[0].reshape(32)
    rows[0, RBDW:RBDW + 512] = f(b_dw)[0]
    rows = np.ascontiguousarray(np.broadcast_to(rows, (128, NR)))
    wr = np.concatenate([f(w_route_group)[0], f(w_route_expert)[0].transpose(1, 0, 2).reshape(1024, 32)], axis=1)
    return {
        "x": f(x[core * nseq:(core + 1) * nseq]).reshape(nseq * S, D),
        "mem": f(mem[core * nseq:(core + 1) * nseq]).reshape(nseq * 256, D),
        "w_in": f(w_in)[0], "w_out": f(w_out)[0], "w_mq": f(w_mq)[0], "w_mkv": f(w_mkv)[0], "w_mo": f(w_mo)[0],
        "w_route": np.ascontiguousarray(wr.reshape(128, 8 * 36)),
        "w_gate": f(w_gate)[0].reshape(32 * 128, 4096), "w_up": f(w_up)[0].reshape(32 * 128, 4096),
        "w_down": f(w_down)[0].reshape(32 * 512, 1024),
        "vecs": vecs, "rows": rows, "constf": cfm, "constb": cbm, "consti": cim,
    }


_NC_CACHE = {}


def kernel(**inputs):
    n_cores = 8
    nseq = 4
    if "full" not in _NC_CACHE:
        _NC_CACHE["full"] = build(nseq=nseq)
    nc = _NC_CACHE["full"]
    in_maps = [_host_inputs(c, nseq, **inputs) for c in range(n_cores)]
    res = run_bass_kernel_spmd(nc, in_maps, core_ids=list(range(n_cores)))
    outs = [np.asarray(r["out"]).reshape(nseq, S, D) for r in res.results]
    return np.concatenate(outs, axis=0).astype(np.float32)
```
